# Optimizing a Trainium2 kernel written in Bass

```python
import math
import jax
import jax.numpy as jnp
from jax import lax
import numpy as np

D_MODEL = 1024
BATCH = 8
SEQ = 4096
DEPTH = 4

GRID_W = 64
CTX_LEN = 256
N_MIXERS = 2
NORM_EPS = 1e-6

MLA_HEADS = 8
QK_NOPE = 128
QK_ROPE = 64
V_HEAD = 128
QK_HEAD = QK_NOPE + QK_ROPE
Q_LORA = 384
KV_LORA = 256
ROPE_BASE = 10000.0
AXIS_PAIRS = QK_ROPE // 4
Q_BLOCK = 128
SM_SCALE = QK_HEAD ** -0.5

RWKV_HEAD = 64
RWKV_HEADS = D_MODEL // RWKV_HEAD
DECAY_LORA = 64
AAA_LORA = 64
MV_LORA = 32
GATE_LORA = 160
LN_X_EPS = 64e-5

D_FF = 2816
N_EXPERTS = 8
TOP_K = 2
D_FF_EXPERT = 3584

N_MLA = (DEPTH + 1) // 2
N_RWKV = DEPTH // 2
N_VRES = max(N_RWKV - 1, 0)
N_DENSE = (DEPTH + 1) // 2
N_MOE = DEPTH // 2

kernel_name = "hybrid_mla_rwkv7_moe_dit_prefix"


def rms_norm(x, g):
    xf = x.astype(jnp.float32)
    y = xf * lax.rsqrt(jnp.mean(xf * xf, axis=-1, keepdims=True) + NORM_EPS)
    return (y * g.astype(jnp.float32)).astype(x.dtype)


def modulate(h, shift, scale):
    return h * (1 + scale) + shift


def axial_rope_tables(rows):
    row_ids = jnp.repeat(jnp.arange(rows, dtype=jnp.float32), GRID_W)
    col_ids = jnp.tile(jnp.arange(GRID_W, dtype=jnp.float32), rows)
    inv_freq = 1.0 / (ROPE_BASE ** (jnp.arange(AXIS_PAIRS, dtype=jnp.float32) / AXIS_PAIRS))
    ang_r = row_ids[:, None] * inv_freq[None, :]
    ang_c = col_ids[:, None] * inv_freq[None, :]
    return (jnp.cos(ang_r), jnp.sin(ang_r), jnp.cos(ang_c), jnp.sin(ang_c))


def _rotate(z, cos, sin):
    m = cos.shape[-1]
    z1, z2 = z[..., :m], z[..., m:]
    cos = cos[:, None, :]
    sin = sin[:, None, :]
    return jnp.concatenate([z1 * cos - z2 * sin, z2 * cos + z1 * sin], axis=-1)


def apply_axial_rope(z, tables):
    cr, sr, cc, sc = tables
    zf = z.astype(jnp.float32)
    half = QK_ROPE // 2
    out = jnp.concatenate([_rotate(zf[..., :half], cr, sr), _rotate(zf[..., half:], cc, sc)], axis=-1)
    return out.astype(z.dtype)


def mla_queries(h, p, rope):
    B, T, _ = h.shape
    cq = rms_norm(h @ p["wqa"], p["qa_norm"])
    q = rms_norm((cq @ p["wqb"]).reshape(B, T, MLA_HEADS, QK_HEAD), p["q_norm"])
    if rope is not None:
        q = jnp.concatenate([q[..., :QK_NOPE], apply_axial_rope(q[..., QK_NOPE:], rope)], axis=-1)
    return q


def mla_keys_values(h, p, rope):
    B, T, _ = h.shape
    kv_a = h @ p["wkva"]
    ckv = rms_norm(kv_a[..., :KV_LORA], p["kva_norm"])
    k_rope = jnp.broadcast_to(kv_a[..., None, KV_LORA:], (B, T, MLA_HEADS, QK_ROPE))
    kv = (ckv @ p["wkvb"]).reshape(B, T, MLA_HEADS, QK_NOPE + V_HEAD)
    k = rms_norm(jnp.concatenate([kv[..., :QK_NOPE], k_rope], axis=-1), p["k_norm"])
    if rope is not None:
        k = jnp.concatenate([k[..., :QK_NOPE], apply_axial_rope(k[..., QK_NOPE:], rope)], axis=-1)
    return k, kv[..., QK_NOPE:]


def softmax_attend(q, k, v):
    s = jnp.einsum("bqhd,bkhd->bhqk", q, k, preferred_element_type=jnp.float32) * SM_SCALE
    pr = jax.nn.softmax(s, axis=-1).astype(v.dtype)
    return jnp.einsum("bhqk,bkhd->bqhd", pr, v)


def mla_mixer(hl, hc, p, rope, need_ctx_out):
    B, T, _ = hl.shape
    k_c, v_c = mla_keys_values(hc, p, None)
    k_l, v_l = mla_keys_values(hl, p, rope)
    q_l = mla_queries(hl, p, rope)
    k_all = jnp.concatenate([k_c, k_l], axis=1)
    v_all = jnp.concatenate([v_c, v_l], axis=1)
    nblk = T // Q_BLOCK
    qb = jnp.moveaxis(q_l.reshape(B, nblk, Q_BLOCK, MLA_HEADS, QK_HEAD), 1, 0)
    ob = lax.map(lambda qq: softmax_attend(qq, k_all, v_all), qb)
    out_l = jnp.moveaxis(ob, 0, 1).reshape(B, T, MLA_HEADS * V_HEAD) @ p["wo"]
    out_c = None
    if need_ctx_out:
        q_c = mla_queries(hc, p, None)
        out_c = softmax_attend(q_c, k_c, v_c).reshape(B, hc.shape[1], MLA_HEADS * V_HEAD) @ p["wo"]
    return out_l, out_c


def token_shift_centred(h):
    hp = jnp.pad(h, ((0, 0), (1, 1), (0, 0)))
    return 0.5 * (hp[:, :-2] + hp[:, 2:]) - h


def rwkv_features(h, p, vres, v_first, need_out):
    B, T, D = h.shape
    heads = lambda z: z.reshape(B, T, RWKV_HEADS, RWKV_HEAD)
    xx = token_shift_centred(h)
    mix = p["mix"]
    xw = h + xx * mix[1]
    xk = h + xx * mix[2]
    xv = h + xx * mix[3]
    xa = h + xx * mix[4]
    k = xk @ p["wk"]
    v = xv @ p["wv"]
    if vres is not None:
        v = v + (v_first - v) * jax.nn.sigmoid(vres["v0"] + (xv @ vres["v1"]) @ vres["v2"])
    kf = k.astype(jnp.float32)
    kk = heads(kf * p["k_k"])
    kk = kk / jnp.maximum(jnp.sqrt(jnp.sum(kk * kk, axis=-1, keepdims=True)), 1e-12)
    decay, a, kd = [], [], []
    for d in range(2):
        w_log = -jax.nn.softplus(-(p["w0"][d] + jnp.tanh(xw @ p["w1"][d]) @ p["w2"][d]).astype(jnp.float32)) - 0.5
        decay.append(heads(jnp.exp(-jnp.exp(w_log))))
        ad = jax.nn.sigmoid((p["a0"][d] + (xa @ p["a1"][d]) @ p["a2"][d]).astype(jnp.float32))
        a.append(heads(ad))
        kd.append(heads(kf * (1 + (ad - 1) * p["k_a"])))
    f = {"v": v, "vh": heads(v.astype(jnp.float32)), "kk": kk, "decay": decay, "a": a, "k": kd}
    if need_out:
        xr = h + xx * mix[0]
        xg = h + xx * mix[5]
        f["r"] = heads((xr @ p["wr"]).astype(jnp.float32))
        f["g"] = jax.nn.sigmoid(xg @ p["g1"]) @ p["g2"]
        f["k_bonus"] = 0.5 * (kd[0] + kd[1])
    return f


def wkv_scan(S0, decay, k, v, kk, a, r, reverse):
    emit = r is not None
    tm = lambda z: jnp.moveaxis(z.astype(jnp.float32), 1, 0)
    xs = (tm(decay), tm(k), tm(v), tm(-kk), tm(kk * a)) + ((tm(r),) if emit else ())

    def step(S, inp):
        w_t, k_t, v_t, a_t, b_t = inp[:5]
        sa = jnp.einsum("bhvk,bhk->bhv", S, a_t)
        S = S * w_t[:, :, None, :] + sa[..., None] * b_t[:, :, None, :] + v_t[..., None] * k_t[:, :, None, :]
        y = jnp.einsum("bhvk,bhk->bhv", S, inp[5]) if emit else None
        return S, y

    S, ys = lax.scan(step, S0, xs, reverse=reverse)
    return S, (jnp.moveaxis(ys, 0, 1) if emit else None)


def rwkv_output(y, f, p, dtype):
    B, T = y.shape[:2]
    mu = jnp.mean(y, axis=-1, keepdims=True)
    var = jnp.mean(jnp.square(y - mu), axis=-1, keepdims=True)
    yn = ((y - mu) * lax.rsqrt(var + LN_X_EPS)).reshape(B, T, D_MODEL) * p["ln_w"] + p["ln_b"]
    bonus = jnp.sum(f["r"] * f["k_bonus"] * p["r_k"], axis=-1, keepdims=True) * f["vh"]
    o = ((yn + bonus.reshape(B, T, D_MODEL)) * f["g"]).astype(dtype)
    return o @ p["wo"]


def rwkv_mixer(hl, hc, p, vres, v_first_l, v_first_c, need_ctx_out):
    B = hl.shape[0]
    fl = rwkv_features(hl, p, vres, v_first_l, True)
    fc = rwkv_features(hc, p, vres, v_first_c, need_ctx_out)
    y_l = 0.0
    y_c = 0.0
    for d, rev in ((0, False), (1, True)):
        S0 = jnp.zeros((B, RWKV_HEADS, RWKV_HEAD, RWKV_HEAD), jnp.float32)
        S_c, yc = wkv_scan(S0, fc["decay"][d], fc["k"][d], fc["vh"], fc["kk"], fc["a"][d], fc.get("r"), rev)
        _, yl = wkv_scan(S_c, fl["decay"][d], fl["k"][d], fl["vh"], fl["kk"], fl["a"][d], fl["r"], rev)
        y_l = y_l + yl
        if need_ctx_out:
            y_c = y_c + yc
    out_l = rwkv_output(y_l, fl, p, hl.dtype)
    out_c = rwkv_output(y_c, fc, p, hc.dtype) if need_ctx_out else None
    return out_l, out_c, fl["v"], fc["v"]


def swiglu(h, w1, w3, w2):
    return (jax.nn.silu(h @ w1) * (h @ w3)) @ w2


def moe_swiglu(h, router, w1, w3, w2):
    logits = (h @ router).astype(jnp.float32)
    top_val, top_idx = lax.top_k(logits, TOP_K)
    top_w = jax.nn.softmax(top_val, axis=-1)
    gates = jnp.sum(jax.nn.one_hot(top_idx, N_EXPERTS, dtype=jnp.float32) * top_w[..., None], axis=1)
    y = jnp.zeros_like(h)
    for e in range(N_EXPERTS):
        y = y + gates[:, e:e + 1].astype(h.dtype) * swiglu(h, w1[e], w3[e], w2[e])
    return y


def setup_inputs(seed: int = 0) -> dict:
    key = jax.random.key(seed)
    keys = jax.random.split(key, 48)
    counter = iter(range(48))
    D = D_MODEL

    def nrm(shape, scale):
        return scale * jax.random.normal(keys[next(counter)], shape, jnp.float32)

    def uni(shape, lo, hi):
        return jax.random.uniform(keys[next(counter)], shape, jnp.float32, lo, hi)

    def gain(shape):
        return 1.0 + nrm(shape, 0.02)

    return {
        "x": nrm((BATCH, SEQ, D), 1.0),
        "c": nrm((BATCH, D), 1.0),
        "ctx": nrm((BATCH, CTX_LEN, D), 1.0),
        "c_ctx": nrm((D,), 1.0),
        "ada_w": nrm((DEPTH, D, 6 * D), 0.5 * D ** -0.5),
        "ada_b": nrm((DEPTH, 6 * D), 0.02),
        "norm1_g": gain((DEPTH, D)),
        "norm2_g": gain((DEPTH, D)),
        "mla_wqa": nrm((N_MLA, D, Q_LORA), D ** -0.5),
        "mla_qa_norm": gain((N_MLA, Q_LORA)),
        "mla_wqb": nrm((N_MLA, Q_LORA, MLA_HEADS * QK_HEAD), Q_LORA ** -0.5),
        "mla_wkva": nrm((N_MLA, D, KV_LORA + QK_ROPE), D ** -0.5),
        "mla_kva_norm": gain((N_MLA, KV_LORA)),
        "mla_wkvb": nrm((N_MLA, KV_LORA, MLA_HEADS * (QK_NOPE + V_HEAD)), KV_LORA ** -0.5),
        "mla_q_norm": gain((N_MLA, QK_HEAD)),
        "mla_k_norm": gain((N_MLA, QK_HEAD)),
        "mla_wo": nrm((N_MLA, MLA_HEADS * V_HEAD, D), (MLA_HEADS * V_HEAD) ** -0.5),
        "rwkv_mix": uni((N_RWKV, 6, D), 0.0, 1.0),
        "rwkv_wr": nrm((N_RWKV, D, D), D ** -0.5),
        "rwkv_wk": nrm((N_RWKV, D, D), D ** -0.5),
        "rwkv_wv": nrm((N_RWKV, D, D), D ** -0.5),
        "rwkv_wo": nrm((N_RWKV, D, D), D ** -0.5),
        "rwkv_w0": uni((N_RWKV, 2, D), -5.5, 0.5),
        "rwkv_w1": nrm((N_RWKV, 2, D, DECAY_LORA), D ** -0.5),
        "rwkv_w2": nrm((N_RWKV, 2, DECAY_LORA, D), 0.1 * DECAY_LORA ** -0.5),
        "rwkv_a0": nrm((N_RWKV, 2, D), 0.1),
        "rwkv_a1": nrm((N_RWKV, 2, D, AAA_LORA), D ** -0.5),
        "rwkv_a2": nrm((N_RWKV, 2, AAA_LORA, D), 0.1 * AAA_LORA ** -0.5),
        "rwkv_g1": nrm((N_RWKV, D, GATE_LORA), D ** -0.5),
        "rwkv_g2": nrm((N_RWKV, GATE_LORA, D), GATE_LORA ** -0.5),
        "rwkv_k_k": 0.85 + nrm((N_RWKV, D), 0.02),
        "rwkv_k_a": gain((N_RWKV, D)),
        "rwkv_r_k": nrm((N_RWKV, RWKV_HEADS, RWKV_HEAD), 0.1),
        "rwkv_ln_w": gain((N_RWKV, D)),
        "rwkv_ln_b": nrm((N_RWKV, D), 0.02),
        "rwkv_v0": 1.0 + nrm((N_VRES, D), 0.1),
        "rwkv_v1": nrm((N_VRES, D, MV_LORA), D ** -0.5),
        "rwkv_v2": nrm((N_VRES, MV_LORA, D), 0.1 * MV_LORA ** -0.5),
        "ffn_w1": nrm((N_DENSE, D, D_FF), D ** -0.5),
        "ffn_w3": nrm((N_DENSE, D, D_FF), D ** -0.5),
        "ffn_w2": nrm((N_DENSE, D_FF, D), D_FF ** -0.5),
        "moe_router": nrm((N_MOE, D, N_EXPERTS), D ** -0.5),
        "moe_w1": nrm((N_MOE, N_EXPERTS, D, D_FF_EXPERT), D ** -0.5),
        "moe_w3": nrm((N_MOE, N_EXPERTS, D, D_FF_EXPERT), D ** -0.5),
        "moe_w2": nrm((N_MOE, N_EXPERTS, D_FF_EXPERT, D), D_FF_EXPERT ** -0.5),
    }


def reference(x, c, ctx, c_ctx, ada_w, ada_b, norm1_g, norm2_g,
              mla_wqa, mla_qa_norm, mla_wqb, mla_wkva, mla_kva_norm, mla_wkvb, mla_q_norm, mla_k_norm, mla_wo,
              rwkv_mix, rwkv_wr, rwkv_wk, rwkv_wv, rwkv_wo, rwkv_w0, rwkv_w1, rwkv_w2, rwkv_a0, rwkv_a1, rwkv_a2,
              rwkv_g1, rwkv_g2, rwkv_k_k, rwkv_k_a, rwkv_r_k, rwkv_ln_w, rwkv_ln_b, rwkv_v0, rwkv_v1, rwkv_v2,
              ffn_w1, ffn_w3, ffn_w2, moe_router, moe_w1, moe_w3, moe_w2):
    B, T, D = x.shape
    L = ctx.shape[1]
    rows = T // GRID_W
    rope = axial_rope_tables(rows)
    sc = jax.nn.silu(c)
    sc_ctx = jax.nn.silu(c_ctx)
    cs = ctx
    v_first_l = None
    v_first_c = None
    for i in range(DEPTH):
        last = i == DEPTH - 1
        j = i // N_MIXERS
        mod_l = (sc @ ada_w[i] + ada_b[i]).reshape(B, 6, 1, D)
        mod_c = (sc_ctx @ ada_w[i] + ada_b[i]).reshape(6, D)

        hl = modulate(rms_norm(x, norm1_g[i]), mod_l[:, 0], mod_l[:, 1])
        hc = modulate(rms_norm(cs, norm1_g[i]), mod_c[0], mod_c[1])
        if i % N_MIXERS == 0:
            p = {"wqa": mla_wqa[j], "qa_norm": mla_qa_norm[j], "wqb": mla_wqb[j], "wkva": mla_wkva[j],
                 "kva_norm": mla_kva_norm[j], "wkvb": mla_wkvb[j], "q_norm": mla_q_norm[j],
                 "k_norm": mla_k_norm[j], "wo": mla_wo[j]}
            out_l, out_c = mla_mixer(hl, hc, p, rope, not last)
        else:
            p = {"mix": rwkv_mix[j], "wr": rwkv_wr[j], "wk": rwkv_wk[j], "wv": rwkv_wv[j], "wo": rwkv_wo[j],
                 "w0": rwkv_w0[j], "w1": rwkv_w1[j], "w2": rwkv_w2[j], "a0": rwkv_a0[j], "a1": rwkv_a1[j],
                 "a2": rwkv_a2[j], "g1": rwkv_g1[j], "g2": rwkv_g2[j], "k_k": rwkv_k_k[j], "k_a": rwkv_k_a[j],
                 "r_k": rwkv_r_k[j], "ln_w": rwkv_ln_w[j], "ln_b": rwkv_ln_b[j]}
            vres = None if j == 0 else {"v0": rwkv_v0[j - 1], "v1": rwkv_v1[j - 1], "v2": rwkv_v2[j - 1]}
            out_l, out_c, v_l, v_c = rwkv_mixer(hl, hc, p, vres, v_first_l, v_first_c, not last)
            if j == 0:
                v_first_l, v_first_c = v_l, v_c
        x = x + mod_l[:, 2] * out_l
        if not last:
            cs = cs + mod_c[2] * out_c

        hl = modulate(rms_norm(x, norm2_g[i]), mod_l[:, 3], mod_l[:, 4]).reshape(B * T, D)
        if not last:
            hc = modulate(rms_norm(cs, norm2_g[i]), mod_c[3], mod_c[4]).reshape(B * L, D)
            tokens = jnp.concatenate([hl, hc], axis=0)
        else:
            tokens = hl
        if i % 2 == 0:
            k = i // 2
            y = swiglu(tokens, ffn_w1[k], ffn_w3[k], ffn_w2[k])
        else:
            k = i // 2
            y = moe_swiglu(tokens, moe_router[k], moe_w1[k], moe_w3[k], moe_w2[k])
        x = x + mod_l[:, 5] * y[:B * T].reshape(B, T, D)
        if not last:
            cs = cs + mod_c[5] * y[B * T:].reshape(B, L, D)
    return x
```

```python
import contextlib
import os
import numpy as np
import concourse.bass as bass
import concourse.mybir as mybir
from concourse.bass_utils import run_bass_kernel_spmd

F32 = mybir.dt.float32
BF16 = mybir.dt.bfloat16
ALU = mybir.AluOpType
AF = mybir.ActivationFunctionType
AX = mybir.AxisListType
NS = 8

D = 1024
KC = 8
CTX = 256
TL = 4096
NT = CTX + TL
EPS = 1e-6
NH = 8
SM_SCALE = 192 ** -0.5
DFF = 2816
DFE = 3584
NE = 8
C32W = 128 * 3 + 8 * 128 + 128 + 64 + 128 + 64 + 128 + 64 + 512
TILES = [(0, 256, 1)] + [(256 + 512 * i, 512, 0) for i in range(8)]


class Sched:
    def __init__(self, nc):
        self.nc = nc
        self.engs = {'pe': nc.tensor, 'dve': nc.vector, 'act': nc.scalar,
                     'pool': nc.gpsimd, 'sp': nc.sync}
        self.sem = {e: nc.alloc_semaphore(name=f"sem_{e}") for e in ['pe', 'dve', 'act', 'pool']}
        self.cnt = {e: 0 for e in self.sem}
        self.dq = {}
        for q, e in [('sp', 'sp'), ('pool', 'pool')]:
            self.dq[q] = dict(eng=e, n=0,
                              sems=[nc.alloc_semaphore(name=f"dsem_{q}{i}") for i in range(NS)])
        self.waited = {}
        self.lastw = {}
        self.readers = {}
        self.nins = 0
        self.mute = False

    def _sid_val(self, tok):
        if tok[0] == 'c':
            return ('c', tok[1]), self.sem[tok[1]], tok[2]
        q = self.dq[tok[1]]
        n = tok[2]
        return ('d', tok[1], n % NS), q['sems'][n % NS], 16 * (n // NS + 1)

    def _wait(self, eng, tok):
        if tok[0] == 'c' and tok[1] == eng and eng == 'pe':
            return
        sid, sem, val = self._sid_val(tok)
        if tok[0] == 'c':
            assert val <= self.cnt[tok[1]], f"wait on unsignalled instr {tok}"
        if self.waited.get((eng, sid), 0) >= val:
            return
        self.engs[eng].wait_ge(sem, val)
        self.waited[(eng, sid)] = val
        self.nins += 1

    def _deps(self, reads, writes):
        deps = set()
        for k in reads:
            if k in self.lastw:
                deps.add(self.lastw[k])
        for k in writes:
            if k in self.lastw:
                deps.add(self.lastw[k])
            for t in self.readers.get(k, {}).values():
                deps.add(t)
        return deps

    def _record(self, tok, reads, writes):
        sid, _, val = self._sid_val(tok)
        for k in reads:
            r = self.readers.setdefault(k, {})
            old = r.get(sid)
            if old is None or self._sid_val(old)[2] < val:
                r[sid] = tok
        for k in writes:
            self.lastw[k] = tok
            self.readers[k] = {}

    def op(self, eng, fn, reads=(), writes=(), inc=True):
        if self.mute:
            return None
        if eng != 'pe':
            psr = [k for k in reads if isinstance(k, tuple) and k[0] in ('ps', 'psb', 'sps')]
            if psr:
                reads = [k for k in reads if k not in psr]
                writes = list(writes) + psr
        for t in self._deps(reads, writes):
            self._wait(eng, t)
        ins = fn(self.engs[eng])
        self.nins += 1
        if inc:
            self.cnt[eng] += 1
            ins.then_inc(self.sem[eng], 1)
            tok = ('c', eng, self.cnt[eng])
        else:
            tok = ('c', eng, self.cnt[eng] + 1)
        self._record(tok, reads, writes)
        return ins

    def dma(self, q, out, in_, reads=(), writes=(), **kw):
        if self.mute:
            return None
        Q = self.dq[q]
        eng = Q['eng']
        n = Q['n']
        for t in self._deps(reads, writes):
            self._wait(eng, t)
        if n >= NS:
            self._wait(eng, ('d', q, n - NS))
        ins = self.engs[eng].dma_start(out=out, in_=in_, **kw)
        ins.then_inc(Q['sems'][n % NS], 16)
        self.nins += 1
        Q['n'] += 1
        self._record(('d', q, n), reads, writes)
        return ins

    def barrier(self):
        toks = []
        for e, c in self.cnt.items():
            if c > 0:
                toks.append(('c', e, c))
        for q, Q in self.dq.items():
            for n in range(max(0, Q['n'] - NS), Q['n']):
                toks.append(('d', q, n))
        for e in ['pe', 'dve', 'act', 'pool', 'sp']:
            for t in toks:
                if t[0] == 'c' and t[1] == e:
                    continue
                self._wait(e, t)
        self.lastw.clear()
        self.readers.clear()


def vec_layout(depth):
    ents = []
    for i in range(depth):
        ents += [(f"adab{i}", 48), (f"n1g{i}", 8), (f"n2g{i}", 8)]
        j = i // 2
        if i % 2 == 0:
            ents += [(f"qan{j}", 3), (f"kvan{j}", 2), (f"qnn{j}", 1), (f"qnr{j}", 1), (f"knn{j}", 1), (f"knr{j}", 1)]
        else:
            ents += [(f"mix{j}", 48), (f"w0{j}", 16), (f"a0{j}", 16), (f"kk{j}", 8), (f"ka{j}", 8),
                     (f"rk{j}", 8), (f"lnw{j}", 8), (f"lnb{j}", 8)]
            if j >= 1:
                ents += [(f"v0{j}", 8)]
    off = {}
    o = 0
    for n, c in ents:
        off[n] = (o, c)
        o += c
    return off, o


def fm(v):
    v = np.asarray(v, np.float32).reshape(-1)
    pad = (-len(v)) % 128
    if pad:
        v = np.concatenate([v, np.zeros(pad, np.float32)])
    return np.ascontiguousarray(v.reshape(-1, 128).T)


def build(depth=4, dbg=None):
    nc = bass.Bass("TRN2", target_bir_lowering=False)
    voff, NV = vec_layout(depth)

    def din(name, shape, dt=F32):
        return nc.dram_tensor(name, list(shape), dt, kind="ExternalInput").ap()

    def dscr(name, shape, dt=F32):
        return nc.dram_tensor(name, list(shape), dt, kind="Internal").ap()

    xs_in = din("xs", [D, NT])
    cvec_d = din("cvec", [128, 16])
    vecs_d = din("vecs", [128, NV])
    c32_d = din("c32", [128, C32W])
    ropec_d = din("ropec", [64, TL])
    ropes_d = din("ropes", [64, TL])
    ada_w = din("ada_w", [4, D, 6 * D])
    mla_wqa = din("mla_wqa", [2, D, 384]); mla_wqb = din("mla_wqb", [2, 384, 1536])
    mla_wkva = din("mla_wkva", [2, D, 320]); mla_wkvb = din("mla_wkvb", [2, 256, 2048])
    mla_wo = din("mla_wo", [2, D, D])
    ffn_w1 = din("ffn_w1", [2, D, DFF]); ffn_w3 = din("ffn_w3", [2, D, DFF]); ffn_w2 = din("ffn_w2", [2, DFF, D])
    moe_router = din("moe_router", [2, D, NE])
    moe_w1 = din("moe_w1", [2, NE, D, DFE]); moe_w3 = din("moe_w3", [2, NE, D, DFE]); moe_w2 = din("moe_w2", [2, NE, DFE, D])
    rwkv_wr = din("rwkv_wr", [2, D, D]); rwkv_wk = din("rwkv_wk", [2, D, D]); rwkv_wv = din("rwkv_wv", [2, D, D]); rwkv_wo = din("rwkv_wo", [2, D, D])
    rwkv_w1 = din("rwkv_w1", [2, 2, D, 64]); rwkv_w2 = din("rwkv_w2", [2, 2, 64, D])
    rwkv_a1 = din("rwkv_a1", [2, 2, D, 64]); rwkv_a2 = din("rwkv_a2", [2, 2, 64, D])
    rwkv_g1 = din("rwkv_g1", [2, D, 160]); rwkv_g2 = din("rwkv_g2", [2, 160, D])
    rwkv_v1 = din("rwkv_v1", [1, D, 32]); rwkv_v2 = din("rwkv_v2", [1, 32, D])
    out_d = nc.dram_tensor("out", [D, TL], F32, kind="ExternalOutput").ap()
    HS = dscr("HS", [D, 258 + 4098])
    ATd = [dscr(f"ATd{d}", [D, NT]) for d in range(2)]; BTd = [dscr(f"BTd{d}", [D, NT]) for d in range(2)]
    KTd = [dscr(f"KTd{d}", [D, NT]) for d in range(2)]; RTd = [dscr(f"RTd{d}", [D, NT]) for d in range(2)]
    WCd = [dscr(f"WCd{d}", [D, NT // 64]) for d in range(2)]
    VT = [dscr(f"VT{d}", [D, NT]) for d in range(2)]
    YD = [dscr(f"YD{d}", [D, NT]) for d in range(2)]
    BON = dscr("BON", [D, NT]); GG = dscr("GG", [D, NT])

    XS = dscr("XS", [D, NT])
    QN = dscr("QN", [NH, 128, NT], BF16); QR = dscr("QR", [NH, 64, NT], BF16)
    KN = dscr("KN", [NH, 128, NT], BF16); KR = dscr("KR", [NH, 64, NT], BF16)
    VV = dscr("VV", [NT, NH, 128], BF16)
    AO = dscr("AO", [D, NT], BF16)

    S = Sched(nc)
    es = contextlib.ExitStack()

    uniq = [0]

    def sb(name, shape, dt=F32, stack=None):
        uniq[0] += 1
        return (stack or es).enter_context(nc.sbuf_tensor(f"s{uniq[0]}_{name}", list(shape), dt))

    with es:
        ps = [es.enter_context(nc.psum_tensor(f"ps{i}", [128, 512], F32)) for i in range(8)]
        vecs = sb("vecs", [128, NV])
        c32 = sb("c32", [128, C32W])
        ones_bf = sb("ones_bf", [128, 128], BF16)
        cvec = sb("cvec", [128, 16])
        modt = sb("modt", [128, depth, 48, 2])
        A1 = sb("A1", [128, depth, 8, 2]); A2 = sb("A2", [128, depth, 8, 2])
        S.dma('sp', vecs[:], vecs_d, writes=['vecs'])
        S.dma('sp', c32[:], c32_d, writes=['c32'])
        S.dma('sp', cvec[:], cvec_d, writes=['cvec'])
        EPSI = {1024: 0, 384: 1, 256: 2, 192: 3, 64: 4}
        epsT = sb("epsT", [128, 8])
        for nf, ci in EPSI.items():
            S.op('pool', lambda e: e.memset(epsT[:, ci:ci + 1], float(nf * EPS)), writes=['epsT'])
        ones32 = c32[:, 0:128]
        ident = c32[:, 128:256]
        rotP = c32[0:64, 256:320]
        selE = c32[0:8, 384:384 + 8 * 128]
        o_ = 384 + 1024
        blockones = c32[:, o_:o_ + 128]
        SI = c32[:, o_ + 128:o_ + 192]
        maskAR_f = c32[:, o_ + 192:o_ + 320]
        maskN_f = c32[:, o_ + 320:o_ + 384]
        maskAR_r = c32[:, o_ + 384:o_ + 512]
        maskN_r = c32[:, o_ + 512:o_ + 576]
        cmask = c32[:, o_ + 576:o_ + 1088]
        S.op('pool', lambda e: e.memset(epsT[:, 5:6], 64e-5), writes=['epsT'])
        S.op('dve', lambda e: e.tensor_copy(out=ones_bf[:], in_=ones32), reads=['c32'], writes=['ones_bf'])

        def V(name, k0=0, k1=None):
            o, c = voff[name]
            k1 = c if k1 is None else k1
            return vecs[:, o + k0:o + k1]

        def phase_mod():
            with contextlib.ExitStack() as st:
                wb = [sb(f"adaw{i}", [128, 8, 1024], F32, st) for i in range(2)]
                sc = sb("sc", [128, 16], F32, st)
                S.op('act', lambda e: e.activation(out=sc[:], in_=cvec[:], func=AF.Silu), reads=['cvec'], writes=['sc'])
                n = 0
                for i in range(depth):
                    for j in range(6):
                        w = wb[n % 2]
                        S.dma('sp', w[:], ada_w[i, :, j * 1024:(j + 1) * 1024].rearrange("(k p) n -> p k n", p=128),
                              writes=[('adaw', n % 2)])
                        for oc in range(8):
                            col = (j * 8 + oc) * 2
                            for kc in range(8):
                                S.op('pe', lambda e: e.matmul(ps[0][:, col:col + 2], lhsT=w[:, kc, oc * 128:(oc + 1) * 128],
                                                              rhs=sc[:, 2 * kc:2 * kc + 2], start=(kc == 0), stop=(kc == 7)),
                                     reads=[('adaw', n % 2), 'sc'], writes=['psmod'], inc=(kc == 7 and oc == 7))
                        n += 1
                    S.op('dve', lambda e: e.tensor_tensor(
                        out=modt[:, i, :, :], in0=ps[0][:, 0:96].rearrange("p (a b) -> p a b", b=2),
                        in1=V(f"adab{i}").unsqueeze(2).to_broadcast([128, 48, 2]), op=ALU.add),
                        reads=['psmod', 'vecs'], writes=['modt'])
                    for (At, gname, jj) in [(A1, f"n1g{i}", 1), (A2, f"n2g{i}", 4)]:
                        S.op('dve', lambda e: e.scalar_tensor_tensor(
                            out=At[:, i, :, :], in0=modt[:, i, jj * 8:(jj + 1) * 8, :], scalar=1.0,
                            in1=V(gname).unsqueeze(2).to_broadcast([128, 8, 2]), op0=ALU.add, op1=ALU.mult),
                            reads=['modt', 'vecs'], writes=['A'])
                        S.op('dve', lambda e: e.tensor_scalar(out=At[:, i, :, :], in0=At[:, i, :, :], scalar1=float(np.sqrt(D)),
                                                              scalar2=None, op0=ALU.mult), reads=['A'], writes=['A'])
            S.barrier()

        def normmod(x32, N, Acol, Scol, outap, sq, rs, psb, kx, tag):
            ksq, krs, kps = ('sq', tag), 'rs', ('ps', psb)
            S.op('pool', lambda e: e.tensor_tensor(out=sq[:, :, :N], in0=x32, in1=x32, op=ALU.mult), reads=[kx], writes=[ksq])
            for k in range(8):
                S.op('pe', lambda e: e.matmul(ps[psb][:, :N], lhsT=ones32, rhs=sq[:, k, :N], start=(k == 0), stop=(k == 7)),
                     reads=[ksq, 'c32'], writes=[kps], inc=(k == 7))
            S.op('act', lambda e: e.activation(out=rs[:, :N], in_=ps[psb][:, :N], func=AF.Sqrt, bias=epsT[:, EPSI[D]:EPSI[D] + 1], scale=1.0),
                 reads=[kps, 'epsT'], writes=[krs])
            S.op('dve', lambda e: e.reciprocal(out=rs[:, :N], in_=rs[:, :N]), reads=[krs], writes=[krs])
            S.op('dve', lambda e: e.tensor_tensor(out=sq[:, :, :N], in0=x32, in1=rs[:, :N].unsqueeze(1).to_broadcast([128, 8, N]),
                                                  op=ALU.mult), reads=[kx, krs], writes=[ksq])
            S.op('pool', lambda e: e.tensor_tensor(out=sq[:, :, :N], in0=sq[:, :, :N], in1=Acol.unsqueeze(2).to_broadcast([128, 8, N]),
                                                   op=ALU.mult), reads=[ksq, 'A'], writes=[ksq])
            return ksq

        def rstd_from_ps(psb, N, nfeat, rs, krs, P=128):
            S.op('act', lambda e: e.activation(out=rs[:P, :N], in_=ps[psb][:P, :N], func=AF.Sqrt, bias=epsT[:P, EPSI[nfeat]:EPSI[nfeat] + 1], scale=1.0),
                 reads=[('ps', psb), 'epsT'], writes=[krs])
            S.op('dve', lambda e: e.reciprocal(out=rs[:P, :N], in_=rs[:P, :N]), reads=[krs], writes=[krs])

        def phase_mla(i, XSin):
            j = i // 2
            with contextlib.ExitStack() as st:
                wqa = sb("wqa", [128, 8, 384], BF16, st); wqb = sb("wqb", [128, 3, 1536], BF16, st)
                wkva = sb("wkva", [128, 8, 320], BF16, st); wkvb = sb("wkvb", [128, 2, 2048], BF16, st)
                S.dma('pool', wqa[:], mla_wqa[j].rearrange("(k p) n -> p k n", p=128), writes=['wqa'])
                S.dma('pool', wqb[:], mla_wqb[j].rearrange("(k p) n -> p k n", p=128), writes=['wqb'])
                S.dma('pool', wkva[:], mla_wkva[j].rearrange("(k p) n -> p k n", p=128), writes=['wkva'])
                S.dma('pool', wkvb[:], mla_wkvb[j].rearrange("(k p) n -> p k n", p=128), writes=['wkvb'])
                xt = [sb("xt0", [128, 8, 512], F32, st)] * 2
                sq = sb("sq", [128, 8, 512], F32, st)
                rs = sb("rs", [128, 512], F32, st)
                hb = sb("hb", [128, 8, 512], BF16, st)
                cq = sb("cq", [128, 3, 512], F32, st); cq2 = sb("cq2", [128, 3, 512], F32, st)
                cqn = sb("cqn", [128, 3, 512], BF16, st)
                ckv = sb("ckv", [128, 2, 512], F32, st); ckv2 = sb("ckv2", [128, 2, 512], F32, st)
                ckvn = sb("ckvn", [128, 2, 512], BF16, st)
                kr = sb("kr", [64, 512], F32, st); kr2 = sb("kr2", [64, 512], F32, st)
                krP = sb("krP", [64, 512], F32, st); krr = sb("krr", [64, 512], F32, st)
                qh = sb("qh", [128, 512], F32, st); qh2 = sb("qh2", [128, 512], F32, st)
                qr = sb("qr", [64, 512], F32, st); qr2 = sb("qr2", [64, 512], F32, st)
                qrP = sb("qrP", [64, 512], F32, st)
                rs2 = sb("rs2", [128, 512], F32, st)
                cosT = sb("cosT", [64, 512], F32, st); sinT = sb("sinT", [64, 512], F32, st)
                qn_o = sb("qn_o", [128, NH, 512], BF16, st); qr_o = sb("qr_o", [64, NH, 512], BF16, st)
                kn_o = sb("kn_o", [128, NH, 512], BF16, st); kr_o = sb("kr_o", [64, NH, 512], BF16, st)
                v_o = sb("v_o", [128, 4, NH, 128], BF16, st)
                sv = sb("sv", [128, 16], F32, st)
                S.op('dve', lambda e: e.tensor_scalar(out=sv[:, 0:3], in0=V(f"qan{j}"), scalar1=float(np.sqrt(384)), scalar2=None, op0=ALU.mult),
                     reads=['vecs'], writes=['sv'])
                S.op('dve', lambda e: e.tensor_scalar(out=sv[:, 3:5], in0=V(f"kvan{j}"), scalar1=float(np.sqrt(256)), scalar2=None, op0=ALU.mult),
                     reads=['vecs'], writes=['sv'])
                S.op('dve', lambda e: e.tensor_scalar(out=sv[:, 5:6], in0=V(f"qnn{j}"), scalar1=float(np.sqrt(192) * SM_SCALE), scalar2=None, op0=ALU.mult),
                     reads=['vecs'], writes=['sv'])
                S.op('dve', lambda e: e.tensor_scalar(out=sv[:, 6:7], in0=V(f"qnr{j}"), scalar1=float(np.sqrt(192) * SM_SCALE), scalar2=None, op0=ALU.mult),
                     reads=['vecs'], writes=['sv'])
                S.op('dve', lambda e: e.tensor_scalar(out=sv[:, 7:8], in0=V(f"knn{j}"), scalar1=float(np.sqrt(192)), scalar2=None, op0=ALU.mult),
                     reads=['vecs'], writes=['sv'])
                S.op('dve', lambda e: e.tensor_scalar(out=sv[:, 8:9], in0=V(f"knr{j}"), scalar1=float(np.sqrt(192)), scalar2=None, op0=ALU.mult),
                     reads=['vecs'], writes=['sv'])

                def rope(src, Pbuf, dst, N, ksrc, kdst, psb):
                    S.op('pe', lambda e: e.matmul(ps[psb][:64, :N], lhsT=rotP, rhs=src[:, :N], start=True, stop=True),
                         reads=[ksrc, 'c32'], writes=[('ps', psb)])
                    S.op('dve', lambda e: e.tensor_tensor(out=Pbuf[:, :N], in0=ps[psb][:64, :N], in1=sinT[:, :N], op=ALU.mult),
                         reads=[('ps', psb), 'rope'], writes=[('rp', kdst)])
                    S.op('pool', lambda e: e.tensor_tensor(out=dst[:, :N], in0=src[:, :N], in1=cosT[:, :N], op=ALU.mult),
                         reads=[ksrc, 'rope'], writes=[kdst])
                    S.op('pool', lambda e: e.tensor_tensor(out=dst[:, :N], in0=dst[:, :N], in1=Pbuf[:, :N], op=ALU.add),
                         reads=[kdst, ('rp', kdst)], writes=[kdst])

                for ti, (t0, N, mc) in enumerate(TILES):
                    x32 = xt[ti % 2]
                    kx = ('xt', ti % 2)
                    S.dma('sp', x32[:, :, :N], XSin[:, t0:t0 + N].rearrange("(k p) n -> p k n", p=128), writes=[kx])
                    if mc == 0:
                        S.dma('sp', cosT[:, :N], ropec_d[:, t0 - CTX:t0 - CTX + N], writes=['rope'])
                        S.dma('sp', sinT[:, :N], ropes_d[:, t0 - CTX:t0 - CTX + N], writes=['rope'])
                    ksq = normmod(x32[:, :, :N], N, A1[:, i, :, mc], None, None, sq, rs, 7, kx, 'm')
                    S.op('dve', lambda e: e.tensor_tensor(out=hb[:, :, :N], in0=sq[:, :, :N],
                                                          in1=modt[:, i, 0:8, mc].unsqueeze(2).to_broadcast([128, 8, N]), op=ALU.add),
                         reads=[ksq, 'modt'], writes=['hb'])
                    for c in range(3):
                        pb = c % 2
                        for kc in range(8):
                            S.op('pe', lambda e: e.matmul(ps[pb][:, :N], lhsT=wqa[:, kc, c * 128:(c + 1) * 128], rhs=hb[:, kc, :N],
                                                          start=(kc == 0), stop=(kc == 7)), reads=['hb', 'wqa'], writes=[('ps', pb)], inc=(kc == 7))
                        S.op('act', lambda e: e.activation(out=cq[:, c, :N], in_=ps[pb][:, :N], func=AF.Identity),
                             reads=[('ps', pb)], writes=[('cq', c)])
                        S.op('pool', lambda e: e.tensor_tensor(out=cq2[:, c, :N], in0=cq[:, c, :N], in1=cq[:, c, :N], op=ALU.mult),
                             reads=[('cq', c)], writes=[('cq2', c)])
                    for c in range(3):
                        S.op('pe', lambda e: e.matmul(ps[7][:, :N], lhsT=ones32, rhs=cq2[:, c, :N], start=(c == 0), stop=(c == 2)),
                             reads=[('cq2', c), 'c32'], writes=[('ps', 7)], inc=(c == 2))
                    rstd_from_ps(7, N, 384, rs, 'rs')
                    for c in range(3):
                        S.op('dve', lambda e: e.scalar_tensor_tensor(out=cqn[:, c, :N], in0=cq[:, c, :N], scalar=sv[:, c:c + 1], in1=rs[:, :N],
                                                                     op0=ALU.mult, op1=ALU.mult), reads=[('cq', c), 'rs', 'sv'], writes=['cqn'])
                    for c in range(3):
                        pb = 2 + c % 2
                        M = 128 if c < 2 else 64
                        for kc in range(8):
                            S.op('pe', lambda e: e.matmul(ps[pb][:M, :N], lhsT=wkva[:, kc, c * 128:c * 128 + M], rhs=hb[:, kc, :N],
                                                          start=(kc == 0), stop=(kc == 7)), reads=['hb', 'wkva'], writes=[('ps', pb)], inc=(kc == 7))
                        if c < 2:
                            S.op('act', lambda e: e.activation(out=ckv[:, c, :N], in_=ps[pb][:, :N], func=AF.Identity),
                                 reads=[('ps', pb)], writes=[('ckv', c)])
                            S.op('pool', lambda e: e.tensor_tensor(out=ckv2[:, c, :N], in0=ckv[:, c, :N], in1=ckv[:, c, :N], op=ALU.mult),
                                 reads=[('ckv', c)], writes=[('ckv2', c)])
                        else:
                            S.op('act', lambda e: e.activation(out=kr[:, :N], in_=ps[pb][:64, :N], func=AF.Identity),
                                 reads=[('ps', pb)], writes=['kr'])
                            S.op('pool', lambda e: e.tensor_tensor(out=kr2[:, :N], in0=kr[:, :N], in1=kr[:, :N], op=ALU.mult),
                                 reads=['kr'], writes=['kr2'])
                            S.op('dve', lambda e: e.tensor_scalar(out=kr[:, :N], in0=kr[:, :N], scalar1=sv[0:64, 8:9], scalar2=None, op0=ALU.mult),
                                 reads=['kr', 'sv', 'kr2'], writes=['kr'])
                    for c in range(2):
                        S.op('pe', lambda e: e.matmul(ps[7][:, :N], lhsT=ones32, rhs=ckv2[:, c, :N], start=(c == 0), stop=(c == 1)),
                             reads=[('ckv2', c), 'c32'], writes=[('ps', 7)], inc=(c == 1))
                    rstd_from_ps(7, N, 256, rs, 'rs')
                    for c in range(2):
                        S.op('dve', lambda e: e.scalar_tensor_tensor(out=ckvn[:, c, :N], in0=ckv[:, c, :N], scalar=sv[:, 3 + c:4 + c], in1=rs[:, :N],
                                                                     op0=ALU.mult, op1=ALU.mult), reads=[('ckv', c), 'rs', 'sv'], writes=['ckvn'])
                    if mc == 0:
                        rope(kr, krP, krr, N, 'kr', 'krr', 6)
                        krsrc, kkr = krr, 'krr'
                    else:
                        krsrc, kkr = kr, 'kr'
                    for h in range(NH):
                        for kc in range(3):
                            S.op('pe', lambda e: e.matmul(ps[0][:, :N], lhsT=wqb[:, kc, h * 192:h * 192 + 128], rhs=cqn[:, kc, :N],
                                                          start=(kc == 0), stop=(kc == 2)), reads=['cqn', 'wqb'], writes=[('ps', 0)], inc=(kc == 2))
                        for kc in range(3):
                            S.op('pe', lambda e: e.matmul(ps[1][:64, :N], lhsT=wqb[:, kc, h * 192 + 128:h * 192 + 192], rhs=cqn[:, kc, :N],
                                                          start=(kc == 0), stop=(kc == 2)), reads=['cqn', 'wqb'], writes=[('ps', 1)], inc=(kc == 2))
                        S.op('act', lambda e: e.activation(out=qh[:, :N], in_=ps[0][:, :N], func=AF.Identity), reads=[('ps', 0)], writes=['qh'])
                        S.op('act', lambda e: e.activation(out=qr[:, :N], in_=ps[1][:64, :N], func=AF.Identity), reads=[('ps', 1)], writes=['qr'])
                        S.op('pool', lambda e: e.tensor_tensor(out=qh2[:, :N], in0=qh[:, :N], in1=qh[:, :N], op=ALU.mult), reads=['qh'], writes=['qh2'])
                        S.op('pool', lambda e: e.tensor_tensor(out=qr2[:, :N], in0=qr[:, :N], in1=qr[:, :N], op=ALU.mult), reads=['qr'], writes=['qr2'])
                        S.op('pe', lambda e: e.matmul(ps[4][:, :N], lhsT=ones32, rhs=qh2[:, :N], start=True, stop=False),
                             reads=['qh2', 'c32'], writes=[('ps', 4)], inc=False)
                        S.op('pe', lambda e: e.matmul(ps[4][:, :N], lhsT=c32[0:64, 0:128], rhs=qr2[:, :N], start=False, stop=True),
                             reads=['qr2', 'c32'], writes=[('ps', 4)])
                        rstd_from_ps(4, N, 192, rs2, 'rs2')
                        S.op('dve', lambda e: e.scalar_tensor_tensor(out=qn_o[:, h, :N], in0=qh[:, :N], scalar=sv[:, 5:6], in1=rs2[:, :N],
                                                                     op0=ALU.mult, op1=ALU.mult), reads=['qh', 'rs2', 'sv'], writes=['qn_o'])
                        S.op('dve', lambda e: e.scalar_tensor_tensor(out=qr[:, :N], in0=qr[:, :N], scalar=sv[0:64, 6:7], in1=rs2[0:64, :N],
                                                                     op0=ALU.mult, op1=ALU.mult), reads=['qr', 'rs2', 'sv', 'qr2'], writes=['qr'])
                        if mc == 0:
                            rope(qr, qrP, qr2, N, 'qr', 'qr2', 6)
                            S.op('act', lambda e: e.activation(out=qr_o[:, h, :N], in_=qr2[:, :N], func=AF.Identity), reads=['qr2'], writes=['qr_o'])
                        else:
                            S.op('act', lambda e: e.activation(out=qr_o[:, h, :N], in_=qr[:, :N], func=AF.Identity), reads=['qr'], writes=['qr_o'])
                        for kc in range(2):
                            S.op('pe', lambda e: e.matmul(ps[2][:, :N], lhsT=wkvb[:, kc, h * 256:h * 256 + 128], rhs=ckvn[:, kc, :N],
                                                          start=(kc == 0), stop=(kc == 1)), reads=['ckvn', 'wkvb'], writes=[('ps', 2)], inc=(kc == 1))
                        S.op('act', lambda e: e.activation(out=qh[:, :N], in_=ps[2][:, :N], func=AF.Identity), reads=[('ps', 2)], writes=['qh'])
                        S.op('pool', lambda e: e.tensor_tensor(out=qh2[:, :N], in0=qh[:, :N], in1=qh[:, :N], op=ALU.mult), reads=['qh'], writes=['qh2'])
                        S.op('pe', lambda e: e.matmul(ps[5][:, :N], lhsT=ones32, rhs=qh2[:, :N], start=True, stop=False),
                             reads=['qh2', 'c32'], writes=[('ps', 5)], inc=False)
                        S.op('pe', lambda e: e.matmul(ps[5][:, :N], lhsT=c32[0:64, 0:128], rhs=kr2[:, :N], start=False, stop=True),
                             reads=['kr2', 'c32'], writes=[('ps', 5)])
                        rstd_from_ps(5, N, 192, rs2, 'rs2')
                        S.op('dve', lambda e: e.scalar_tensor_tensor(out=kn_o[:, h, :N], in0=qh[:, :N], scalar=sv[:, 7:8], in1=rs2[:, :N],
                                                                     op0=ALU.mult, op1=ALU.mult), reads=['qh', 'rs2', 'sv'], writes=['kn_o'])
                        S.op('dve', lambda e: e.tensor_tensor(out=kr_o[:, h, :N], in0=krsrc[:, :N], in1=rs2[0:64, :N], op=ALU.mult),
                             reads=[kkr, 'rs2'], writes=['kr_o'])
                        for blk in range(N // 128):
                            for kc in range(2):
                                S.op('pe', lambda e: e.matmul(ps[3][:, blk * 128:(blk + 1) * 128], lhsT=ckvn[:, kc, blk * 128:(blk + 1) * 128],
                                                              rhs=wkvb[:, kc, h * 256 + 128:h * 256 + 256], start=(kc == 0), stop=(kc == 1)),
                                     reads=['ckvn', 'wkvb'], writes=[('ps', 3)], inc=(kc == 1 and blk == N // 128 - 1))
                        S.op('act', lambda e: e.activation(out=v_o[:, 0:N // 128, h, :], in_=ps[3][:, :N].rearrange("p (b d) -> p b d", d=128),
                                                           func=AF.Identity), reads=[('ps', 3)], writes=['v_o'])
                    S.dma('sp', QN[:, :, t0:t0 + N].rearrange("h p n -> p h n"), qn_o[:, :, :N], reads=['qn_o'], writes=['QN'])
                    S.dma('sp', QR[:, :, t0:t0 + N].rearrange("h p n -> p h n"), qr_o[:, :, :N], reads=['qr_o'], writes=['QR'])
                    S.dma('sp', KN[:, :, t0:t0 + N].rearrange("h p n -> p h n"), kn_o[:, :, :N], reads=['kn_o'], writes=['KN'])
                    S.dma('sp', KR[:, :, t0:t0 + N].rearrange("h p n -> p h n"), kr_o[:, :, :N], reads=['kr_o'], writes=['KR'])
                    S.dma('sp', VV[t0:t0 + N].rearrange("(b p) h d -> p b h d", p=128), v_o[:, 0:N // 128], reads=['v_o'], writes=['VV'])
            S.barrier()
            with contextlib.ExitStack() as st:
                kn = [sb(f"a_kn{b}", [128, NT], BF16, st) for b in range(2)]
                krt = [sb(f"a_kr{b}", [64, NT], BF16, st) for b in range(2)]
                qn = [sb(f"a_qn{b}", [128, NT], BF16, st) for b in range(2)]
                qrt = [sb(f"a_qr{b}", [64, NT], BF16, st) for b in range(2)]
                vt = [sb(f"a_v{b}", [128, NT // 128, 128], BF16, st) for b in range(2)]
                pT = [sb(f"a_p{b}", [128, 512], BF16, st) for b in range(3)]
                rd = sb("a_rd", [128, 512], F32, st)
                ao = [sb(f"a_o{b}", [128, 512], BF16, st) for b in range(2)]
                npt = 0
                nq = 0
                for h in range(NH):
                    b = h % 2
                    kh = ('ah', b)
                    S.dma('sp', kn[b][:], KN[h], writes=[kh])
                    S.dma('sp', krt[b][:], KR[h], writes=[kh])
                    S.dma('sp', qn[b][:], QN[h], writes=[kh])
                    S.dma('sp', qrt[b][:], QR[h], writes=[kh])
                    S.dma('sp', vt[b][:], VV[:, h, :].rearrange("(b p) d -> p b d", p=128), writes=[kh])
                    for (t0, N, mc) in TILES:
                        nkb = 2 if mc == 1 else NT // 128
                        po, pd = 4 + (nq % 2), 6 + (nq % 2)

                        def scores(kb):
                            sbk = kb % 3
                            S.op('pe', lambda e: e.matmul(ps[sbk][:, :N], lhsT=kn[b][:, kb * 128:(kb + 1) * 128], rhs=qn[b][:, t0:t0 + N],
                                                          start=True, stop=False), reads=[kh], writes=[('ps', sbk)], inc=False)
                            S.op('pe', lambda e: e.matmul(ps[sbk][:, :N], lhsT=krt[b][:, kb * 128:(kb + 1) * 128], rhs=qrt[b][:, t0:t0 + N],
                                                          start=False, stop=True), reads=[kh], writes=[('ps', sbk)])
                        scores(0)
                        for kb in range(nkb):
                            if kb + 1 < nkb:
                                scores(kb + 1)
                            sbk = kb % 3
                            pb = npt % 3
                            npt += 1
                            S.op('act', lambda e: e.activation(out=pT[pb][:, :N], in_=ps[sbk][:, :N], func=AF.Exp),
                                 reads=[('ps', sbk)], writes=[('pT', pb)])
                            S.op('pe', lambda e: e.matmul(ps[po][:, :N], lhsT=vt[b][:, kb, :], rhs=pT[pb][:, :N], start=(kb == 0), stop=(kb == nkb - 1)),
                                 reads=[kh, ('pT', pb)], writes=[('ps', po)], inc=False)
                            S.op('pe', lambda e: e.matmul(ps[pd][:, :N], lhsT=ones_bf[:], rhs=pT[pb][:, :N], start=(kb == 0), stop=(kb == nkb - 1)),
                                 reads=['ones_bf', ('pT', pb)], writes=[('ps', pd)])
                        S.op('dve', lambda e: e.reciprocal(out=rd[:, :N], in_=ps[pd][:, :N]), reads=[('ps', pd)], writes=['rd'])
                        ob = nq % 2
                        S.op('dve', lambda e: e.tensor_tensor(out=ao[ob][:, :N], in0=ps[po][:, :N], in1=rd[:, :N], op=ALU.mult),
                             reads=[('ps', po), 'rd'], writes=[('ao', ob)])
                        S.dma('sp', AO[h * 128:(h + 1) * 128, t0:t0 + N], ao[ob][:, :N], reads=[('ao', ob)], writes=['AO'])
                        nq += 1
            S.barrier()
            with contextlib.ExitStack() as st:
                wo = sb("wo", [128, 8, 1024], BF16, st)
                S.dma('pool', wo[:], mla_wo[j].rearrange("(k p) n -> p k n", p=128), writes=['wo'])
                xt = [sb(f"c_xt{b}", [128, 8, 512], F32, st) for b in range(2)]
                at = [sb(f"c_at{b}", [128, 8, 512], BF16, st) for b in range(2)]
                for ti, (t0, N, mc) in enumerate(TILES):
                    b = ti % 2
                    S.dma('sp', xt[b][:, :, :N], XSin[:, t0:t0 + N].rearrange("(k p) n -> p k n", p=128), writes=[('cx', b)])
                    S.dma('sp', at[b][:, :, :N], AO[:, t0:t0 + N].rearrange("(k p) n -> p k n", p=128), writes=[('ca', b)])
                    for oc in range(8):
                        pb = oc % 4
                        for kc in range(8):
                            S.op('pe', lambda e: e.matmul(ps[pb][:, :N], lhsT=wo[:, kc, oc * 128:(oc + 1) * 128], rhs=at[b][:, kc, :N],
                                                          start=(kc == 0), stop=(kc == 7)), reads=['wo', ('ca', b)], writes=[('ps', pb)], inc=(kc == 7))
                        S.op('dve', lambda e: e.scalar_tensor_tensor(out=xt[b][:, oc, :N], in0=ps[pb][:, :N], scalar=modt[:, i, 16 + oc, mc:mc + 1],
                                                                     in1=xt[b][:, oc, :N], op0=ALU.mult, op1=ALU.add),
                             reads=[('ps', pb), ('cx', b), 'modt'], writes=[('cx', b)])
                    S.dma('sp', XS[:, t0:t0 + N].rearrange("(k p) n -> p k n", p=128), xt[b][:, :, :N], reads=[('cx', b)], writes=['XS'])
            S.barrier()

        def phase_rwkv(i, last):
            j = i // 2
            NCH = NT // 64
            RT256 = [(0, 256, 1)] + [(256 + 256 * a, 256, 0) for a in range(16)]
            HC = HS[:, 0:258]
            HL = HS[:, 258:258 + 4098]
            with contextlib.ExitStack() as st:
                xt = [sb(f"r1x{b}", [128, 8, 512], F32, st) for b in range(2)]
                sq = sb("r1sq", [128, 8, 512], F32, st)
                rs = sb("r1rs", [128, 512], F32, st)
                zt = sb("r1z", [128, 8, 1], F32, st)
                S.op('pool', lambda e: e.memset(zt[:], 0.0), writes=['zt'])
                for col in (0, 257, 258, 258 + 4097):
                    S.dma('sp', HS[:, col:col + 1].rearrange("(k p) n -> p k n", p=128), zt[:], reads=['zt'], writes=['HS'], allow_slow_non_contiguous=True)
                for ti, (t0, N, mc) in enumerate(TILES):
                    b = ti % 2
                    kx = ('r1x', b)
                    S.dma('sp', xt[b][:, :, :N], XS[:, t0:t0 + N].rearrange("(k p) n -> p k n", p=128), writes=[kx])
                    ksq = normmod(xt[b][:, :, :N], N, A1[:, i, :, mc], None, None, sq, rs, 7, kx, 'r1')
                    S.op('dve', lambda e: e.tensor_tensor(out=xt[b][:, :, :N], in0=sq[:, :, :N],
                                                          in1=modt[:, i, 0:8, mc].unsqueeze(2).to_broadcast([128, 8, N]), op=ALU.add),
                         reads=[ksq, 'modt'], writes=[kx])
                    dst = HC[:, 1:257] if mc == 1 else HL[:, 1 + t0 - CTX:1 + t0 - CTX + N]
                    S.dma('sp', dst.rearrange("(k p) n -> p k n", p=128), xt[b][:, :, :N], reads=[kx], writes=['HS'])
            S.barrier()
            if os.environ.get('RSTOP') == '1':
                return
            class _Stop(Exception):
                pass

            def stg(x):
                if os.environ.get('R2STOP') == x:
                    S.mute = True
            try:
              with contextlib.ExitStack() as st:
                  N = 256
                  wr = sb("wr", [128, 8, 1024], BF16, st); wk = sb("wk", [128, 8, 1024], BF16, st); wv = sb("wv", [128, 8, 1024], BF16, st)
                  w1c = sb("w1c", [128, 8, 128], BF16, st); a1c = sb("a1c", [128, 8, 128], BF16, st)
                  g1 = sb("g1", [128, 8, 160], BF16, st); g2a = sb("g2a", [128, 1024], BF16, st); g2b = sb("g2b", [32, 1024], BF16, st)
                  w2p = sb("w2p", [128, 2, 1024], BF16, st); a2p = sb("a2p", [128, 2, 1024], BF16, st)
                  for (wt_, src, kk_) in [(wr, rwkv_wr, 'wr'), (wk, rwkv_wk, 'wk'), (wv, rwkv_wv, 'wv')]:
                      S.dma('pool', wt_[:], src[j].rearrange("(k p) n -> p k n", p=128), writes=[kk_])
                  for d in range(2):
                      S.dma('pool', w1c[:, :, d * 64:(d + 1) * 64], rwkv_w1[j, d].rearrange("(k p) n -> p k n", p=128), writes=['w1c'])
                      S.dma('pool', a1c[:, :, d * 64:(d + 1) * 64], rwkv_a1[j, d].rearrange("(k p) n -> p k n", p=128), writes=['a1c'])
                  S.dma('pool', g1[:], rwkv_g1[j].rearrange("(k p) n -> p k n", p=128), writes=['g1'])
                  S.dma('pool', g2a[:], rwkv_g2[j, 0:128, :], writes=['g2a'])
                  S.dma('pool', g2b[:], rwkv_g2[j, 128:160, :], writes=['g2b'])
                  S.op('pool', lambda e: e.memset(w2p[:], 0.0), writes=['w2p'])
                  S.op('pool', lambda e: e.memset(a2p[:], 0.0), writes=['a2p'])
                  for d in range(2):
                      S.dma('pool', w2p[d * 64:(d + 1) * 64, d, :], rwkv_w2[j, d], writes=['w2p'])
                      S.dma('pool', a2p[d * 64:(d + 1) * 64, d, :], rwkv_a2[j, d], writes=['a2p'])
                  vres = j >= 1
                  if vres:
                      v1 = sb("v1", [128, 8, 32], BF16, st); v2 = sb("v2", [32, 1024], BF16, st)
                      S.dma('pool', v1[:], rwkv_v1[j - 1].rearrange("(k p) n -> p k n", p=128), writes=['v1'])
                      S.dma('pool', v2[:], rwkv_v2[j - 1], writes=['v2'])
                      vf = sb("vf", [128, N], F32, st)
                  dv_ = sb("dv", [128, 16], F32, st)
                  S.op('dve', lambda e: e.tensor_scalar(out=dv_[:, 0:8], in0=V(f"ka{j}"), scalar1=-1.0, scalar2=1.0, op0=ALU.mult, op1=ALU.add),
                       reads=['vecs'], writes=['dv'])
                  S.op('dve', lambda e: e.tensor_scalar(out=dv_[:, 8:16], in0=V(f"rk{j}"), scalar1=0.5, scalar2=None, op0=ALU.mult),
                       reads=['vecs'], writes=['dv'])
                  hx = sb("hx", [128, 8, N + 2], F32, st)
                  xx = sb("xx", [128, 8, N], F32, st)
                  xm = [sb(f"xm{m}", [128, 8, N], BF16, st) for m in range(6)]
                  lwm = sb("lwm", [128, N], BF16, st); am = sb("am", [128, N], BF16, st)
                  gma = sb("gma", [128, N], BF16, st); gmb = sb("gmb", [32, N], BF16, st); vm = sb("vm", [32, N], BF16, st)
                  T = {}
                  for nm in ["r32", "k32", "v32", "g32", "vv", "dvv", "kkc", "kk2", "rn", "kkn", "aneg", "sig", "lw", "al", "tk", "kd", "bb",
                             "Lp", "Lc", "E1", "E2", "E3", "t2", "At", "Rt", "Bt", "Kt", "kb", "t3", "bon"]:
                      T[nm] = sb("f_" + nm, [128, N], F32, st)
                  wct = sb("wct", [128, 4], F32, st)

                  def hb_(n):
                      return ps[n // 2][:, (n % 2) * 256:(n % 2) * 256 + 256]

                  def hk(n):
                      return ('psb', n // 2)

                  for ti, (t0, N_, mc) in enumerate(RT256):
                      src = HC[:, 0:258] if mc == 1 else HL[:, t0 - CTX:t0 - CTX + N + 2]
                      S.dma('sp', hx[:], src.rearrange("(k p) n -> p k n", p=128), writes=['hx'])
                      S.op('dve', lambda e: e.tensor_tensor(out=xx[:], in0=hx[:, :, 0:N], in1=hx[:, :, 2:N + 2], op=ALU.add), reads=['hx'], writes=['xx'])
                      S.op('dve', lambda e: e.scalar_tensor_tensor(out=xx[:], in0=xx[:], scalar=0.5, in1=hx[:, :, 1:N + 1], op0=ALU.mult, op1=ALU.subtract),
                           reads=['hx', 'xx'], writes=['xx'])
                      for m in range(6):
                          for kc in range(8):
                              S.op('dve', lambda e: e.scalar_tensor_tensor(out=xm[m][:, kc, :], in0=xx[:, kc, :], scalar=V(f"mix{j}", m * 8 + kc, m * 8 + kc + 1),
                                                                           in1=hx[:, kc, 1:N + 1], op0=ALU.mult, op1=ALU.add),
                                   reads=['hx', 'xx', 'vecs'], writes=[('xm', m)])
                      stg('a')
                      for kc in range(8):
                          S.op('pe', lambda e: e.matmul(hb_(0), lhsT=w1c[:, kc, :], rhs=xm[1][:, kc, :], start=(kc == 0), stop=(kc == 7)),
                               reads=['w1c', ('xm', 1)], writes=[hk(0)], inc=(kc == 7))
                      S.op('act', lambda e: e.activation(out=T["sig"][:], in_=hb_(0), func=AF.Sigmoid, scale=2.0), reads=[hk(0)], writes=['sig'])
                      S.op('dve', lambda e: e.tensor_scalar(out=lwm[:], in0=T["sig"][:], scalar1=2.0, scalar2=-1.0, op0=ALU.mult, op1=ALU.add),
                           reads=['sig'], writes=['lwm'])
                      for kc in range(8):
                          S.op('pe', lambda e: e.matmul(hb_(1), lhsT=a1c[:, kc, :], rhs=xm[4][:, kc, :], start=(kc == 0), stop=(kc == 7)),
                               reads=['a1c', ('xm', 4)], writes=[hk(1)], inc=(kc == 7))
                      S.op('act', lambda e: e.activation(out=am[:], in_=hb_(1), func=AF.Identity), reads=[hk(1)], writes=['am'])
                      for kc in range(8):
                          S.op('pe', lambda e: e.matmul(hb_(2), lhsT=g1[:, kc, 0:128], rhs=xm[5][:, kc, :], start=(kc == 0), stop=(kc == 7)),
                               reads=['g1', ('xm', 5)], writes=[hk(2)], inc=(kc == 7))
                      S.op('act', lambda e: e.activation(out=gma[:], in_=hb_(2), func=AF.Sigmoid), reads=[hk(2)], writes=['gma'])
                      for kc in range(8):
                          S.op('pe', lambda e: e.matmul(hb_(3)[0:32, :], lhsT=g1[:, kc, 128:160], rhs=xm[5][:, kc, :], start=(kc == 0), stop=(kc == 7)),
                               reads=['g1', ('xm', 5)], writes=[hk(3)], inc=(kc == 7))
                      S.op('act', lambda e: e.activation(out=gmb[:], in_=hb_(3)[0:32, :], func=AF.Sigmoid), reads=[hk(3)], writes=['gmb'])
                      if vres:
                          for kc in range(8):
                              S.op('pe', lambda e: e.matmul(hb_(4)[0:32, :], lhsT=v1[:, kc, :], rhs=xm[3][:, kc, :], start=(kc == 0), stop=(kc == 7)),
                                   reads=['v1', ('xm', 3)], writes=[hk(4)], inc=(kc == 7))
                          S.op('act', lambda e: e.activation(out=vm[:], in_=hb_(4)[0:32, :], func=AF.Identity), reads=[hk(4)], writes=['vm'])
                      for c in range(8):
                          cs = slice(c * 128, (c + 1) * 128)
                          rows = slice(c * 128, (c + 1) * 128)
                          stg('b')
                          for (hbn, w_, m) in [(5, wr, 0), (6, wk, 2), (7, wv, 3)]:
                              for kc in range(8):
                                  S.op('pe', lambda e: e.matmul(hb_(hbn), lhsT=w_[:, kc, cs], rhs=xm[m][:, kc, :], start=(kc == 0), stop=(kc == 7)),
                                       reads=[('xm', m), 'wr', 'wk', 'wv'], writes=[hk(hbn)], inc=(kc == 7))
                          S.op('pe', lambda e: e.matmul(hb_(8), lhsT=g2a[:, cs], rhs=gma[:], start=True, stop=False), reads=['g2a', 'gma'], writes=[hk(8)], inc=False)
                          S.op('pe', lambda e: e.matmul(hb_(8), lhsT=g2b[:, cs], rhs=gmb[:], start=False, stop=True), reads=['g2b', 'gmb'], writes=[hk(8)])
                          for d in range(2):
                              S.op('pe', lambda e: e.matmul(hb_(9 + d), lhsT=w2p[:, d, cs], rhs=lwm[:], start=True, stop=True), reads=['w2p', 'lwm'], writes=[hk(9 + d)])
                              S.op('pe', lambda e: e.matmul(hb_(11 + d), lhsT=a2p[:, d, cs], rhs=am[:], start=True, stop=True), reads=['a2p', 'am'], writes=[hk(11 + d)])
                          if vres:
                              S.op('pe', lambda e: e.matmul(hb_(13), lhsT=v2[:, cs], rhs=vm[:], start=True, stop=True), reads=['v2', 'vm'], writes=[hk(13)])
                          S.op('act', lambda e: e.activation(out=T["r32"][:], in_=hb_(5), func=AF.Identity), reads=[hk(5)], writes=['r32'])
                          S.op('act', lambda e: e.activation(out=T["k32"][:], in_=hb_(6), func=AF.Identity), reads=[hk(6)], writes=['k32'])
                          S.op('act', lambda e: e.activation(out=T["v32"][:], in_=hb_(7), func=AF.Identity), reads=[hk(7)], writes=['v32'])
                          S.op('act', lambda e: e.activation(out=T["g32"][:], in_=hb_(8), func=AF.Identity), reads=[hk(8)], writes=['g32'])
                          if vres:
                              S.dma('sp', vf[:], VT[0][rows, t0:t0 + N], writes=['vf'])
                              S.op('act', lambda e: e.activation(out=T["vv"][:], in_=hb_(13), func=AF.Sigmoid, bias=V(f"v0{j}", c, c + 1), scale=1.0),
                                   reads=[hk(13), 'vecs'], writes=['vv'])
                              S.op('pool', lambda e: e.tensor_tensor(out=T["dvv"][:], in0=vf[:], in1=T["v32"][:], op=ALU.subtract), reads=['vf', 'v32'], writes=['dvv'])
                              S.op('pool', lambda e: e.tensor_tensor(out=T["dvv"][:], in0=T["dvv"][:], in1=T["vv"][:], op=ALU.mult), reads=['dvv', 'vv'], writes=['dvv'])
                              S.op('pool', lambda e: e.tensor_tensor(out=T["v32"][:], in0=T["v32"][:], in1=T["dvv"][:], op=ALU.add), reads=['dvv', 'v32'], writes=['v32'])
                          stg('c')
                          S.op('dve', lambda e: e.tensor_scalar(out=T["kkc"][:], in0=T["k32"][:], scalar1=V(f"kk{j}", c, c + 1), scalar2=None, op0=ALU.mult),
                               reads=['k32', 'vecs'], writes=['kkc'])
                          S.op('pool', lambda e: e.tensor_tensor(out=T["kk2"][:], in0=T["kkc"][:], in1=T["kkc"][:], op=ALU.mult), reads=['kkc'], writes=['kk2'])
                          S.op('pe', lambda e: e.matmul(hb_(14), lhsT=blockones, rhs=T["kk2"][:], start=True, stop=True), reads=['kk2', 'c32'], writes=[hk(14)])
                          S.op('act', lambda e: e.activation(out=T["rn"][:], in_=hb_(14), func=AF.Sqrt), reads=[hk(14)], writes=['rn'])
                          S.op('dve', lambda e: e.tensor_scalar(out=T["rn"][:], in0=T["rn"][:], scalar1=1e-12, scalar2=None, op0=ALU.max), reads=['rn'], writes=['rn'])
                          S.op('dve', lambda e: e.reciprocal(out=T["rn"][:], in_=T["rn"][:]), reads=['rn'], writes=['rn'])
                          S.op('pool', lambda e: e.tensor_tensor(out=T["kkn"][:], in0=T["kkc"][:], in1=T["rn"][:], op=ALU.mult), reads=['kkc', 'rn'], writes=['kkn'])
                          S.op('pool', lambda e: e.tensor_scalar(out=T["aneg"][:], in0=T["kkn"][:], scalar1=-1.0, scalar2=None, op0=ALU.mult), reads=['kkn'], writes=['aneg'])
                          stg('d')
                          for d in range(2):
                              S.op('act', lambda e: e.activation(out=T["sig"][:], in_=hb_(9 + d), func=AF.Sigmoid, bias=V(f"w0{j}", d * 8 + c, d * 8 + c + 1), scale=1.0),
                                   reads=[hk(9 + d), 'vecs'], writes=['sig'])
                              S.op('pool', lambda e: e.tensor_scalar(out=T["lw"][:], in0=T["sig"][:], scalar1=float(-np.exp(-0.5)), scalar2=None, op0=ALU.mult),
                                   reads=['sig'], writes=['lw'])
                              S.op('act', lambda e: e.activation(out=T["al"][:], in_=hb_(11 + d), func=AF.Sigmoid, bias=V(f"a0{j}", d * 8 + c, d * 8 + c + 1), scale=1.0),
                                   reads=[hk(11 + d), 'vecs'], writes=['al'])
                              S.op('dve', lambda e: e.tensor_scalar(out=T["tk"][:], in0=T["al"][:], scalar1=V(f"ka{j}", c, c + 1), scalar2=dv_[:, c:c + 1],
                                                                    op0=ALU.mult, op1=ALU.add), reads=['al', 'vecs', 'dv'], writes=['tk'])
                              S.op('pool', lambda e: e.tensor_tensor(out=T["kd"][:], in0=T["k32"][:], in1=T["tk"][:], op=ALU.mult), reads=['k32', 'tk'], writes=['kd'])
                              S.op('pool', lambda e: e.tensor_tensor(out=T["bb"][:], in0=T["kkn"][:], in1=T["al"][:], op=ALU.mult), reads=['kkn', 'al'], writes=['bb'])
                              S.op('dve', lambda e: e.tensor_tensor_scan(out=T["Lp"][:], data0=cmask[:, :N], data1=T["lw"][:], initial=0.0, op0=ALU.mult, op1=ALU.add),
                                   reads=['lw', 'c32'], writes=['Lp'])
                              if d == 0:
                                  Lc, kLc = T["Lp"], 'Lp'
                              else:
                                  S.op('pool', lambda e: e.tensor_tensor(out=T["Lc"][:], in0=T["lw"][:], in1=T["Lp"][:], op=ALU.subtract), reads=['lw', 'Lp'], writes=['Lc'])
                                  S.op('pool', lambda e: e.tensor_tensor(
                                      out=T["Lc"][:].rearrange("p (c t) -> p c t", t=64), in0=T["Lc"][:].rearrange("p (c t) -> p c t", t=64),
                                      in1=T["Lp"][:].rearrange("p (c t) -> p c t", t=64)[:, :, 63:64].to_broadcast([128, N // 64, 64]), op=ALU.add),
                                      reads=['Lc', 'Lp'], writes=['Lc'])
                                  Lc, kLc = T["Lc"], 'Lc'
                              S.op('act', lambda e: e.activation(out=T["E1"][:], in_=Lc[:], func=AF.Exp), reads=[kLc], writes=['E1'])
                              S.op('act', lambda e: e.activation(out=T["E2"][:], in_=Lc[:], func=AF.Exp, scale=-1.0), reads=[kLc], writes=['E2'])
                              S.op('pool', lambda e: e.tensor_tensor(out=T["t2"][:], in0=Lc[:], in1=T["lw"][:], op=ALU.subtract), reads=[kLc, 'lw'], writes=['t2'])
                              S.op('act', lambda e: e.activation(out=T["E3"][:], in_=T["t2"][:], func=AF.Exp), reads=['t2'], writes=['E3'])
                              S.op('pool', lambda e: e.tensor_tensor(out=T["At"][:], in0=T["aneg"][:], in1=T["E3"][:], op=ALU.mult), reads=['aneg', 'E3'], writes=['At'])
                              S.op('pool', lambda e: e.tensor_tensor(out=T["Rt"][:], in0=T["r32"][:], in1=T["E1"][:], op=ALU.mult), reads=['r32', 'E1'], writes=['Rt'])
                              S.op('dve', lambda e: e.tensor_tensor(out=T["Bt"][:], in0=T["bb"][:], in1=T["E2"][:], op=ALU.mult), reads=['bb', 'E2'], writes=['Bt'])
                              S.op('dve', lambda e: e.tensor_tensor(out=T["Kt"][:], in0=T["kd"][:], in1=T["E2"][:], op=ALU.mult), reads=['kd', 'E2'], writes=['Kt'])
                              wcol = 63 if d == 0 else 0
                              S.op('dve', lambda e: e.tensor_copy(out=wct[:, 0:N // 64], in_=T["E1"][:].rearrange("p (c t) -> p c t", t=64)[:, :, wcol]),
                                   reads=['E1'], writes=['wct'])
                              S.dma('sp', ATd[d][rows, t0:t0 + N], T["At"][:], reads=['At'], writes=['ATd'])
                              S.dma('sp', RTd[d][rows, t0:t0 + N], T["Rt"][:], reads=['Rt'], writes=['RTd'])
                              S.dma('sp', BTd[d][rows, t0:t0 + N], T["Bt"][:], reads=['Bt'], writes=['BTd'])
                              S.dma('sp', KTd[d][rows, t0:t0 + N], T["Kt"][:], reads=['Kt'], writes=['KTd'])
                              S.dma('sp', WCd[d][rows, t0 // 64:t0 // 64 + N // 64], wct[:, 0:N // 64], reads=['wct'], writes=['WCd'])
                              if d == 0:
                                  S.op('pool', lambda e: e.tensor_copy(out=T["kb"][:], in_=T["kd"][:]), reads=['kd'], writes=['kb'])
                              else:
                                  S.op('pool', lambda e: e.tensor_tensor(out=T["kb"][:], in0=T["kb"][:], in1=T["kd"][:], op=ALU.add), reads=['kd', 'kb'], writes=['kb'])
                          stg('e')
                          S.op('pool', lambda e: e.tensor_tensor(out=T["t3"][:], in0=T["r32"][:], in1=T["kb"][:], op=ALU.mult), reads=['r32', 'kb'], writes=['t3'])
                          S.op('dve', lambda e: e.tensor_scalar(out=T["t3"][:], in0=T["t3"][:], scalar1=dv_[:, 8 + c:9 + c], scalar2=None, op0=ALU.mult),
                               reads=['t3', 'dv'], writes=['t3'])
                          S.op('pe', lambda e: e.matmul(hb_(15), lhsT=blockones, rhs=T["t3"][:], start=True, stop=True), reads=['t3', 'c32'], writes=[hk(15)])
                          S.op('dve', lambda e: e.tensor_tensor(out=T["bon"][:], in0=hb_(15), in1=T["v32"][:], op=ALU.mult), reads=[hk(15), 'v32'], writes=['bon'])
                          S.dma('sp', BON[rows, t0:t0 + N], T["bon"][:], reads=['bon'], writes=['BON'])
                          S.dma('sp', VT[j][rows, t0:t0 + N], T["v32"][:], reads=['v32'], writes=['VT'])
                          S.dma('sp', GG[rows, t0:t0 + N], T["g32"][:], reads=['g32'], writes=['GG'])

            except _Stop:
                pass
            S.mute = False
            S.barrier()
            if os.environ.get('RSTOP') == '2':
                return
            for d in range(2):
                order = ([0, 1, 2, 3] + list(range(4, NCH))) if d == 0 else ([3, 2, 1, 0] + list(range(NCH - 1, 3, -1)))
                mAR = maskAR_f if d == 0 else maskAR_r
                mN = maskN_f if d == 0 else maskN_r
                with contextlib.ExitStack() as st:
                    def stream(hh):
                        P_ = f"s{hh}_"
                        pb = [ps[4 * hh + q] for q in range(4)]

                        def pk(bank, half=None):
                            return [('sps', hh, bank)]
                        BD = {n: sb(P_ + n, [128, 4, 128], F32, st) for n in ["bdA", "T2", "T3", "T4", "tbB", "tbK", "tbV", "T8", "bdMak", "bdU", "bdST"]}
                        for n, t_ in BD.items():
                            S.op('pool', lambda e: e.memset(t_[:], 0.0), writes=[P_ + n])
                        AR = [sb(P_ + f"AR{b}", [128, 4, 2, 64], F32, st) for b in range(2)]
                        Bi = [sb(P_ + f"Bi{b}", [128, 4, 64], F32, st) for b in range(2)]
                        Ki = [sb(P_ + f"Ki{b}", [128, 4, 64], F32, st) for b in range(2)]
                        Vi = [sb(P_ + f"Vi{b}", [128, 4, 64], F32, st) for b in range(2)]
                        WCall = sb(P_ + "WCall", [128, 4, NCH], F32, st)
                        S.dma('sp', WCall[:], WCd[d][hh * 512:hh * 512 + 512, :].rearrange("(c p) n -> p c n", p=128), writes=[P_ + "WCall"])
                        ARm = sb(P_ + "ARm", [128, 4, 128], F32, st)
                        AKm = sb(P_ + "AKm", [128, 4, 128], F32, st)
                        Ns = sb(P_ + "Ns", [128, 4, 64], F32, st)
                        Vs = sb(P_ + "Vs", [128, 4, 64], F32, st)
                        PG = [sb(P_ + f"PG{b}", [128, 4, 128], F32, st) for b in range(2)]
                        Pst = [sb(P_ + f"Pst{b}", [128, 4, 64], F32, st) for b in range(2)]
                        Xs = sb(P_ + "Xs", [128, 4, 64], F32, st); Us = sb(P_ + "Us", [128, 4, 64], F32, st)
                        Yb = sb(P_ + "Yb", [128, 4, 64], F32, st); STs = sb(P_ + "STs", [128, 4, 64], F32, st)
                        tmpS = sb(P_ + "tmpS", [128, 4, 64], F32, st)
                        S.op('pool', lambda e: e.memset(STs[:], 0.0), writes=[P_ + "STs"])
                        r0 = hh * 512

                        def diag(eng, dst, kdst, src, ksrc, cols=64):
                            for half in range(2):
                                pslice = slice(half * 64, half * 64 + 64)
                                if eng == 'act':
                                    S.op('act', lambda e: e.activation(out=dst[pslice, :, half * 64:half * 64 + 64], in_=src[pslice], func=AF.Identity),
                                         reads=ksrc, writes=[kdst])
                                else:
                                    S.op(eng, lambda e: e.tensor_copy(out=dst[pslice, :, half * 64:half * 64 + 64], in_=src[pslice]),
                                         reads=ksrc, writes=[kdst])

                        def load(n):
                            g = order[n]
                            b = n % 2
                            cols = slice(g * 64, g * 64 + 64)
                            kin = P_ + f"in{b}"
                            for (dst, srcd) in [(AR[b][:, :, 0, :], ATd[d]), (AR[b][:, :, 1, :], RTd[d]), (Bi[b][:], BTd[d]), (Ki[b][:], KTd[d]), (Vi[b][:], VT[j])]:
                                S.dma('sp', dst, srcd[r0:r0 + 512, cols].rearrange("(c p) n -> p c n", p=128), writes=[kin])

                        load(0)
                        for n in range(NCH):
                            g = order[n]
                            b = n % 2
                            kin = P_ + f"in{b}"
                            if n + 1 < NCH:
                                load(n + 1)
                            diag('pool', BD["bdA"], P_ + "bdA", AR[b][:, :, 0, :], [kin])
                            diag('pool', BD["T2"], P_ + "T2", Bi[b], [kin])
                            diag('pool', BD["T3"], P_ + "T3", Ki[b], [kin])
                            diag('pool', BD["T4"], P_ + "T4", Vi[b], [kin])
                            ARf = AR[b][:].rearrange("p c a t -> p c (a t)")
                            for hp in range(4):
                                S.op('pe', lambda e: e.matmul(pb[0][:, hp * 128:(hp + 1) * 128], lhsT=BD["T2"][:, hp, :], rhs=ARf[:, hp, :], start=True, stop=True),
                                     reads=[P_ + "T2", kin], writes=pk(0), inc=(hp == 3))
                            for hp in range(4):
                                S.op('pe', lambda e: e.matmul(pb[1][:, hp * 128:(hp + 1) * 128], lhsT=BD["T3"][:, hp, :], rhs=ARf[:, hp, :], start=True, stop=True),
                                     reads=[P_ + "T3", kin], writes=pk(1), inc=(hp == 3))
                            for hp in range(4):
                                S.op('pe', lambda e: e.matmul(pb[2][:, hp * 64:(hp + 1) * 64], lhsT=BD["bdA"][:, hp, :], rhs=Bi[b][:, hp, :], start=True, stop=True),
                                     reads=[P_ + "bdA", kin], writes=pk(2, 0), inc=(hp == 3))
                            yield
                            S.op('dve', lambda e: e.tensor_tensor(out=ARm[:], in0=pb[0][:].rearrange("p (c t) -> p c t", t=128),
                                                                  in1=mAR.unsqueeze(1).to_broadcast([128, 4, 128]), op=ALU.mult),
                                 reads=pk(0) + ['c32'], writes=[P_ + "ARm"])
                            S.op('dve', lambda e: e.tensor_tensor(out=AKm[:], in0=pb[1][:].rearrange("p (c t) -> p c t", t=128),
                                                                  in1=mAR.unsqueeze(1).to_broadcast([128, 4, 128]), op=ALU.mult),
                                 reads=pk(1) + ['c32'], writes=[P_ + "AKm"])
                            S.op('dve', lambda e: e.tensor_tensor(out=Pst[0][:], in0=pb[2][:, 0:256].rearrange("p (c t) -> p c t", t=64),
                                                                  in1=mN.unsqueeze(1).to_broadcast([128, 4, 64]), op=ALU.mult),
                                 reads=pk(2, 0) + ['c32'], writes=[P_ + "Pst0"])
                            for hp in range(4):
                                S.op('pe', lambda e: e.matmul(pb[0][:, hp * 128:(hp + 1) * 128], lhsT=BD["T2"][:, hp, :], rhs=ident, start=True, stop=True),
                                     reads=[P_ + "T2", 'c32'], writes=pk(0), inc=(hp == 3))
                            for hp in range(4):
                                S.op('pe', lambda e: e.matmul(pb[1][:, hp * 128:(hp + 1) * 128], lhsT=BD["T3"][:, hp, :], rhs=ident, start=True, stop=True),
                                     reads=[P_ + "T3", 'c32'], writes=pk(1), inc=(hp == 3))
                            yield
                            S.op('act', lambda e: e.activation(out=BD["tbB"][:], in_=pb[0][:].rearrange("p (c t) -> p c t", t=128), func=AF.Identity),
                                 reads=pk(0), writes=[P_ + "tbB"])
                            S.op('act', lambda e: e.activation(out=BD["tbK"][:], in_=pb[1][:].rearrange("p (c t) -> p c t", t=128), func=AF.Identity),
                                 reads=pk(1), writes=[P_ + "tbK"])
                            for hp in range(4):
                                S.op('pe', lambda e: e.matmul(pb[0][:, hp * 128:(hp + 1) * 128], lhsT=BD["T4"][:, hp, :], rhs=ident, start=True, stop=True),
                                     reads=[P_ + "T4", 'c32'], writes=pk(0), inc=(hp == 3))
                            S.op('pool', lambda e: e.tensor_copy(out=PG[0][:, :, 0:64], in_=ARm[:, :, 0:64]), reads=[P_ + "ARm"], writes=[P_ + "PG0"])
                            S.op('pool', lambda e: e.tensor_copy(out=PG[0][:, :, 64:128], in_=SI.unsqueeze(1).to_broadcast([128, 4, 64])),
                                 reads=['c32'], writes=[P_ + "PG0"])
                            diag('pool', BD["bdMak"], P_ + "bdMak", AKm[:, :, 0:64], [P_ + "AKm"])
                            yield
                            S.op('act', lambda e: e.activation(out=BD["tbV"][:], in_=pb[0][:].rearrange("p (c t) -> p c t", t=128), func=AF.Identity),
                                 reads=pk(0), writes=[P_ + "tbV"])
                            for half in range(2):
                                pslice = slice(half * 64, half * 64 + 64)
                                S.op('pool', lambda e: e.tensor_copy(out=Vs[pslice], in_=BD["tbV"][pslice, :, half * 64:half * 64 + 64]),
                                     reads=[P_ + "tbV"], writes=[P_ + "Vs"])
                            diag('pool', BD["T2"], P_ + "T2", Pst[0], [P_ + "Pst0"])
                            diag('pool', BD["T3"], P_ + "T3", PG[0][:, :, 0:64], [P_ + "PG0"])
                            sets = [("T2", "T3"), ("T4", "T8")]
                            for lv in range(6):
                                cur, nxt = lv % 2, (lv + 1) % 2
                                bP, bPT = sets[cur]
                                nP, nPT = sets[nxt]
                                kPG, kPGn = P_ + f"PG{cur}", P_ + f"PG{nxt}"
                                if lv < 5:
                                    for hp in range(4):
                                        S.op('pe', lambda e: e.matmul(pb[1][:, hp * 64:(hp + 1) * 64], lhsT=BD[bPT][:, hp, :], rhs=Pst[cur][:, hp, :], start=True, stop=True),
                                             reads=[P_ + bPT, P_ + f"Pst{cur}"], writes=pk(1, 0), inc=(hp == 3))
                                    for hp in range(4):
                                        S.op('pe', lambda e: e.matmul(pb[0][:, hp * 128:(hp + 1) * 128], lhsT=BD[bP][:, hp, :], rhs=PG[cur][:, hp, :], start=True, stop=True),
                                             reads=[P_ + bP, kPG], writes=pk(0), inc=(hp == 3))
                                else:
                                    for hp in range(4):
                                        S.op('pe', lambda e: e.matmul(pb[0][:, hp * 128 + 64:(hp + 1) * 128], lhsT=BD[bP][:, hp, :], rhs=PG[cur][:, hp, 64:128], start=True, stop=True),
                                             reads=[P_ + bP, kPG], writes=pk(0), inc=(hp == 3))
                                yield
                                psv = pb[0][:].rearrange("p (c t) -> p c t", t=128)
                                S.op('dve', lambda e: e.tensor_tensor(out=PG[nxt][:, :, 64:128], in0=PG[cur][:, :, 64:128], in1=psv[:, :, 64:128], op=ALU.add),
                                     reads=pk(0) + [kPG], writes=[kPGn])
                                if lv < 5:
                                    S.op('act', lambda e: e.activation(out=PG[nxt][:, :, 0:64], in_=psv[:, :, 0:64], func=AF.Identity), reads=pk(0), writes=[kPGn])
                                    S.op('dve', lambda e: e.tensor_copy(out=Pst[nxt][:], in_=pb[1][:, 0:256].rearrange("p (c t) -> p c t", t=64)),
                                         reads=pk(1, 0), writes=[P_ + f"Pst{nxt}"])
                                    diag('pool', BD[nP], P_ + nP, Pst[nxt], [P_ + f"Pst{nxt}"])
                                    if lv < 4:
                                        diag('pool', BD[nPT], P_ + nPT, PG[nxt][:, :, 0:64], [kPGn])
                            diag('pool', BD["T8"], P_ + "T8", PG[0][:, :, 64:128], [P_ + "PG0"])
                            for hp in range(4):
                                S.op('pe', lambda e: e.matmul(pb[2][:, 256 + hp * 64:256 + (hp + 1) * 64], lhsT=BD["bdA"][:, hp, :], rhs=STs[:, hp, :], start=True, stop=False),
                                     reads=[P_ + "bdA", P_ + "STs"], writes=pk(2, 1), inc=False)
                                S.op('pe', lambda e: e.matmul(pb[2][:, 256 + hp * 64:256 + (hp + 1) * 64], lhsT=BD["bdMak"][:, hp, :], rhs=Vs[:, hp, :], start=False, stop=True),
                                     reads=[P_ + "bdMak", P_ + "Vs"], writes=pk(2, 1), inc=(hp == 3))
                            yield
                            S.op('act', lambda e: e.activation(out=Xs[:], in_=pb[2][:, 256:512].rearrange("p (c t) -> p c t", t=64), func=AF.Identity),
                                 reads=pk(2, 1), writes=[P_ + "Xs"])
                            for hp in range(4):
                                S.op('pe', lambda e: e.matmul(pb[3][:, hp * 64:(hp + 1) * 64], lhsT=BD["T8"][:, hp, :], rhs=Xs[:, hp, :], start=True, stop=True),
                                     reads=[P_ + "T8", P_ + "Xs"], writes=pk(3, 0), inc=(hp == 3))
                            yield
                            S.op('dve', lambda e: e.tensor_copy(out=Us[:], in_=pb[3][:, 0:256].rearrange("p (c t) -> p c t", t=64)), reads=pk(3, 0), writes=[P_ + "Us"])
                            diag('act', BD["bdU"], P_ + "bdU", pb[3][:, 0:256].rearrange("p (c t) -> p c t", t=64), pk(3, 0))
                            for hp in range(4):
                                S.op('pe', lambda e: e.matmul(pb[3][:, 256 + hp * 64:256 + (hp + 1) * 64], lhsT=BD["bdST"][:, hp, :], rhs=AR[b][:, hp, 1, :], start=True, stop=False),
                                     reads=[P_ + "bdST", kin], writes=pk(3, 1), inc=False)
                                S.op('pe', lambda e: e.matmul(pb[3][:, 256 + hp * 64:256 + (hp + 1) * 64], lhsT=BD["bdU"][:, hp, :], rhs=ARm[:, hp, 64:128], start=False, stop=False),
                                     reads=[P_ + "bdU", P_ + "ARm"], writes=pk(3, 1), inc=False)
                                S.op('pe', lambda e: e.matmul(pb[3][:, 256 + hp * 64:256 + (hp + 1) * 64], lhsT=BD["tbV"][:, hp, :], rhs=AKm[:, hp, 64:128], start=False, stop=True),
                                     reads=[P_ + "tbV", P_ + "AKm"], writes=pk(3, 1), inc=(hp == 3))
                            for hp in range(4):
                                S.op('pe', lambda e: e.matmul(pb[1][:, 256 + hp * 64:256 + (hp + 1) * 64], lhsT=BD["tbB"][:, hp, :], rhs=Us[:, hp, :], start=True, stop=False),
                                     reads=[P_ + "tbB", P_ + "Us"], writes=pk(1, 1), inc=False)
                                S.op('pe', lambda e: e.matmul(pb[1][:, 256 + hp * 64:256 + (hp + 1) * 64], lhsT=BD["tbK"][:, hp, :], rhs=Vs[:, hp, :], start=False, stop=True),
                                     reads=[P_ + "tbK", P_ + "Vs"], writes=pk(1, 1), inc=(hp == 3))
                            yield
                            S.op('act', lambda e: e.activation(out=Yb[:], in_=pb[3][:, 256:512].rearrange("p (c t) -> p c t", t=64), func=AF.Identity),
                                 reads=pk(3, 1), writes=[P_ + "Yb"])
                            S.dma('sp', YD[d][r0:r0 + 512, g * 64:g * 64 + 64].rearrange("(c p) n -> p c n", p=128), Yb[:], reads=[P_ + "Yb"], writes=['YD'])
                            S.op('dve', lambda e: e.tensor_tensor(out=tmpS[:], in0=pb[1][:, 256:512].rearrange("p (c t) -> p c t", t=64), in1=STs[:], op=ALU.add),
                                 reads=pk(1, 1) + [P_ + "STs"], writes=[P_ + "tmpS"])
                            S.op('dve', lambda e: e.tensor_tensor(out=STs[:], in0=tmpS[:], in1=WCall[:, :, g:g + 1].to_broadcast([128, 4, 64]), op=ALU.mult),
                                 reads=[P_ + "tmpS", P_ + "WCall"], writes=[P_ + "STs"])
                            diag('pool', BD["bdST"], P_ + "bdST", STs, [P_ + "STs"])
                            yield

                    gens = [stream(0), stream(1)]
                    alive = [True, True]
                    while any(alive):
                        for q in range(2):
                            if alive[q]:
                                try:
                                    next(gens[q])
                                except StopIteration:
                                    alive[q] = False
                S.barrier()
            if os.environ.get('RSTOP') == '3':
                return
            with contextlib.ExitStack() as st:
                wo = sb("r_wo", [128, 8, 1024], BF16, st)
                S.dma('pool', wo[:], rwkv_wo[j].rearrange("(k p) n -> p k n", p=128), writes=['r_wo'])
                y0 = sb("r_y0", [128, 8, 512], F32, st); y1 = sb("r_y1", [128, 8, 512], F32, st)
                bo = sb("r_bo", [128, 8, 512], F32, st); gg = sb("r_gg", [128, 8, 512], F32, st)
                xt = sb("r_xt", [128, 8, 512], F32, st)
                ob = sb("r_ob", [128, 8, 512], BF16, st)
                yc = sb("r_yc", [128, 512], F32, st); y2 = sb("r_y2", [128, 512], F32, st); sd = sb("r_sd", [128, 512], F32, st)
                tiles = TILES[1:] if last else TILES
                for ti, (t0, N, mc) in enumerate(tiles):
                    for (dst, srcd, kk_) in [(y0, YD[0], 'y0'), (y1, YD[1], 'y1'), (bo, BON, 'bo'), (gg, GG, 'gg'), (xt, XS, 'r_xt')]:
                        S.dma('sp', dst[:, :, :N], srcd[:, t0:t0 + N].rearrange("(k p) n -> p k n", p=128), writes=[kk_])
                    for c in range(8):
                        pa, pv = c % 2, 2 + c % 2
                        S.op('dve', lambda e: e.tensor_tensor(out=y0[:, c, :N], in0=y0[:, c, :N], in1=y1[:, c, :N], op=ALU.add), reads=['y0', 'y1'], writes=['y0'])
                        S.op('pe', lambda e: e.matmul(ps[pa][:, :N], lhsT=blockones, rhs=y0[:, c, :N], start=True, stop=True), reads=['y0', 'c32'], writes=[('ps', pa)])
                        S.op('dve', lambda e: e.scalar_tensor_tensor(out=yc[:, :N], in0=ps[pa][:, :N], scalar=-1.0 / 64, in1=y0[:, c, :N], op0=ALU.mult, op1=ALU.add),
                             reads=[('ps', pa), 'y0'], writes=['yc'])
                        S.op('pool', lambda e: e.tensor_tensor(out=y2[:, :N], in0=yc[:, :N], in1=yc[:, :N], op=ALU.mult), reads=['yc'], writes=['y2'])
                        S.op('pe', lambda e: e.matmul(ps[pv][:, :N], lhsT=blockones, rhs=y2[:, :N], start=True, stop=True), reads=['y2', 'c32'], writes=[('ps', pv)])
                        S.op('act', lambda e: e.activation(out=sd[:, :N], in_=ps[pv][:, :N], func=AF.Sqrt, bias=epsT[:, 5:6], scale=1.0 / 64),
                             reads=[('ps', pv), 'epsT'], writes=['sd'])
                        S.op('dve', lambda e: e.reciprocal(out=sd[:, :N], in_=sd[:, :N]), reads=['sd'], writes=['sd'])
                        S.op('pool', lambda e: e.tensor_tensor(out=yc[:, :N], in0=yc[:, :N], in1=sd[:, :N], op=ALU.mult), reads=['yc', 'sd'], writes=['yc'])
                        S.op('dve', lambda e: e.tensor_scalar(out=yc[:, :N], in0=yc[:, :N], scalar1=V(f"lnw{j}", c, c + 1), scalar2=V(f"lnb{j}", c, c + 1),
                                                              op0=ALU.mult, op1=ALU.add), reads=['yc', 'vecs'], writes=['yc'])
                        S.op('pool', lambda e: e.tensor_tensor(out=yc[:, :N], in0=yc[:, :N], in1=bo[:, c, :N], op=ALU.add), reads=['yc', 'bo'], writes=['yc'])
                        S.op('pool', lambda e: e.tensor_tensor(out=ob[:, c, :N], in0=yc[:, :N], in1=gg[:, c, :N], op=ALU.mult), reads=['yc', 'gg'], writes=['ob'])
                    for oc in range(8):
                        pb_ = 4 + oc % 4
                        for kc in range(8):
                            S.op('pe', lambda e: e.matmul(ps[pb_][:, :N], lhsT=wo[:, kc, oc * 128:(oc + 1) * 128], rhs=ob[:, kc, :N], start=(kc == 0), stop=(kc == 7)),
                                 reads=['r_wo', 'ob'], writes=[('ps', pb_)], inc=(kc == 7))
                        S.op('dve', lambda e: e.scalar_tensor_tensor(out=xt[:, oc, :N], in0=ps[pb_][:, :N], scalar=modt[:, i, 16 + oc, mc:mc + 1],
                                                                     in1=xt[:, oc, :N], op0=ALU.mult, op1=ALU.add),
                             reads=[('ps', pb_), 'r_xt', 'modt'], writes=['r_xt'])
                    S.dma('sp', XS[:, t0:t0 + N].rearrange("(k p) n -> p k n", p=128), xt[:, :, :N], reads=['r_xt'], writes=['XS'])
            S.barrier()

        def phase_ffn(i, last):
            moe = (i % 2 == 1)
            k = i // 2
            E = NE if moe else 1
            F = DFE if moe else DFF
            nchunk = F // 128
            blocks = [(c0, min(4, nchunk - c0)) for c0 in range(0, nchunk, 4)]
            tiles = TILES[1:] if last else TILES
            groups = [tiles[0:len(tiles) - 6], tiles[-6:-3], tiles[-3:]]
            for gi, grp in enumerate(groups):
                Sg = sum(t[1] for t in grp)
                offs = [sum(t[1] for t in grp[:a]) for a in range(len(grp))]
                with contextlib.ExitStack() as st:
                    hb = sb("f_hb", [128, 8, 1536], BF16, st)
                    yacc = sb("f_yacc", [128, 8, 1536], F32, st)
                    GT = sb("f_GT", [8, 1536], F32, st)
                    gbc = sb("f_gbc", [128, 1536], F32, st)
                    with contextlib.ExitStack() as st2:
                        xt = [sb(f"f_xt{b}", [128, 8, 512], F32, st2) for b in range(2)]
                        sq = sb("f_sq", [128, 8, 512], F32, st2)
                        rs = sb("f_rs", [128, 512], F32, st2)
                        if moe:
                            rt = sb("f_rt", [128, 8, 8], F32, st2)
                            S.dma('sp', rt[:], moe_router[k].rearrange("(k p) n -> p k n", p=128), writes=['rt'])
                            lg = sb("f_lg", [128, 8], F32, st2); m8 = sb("f_m8", [128, 8], F32, st2)
                            sel = sb("f_sel", [128, 8], F32, st2); ex = sb("f_ex", [128, 8], F32, st2)
                            sm = sb("f_sm", [128, 4], F32, st2); G = sb("f_G", [128, 4, 8], F32, st2)
                        for ti, (t0, N, mc) in enumerate(grp):
                            b = ti % 2
                            kx = ('fx', b)
                            o = offs[ti]
                            S.dma('sp', xt[b][:, :, :N], XS[:, t0:t0 + N].rearrange("(k p) n -> p k n", p=128), writes=[kx])
                            ksq = normmod(xt[b][:, :, :N], N, A2[:, i, :, mc], None, None, sq, rs, 7, kx, 'f')
                            S.op('dve', lambda e: e.tensor_tensor(out=sq[:, :, :N], in0=sq[:, :, :N],
                                                                  in1=modt[:, i, 24:32, mc].unsqueeze(2).to_broadcast([128, 8, N]), op=ALU.add),
                                 reads=[ksq, 'modt'], writes=[ksq])
                            S.op('act', lambda e: e.activation(out=hb[:, :, o:o + N], in_=sq[:, :, :N], func=AF.Identity),
                                 reads=[ksq], writes=['hb'])
                            if moe:
                                for blk in range(N // 128):
                                    for kc in range(8):
                                        S.op('pe', lambda e: e.matmul(ps[6][:, 0:8], lhsT=sq[:, kc, blk * 128:(blk + 1) * 128], rhs=rt[:, kc, :],
                                                                      start=(kc == 0), stop=(kc == 7)), reads=[ksq, 'rt'], writes=[('ps', 6)], inc=(kc == 7))
                                    S.op('dve', lambda e: e.tensor_copy(out=lg[:], in_=ps[6][:, 0:8]), reads=[('ps', 6)], writes=['lg'])
                                    S.op('dve', lambda e: e.max(out=m8[:], in_=lg[:]), reads=['lg'], writes=['m8'])
                                    S.op('dve', lambda e: e.tensor_scalar(out=sel[:], in0=lg[:], scalar1=m8[:, 1:2], scalar2=None, op0=ALU.is_ge),
                                         reads=['lg', 'm8'], writes=['sel'])
                                    S.op('dve', lambda e: e.tensor_scalar(out=sm[:, 0:1], in0=m8[:, 0:1], scalar1=-1.0, scalar2=None, op0=ALU.mult),
                                         reads=['m8'], writes=['sm'])
                                    S.op('act', lambda e: e.activation(out=ex[:], in_=lg[:], func=AF.Exp, bias=sm[:, 0:1], scale=1.0),
                                         reads=['lg', 'sm'], writes=['ex'])
                                    S.op('dve', lambda e: e.tensor_tensor(out=ex[:], in0=ex[:], in1=sel[:], op=ALU.mult), reads=['ex', 'sel'], writes=['ex'])
                                    S.op('dve', lambda e: e.tensor_reduce(out=sm[:, 1:2], in_=ex[:], axis=AX.X, op=ALU.add), reads=['ex'], writes=['sm'])
                                    S.op('dve', lambda e: e.reciprocal(out=sm[:, 2:3], in_=sm[:, 1:2]), reads=['sm'], writes=['sm'])
                                    S.op('dve', lambda e: e.tensor_scalar(out=G[:, blk, :], in0=ex[:], scalar1=sm[:, 2:3], scalar2=None, op0=ALU.mult),
                                         reads=['ex', 'sm'], writes=['G'])
                                    S.op('pe', lambda e: e.transpose(out=ps[5][0:8, blk * 128:(blk + 1) * 128], in_=G[:, blk, :], identity=ident),
                                         reads=['G', 'c32'], writes=[('ps', 5)])
                                S.op('act', lambda e: e.activation(out=GT[:, o:o + N], in_=ps[5][0:8, :N], func=AF.Identity),
                                     reads=[('ps', 5)], writes=['GT'])
                    S.barrier()
                    with contextlib.ExitStack() as st2:
                        w1b = [sb(f"f_w1{b}", [128, 8, 512], BF16, st2) for b in range(2)]
                        w3b = [sb(f"f_w3{b}", [128, 8, 512], BF16, st2) for b in range(2)]
                        w2b = [sb(f"f_w2{b}", [128, 4, 1024], BF16, st2) for b in range(2)]
                        gt = [sb(f"f_g{b}", [128, 4, 512], BF16, st2) for b in range(2)]
                        s1 = [sb(f"f_s1{b}", [128, 512], F32, st2) for b in range(2)]
                        s1g = [sb(f"f_s1g{b}", [128, 512], F32, st2) for b in range(2)]
                        nw = 0
                        ng = 0
                        nhc = 0
                        ny = 0
                        first = True
                        for ex_i in range(E):
                            if moe:
                                for ti, (t0, N, mc) in enumerate(grp):
                                    o = offs[ti]
                                    S.op('pe', lambda e: e.matmul(ps[6][:, :N], lhsT=selE[:, ex_i * 128:(ex_i + 1) * 128], rhs=GT[:, o:o + N],
                                                                  start=True, stop=True), reads=['GT', 'c32'], writes=[('ps', 6)])
                                    S.op('act', lambda e: e.activation(out=gbc[:, o:o + N], in_=ps[6][:, :N], func=AF.Identity),
                                         reads=[('ps', 6)], writes=['gbc'])
                            for (c0, nh) in blocks:
                                wb = nw % 2
                                nw += 1
                                kw = ('fw', wb)
                                if moe:
                                    W1, W3, W2 = moe_w1[k, ex_i], moe_w3[k, ex_i], moe_w2[k, ex_i]
                                else:
                                    W1, W3, W2 = ffn_w1[k], ffn_w3[k], ffn_w2[k]
                                S.dma('pool', w1b[wb][:, :, :nh * 128], W1[:, c0 * 128:(c0 + nh) * 128].rearrange("(k p) n -> p k n", p=128), writes=[kw])
                                S.dma('pool', w3b[wb][:, :, :nh * 128], W3[:, c0 * 128:(c0 + nh) * 128].rearrange("(k p) n -> p k n", p=128), writes=[kw])
                                S.dma('pool', w2b[wb][:, :nh, :], W2[c0 * 128:(c0 + nh) * 128, :].rearrange("(k p) n -> p k n", p=128), writes=[kw])
                                for ti, (t0, N, mc) in enumerate(grp):
                                    o = offs[ti]
                                    gb = ng % 2
                                    ng += 1
                                    for hc in range(nh):
                                        pa = (nhc % 2) * 2
                                        sbi = nhc % 2
                                        nhc += 1
                                        for kc in range(8):
                                            S.op('pe', lambda e: e.matmul(ps[pa][:, :N], lhsT=w1b[wb][:, kc, hc * 128:(hc + 1) * 128], rhs=hb[:, kc, o:o + N],
                                                                          start=(kc == 0), stop=(kc == 7)), reads=[kw, 'hb'], writes=[('ps', pa)], inc=(kc == 7))
                                        for kc in range(8):
                                            S.op('pe', lambda e: e.matmul(ps[pa + 1][:, :N], lhsT=w3b[wb][:, kc, hc * 128:(hc + 1) * 128], rhs=hb[:, kc, o:o + N],
                                                                          start=(kc == 0), stop=(kc == 7)), reads=[kw, 'hb'], writes=[('ps', pa + 1)], inc=(kc == 7))
                                        S.op('act', lambda e: e.activation(out=s1[sbi][:, :N], in_=ps[pa][:, :N], func=AF.Silu),
                                             reads=[('ps', pa)], writes=[('s1', sbi)])
                                        src, ksrc = s1[sbi], ('s1', sbi)
                                        if moe:
                                            S.op('pool', lambda e: e.tensor_tensor(out=s1g[sbi][:, :N], in0=s1[sbi][:, :N], in1=gbc[:, o:o + N], op=ALU.mult),
                                                 reads=[('s1', sbi), 'gbc'], writes=[('s1g', sbi)])
                                            src, ksrc = s1g[sbi], ('s1g', sbi)
                                        S.op('dve', lambda e: e.tensor_tensor(out=gt[gb][:, hc, :N], in0=src[:, :N], in1=ps[pa + 1][:, :N], op=ALU.mult),
                                             reads=[ksrc, ('ps', pa + 1)], writes=[('g', gb)])
                                    for oc in range(8):
                                        py = 4 + ny % 2
                                        ny += 1
                                        for hc in range(nh):
                                            S.op('pe', lambda e: e.matmul(ps[py][:, :N], lhsT=w2b[wb][:, hc, oc * 128:(oc + 1) * 128], rhs=gt[gb][:, hc, :N],
                                                                          start=(hc == 0), stop=(hc == nh - 1)), reads=[kw, ('g', gb)], writes=[('ps', py)],
                                                 inc=(hc == nh - 1))
                                        ky = ('y', o, oc)
                                        if first:
                                            S.op('act', lambda e: e.activation(out=yacc[:, oc, o:o + N], in_=ps[py][:, :N], func=AF.Identity),
                                                 reads=[('ps', py)], writes=[ky])
                                        else:
                                            S.op('dve', lambda e: e.tensor_tensor(out=yacc[:, oc, o:o + N], in0=yacc[:, oc, o:o + N], in1=ps[py][:, :N], op=ALU.add),
                                                 reads=[('ps', py), ky], writes=[ky])
                                first = False
                    S.barrier()
                    with contextlib.ExitStack() as st2:
                        xt = [sb(f"f_cx{b}", [128, 8, 512], F32, st2) for b in range(2)]
                        for ti, (t0, N, mc) in enumerate(grp):
                            b = ti % 2
                            o = offs[ti]
                            S.dma('sp', xt[b][:, :, :N], XS[:, t0:t0 + N].rearrange("(k p) n -> p k n", p=128), writes=[('fcx', b)])
                            for oc in range(8):
                                S.op('dve', lambda e: e.scalar_tensor_tensor(
                                    out=xt[b][:, oc, :N], in0=yacc[:, oc, o:o + N], scalar=modt[:, i, 40 + oc, mc:mc + 1],
                                    in1=xt[b][:, oc, :N], op0=ALU.mult, op1=ALU.add), reads=[('fcx', b), 'modt'], writes=[('fcx', b)])
                            if last:
                                S.dma('sp', out_d[:, t0 - CTX:t0 - CTX + N].rearrange("(k p) n -> p k n", p=128), xt[b][:, :, :N],
                                      reads=[('fcx', b)], writes=['out'])
                            else:
                                S.dma('sp', XS[:, t0:t0 + N].rearrange("(k p) n -> p k n", p=128), xt[b][:, :, :N],
                                      reads=[('fcx', b)], writes=['XS'])
                    S.barrier()

        if dbg:
            dbg_d = nc.dram_tensor("dbg", [D, NT], F32, kind="ExternalOutput").ap()
        phase_mod()
        for i in range(depth):
            last = (i == depth - 1)
            if i == 0 and os.environ.get('SKIP0'):
                S.dma('sp', XS, xs_in, writes=['XS'])
                S.barrier()
                continue
            if i % 2 == 0:
                phase_mla(i, xs_in if i == 0 else XS)
            else:
                phase_rwkv(i, last and dbg != 'r')
            if not (dbg == 'r' and i == depth - 1):
                phase_ffn(i, last)
        if dbg:
            S.dma('sp', dbg_d, XS, writes=['dbg'])
        S.barrier()
    return nc, S


def host_consts():
    c = np.zeros((128, C32W), np.float32)
    c[:, 0:128] = 1.0
    c[:, 128:256] = np.eye(128, dtype=np.float32)
    P = np.zeros((64, 64), np.float32)
    for base in (0, 32):
        for f in range(16):
            P[base + 16 + f, base + f] = -1.0
            P[base + f, base + 16 + f] = 1.0
    c[0:64, 256:320] = P
    for e in range(8):
        c[e, 384 + e * 128:384 + (e + 1) * 128] = 1.0
    o_ = 384 + 1024
    p = np.arange(128)
    c[:, o_:o_ + 128] = (p[:, None] // 64 == p[None, :] // 64)
    tt = np.arange(64)
    c[:, o_ + 128:o_ + 192] = (p[:, None] % 64 == tt[None, :])
    sidx = (p % 64)[:, None]
    c[:, o_ + 192:o_ + 256] = (sidx < tt[None, :])
    c[:, o_ + 256:o_ + 320] = (sidx <= tt[None, :])
    c[:, o_ + 320:o_ + 384] = (tt[None, :] < sidx)
    c[:, o_ + 384:o_ + 448] = (sidx > tt[None, :])
    c[:, o_ + 448:o_ + 512] = (sidx >= tt[None, :])
    c[:, o_ + 512:o_ + 576] = (tt[None, :] > sidx)
    cm = np.ones(512, np.float32); cm[0::64] = 0.0
    c[:, o_ + 576:o_ + 1088] = cm[None, :]
    rows = TL // 64
    row_ids = np.repeat(np.arange(rows, dtype=np.float32), 64)
    col_ids = np.tile(np.arange(64, dtype=np.float32), rows)
    inv_freq = (1.0 / (np.float32(10000.0) ** (np.arange(16, dtype=np.float32) / np.float32(16)))).astype(np.float32)
    ang_r = (row_ids[:, None] * inv_freq[None, :]).astype(np.float32)
    ang_c = (col_ids[:, None] * inv_freq[None, :]).astype(np.float32)
    cosT = np.zeros((64, TL), np.float32)
    sinT = np.zeros((64, TL), np.float32)
    for base, ang in ((0, ang_r), (32, ang_c)):
        for half in (0, 16):
            cosT[base + half:base + half + 16] = np.cos(ang).T
            sinT[base + half:base + half + 16] = np.sin(ang).T
    return c, cosT, sinT


def host_vecs(inp, depth):
    voff, NV = vec_layout(depth)
    vecs = np.zeros((128, NV), np.float32)

    def put(name, arr):
        o, c = voff[name]
        assert arr.shape == (128, c), (name, arr.shape, c)
        vecs[:, o:o + c] = arr
    for i in range(depth):
        put(f"adab{i}", fm(inp["ada_b"][i]))
        put(f"n1g{i}", fm(inp["norm1_g"][i]))
        put(f"n2g{i}", fm(inp["norm2_g"][i]))
        j = i // 2
        if i % 2 == 0:
            put(f"qan{j}", fm(inp["mla_qa_norm"][j]))
            put(f"kvan{j}", fm(inp["mla_kva_norm"][j]))
            put(f"qnn{j}", fm(inp["mla_q_norm"][j][:128]))
            put(f"qnr{j}", fm(inp["mla_q_norm"][j][128:]))
            put(f"knn{j}", fm(inp["mla_k_norm"][j][:128]))
            put(f"knr{j}", fm(inp["mla_k_norm"][j][128:]))
        else:
            put(f"mix{j}", fm(inp["rwkv_mix"][j]))
            put(f"w0{j}", fm(inp["rwkv_w0"][j]))
            put(f"a0{j}", fm(inp["rwkv_a0"][j]))
            put(f"kk{j}", fm(inp["rwkv_k_k"][j]))
            put(f"ka{j}", fm(inp["rwkv_k_a"][j]))
            put(f"rk{j}", fm(inp["rwkv_r_k"][j]))
            put(f"lnw{j}", fm(inp["rwkv_ln_w"][j]))
            put(f"lnb{j}", fm(inp["rwkv_ln_b"][j]))
            if j >= 1:
                put(f"v0{j}", fm(inp["rwkv_v0"][j - 1]))
    return vecs


WNAMES = ["ada_w", "mla_wqa", "mla_wqb", "mla_wkva", "mla_wkvb", "mla_wo", "ffn_w1", "ffn_w3", "ffn_w2",
          "moe_router", "moe_w1", "moe_w3", "moe_w2",
          "rwkv_wr", "rwkv_wk", "rwkv_wv", "rwkv_wo", "rwkv_w1", "rwkv_w2", "rwkv_a1", "rwkv_a2", "rwkv_g1", "rwkv_g2", "rwkv_v1", "rwkv_v2"]


def make_in_maps(inp, depth, cores):
    c32, cosT, sinT = host_consts()
    vecs = host_vecs(inp, depth)
    shared = {n: np.ascontiguousarray(inp[n], dtype=np.float32) for n in WNAMES}
    maps = []
    for b in cores:
        xs = np.ascontiguousarray(np.concatenate([inp["ctx"][b], inp["x"][b]], axis=0).T.astype(np.float32))
        cv = np.zeros((128, 16), np.float32)
        cv[:, 0::2] = fm(inp["c"][b])
        cv[:, 1::2] = fm(inp["c_ctx"])
        m = dict(shared)
        m.update(xs=xs, cvec=cv, vecs=vecs, c32=c32, ropec=cosT, ropes=sinT)
        maps.append(m)
    return maps


def kernel(**inp):
    depth = 4
    nc, S = build(depth)
    maps = make_in_maps(inp, depth, list(range(8)))
    res = run_bass_kernel_spmd(nc, maps, core_ids=list(range(8)))
    out = np.stack([np.ascontiguousarray(r["out"].T) for r in res.results], axis=0)
    return out.astype(np.float32)
```

```python
import contextlib
import os
import numpy as np
import concourse.bass as bass
import concourse.mybir as mybir
from concourse.bass_utils import run_bass_kernel_spmd

F32 = mybir.dt.float32
BF16 = mybir.dt.bfloat16
ALU = mybir.AluOpType
AF = mybir.ActivationFunctionType
AX = mybir.AxisListType
NS = 8

D = 1024
KC = 8
CTX = 256
TL = 4096
NT = CTX + TL
EPS = 1e-6
NH = 8
SM_SCALE = 192 ** -0.5
DFF = 2816
DFE = 3584
NE = 8
C32W = 128 * 3 + 8 * 128 + 128 + 64 + 128 + 64 + 128 + 64 + 512
TILES = [(0, 256, 1)] + [(256 + 512 * i, 512, 0) for i in range(8)]


class Sched:
    def __init__(self, nc):
        self.nc = nc
        self.engs = {'pe': nc.tensor, 'dve': nc.vector, 'act': nc.scalar,
                     'pool': nc.gpsimd, 'sp': nc.sync}
        self.sem = {e: nc.alloc_semaphore(name=f"sem_{e}") for e in ['pe', 'dve', 'act', 'pool']}
        self.cnt = {e: 0 for e in self.sem}
        self.dq = {}
        for q, e in [('sp', 'sp'), ('pool', 'pool')]:
            self.dq[q] = dict(eng=e, n=0,
                              sems=[nc.alloc_semaphore(name=f"dsem_{q}{i}") for i in range(NS)])
        self.waited = {}
        self.lastw = {}
        self.readers = {}
        self.nins = 0
        self.mute = False

    def _sid_val(self, tok):
        if tok[0] == 'c':
            return ('c', tok[1]), self.sem[tok[1]], tok[2]
        q = self.dq[tok[1]]
        n = tok[2]
        return ('d', tok[1], n % NS), q['sems'][n % NS], 16 * (n // NS + 1)

    def _wait(self, eng, tok):
        if tok[0] == 'c' and tok[1] == eng and eng == 'pe':
            return
        sid, sem, val = self._sid_val(tok)
        if tok[0] == 'c':
            assert val <= self.cnt[tok[1]], f"wait on unsignalled instr {tok}"
        if self.waited.get((eng, sid), 0) >= val:
            return
        self.engs[eng].wait_ge(sem, val)
        self.waited[(eng, sid)] = val
        self.nins += 1

    def _deps(self, reads, writes):
        deps = set()
        for k in reads:
            if k in self.lastw:
                deps.add(self.lastw[k])
        for k in writes:
            if k in self.lastw:
                deps.add(self.lastw[k])
            for t in self.readers.get(k, {}).values():
                deps.add(t)
        return deps

    def _record(self, tok, reads, writes):
        sid, _, val = self._sid_val(tok)
        for k in reads:
            r = self.readers.setdefault(k, {})
            old = r.get(sid)
            if old is None or self._sid_val(old)[2] < val:
                r[sid] = tok
        for k in writes:
            self.lastw[k] = tok
            self.readers[k] = {}

    def op(self, eng, fn, reads=(), writes=(), inc=True):
        if self.mute:
            return None
        if eng != 'pe':
            psr = [k for k in reads if isinstance(k, tuple) and k[0] in ('ps', 'psb', 'sps')]
            if psr:
                reads = [k for k in reads if k not in psr]
                writes = list(writes) + psr
        for t in self._deps(reads, writes):
            self._wait(eng, t)
        ins = fn(self.engs[eng])
        self.nins += 1
        if inc:
            self.cnt[eng] += 1
            ins.then_inc(self.sem[eng], 1)
            tok = ('c', eng, self.cnt[eng])
        else:
            tok = ('c', eng, self.cnt[eng] + 1)
        self._record(tok, reads, writes)
        return ins

    def dma(self, q, out, in_, reads=(), writes=(), **kw):
        if self.mute:
            return None
        Q = self.dq[q]
        eng = Q['eng']
        n = Q['n']
        for t in self._deps(reads, writes):
            self._wait(eng, t)
        if n >= NS:
            self._wait(eng, ('d', q, n - NS))
        ins = self.engs[eng].dma_start(out=out, in_=in_, **kw)
        ins.then_inc(Q['sems'][n % NS], 16)
        self.nins += 1
        Q['n'] += 1
        self._record(('d', q, n), reads, writes)
        return ins

    def barrier(self):
        toks = []
        for e, c in self.cnt.items():
            if c > 0:
                toks.append(('c', e, c))
        for q, Q in self.dq.items():
            for n in range(max(0, Q['n'] - NS), Q['n']):
                toks.append(('d', q, n))
        for e in ['pe', 'dve', 'act', 'pool', 'sp']:
            for t in toks:
                if t[0] == 'c' and t[1] == e:
                    continue
                self._wait(e, t)
        self.lastw.clear()
        self.readers.clear()


def vec_layout(depth):
    ents = []
    for i in range(depth):
        ents += [(f"adab{i}", 48), (f"n1g{i}", 8), (f"n2g{i}", 8)]
        j = i // 2
        if i % 2 == 0:
            ents += [(f"qan{j}", 3), (f"kvan{j}", 2), (f"qnn{j}", 1), (f"qnr{j}", 1), (f"knn{j}", 1), (f"knr{j}", 1)]
        else:
            ents += [(f"mix{j}", 48), (f"w0{j}", 16), (f"a0{j}", 16), (f"kk{j}", 8), (f"ka{j}", 8),
                     (f"rk{j}", 8), (f"lnw{j}", 8), (f"lnb{j}", 8)]
            if j >= 1:
                ents += [(f"v0{j}", 8)]
    off = {}
    o = 0
    for n, c in ents:
        off[n] = (o, c)
        o += c
    return off, o


def fm(v):
    v = np.asarray(v, np.float32).reshape(-1)
    pad = (-len(v)) % 128
    if pad:
        v = np.concatenate([v, np.zeros(pad, np.float32)])
    return np.ascontiguousarray(v.reshape(-1, 128).T)


def build(depth=4, dbg=None):
    nc = bass.Bass("TRN2", target_bir_lowering=False)
    voff, NV = vec_layout(depth)

    def din(name, shape, dt=F32):
        return nc.dram_tensor(name, list(shape), dt, kind="ExternalInput").ap()

    def dscr(name, shape, dt=F32):
        return nc.dram_tensor(name, list(shape), dt, kind="Internal").ap()

    xs_in = din("xs", [D, NT])
    cvec_d = din("cvec", [128, 16])
    vecs_d = din("vecs", [128, NV])
    c32_d = din("c32", [128, C32W])
    ropec_d = din("ropec", [64, TL])
    ropes_d = din("ropes", [64, TL])
    ada_w = din("ada_w", [4, D, 6 * D])
    mla_wqa = din("mla_wqa", [2, D, 384]); mla_wqb = din("mla_wqb", [2, 384, 1536])
    mla_wkva = din("mla_wkva", [2, D, 320]); mla_wkvb = din("mla_wkvb", [2, 256, 2048])
    mla_wo = din("mla_wo", [2, D, D])
    ffn_w1 = din("ffn_w1", [2, D, DFF]); ffn_w3 = din("ffn_w3", [2, D, DFF]); ffn_w2 = din("ffn_w2", [2, DFF, D])
    moe_router = din("moe_router", [2, D, NE])
    moe_w1 = din("moe_w1", [2, NE, D, DFE]); moe_w3 = din("moe_w3", [2, NE, D, DFE]); moe_w2 = din("moe_w2", [2, NE, DFE, D])
    rwkv_wr = din("rwkv_wr", [2, D, D]); rwkv_wk = din("rwkv_wk", [2, D, D]); rwkv_wv = din("rwkv_wv", [2, D, D]); rwkv_wo = din("rwkv_wo", [2, D, D])
    rwkv_w1 = din("rwkv_w1", [2, 2, D, 64]); rwkv_w2 = din("rwkv_w2", [2, 2, 64, D])
    rwkv_a1 = din("rwkv_a1", [2, 2, D, 64]); rwkv_a2 = din("rwkv_a2", [2, 2, 64, D])
    rwkv_g1 = din("rwkv_g1", [2, D, 160]); rwkv_g2 = din("rwkv_g2", [2, 160, D])
    rwkv_v1 = din("rwkv_v1", [1, D, 32]); rwkv_v2 = din("rwkv_v2", [1, 32, D])
    out_d = nc.dram_tensor("out", [D, TL], F32, kind="ExternalOutput").ap()
    HS = dscr("HS", [D, 258 + 4098])
    ATd = [dscr(f"ATd{d}", [D, NT]) for d in range(2)]; BTd = [dscr(f"BTd{d}", [D, NT]) for d in range(2)]
    KTd = [dscr(f"KTd{d}", [D, NT]) for d in range(2)]; RTd = [dscr(f"RTd{d}", [D, NT]) for d in range(2)]
    WCd = [dscr(f"WCd{d}", [D, NT // 64]) for d in range(2)]
    VT = [dscr(f"VT{d}", [D, NT]) for d in range(2)]
    YD = [dscr(f"YD{d}", [D, NT]) for d in range(2)]
    BON = dscr("BON", [D, NT]); GG = dscr("GG", [D, NT])

    XS = dscr("XS", [D, NT])
    QN = dscr("QN", [NH, 128, NT], BF16); QR = dscr("QR", [NH, 64, NT], BF16)
    KN = dscr("KN", [NH, 128, NT], BF16); KR = dscr("KR", [NH, 64, NT], BF16)
    VV = dscr("VV", [NT, NH, 128], BF16)
    AO = dscr("AO", [D, NT], BF16)

    S = Sched(nc)
    es = contextlib.ExitStack()

    uniq = [0]

    def sb(name, shape, dt=F32, stack=None):
        uniq[0] += 1
        return (stack or es).enter_context(nc.sbuf_tensor(f"s{uniq[0]}_{name}", list(shape), dt))

    with es:
        ps = [es.enter_context(nc.psum_tensor(f"ps{i}", [128, 512], F32)) for i in range(8)]
        vecs = sb("vecs", [128, NV])
        c32 = sb("c32", [128, C32W])
        ones_bf = sb("ones_bf", [128, 128], BF16)
        cvec = sb("cvec", [128, 16])
        modt = sb("modt", [128, depth, 48, 2])
        A1 = sb("A1", [128, depth, 8, 2]); A2 = sb("A2", [128, depth, 8, 2])
        S.dma('sp', vecs[:], vecs_d, writes=['vecs'])
        S.dma('sp', c32[:], c32_d, writes=['c32'])
        S.dma('sp', cvec[:], cvec_d, writes=['cvec'])
        EPSI = {1024: 0, 384: 1, 256: 2, 192: 3, 64: 4}
        epsT = sb("epsT", [128, 8])
        for nf, ci in EPSI.items():
            S.op('pool', lambda e: e.memset(epsT[:, ci:ci + 1], float(nf * EPS)), writes=['epsT'])
        ones32 = c32[:, 0:128]
        ident = c32[:, 128:256]
        rotP = c32[0:64, 256:320]
        selE = c32[0:8, 384:384 + 8 * 128]
        o_ = 384 + 1024
        blockones = c32[:, o_:o_ + 128]
        SI = c32[:, o_ + 128:o_ + 192]
        maskAR_f = c32[:, o_ + 192:o_ + 320]
        maskN_f = c32[:, o_ + 320:o_ + 384]
        maskAR_r = c32[:, o_ + 384:o_ + 512]
        maskN_r = c32[:, o_ + 512:o_ + 576]
        cmask = c32[:, o_ + 576:o_ + 1088]
        S.op('pool', lambda e: e.memset(epsT[:, 5:6], 64e-5), writes=['epsT'])
        S.op('dve', lambda e: e.tensor_copy(out=ones_bf[:], in_=ones32), reads=['c32'], writes=['ones_bf'])

        def V(name, k0=0, k1=None):
            o, c = voff[name]
            k1 = c if k1 is None else k1
            return vecs[:, o + k0:o + k1]

        def phase_mod():
            with contextlib.ExitStack() as st:
                wb = [sb(f"adaw{i}", [128, 8, 1024], F32, st) for i in range(2)]
                sc = sb("sc", [128, 16], F32, st)
                S.op('act', lambda e: e.activation(out=sc[:], in_=cvec[:], func=AF.Silu), reads=['cvec'], writes=['sc'])
                n = 0
                for i in range(depth):
                    for j in range(6):
                        w = wb[n % 2]
                        S.dma('sp', w[:], ada_w[i, :, j * 1024:(j + 1) * 1024].rearrange("(k p) n -> p k n", p=128),
                              writes=[('adaw', n % 2)])
                        for oc in range(8):
                            col = (j * 8 + oc) * 2
                            for kc in range(8):
                                S.op('pe', lambda e: e.matmul(ps[0][:, col:col + 2], lhsT=w[:, kc, oc * 128:(oc + 1) * 128],
                                                              rhs=sc[:, 2 * kc:2 * kc + 2], start=(kc == 0), stop=(kc == 7)),
                                     reads=[('adaw', n % 2), 'sc'], writes=['psmod'], inc=(kc == 7 and oc == 7))
                        n += 1
                    S.op('dve', lambda e: e.tensor_tensor(
                        out=modt[:, i, :, :], in0=ps[0][:, 0:96].rearrange("p (a b) -> p a b", b=2),
                        in1=V(f"adab{i}").unsqueeze(2).to_broadcast([128, 48, 2]), op=ALU.add),
                        reads=['psmod', 'vecs'], writes=['modt'])
                    for (At, gname, jj) in [(A1, f"n1g{i}", 1), (A2, f"n2g{i}", 4)]:
                        S.op('dve', lambda e: e.scalar_tensor_tensor(
                            out=At[:, i, :, :], in0=modt[:, i, jj * 8:(jj + 1) * 8, :], scalar=1.0,
                            in1=V(gname).unsqueeze(2).to_broadcast([128, 8, 2]), op0=ALU.add, op1=ALU.mult),
                            reads=['modt', 'vecs'], writes=['A'])
                        S.op('dve', lambda e: e.tensor_scalar(out=At[:, i, :, :], in0=At[:, i, :, :], scalar1=float(np.sqrt(D)),
                                                              scalar2=None, op0=ALU.mult), reads=['A'], writes=['A'])
            S.barrier()

        def normmod(x32, N, Acol, Scol, outap, sq, rs, psb, kx, tag):
            ksq, krs, kps = ('sq', tag), 'rs', ('ps', psb)
            S.op('pool', lambda e: e.tensor_tensor(out=sq[:, :, :N], in0=x32, in1=x32, op=ALU.mult), reads=[kx], writes=[ksq])
            for k in range(8):
                S.op('pe', lambda e: e.matmul(ps[psb][:, :N], lhsT=ones32, rhs=sq[:, k, :N], start=(k == 0), stop=(k == 7)),
                     reads=[ksq, 'c32'], writes=[kps], inc=(k == 7))
            S.op('act', lambda e: e.activation(out=rs[:, :N], in_=ps[psb][:, :N], func=AF.Sqrt, bias=epsT[:, EPSI[D]:EPSI[D] + 1], scale=1.0),
                 reads=[kps, 'epsT'], writes=[krs])
            S.op('dve', lambda e: e.reciprocal(out=rs[:, :N], in_=rs[:, :N]), reads=[krs], writes=[krs])
            S.op('dve', lambda e: e.tensor_tensor(out=sq[:, :, :N], in0=x32, in1=rs[:, :N].unsqueeze(1).to_broadcast([128, 8, N]),
                                                  op=ALU.mult), reads=[kx, krs], writes=[ksq])
            S.op('pool', lambda e: e.tensor_tensor(out=sq[:, :, :N], in0=sq[:, :, :N], in1=Acol.unsqueeze(2).to_broadcast([128, 8, N]),
                                                   op=ALU.mult), reads=[ksq, 'A'], writes=[ksq])
            return ksq

        def rstd_from_ps(psb, N, nfeat, rs, krs, P=128):
            S.op('act', lambda e: e.activation(out=rs[:P, :N], in_=ps[psb][:P, :N], func=AF.Sqrt, bias=epsT[:P, EPSI[nfeat]:EPSI[nfeat] + 1], scale=1.0),
                 reads=[('ps', psb), 'epsT'], writes=[krs])
            S.op('dve', lambda e: e.reciprocal(out=rs[:P, :N], in_=rs[:P, :N]), reads=[krs], writes=[krs])

        def phase_mla(i, XSin):
            j = i // 2
            with contextlib.ExitStack() as st:
                wqa = sb("wqa", [128, 8, 384], BF16, st); wqb = sb("wqb", [128, 3, 1536], BF16, st)
                wkva = sb("wkva", [128, 8, 320], BF16, st); wkvb = sb("wkvb", [128, 2, 2048], BF16, st)
                S.dma('pool', wqa[:], mla_wqa[j].rearrange("(k p) n -> p k n", p=128), writes=['wqa'])
                S.dma('pool', wqb[:], mla_wqb[j].rearrange("(k p) n -> p k n", p=128), writes=['wqb'])
                S.dma('pool', wkva[:], mla_wkva[j].rearrange("(k p) n -> p k n", p=128), writes=['wkva'])
                S.dma('pool', wkvb[:], mla_wkvb[j].rearrange("(k p) n -> p k n", p=128), writes=['wkvb'])
                xt = [sb("xt0", [128, 8, 512], F32, st)] * 2
                sq = sb("sq", [128, 8, 512], F32, st)
                rs = sb("rs", [128, 512], F32, st)
                hb = sb("hb", [128, 8, 512], BF16, st)
                cq = sb("cq", [128, 3, 512], F32, st); cq2 = sb("cq2", [128, 3, 512], F32, st)
                cqn = sb("cqn", [128, 3, 512], BF16, st)
                ckv = sb("ckv", [128, 2, 512], F32, st); ckv2 = sb("ckv2", [128, 2, 512], F32, st)
                ckvn = sb("ckvn", [128, 2, 512], BF16, st)
                kr = sb("kr", [64, 512], F32, st); kr2 = sb("kr2", [64, 512], F32, st)
                krP = sb("krP", [64, 512], F32, st); krr = sb("krr", [64, 512], F32, st)
                qh = sb("qh", [128, 512], F32, st); qh2 = sb("qh2", [128, 512], F32, st)
                qr = sb("qr", [64, 512], F32, st); qr2 = sb("qr2", [64, 512], F32, st)
                qrP = sb("qrP", [64, 512], F32, st)
                rs2 = sb("rs2", [128, 512], F32, st)
                cosT = sb("cosT", [64, 512], F32, st); sinT = sb("sinT", [64, 512], F32, st)
                qn_o = sb("qn_o", [128, NH, 512], BF16, st); qr_o = sb("qr_o", [64, NH, 512], BF16, st)
                kn_o = sb("kn_o", [128, NH, 512], BF16, st); kr_o = sb("kr_o", [64, NH, 512], BF16, st)
                v_o = sb("v_o", [128, 4, NH, 128], BF16, st)
                sv = sb("sv", [128, 16], F32, st)
                S.op('dve', lambda e: e.tensor_scalar(out=sv[:, 0:3], in0=V(f"qan{j}"), scalar1=float(np.sqrt(384)), scalar2=None, op0=ALU.mult),
                     reads=['vecs'], writes=['sv'])
                S.op('dve', lambda e: e.tensor_scalar(out=sv[:, 3:5], in0=V(f"kvan{j}"), scalar1=float(np.sqrt(256)), scalar2=None, op0=ALU.mult),
                     reads=['vecs'], writes=['sv'])
                S.op('dve', lambda e: e.tensor_scalar(out=sv[:, 5:6], in0=V(f"qnn{j}"), scalar1=float(np.sqrt(192) * SM_SCALE), scalar2=None, op0=ALU.mult),
                     reads=['vecs'], writes=['sv'])
                S.op('dve', lambda e: e.tensor_scalar(out=sv[:, 6:7], in0=V(f"qnr{j}"), scalar1=float(np.sqrt(192) * SM_SCALE), scalar2=None, op0=ALU.mult),
                     reads=['vecs'], writes=['sv'])
                S.op('dve', lambda e: e.tensor_scalar(out=sv[:, 7:8], in0=V(f"knn{j}"), scalar1=float(np.sqrt(192)), scalar2=None, op0=ALU.mult),
                     reads=['vecs'], writes=['sv'])
                S.op('dve', lambda e: e.tensor_scalar(out=sv[:, 8:9], in0=V(f"knr{j}"), scalar1=float(np.sqrt(192)), scalar2=None, op0=ALU.mult),
                     reads=['vecs'], writes=['sv'])

                def rope(src, Pbuf, dst, N, ksrc, kdst, psb):
                    S.op('pe', lambda e: e.matmul(ps[psb][:64, :N], lhsT=rotP, rhs=src[:, :N], start=True, stop=True),
                         reads=[ksrc, 'c32'], writes=[('ps', psb)])
                    S.op('dve', lambda e: e.tensor_tensor(out=Pbuf[:, :N], in0=ps[psb][:64, :N], in1=sinT[:, :N], op=ALU.mult),
                         reads=[('ps', psb), 'rope'], writes=[('rp', kdst)])
                    S.op('pool', lambda e: e.tensor_tensor(out=dst[:, :N], in0=src[:, :N], in1=cosT[:, :N], op=ALU.mult),
                         reads=[ksrc, 'rope'], writes=[kdst])
                    S.op('pool', lambda e: e.tensor_tensor(out=dst[:, :N], in0=dst[:, :N], in1=Pbuf[:, :N], op=ALU.add),
                         reads=[kdst, ('rp', kdst)], writes=[kdst])

                for ti, (t0, N, mc) in enumerate(TILES):
                    x32 = xt[ti % 2]
                    kx = ('xt', ti % 2)
                    S.dma('sp', x32[:, :, :N], XSin[:, t0:t0 + N].rearrange("(k p) n -> p k n", p=128), writes=[kx])
                    if mc == 0:
                        S.dma('sp', cosT[:, :N], ropec_d[:, t0 - CTX:t0 - CTX + N], writes=['rope'])
                        S.dma('sp', sinT[:, :N], ropes_d[:, t0 - CTX:t0 - CTX + N], writes=['rope'])
                    ksq = normmod(x32[:, :, :N], N, A1[:, i, :, mc], None, None, sq, rs, 7, kx, 'm')
                    S.op('dve', lambda e: e.tensor_tensor(out=hb[:, :, :N], in0=sq[:, :, :N],
                                                          in1=modt[:, i, 0:8, mc].unsqueeze(2).to_broadcast([128, 8, N]), op=ALU.add),
                         reads=[ksq, 'modt'], writes=['hb'])
                    for c in range(3):
                        pb = c % 2
                        for kc in range(8):
                            S.op('pe', lambda e: e.matmul(ps[pb][:, :N], lhsT=wqa[:, kc, c * 128:(c + 1) * 128], rhs=hb[:, kc, :N],
                                                          start=(kc == 0), stop=(kc == 7)), reads=['hb', 'wqa'], writes=[('ps', pb)], inc=(kc == 7))
                        S.op('act', lambda e: e.activation(out=cq[:, c, :N], in_=ps[pb][:, :N], func=AF.Identity),
                             reads=[('ps', pb)], writes=[('cq', c)])
                        S.op('pool', lambda e: e.tensor_tensor(out=cq2[:, c, :N], in0=cq[:, c, :N], in1=cq[:, c, :N], op=ALU.mult),
                             reads=[('cq', c)], writes=[('cq2', c)])
                    for c in range(3):
                        S.op('pe', lambda e: e.matmul(ps[7][:, :N], lhsT=ones32, rhs=cq2[:, c, :N], start=(c == 0), stop=(c == 2)),
                             reads=[('cq2', c), 'c32'], writes=[('ps', 7)], inc=(c == 2))
                    rstd_from_ps(7, N, 384, rs, 'rs')
                    for c in range(3):
                        S.op('dve', lambda e: e.scalar_tensor_tensor(out=cqn[:, c, :N], in0=cq[:, c, :N], scalar=sv[:, c:c + 1], in1=rs[:, :N],
                                                                     op0=ALU.mult, op1=ALU.mult), reads=[('cq', c), 'rs', 'sv'], writes=['cqn'])
                    for c in range(3):
                        pb = 2 + c % 2
                        M = 128 if c < 2 else 64
                        for kc in range(8):
                            S.op('pe', lambda e: e.matmul(ps[pb][:M, :N], lhsT=wkva[:, kc, c * 128:c * 128 + M], rhs=hb[:, kc, :N],
                                                          start=(kc == 0), stop=(kc == 7)), reads=['hb', 'wkva'], writes=[('ps', pb)], inc=(kc == 7))
                        if c < 2:
                            S.op('act', lambda e: e.activation(out=ckv[:, c, :N], in_=ps[pb][:, :N], func=AF.Identity),
                                 reads=[('ps', pb)], writes=[('ckv', c)])
                            S.op('pool', lambda e: e.tensor_tensor(out=ckv2[:, c, :N], in0=ckv[:, c, :N], in1=ckv[:, c, :N], op=ALU.mult),
                                 reads=[('ckv', c)], writes=[('ckv2', c)])
                        else:
                            S.op('act', lambda e: e.activation(out=kr[:, :N], in_=ps[pb][:64, :N], func=AF.Identity),
                                 reads=[('ps', pb)], writes=['kr'])
                            S.op('pool', lambda e: e.tensor_tensor(out=kr2[:, :N], in0=kr[:, :N], in1=kr[:, :N], op=ALU.mult),
                                 reads=['kr'], writes=['kr2'])
                            S.op('dve', lambda e: e.tensor_scalar(out=kr[:, :N], in0=kr[:, :N], scalar1=sv[0:64, 8:9], scalar2=None, op0=ALU.mult),
                                 reads=['kr', 'sv', 'kr2'], writes=['kr'])
                    for c in range(2):
                        S.op('pe', lambda e: e.matmul(ps[7][:, :N], lhsT=ones32, rhs=ckv2[:, c, :N], start=(c == 0), stop=(c == 1)),
                             reads=[('ckv2', c), 'c32'], writes=[('ps', 7)], inc=(c == 1))
                    rstd_from_ps(7, N, 256, rs, 'rs')
                    for c in range(2):
                        S.op('dve', lambda e: e.scalar_tensor_tensor(out=ckvn[:, c, :N], in0=ckv[:, c, :N], scalar=sv[:, 3 + c:4 + c], in1=rs[:, :N],
                                                                     op0=ALU.mult, op1=ALU.mult), reads=[('ckv', c), 'rs', 'sv'], writes=['ckvn'])
                    if mc == 0:
                        rope(kr, krP, krr, N, 'kr', 'krr', 6)
                        krsrc, kkr = krr, 'krr'
                    else:
                        krsrc, kkr = kr, 'kr'
                    for h in range(NH):
                        for kc in range(3):
                            S.op('pe', lambda e: e.matmul(ps[0][:, :N], lhsT=wqb[:, kc, h * 192:h * 192 + 128], rhs=cqn[:, kc, :N],
                                                          start=(kc == 0), stop=(kc == 2)), reads=['cqn', 'wqb'], writes=[('ps', 0)], inc=(kc == 2))
                        for kc in range(3):
                            S.op('pe', lambda e: e.matmul(ps[1][:64, :N], lhsT=wqb[:, kc, h * 192 + 128:h * 192 + 192], rhs=cqn[:, kc, :N],
                                                          start=(kc == 0), stop=(kc == 2)), reads=['cqn', 'wqb'], writes=[('ps', 1)], inc=(kc == 2))
                        S.op('act', lambda e: e.activation(out=qh[:, :N], in_=ps[0][:, :N], func=AF.Identity), reads=[('ps', 0)], writes=['qh'])
                        S.op('act', lambda e: e.activation(out=qr[:, :N], in_=ps[1][:64, :N], func=AF.Identity), reads=[('ps', 1)], writes=['qr'])
                        S.op('pool', lambda e: e.tensor_tensor(out=qh2[:, :N], in0=qh[:, :N], in1=qh[:, :N], op=ALU.mult), reads=['qh'], writes=['qh2'])
                        S.op('pool', lambda e: e.tensor_tensor(out=qr2[:, :N], in0=qr[:, :N], in1=qr[:, :N], op=ALU.mult), reads=['qr'], writes=['qr2'])
                        S.op('pe', lambda e: e.matmul(ps[4][:, :N], lhsT=ones32, rhs=qh2[:, :N], start=True, stop=False),
                             reads=['qh2', 'c32'], writes=[('ps', 4)], inc=False)
                        S.op('pe', lambda e: e.matmul(ps[4][:, :N], lhsT=c32[0:64, 0:128], rhs=qr2[:, :N], start=False, stop=True),
                             reads=['qr2', 'c32'], writes=[('ps', 4)])
                        rstd_from_ps(4, N, 192, rs2, 'rs2')
                        S.op('dve', lambda e: e.scalar_tensor_tensor(out=qn_o[:, h, :N], in0=qh[:, :N], scalar=sv[:, 5:6], in1=rs2[:, :N],
                                                                     op0=ALU.mult, op1=ALU.mult), reads=['qh', 'rs2', 'sv'], writes=['qn_o'])
                        S.op('dve', lambda e: e.scalar_tensor_tensor(out=qr[:, :N], in0=qr[:, :N], scalar=sv[0:64, 6:7], in1=rs2[0:64, :N],
                                                                     op0=ALU.mult, op1=ALU.mult), reads=['qr', 'rs2', 'sv', 'qr2'], writes=['qr'])
                        if mc == 0:
                            rope(qr, qrP, qr2, N, 'qr', 'qr2', 6)
                            S.op('act', lambda e: e.activation(out=qr_o[:, h, :N], in_=qr2[:, :N], func=AF.Identity), reads=['qr2'], writes=['qr_o'])
                        else:
                            S.op('act', lambda e: e.activation(out=qr_o[:, h, :N], in_=qr[:, :N], func=AF.Identity), reads=['qr'], writes=['qr_o'])
                        for kc in range(2):
                            S.op('pe', lambda e: e.matmul(ps[2][:, :N], lhsT=wkvb[:, kc, h * 256:h * 256 + 128], rhs=ckvn[:, kc, :N],
                                                          start=(kc == 0), stop=(kc == 1)), reads=['ckvn', 'wkvb'], writes=[('ps', 2)], inc=(kc == 1))
                        S.op('act', lambda e: e.activation(out=qh[:, :N], in_=ps[2][:, :N], func=AF.Identity), reads=[('ps', 2)], writes=['qh'])
                        S.op('pool', lambda e: e.tensor_tensor(out=qh2[:, :N], in0=qh[:, :N], in1=qh[:, :N], op=ALU.mult), reads=['qh'], writes=['qh2'])
                        S.op('pe', lambda e: e.matmul(ps[5][:, :N], lhsT=ones32, rhs=qh2[:, :N], start=True, stop=False),
                             reads=['qh2', 'c32'], writes=[('ps', 5)], inc=False)
                        S.op('pe', lambda e: e.matmul(ps[5][:, :N], lhsT=c32[0:64, 0:128], rhs=kr2[:, :N], start=False, stop=True),
                             reads=['kr2', 'c32'], writes=[('ps', 5)])
                        rstd_from_ps(5, N, 192, rs2, 'rs2')
                        S.op('dve', lambda e: e.scalar_tensor_tensor(out=kn_o[:, h, :N], in0=qh[:, :N], scalar=sv[:, 7:8], in1=rs2[:, :N],
                                                                     op0=ALU.mult, op1=ALU.mult), reads=['qh', 'rs2', 'sv'], writes=['kn_o'])
                        S.op('dve', lambda e: e.tensor_tensor(out=kr_o[:, h, :N], in0=krsrc[:, :N], in1=rs2[0:64, :N], op=ALU.mult),
                             reads=[kkr, 'rs2'], writes=['kr_o'])
                        for blk in range(N // 128):
                            for kc in range(2):
                                S.op('pe', lambda e: e.matmul(ps[3][:, blk * 128:(blk + 1) * 128], lhsT=ckvn[:, kc, blk * 128:(blk + 1) * 128],
                                                              rhs=wkvb[:, kc, h * 256 + 128:h * 256 + 256], start=(kc == 0), stop=(kc == 1)),
                                     reads=['ckvn', 'wkvb'], writes=[('ps', 3)], inc=(kc == 1 and blk == N // 128 - 1))
                        S.op('act', lambda e: e.activation(out=v_o[:, 0:N // 128, h, :], in_=ps[3][:, :N].rearrange("p (b d) -> p b d", d=128),
                                                           func=AF.Identity), reads=[('ps', 3)], writes=['v_o'])
                    S.dma('sp', QN[:, :, t0:t0 + N].rearrange("h p n -> p h n"), qn_o[:, :, :N], reads=['qn_o'], writes=['QN'])
                    S.dma('sp', QR[:, :, t0:t0 + N].rearrange("h p n -> p h n"), qr_o[:, :, :N], reads=['qr_o'], writes=['QR'])
                    S.dma('sp', KN[:, :, t0:t0 + N].rearrange("h p n -> p h n"), kn_o[:, :, :N], reads=['kn_o'], writes=['KN'])
                    S.dma('sp', KR[:, :, t0:t0 + N].rearrange("h p n -> p h n"), kr_o[:, :, :N], reads=['kr_o'], writes=['KR'])
                    S.dma('sp', VV[t0:t0 + N].rearrange("(b p) h d -> p b h d", p=128), v_o[:, 0:N // 128], reads=['v_o'], writes=['VV'])
            S.barrier()
            with contextlib.ExitStack() as st:
                kn = [sb(f"a_kn{b}", [128, NT], BF16, st) for b in range(2)]
                krt = [sb(f"a_kr{b}", [64, NT], BF16, st) for b in range(2)]
                qn = [sb(f"a_qn{b}", [128, NT], BF16, st) for b in range(2)]
                qrt = [sb(f"a_qr{b}", [64, NT], BF16, st) for b in range(2)]
                vt = [sb(f"a_v{b}", [128, NT // 128, 128], BF16, st) for b in range(2)]
                pT = [sb(f"a_p{b}", [128, 512], BF16, st) for b in range(3)]
                rd = sb("a_rd", [128, 512], F32, st)
                ao = [sb(f"a_o{b}", [128, 512], BF16, st) for b in range(2)]
                npt = 0
                nq = 0
                for h in range(NH):
                    b = h % 2
                    kh = ('ah', b)
                    S.dma('sp', kn[b][:], KN[h], writes=[kh])
                    S.dma('sp', krt[b][:], KR[h], writes=[kh])
                    S.dma('sp', qn[b][:], QN[h], writes=[kh])
                    S.dma('sp', qrt[b][:], QR[h], writes=[kh])
                    S.dma('sp', vt[b][:], VV[:, h, :].rearrange("(b p) d -> p b d", p=128), writes=[kh])
                    for (t0, N, mc) in TILES:
                        nkb = 2 if mc == 1 else NT // 128
                        po, pd = 4 + (nq % 2), 6 + (nq % 2)

                        def scores(kb):
                            sbk = kb % 3
                            S.op('pe', lambda e: e.matmul(ps[sbk][:, :N], lhsT=kn[b][:, kb * 128:(kb + 1) * 128], rhs=qn[b][:, t0:t0 + N],
                                                          start=True, stop=False), reads=[kh], writes=[('ps', sbk)], inc=False)
                            S.op('pe', lambda e: e.matmul(ps[sbk][:, :N], lhsT=krt[b][:, kb * 128:(kb + 1) * 128], rhs=qrt[b][:, t0:t0 + N],
                                                          start=False, stop=True), reads=[kh], writes=[('ps', sbk)])
                        scores(0)
                        for kb in range(nkb):
                            if kb + 1 < nkb:
                                scores(kb + 1)
                            sbk = kb % 3
                            pb = npt % 3
                            npt += 1
                            S.op('act', lambda e: e.activation(out=pT[pb][:, :N], in_=ps[sbk][:, :N], func=AF.Exp),
                                 reads=[('ps', sbk)], writes=[('pT', pb)])
                            S.op('pe', lambda e: e.matmul(ps[po][:, :N], lhsT=vt[b][:, kb, :], rhs=pT[pb][:, :N], start=(kb == 0), stop=(kb == nkb - 1)),
                                 reads=[kh, ('pT', pb)], writes=[('ps', po)], inc=False)
                            S.op('pe', lambda e: e.matmul(ps[pd][:, :N], lhsT=ones_bf[:], rhs=pT[pb][:, :N], start=(kb == 0), stop=(kb == nkb - 1)),
                                 reads=['ones_bf', ('pT', pb)], writes=[('ps', pd)])
                        S.op('dve', lambda e: e.reciprocal(out=rd[:, :N], in_=ps[pd][:, :N]), reads=[('ps', pd)], writes=['rd'])
                        ob = nq % 2
                        S.op('dve', lambda e: e.tensor_tensor(out=ao[ob][:, :N], in0=ps[po][:, :N], in1=rd[:, :N], op=ALU.mult),
                             reads=[('ps', po), 'rd'], writes=[('ao', ob)])
                        S.dma('sp', AO[h * 128:(h + 1) * 128, t0:t0 + N], ao[ob][:, :N], reads=[('ao', ob)], writes=['AO'])
                        nq += 1
            S.barrier()
            with contextlib.ExitStack() as st:
                wo = sb("wo", [128, 8, 1024], BF16, st)
                S.dma('pool', wo[:], mla_wo[j].rearrange("(k p) n -> p k n", p=128), writes=['wo'])
                xt = [sb(f"c_xt{b}", [128, 8, 512], F32, st) for b in range(2)]
                at = [sb(f"c_at{b}", [128, 8, 512], BF16, st) for b in range(2)]
                for ti, (t0, N, mc) in enumerate(TILES):
                    b = ti % 2
                    S.dma('sp', xt[b][:, :, :N], XSin[:, t0:t0 + N].rearrange("(k p) n -> p k n", p=128), writes=[('cx', b)])
                    S.dma('sp', at[b][:, :, :N], AO[:, t0:t0 + N].rearrange("(k p) n -> p k n", p=128), writes=[('ca', b)])
                    for oc in range(8):
                        pb = oc % 4
                        for kc in range(8):
                            S.op('pe', lambda e: e.matmul(ps[pb][:, :N], lhsT=wo[:, kc, oc * 128:(oc + 1) * 128], rhs=at[b][:, kc, :N],
                                                          start=(kc == 0), stop=(kc == 7)), reads=['wo', ('ca', b)], writes=[('ps', pb)], inc=(kc == 7))
                        S.op('dve', lambda e: e.scalar_tensor_tensor(out=xt[b][:, oc, :N], in0=ps[pb][:, :N], scalar=modt[:, i, 16 + oc, mc:mc + 1],
                                                                     in1=xt[b][:, oc, :N], op0=ALU.mult, op1=ALU.add),
                             reads=[('ps', pb), ('cx', b), 'modt'], writes=[('cx', b)])
                    S.dma('sp', XS[:, t0:t0 + N].rearrange("(k p) n -> p k n", p=128), xt[b][:, :, :N], reads=[('cx', b)], writes=['XS'])
            S.barrier()

        def phase_rwkv(i, last):
            j = i // 2
            NCH = NT // 64
            RT256 = [(0, 256, 1)] + [(256 + 256 * a, 256, 0) for a in range(16)]
            HC = HS[:, 0:258]
            HL = HS[:, 258:258 + 4098]
            with contextlib.ExitStack() as st:
                xt = [sb(f"r1x{b}", [128, 8, 512], F32, st) for b in range(2)]
                sq = sb("r1sq", [128, 8, 512], F32, st)
                rs = sb("r1rs", [128, 512], F32, st)
                zt = sb("r1z", [128, 8, 1], F32, st)
                S.op('pool', lambda e: e.memset(zt[:], 0.0), writes=['zt'])
                for col in (0, 257, 258, 258 + 4097):
                    S.dma('sp', HS[:, col:col + 1].rearrange("(k p) n -> p k n", p=128), zt[:], reads=['zt'], writes=['HS'], allow_slow_non_contiguous=True)
                for ti, (t0, N, mc) in enumerate(TILES):
                    b = ti % 2
                    kx = ('r1x', b)
                    S.dma('sp', xt[b][:, :, :N], XS[:, t0:t0 + N].rearrange("(k p) n -> p k n", p=128), writes=[kx])
                    ksq = normmod(xt[b][:, :, :N], N, A1[:, i, :, mc], None, None, sq, rs, 7, kx, 'r1')
                    S.op('dve', lambda e: e.tensor_tensor(out=xt[b][:, :, :N], in0=sq[:, :, :N],
                                                          in1=modt[:, i, 0:8, mc].unsqueeze(2).to_broadcast([128, 8, N]), op=ALU.add),
                         reads=[ksq, 'modt'], writes=[kx])
                    dst = HC[:, 1:257] if mc == 1 else HL[:, 1 + t0 - CTX:1 + t0 - CTX + N]
                    S.dma('sp', dst.rearrange("(k p) n -> p k n", p=128), xt[b][:, :, :N], reads=[kx], writes=['HS'])
            S.barrier()
            if os.environ.get('RSTOP') == '1':
                return
            class _Stop(Exception):
                pass

            def stg(x):
                if os.environ.get('R2STOP') == x:
                    S.mute = True
            try:
              with contextlib.ExitStack() as st:
                  N = 256
                  wr = sb("wr", [128, 8, 1024], BF16, st); wk = sb("wk", [128, 8, 1024], BF16, st); wv = sb("wv", [128, 8, 1024], BF16, st)
                  w1c = sb("w1c", [128, 8, 128], BF16, st); a1c = sb("a1c", [128, 8, 128], BF16, st)
                  g1 = sb("g1", [128, 8, 160], BF16, st); g2a = sb("g2a", [128, 1024], BF16, st); g2b = sb("g2b", [32, 1024], BF16, st)
                  w2p = sb("w2p", [128, 2, 1024], BF16, st); a2p = sb("a2p", [128, 2, 1024], BF16, st)
                  for (wt_, src, kk_) in [(wr, rwkv_wr, 'wr'), (wk, rwkv_wk, 'wk'), (wv, rwkv_wv, 'wv')]:
                      S.dma('pool', wt_[:], src[j].rearrange("(k p) n -> p k n", p=128), writes=[kk_])
                  for d in range(2):
                      S.dma('pool', w1c[:, :, d * 64:(d + 1) * 64], rwkv_w1[j, d].rearrange("(k p) n -> p k n", p=128), writes=['w1c'])
                      S.dma('pool', a1c[:, :, d * 64:(d + 1) * 64], rwkv_a1[j, d].rearrange("(k p) n -> p k n", p=128), writes=['a1c'])
                  S.dma('pool', g1[:], rwkv_g1[j].rearrange("(k p) n -> p k n", p=128), writes=['g1'])
                  S.dma('pool', g2a[:], rwkv_g2[j, 0:128, :], writes=['g2a'])
                  S.dma('pool', g2b[:], rwkv_g2[j, 128:160, :], writes=['g2b'])
                  S.op('pool', lambda e: e.memset(w2p[:], 0.0), writes=['w2p'])
                  S.op('pool', lambda e: e.memset(a2p[:], 0.0), writes=['a2p'])
                  for d in range(2):
                      S.dma('pool', w2p[d * 64:(d + 1) * 64, d, :], rwkv_w2[j, d], writes=['w2p'])
                      S.dma('pool', a2p[d * 64:(d + 1) * 64, d, :], rwkv_a2[j, d], writes=['a2p'])
                  vres = j >= 1
                  if vres:
                      v1 = sb("v1", [128, 8, 32], BF16, st); v2 = sb("v2", [32, 1024], BF16, st)
                      S.dma('pool', v1[:], rwkv_v1[j - 1].rearrange("(k p) n -> p k n", p=128), writes=['v1'])
                      S.dma('pool', v2[:], rwkv_v2[j - 1], writes=['v2'])
                      vf = sb("vf", [128, N], F32, st)
                  dv_ = sb("dv", [128, 16], F32, st)
                  S.op('dve', lambda e: e.tensor_scalar(out=dv_[:, 0:8], in0=V(f"ka{j}"), scalar1=-1.0, scalar2=1.0, op0=ALU.mult, op1=ALU.add),
                       reads=['vecs'], writes=['dv'])
                  S.op('dve', lambda e: e.tensor_scalar(out=dv_[:, 8:16], in0=V(f"rk{j}"), scalar1=0.5, scalar2=None, op0=ALU.mult),
                       reads=['vecs'], writes=['dv'])
                  hx = sb("hx", [128, 8, N + 2], F32, st)
                  xx = sb("xx", [128, 8, N], F32, st)
                  xm = [sb(f"xm{m}", [128, 8, N], BF16, st) for m in range(6)]
                  lwm = sb("lwm", [128, N], BF16, st); am = sb("am", [128, N], BF16, st)
                  gma = sb("gma", [128, N], BF16, st); gmb = sb("gmb", [32, N], BF16, st); vm = sb("vm", [32, N], BF16, st)
                  T = {}
                  for nm in ["r32", "k32", "v32", "g32", "vv", "dvv", "kkc", "kk2", "rn", "kkn", "aneg", "sig", "lw", "al", "tk", "kd", "bb",
                             "Lp", "Lc", "E1", "E2", "E3", "t2", "At", "Rt", "Bt", "Kt", "kb", "t3", "bon"]:
                      T[nm] = sb("f_" + nm, [128, N], F32, st)
                  wct = sb("wct", [128, 4], F32, st)

                  def hb_(n):
                      return ps[n // 2][:, (n % 2) * 256:(n % 2) * 256 + 256]

                  def hk(n):
                      return ('psb', n // 2)

                  for ti, (t0, N_, mc) in enumerate(RT256):
                      src = HC[:, 0:258] if mc == 1 else HL[:, t0 - CTX:t0 - CTX + N + 2]
                      S.dma('sp', hx[:], src.rearrange("(k p) n -> p k n", p=128), writes=['hx'])
                      S.op('dve', lambda e: e.tensor_tensor(out=xx[:], in0=hx[:, :, 0:N], in1=hx[:, :, 2:N + 2], op=ALU.add), reads=['hx'], writes=['xx'])
                      S.op('dve', lambda e: e.scalar_tensor_tensor(out=xx[:], in0=xx[:], scalar=0.5, in1=hx[:, :, 1:N + 1], op0=ALU.mult, op1=ALU.subtract),
                           reads=['hx', 'xx'], writes=['xx'])
                      for m in range(6):
                          for kc in range(8):
                              S.op('dve', lambda e: e.scalar_tensor_tensor(out=xm[m][:, kc, :], in0=xx[:, kc, :], scalar=V(f"mix{j}", m * 8 + kc, m * 8 + kc + 1),
                                                                           in1=hx[:, kc, 1:N + 1], op0=ALU.mult, op1=ALU.add),
                                   reads=['hx', 'xx', 'vecs'], writes=[('xm', m)])
                      stg('a')
                      for kc in range(8):
                          S.op('pe', lambda e: e.matmul(hb_(0), lhsT=w1c[:, kc, :], rhs=xm[1][:, kc, :], start=(kc == 0), stop=(kc == 7)),
                               reads=['w1c', ('xm', 1)], writes=[hk(0)], inc=(kc == 7))
                      S.op('act', lambda e: e.activation(out=T["sig"][:], in_=hb_(0), func=AF.Sigmoid, scale=2.0), reads=[hk(0)], writes=['sig'])
                      S.op('dve', lambda e: e.tensor_scalar(out=lwm[:], in0=T["sig"][:], scalar1=2.0, scalar2=-1.0, op0=ALU.mult, op1=ALU.add),
                           reads=['sig'], writes=['lwm'])
                      for kc in range(8):
                          S.op('pe', lambda e: e.matmul(hb_(1), lhsT=a1c[:, kc, :], rhs=xm[4][:, kc, :], start=(kc == 0), stop=(kc == 7)),
                               reads=['a1c', ('xm', 4)], writes=[hk(1)], inc=(kc == 7))
                      S.op('act', lambda e: e.activation(out=am[:], in_=hb_(1), func=AF.Identity), reads=[hk(1)], writes=['am'])
                      for kc in range(8):
                          S.op('pe', lambda e: e.matmul(hb_(2), lhsT=g1[:, kc, 0:128], rhs=xm[5][:, kc, :], start=(kc == 0), stop=(kc == 7)),
                               reads=['g1', ('xm', 5)], writes=[hk(2)], inc=(kc == 7))
                      S.op('act', lambda e: e.activation(out=gma[:], in_=hb_(2), func=AF.Sigmoid), reads=[hk(2)], writes=['gma'])
                      for kc in range(8):
                          S.op('pe', lambda e: e.matmul(hb_(3)[0:32, :], lhsT=g1[:, kc, 128:160], rhs=xm[5][:, kc, :], start=(kc == 0), stop=(kc == 7)),
                               reads=['g1', ('xm', 5)], writes=[hk(3)], inc=(kc == 7))
                      S.op('act', lambda e: e.activation(out=gmb[:], in_=hb_(3)[0:32, :], func=AF.Sigmoid), reads=[hk(3)], writes=['gmb'])
                      if vres:
                          for kc in range(8):
                              S.op('pe', lambda e: e.matmul(hb_(4)[0:32, :], lhsT=v1[:, kc, :], rhs=xm[3][:, kc, :], start=(kc == 0), stop=(kc == 7)),
                                   reads=['v1', ('xm', 3)], writes=[hk(4)], inc=(kc == 7))
                          S.op('act', lambda e: e.activation(out=vm[:], in_=hb_(4)[0:32, :], func=AF.Identity), reads=[hk(4)], writes=['vm'])
                      for c in range(8):
                          cs = slice(c * 128, (c + 1) * 128)
                          rows = slice(c * 128, (c + 1) * 128)
                          stg('b')
                          for (hbn, w_, m) in [(5, wr, 0), (6, wk, 2), (7, wv, 3)]:
                              for kc in range(8):
                                  S.op('pe', lambda e: e.matmul(hb_(hbn), lhsT=w_[:, kc, cs], rhs=xm[m][:, kc, :], start=(kc == 0), stop=(kc == 7)),
                                       reads=[('xm', m), 'wr', 'wk', 'wv'], writes=[hk(hbn)], inc=(kc == 7))
                          S.op('pe', lambda e: e.matmul(hb_(8), lhsT=g2a[:, cs], rhs=gma[:], start=True, stop=False), reads=['g2a', 'gma'], writes=[hk(8)], inc=False)
                          S.op('pe', lambda e: e.matmul(hb_(8), lhsT=g2b[:, cs], rhs=gmb[:], start=False, stop=True), reads=['g2b', 'gmb'], writes=[hk(8)])
                          for d in range(2):
                              S.op('pe', lambda e: e.matmul(hb_(9 + d), lhsT=w2p[:, d, cs], rhs=lwm[:], start=True, stop=True), reads=['w2p', 'lwm'], writes=[hk(9 + d)])
                              S.op('pe', lambda e: e.matmul(hb_(11 + d), lhsT=a2p[:, d, cs], rhs=am[:], start=True, stop=True), reads=['a2p', 'am'], writes=[hk(11 + d)])
                          if vres:
                              S.op('pe', lambda e: e.matmul(hb_(13), lhsT=v2[:, cs], rhs=vm[:], start=True, stop=True), reads=['v2', 'vm'], writes=[hk(13)])
                          S.op('act', lambda e: e.activation(out=T["r32"][:], in_=hb_(5), func=AF.Identity), reads=[hk(5)], writes=['r32'])
                          S.op('act', lambda e: e.activation(out=T["k32"][:], in_=hb_(6), func=AF.Identity), reads=[hk(6)], writes=['k32'])
                          S.op('act', lambda e: e.activation(out=T["v32"][:], in_=hb_(7), func=AF.Identity), reads=[hk(7)], writes=['v32'])
                          S.op('act', lambda e: e.activation(out=T["g32"][:], in_=hb_(8), func=AF.Identity), reads=[hk(8)], writes=['g32'])
                          if vres:
                              S.dma('sp', vf[:], VT[0][rows, t0:t0 + N], writes=['vf'])
                              S.op('act', lambda e: e.activation(out=T["vv"][:], in_=hb_(13), func=AF.Sigmoid, bias=V(f"v0{j}", c, c + 1), scale=1.0),
                                   reads=[hk(13), 'vecs'], writes=['vv'])
                              S.op('pool', lambda e: e.tensor_tensor(out=T["dvv"][:], in0=vf[:], in1=T["v32"][:], op=ALU.subtract), reads=['vf', 'v32'], writes=['dvv'])
                              S.op('pool', lambda e: e.tensor_tensor(out=T["dvv"][:], in0=T["dvv"][:], in1=T["vv"][:], op=ALU.mult), reads=['dvv', 'vv'], writes=['dvv'])
                              S.op('pool', lambda e: e.tensor_tensor(out=T["v32"][:], in0=T["v32"][:], in1=T["dvv"][:], op=ALU.add), reads=['dvv', 'v32'], writes=['v32'])
                          stg('c')
                          S.op('dve', lambda e: e.tensor_scalar(out=T["kkc"][:], in0=T["k32"][:], scalar1=V(f"kk{j}", c, c + 1), scalar2=None, op0=ALU.mult),
                               reads=['k32', 'vecs'], writes=['kkc'])
                          S.op('pool', lambda e: e.tensor_tensor(out=T["kk2"][:], in0=T["kkc"][:], in1=T["kkc"][:], op=ALU.mult), reads=['kkc'], writes=['kk2'])
                          S.op('pe', lambda e: e.matmul(hb_(14), lhsT=blockones, rhs=T["kk2"][:], start=True, stop=True), reads=['kk2', 'c32'], writes=[hk(14)])
                          S.op('act', lambda e: e.activation(out=T["rn"][:], in_=hb_(14), func=AF.Sqrt), reads=[hk(14)], writes=['rn'])
                          S.op('dve', lambda e: e.tensor_scalar(out=T["rn"][:], in0=T["rn"][:], scalar1=1e-12, scalar2=None, op0=ALU.max), reads=['rn'], writes=['rn'])
                          S.op('dve', lambda e: e.reciprocal(out=T["rn"][:], in_=T["rn"][:]), reads=['rn'], writes=['rn'])
                          S.op('pool', lambda e: e.tensor_tensor(out=T["kkn"][:], in0=T["kkc"][:], in1=T["rn"][:], op=ALU.mult), reads=['kkc', 'rn'], writes=['kkn'])
                          S.op('pool', lambda e: e.tensor_scalar(out=T["aneg"][:], in0=T["kkn"][:], scalar1=-1.0, scalar2=None, op0=ALU.mult), reads=['kkn'], writes=['aneg'])
                          stg('d')
                          for d in range(2):
                              S.op('act', lambda e: e.activation(out=T["sig"][:], in_=hb_(9 + d), func=AF.Sigmoid, bias=V(f"w0{j}", d * 8 + c, d * 8 + c + 1), scale=1.0),
                                   reads=[hk(9 + d), 'vecs'], writes=['sig'])
                              S.op('pool', lambda e: e.tensor_scalar(out=T["lw"][:], in0=T["sig"][:], scalar1=float(-np.exp(-0.5)), scalar2=None, op0=ALU.mult),
                                   reads=['sig'], writes=['lw'])
                              S.op('act', lambda e: e.activation(out=T["al"][:], in_=hb_(11 + d), func=AF.Sigmoid, bias=V(f"a0{j}", d * 8 + c, d * 8 + c + 1), scale=1.0),
                                   reads=[hk(11 + d), 'vecs'], writes=['al'])
                              S.op('dve', lambda e: e.tensor_scalar(out=T["tk"][:], in0=T["al"][:], scalar1=V(f"ka{j}", c, c + 1), scalar2=dv_[:, c:c + 1],
                                                                    op0=ALU.mult, op1=ALU.add), reads=['al', 'vecs', 'dv'], writes=['tk'])
                              S.op('pool', lambda e: e.tensor_tensor(out=T["kd"][:], in0=T["k32"][:], in1=T["tk"][:], op=ALU.mult), reads=['k32', 'tk'], writes=['kd'])
                              S.op('pool', lambda e: e.tensor_tensor(out=T["bb"][:], in0=T["kkn"][:], in1=T["al"][:], op=ALU.mult), reads=['kkn', 'al'], writes=['bb'])
                              S.op('dve', lambda e: e.tensor_tensor_scan(out=T["Lp"][:], data0=cmask[:, :N], data1=T["lw"][:], initial=0.0, op0=ALU.mult, op1=ALU.add),
                                   reads=['lw', 'c32'], writes=['Lp'])
                              if d == 0:
                                  Lc, kLc = T["Lp"], 'Lp'
                              else:
                                  S.op('pool', lambda e: e.tensor_tensor(out=T["Lc"][:], in0=T["lw"][:], in1=T["Lp"][:], op=ALU.subtract), reads=['lw', 'Lp'], writes=['Lc'])
                                  S.op('pool', lambda e: e.tensor_tensor(
                                      out=T["Lc"][:].rearrange("p (c t) -> p c t", t=64), in0=T["Lc"][:].rearrange("p (c t) -> p c t", t=64),
                                      in1=T["Lp"][:].rearrange("p (c t) -> p c t", t=64)[:, :, 63:64].to_broadcast([128, N // 64, 64]), op=ALU.add),
                                      reads=['Lc', 'Lp'], writes=['Lc'])
                                  Lc, kLc = T["Lc"], 'Lc'
                              S.op('act', lambda e: e.activation(out=T["E1"][:], in_=Lc[:], func=AF.Exp), reads=[kLc], writes=['E1'])
                              S.op('act', lambda e: e.activation(out=T["E2"][:], in_=Lc[:], func=AF.Exp, scale=-1.0), reads=[kLc], writes=['E2'])
                              S.op('pool', lambda e: e.tensor_tensor(out=T["t2"][:], in0=Lc[:], in1=T["lw"][:], op=ALU.subtract), reads=[kLc, 'lw'], writes=['t2'])
                              S.op('act', lambda e: e.activation(out=T["E3"][:], in_=T["t2"][:], func=AF.Exp), reads=['t2'], writes=['E3'])
                              S.op('pool', lambda e: e.tensor_tensor(out=T["At"][:], in0=T["aneg"][:], in1=T["E3"][:], op=ALU.mult), reads=['aneg', 'E3'], writes=['At'])
                              S.op('pool', lambda e: e.tensor_tensor(out=T["Rt"][:], in0=T["r32"][:], in1=T["E1"][:], op=ALU.mult), reads=['r32', 'E1'], writes=['Rt'])
                              S.op('dve', lambda e: e.tensor_tensor(out=T["Bt"][:], in0=T["bb"][:], in1=T["E2"][:], op=ALU.mult), reads=['bb', 'E2'], writes=['Bt'])
                              S.op('dve', lambda e: e.tensor_tensor(out=T["Kt"][:], in0=T["kd"][:], in1=T["E2"][:], op=ALU.mult), reads=['kd', 'E2'], writes=['Kt'])
                              wcol = 63 if d == 0 else 0
                              S.op('dve', lambda e: e.tensor_copy(out=wct[:, 0:N // 64], in_=T["E1"][:].rearrange("p (c t) -> p c t", t=64)[:, :, wcol]),
                                   reads=['E1'], writes=['wct'])
                              S.dma('sp', ATd[d][rows, t0:t0 + N], T["At"][:], reads=['At'], writes=['ATd'])
                              S.dma('sp', RTd[d][rows, t0:t0 + N], T["Rt"][:], reads=['Rt'], writes=['RTd'])
                              S.dma('sp', BTd[d][rows, t0:t0 + N], T["Bt"][:], reads=['Bt'], writes=['BTd'])
                              S.dma('sp', KTd[d][rows, t0:t0 + N], T["Kt"][:], reads=['Kt'], writes=['KTd'])
                              S.dma('sp', WCd[d][rows, t0 // 64:t0 // 64 + N // 64], wct[:, 0:N // 64], reads=['wct'], writes=['WCd'])
                              if d == 0:
                                  S.op('pool', lambda e: e.tensor_copy(out=T["kb"][:], in_=T["kd"][:]), reads=['kd'], writes=['kb'])
                              else:
                                  S.op('pool', lambda e: e.tensor_tensor(out=T["kb"][:], in0=T["kb"][:], in1=T["kd"][:], op=ALU.add), reads=['kd', 'kb'], writes=['kb'])
                          stg('e')
                          S.op('pool', lambda e: e.tensor_tensor(out=T["t3"][:], in0=T["r32"][:], in1=T["kb"][:], op=ALU.mult), reads=['r32', 'kb'], writes=['t3'])
                          S.op('dve', lambda e: e.tensor_scalar(out=T["t3"][:], in0=T["t3"][:], scalar1=dv_[:, 8 + c:9 + c], scalar2=None, op0=ALU.mult),
                               reads=['t3', 'dv'], writes=['t3'])
                          S.op('pe', lambda e: e.matmul(hb_(15), lhsT=blockones, rhs=T["t3"][:], start=True, stop=True), reads=['t3', 'c32'], writes=[hk(15)])
                          S.op('dve', lambda e: e.tensor_tensor(out=T["bon"][:], in0=hb_(15), in1=T["v32"][:], op=ALU.mult), reads=[hk(15), 'v32'], writes=['bon'])
                          S.dma('sp', BON[rows, t0:t0 + N], T["bon"][:], reads=['bon'], writes=['BON'])
                          S.dma('sp', VT[j][rows, t0:t0 + N], T["v32"][:], reads=['v32'], writes=['VT'])
                          S.dma('sp', GG[rows, t0:t0 + N], T["g32"][:], reads=['g32'], writes=['GG'])

            except _Stop:
                pass
            S.mute = False
            S.barrier()
            if os.environ.get('RSTOP') == '2':
                return
            for d in range(2):
                order = ([0, 1, 2, 3] + list(range(4, NCH))) if d == 0 else ([3, 2, 1, 0] + list(range(NCH - 1, 3, -1)))
                mAR = maskAR_f if d == 0 else maskAR_r
                mN = maskN_f if d == 0 else maskN_r
                with contextlib.ExitStack() as st:
                    def stream(hh):
                        P_ = f"s{hh}_"
                        pb = [ps[4 * hh + q] for q in range(4)]

                        def pk(bank, half=None):
                            return [('sps', hh, bank)]
                        BD = {n: sb(P_ + n, [128, 4, 128], F32, st) for n in ["bdA", "T2", "T3", "T4", "tbB", "tbK", "tbV", "T8", "bdMak", "bdU", "bdST"]}
                        for n, t_ in BD.items():
                            S.op('pool', lambda e: e.memset(t_[:], 0.0), writes=[P_ + n])
                        AR = [sb(P_ + f"AR{b}", [128, 4, 2, 64], F32, st) for b in range(2)]
                        Bi = [sb(P_ + f"Bi{b}", [128, 4, 64], F32, st) for b in range(2)]
                        Ki = [sb(P_ + f"Ki{b}", [128, 4, 64], F32, st) for b in range(2)]
                        Vi = [sb(P_ + f"Vi{b}", [128, 4, 64], F32, st) for b in range(2)]
                        WCall = sb(P_ + "WCall", [128, 4, NCH], F32, st)
                        S.dma('sp', WCall[:], WCd[d][hh * 512:hh * 512 + 512, :].rearrange("(c p) n -> p c n", p=128), writes=[P_ + "WCall"])
                        ARm = sb(P_ + "ARm", [128, 4, 128], F32, st)
                        AKm = sb(P_ + "AKm", [128, 4, 128], F32, st)
                        Ns = sb(P_ + "Ns", [128, 4, 64], F32, st)
                        Vs = sb(P_ + "Vs", [128, 4, 64], F32, st)
                        PG = [sb(P_ + f"PG{b}", [128, 4, 128], F32, st) for b in range(2)]
                        Pst = [sb(P_ + f"Pst{b}", [128, 4, 64], F32, st) for b in range(2)]
                        Xs = sb(P_ + "Xs", [128, 4, 64], F32, st); Us = sb(P_ + "Us", [128, 4, 64], F32, st)
                        Yb = sb(P_ + "Yb", [128, 4, 64], F32, st); STs = sb(P_ + "STs", [128, 4, 64], F32, st)
                        tmpS = sb(P_ + "tmpS", [128, 4, 64], F32, st)
                        S.op('pool', lambda e: e.memset(STs[:], 0.0), writes=[P_ + "STs"])
                        r0 = hh * 512

                        def diag(eng, dst, kdst, src, ksrc, cols=64):
                            for half in range(2):
                                pslice = slice(half * 64, half * 64 + 64)
                                if eng == 'act':
                                    S.op('act', lambda e: e.activation(out=dst[pslice, :, half * 64:half * 64 + 64], in_=src[pslice], func=AF.Identity),
                                         reads=ksrc, writes=[kdst])
                                else:
                                    S.op(eng, lambda e: e.tensor_copy(out=dst[pslice, :, half * 64:half * 64 + 64], in_=src[pslice]),
                                         reads=ksrc, writes=[kdst])

                        def load(n):
                            g = order[n]
                            b = n % 2
                            cols = slice(g * 64, g * 64 + 64)
                            kin = P_ + f"in{b}"
                            for (dst, srcd) in [(AR[b][:, :, 0, :], ATd[d]), (AR[b][:, :, 1, :], RTd[d]), (Bi[b][:], BTd[d]), (Ki[b][:], KTd[d]), (Vi[b][:], VT[j])]:
                                S.dma('sp', dst, srcd[r0:r0 + 512, cols].rearrange("(c p) n -> p c n", p=128), writes=[kin])

                        load(0)
                        for n in range(NCH):
                            g = order[n]
                            b = n % 2
                            kin = P_ + f"in{b}"
                            if n + 1 < NCH:
                                load(n + 1)
                            diag('dve', BD["bdA"], P_ + "bdA", AR[b][:, :, 0, :], [kin])
                            diag('dve', BD["T2"], P_ + "T2", Bi[b], [kin])
                            diag('act', BD["T3"], P_ + "T3", Ki[b], [kin])
                            diag('act', BD["T4"], P_ + "T4", Vi[b], [kin])
                            ARf = AR[b][:].rearrange("p c a t -> p c (a t)")
                            for hp in range(4):
                                S.op('pe', lambda e: e.matmul(pb[0][:, hp * 128:(hp + 1) * 128], lhsT=BD["T2"][:, hp, :], rhs=ARf[:, hp, :], start=True, stop=True),
                                     reads=[P_ + "T2", kin], writes=pk(0), inc=(hp == 3))
                            for hp in range(4):
                                S.op('pe', lambda e: e.matmul(pb[1][:, hp * 128:(hp + 1) * 128], lhsT=BD["T3"][:, hp, :], rhs=ARf[:, hp, :], start=True, stop=True),
                                     reads=[P_ + "T3", kin], writes=pk(1), inc=(hp == 3))
                            for hp in range(4):
                                S.op('pe', lambda e: e.matmul(pb[2][:, hp * 64:(hp + 1) * 64], lhsT=BD["bdA"][:, hp, :], rhs=Bi[b][:, hp, :], start=True, stop=True),
                                     reads=[P_ + "bdA", kin], writes=pk(2, 0), inc=(hp == 3))
                            yield
                            S.op('dve', lambda e: e.tensor_tensor(out=ARm[:], in0=pb[0][:].rearrange("p (c t) -> p c t", t=128),
                                                                  in1=mAR.unsqueeze(1).to_broadcast([128, 4, 128]), op=ALU.mult),
                                 reads=pk(0) + ['c32'], writes=[P_ + "ARm"])
                            S.op('dve', lambda e: e.tensor_tensor(out=AKm[:], in0=pb[1][:].rearrange("p (c t) -> p c t", t=128),
                                                                  in1=mAR.unsqueeze(1).to_broadcast([128, 4, 128]), op=ALU.mult),
                                 reads=pk(1) + ['c32'], writes=[P_ + "AKm"])
                            S.op('dve', lambda e: e.tensor_tensor(out=Pst[0][:], in0=pb[2][:, 0:256].rearrange("p (c t) -> p c t", t=64),
                                                                  in1=mN.unsqueeze(1).to_broadcast([128, 4, 64]), op=ALU.mult),
                                 reads=pk(2, 0) + ['c32'], writes=[P_ + "Pst0"])
                            for hp in range(4):
                                S.op('pe', lambda e: e.matmul(pb[0][:, hp * 128:(hp + 1) * 128], lhsT=BD["T2"][:, hp, :], rhs=ident, start=True, stop=True),
                                     reads=[P_ + "T2", 'c32'], writes=pk(0), inc=(hp == 3))
                            for hp in range(4):
                                S.op('pe', lambda e: e.matmul(pb[1][:, hp * 128:(hp + 1) * 128], lhsT=BD["T3"][:, hp, :], rhs=ident, start=True, stop=True),
                                     reads=[P_ + "T3", 'c32'], writes=pk(1), inc=(hp == 3))
                            yield
                            S.op('act', lambda e: e.activation(out=BD["tbB"][:], in_=pb[0][:].rearrange("p (c t) -> p c t", t=128), func=AF.Identity),
                                 reads=pk(0), writes=[P_ + "tbB"])
                            S.op('act', lambda e: e.activation(out=BD["tbK"][:], in_=pb[1][:].rearrange("p (c t) -> p c t", t=128), func=AF.Identity),
                                 reads=pk(1), writes=[P_ + "tbK"])
                            for hp in range(4):
                                S.op('pe', lambda e: e.matmul(pb[0][:, hp * 128:(hp + 1) * 128], lhsT=BD["T4"][:, hp, :], rhs=ident, start=True, stop=True),
                                     reads=[P_ + "T4", 'c32'], writes=pk(0), inc=(hp == 3))
                            S.op('pool', lambda e: e.tensor_copy(out=PG[0][:, :, 0:64], in_=ARm[:, :, 0:64]), reads=[P_ + "ARm"], writes=[P_ + "PG0"])
                            S.op('pool', lambda e: e.tensor_copy(out=PG[0][:, :, 64:128], in_=SI.unsqueeze(1).to_broadcast([128, 4, 64])),
                                 reads=['c32'], writes=[P_ + "PG0"])
                            diag('pool', BD["bdMak"], P_ + "bdMak", AKm[:, :, 0:64], [P_ + "AKm"])
                            yield
                            S.op('act', lambda e: e.activation(out=BD["tbV"][:], in_=pb[0][:].rearrange("p (c t) -> p c t", t=128), func=AF.Identity),
                                 reads=pk(0), writes=[P_ + "tbV"])
                            for half in range(2):
                                pslice = slice(half * 64, half * 64 + 64)
                                S.op('pool', lambda e: e.tensor_copy(out=Vs[pslice], in_=BD["tbV"][pslice, :, half * 64:half * 64 + 64]),
                                     reads=[P_ + "tbV"], writes=[P_ + "Vs"])
                            diag('pool', BD["T2"], P_ + "T2", Pst[0], [P_ + "Pst0"])
                            diag('pool', BD["T3"], P_ + "T3", PG[0][:, :, 0:64], [P_ + "PG0"])
                            sets = [("T2", "T3"), ("T4", "T8")]
                            for lv in range(6):
                                cur, nxt = lv % 2, (lv + 1) % 2
                                bP, bPT = sets[cur]
                                nP, nPT = sets[nxt]
                                kPG, kPGn = P_ + f"PG{cur}", P_ + f"PG{nxt}"
                                if lv < 5:
                                    for hp in range(4):
                                        S.op('pe', lambda e: e.matmul(pb[1][:, hp * 64:(hp + 1) * 64], lhsT=BD[bPT][:, hp, :], rhs=Pst[cur][:, hp, :], start=True, stop=True),
                                             reads=[P_ + bPT, P_ + f"Pst{cur}"], writes=pk(1, 0), inc=(hp == 3))
                                    for hp in range(4):
                                        S.op('pe', lambda e: e.matmul(pb[0][:, hp * 128:(hp + 1) * 128], lhsT=BD[bP][:, hp, :], rhs=PG[cur][:, hp, :], start=True, stop=True),
                                             reads=[P_ + bP, kPG], writes=pk(0), inc=(hp == 3))
                                else:
                                    for hp in range(4):
                                        S.op('pe', lambda e: e.matmul(pb[0][:, hp * 128 + 64:(hp + 1) * 128], lhsT=BD[bP][:, hp, :], rhs=PG[cur][:, hp, 64:128], start=True, stop=True),
                                             reads=[P_ + bP, kPG], writes=pk(0), inc=(hp == 3))
                                yield
                                psv = pb[0][:].rearrange("p (c t) -> p c t", t=128)
                                S.op('dve', lambda e: e.tensor_tensor(out=PG[nxt][:, :, 64:128], in0=PG[cur][:, :, 64:128], in1=psv[:, :, 64:128], op=ALU.add),
                                     reads=pk(0) + [kPG], writes=[kPGn])
                                if lv < 5:
                                    S.op('act', lambda e: e.activation(out=PG[nxt][:, :, 0:64], in_=psv[:, :, 0:64], func=AF.Identity), reads=pk(0), writes=[kPGn])
                                    S.op('dve', lambda e: e.tensor_copy(out=Pst[nxt][:], in_=pb[1][:, 0:256].rearrange("p (c t) -> p c t", t=64)),
                                         reads=pk(1, 0), writes=[P_ + f"Pst{nxt}"])
                                    diag('act', BD[nP], P_ + nP, Pst[nxt], [P_ + f"Pst{nxt}"])
                                    if lv < 4:
                                        diag('pool', BD[nPT], P_ + nPT, PG[nxt][:, :, 0:64], [kPGn])
                            diag('pool', BD["T8"], P_ + "T8", PG[0][:, :, 64:128], [P_ + "PG0"])
                            for hp in range(4):
                                S.op('pe', lambda e: e.matmul(pb[2][:, 256 + hp * 64:256 + (hp + 1) * 64], lhsT=BD["bdA"][:, hp, :], rhs=STs[:, hp, :], start=True, stop=False),
                                     reads=[P_ + "bdA", P_ + "STs"], writes=pk(2, 1), inc=False)
                                S.op('pe', lambda e: e.matmul(pb[2][:, 256 + hp * 64:256 + (hp + 1) * 64], lhsT=BD["bdMak"][:, hp, :], rhs=Vs[:, hp, :], start=False, stop=True),
                                     reads=[P_ + "bdMak", P_ + "Vs"], writes=pk(2, 1), inc=(hp == 3))
                            yield
                            S.op('act', lambda e: e.activation(out=Xs[:], in_=pb[2][:, 256:512].rearrange("p (c t) -> p c t", t=64), func=AF.Identity),
                                 reads=pk(2, 1), writes=[P_ + "Xs"])
                            for hp in range(4):
                                S.op('pe', lambda e: e.matmul(pb[3][:, hp * 64:(hp + 1) * 64], lhsT=BD["T8"][:, hp, :], rhs=Xs[:, hp, :], start=True, stop=True),
                                     reads=[P_ + "T8", P_ + "Xs"], writes=pk(3, 0), inc=(hp == 3))
                            yield
                            S.op('dve', lambda e: e.tensor_copy(out=Us[:], in_=pb[3][:, 0:256].rearrange("p (c t) -> p c t", t=64)), reads=pk(3, 0), writes=[P_ + "Us"])
                            diag('act', BD["bdU"], P_ + "bdU", pb[3][:, 0:256].rearrange("p (c t) -> p c t", t=64), pk(3, 0))
                            for hp in range(4):
                                S.op('pe', lambda e: e.matmul(pb[3][:, 256 + hp * 64:256 + (hp + 1) * 64], lhsT=BD["bdST"][:, hp, :], rhs=AR[b][:, hp, 1, :], start=True, stop=False),
                                     reads=[P_ + "bdST", kin], writes=pk(3, 1), inc=False)
                                S.op('pe', lambda e: e.matmul(pb[3][:, 256 + hp * 64:256 + (hp + 1) * 64], lhsT=BD["bdU"][:, hp, :], rhs=ARm[:, hp, 64:128], start=False, stop=False),
                                     reads=[P_ + "bdU", P_ + "ARm"], writes=pk(3, 1), inc=False)
                                S.op('pe', lambda e: e.matmul(pb[3][:, 256 + hp * 64:256 + (hp + 1) * 64], lhsT=BD["tbV"][:, hp, :], rhs=AKm[:, hp, 64:128], start=False, stop=True),
                                     reads=[P_ + "tbV", P_ + "AKm"], writes=pk(3, 1), inc=(hp == 3))
                            for hp in range(4):
                                S.op('pe', lambda e: e.matmul(pb[1][:, 256 + hp * 64:256 + (hp + 1) * 64], lhsT=BD["tbB"][:, hp, :], rhs=Us[:, hp, :], start=True, stop=False),
                                     reads=[P_ + "tbB", P_ + "Us"], writes=pk(1, 1), inc=False)
                                S.op('pe', lambda e: e.matmul(pb[1][:, 256 + hp * 64:256 + (hp + 1) * 64], lhsT=BD["tbK"][:, hp, :], rhs=Vs[:, hp, :], start=False, stop=True),
                                     reads=[P_ + "tbK", P_ + "Vs"], writes=pk(1, 1), inc=(hp == 3))
                            yield
                            S.op('act', lambda e: e.activation(out=Yb[:], in_=pb[3][:, 256:512].rearrange("p (c t) -> p c t", t=64), func=AF.Identity),
                                 reads=pk(3, 1), writes=[P_ + "Yb"])
                            S.dma('sp', YD[d][r0:r0 + 512, g * 64:g * 64 + 64].rearrange("(c p) n -> p c n", p=128), Yb[:], reads=[P_ + "Yb"], writes=['YD'])
                            S.op('dve', lambda e: e.tensor_tensor(out=tmpS[:], in0=pb[1][:, 256:512].rearrange("p (c t) -> p c t", t=64), in1=STs[:], op=ALU.add),
                                 reads=pk(1, 1) + [P_ + "STs"], writes=[P_ + "tmpS"])
                            S.op('dve', lambda e: e.tensor_tensor(out=STs[:], in0=tmpS[:], in1=WCall[:, :, g:g + 1].to_broadcast([128, 4, 64]), op=ALU.mult),
                                 reads=[P_ + "tmpS", P_ + "WCall"], writes=[P_ + "STs"])
                            diag('pool', BD["bdST"], P_ + "bdST", STs, [P_ + "STs"])
                            yield

                    gens = [stream(0), stream(1)]
                    alive = [True, True]
                    while any(alive):
                        for q in range(2):
                            if alive[q]:
                                try:
                                    next(gens[q])
                                except StopIteration:
                                    alive[q] = False
                S.barrier()
            if os.environ.get('RSTOP') == '3':
                return
            with contextlib.ExitStack() as st:
                wo = sb("r_wo", [128, 8, 1024], BF16, st)
                S.dma('pool', wo[:], rwkv_wo[j].rearrange("(k p) n -> p k n", p=128), writes=['r_wo'])
                y0 = sb("r_y0", [128, 8, 512], F32, st); y1 = sb("r_y1", [128, 8, 512], F32, st)
                bo = sb("r_bo", [128, 8, 512], F32, st); gg = sb("r_gg", [128, 8, 512], F32, st)
                xt = sb("r_xt", [128, 8, 512], F32, st)
                ob = sb("r_ob", [128, 8, 512], BF16, st)
                yc = sb("r_yc", [128, 512], F32, st); y2 = sb("r_y2", [128, 512], F32, st); sd = sb("r_sd", [128, 512], F32, st)
                tiles = TILES[1:] if last else TILES
                for ti, (t0, N, mc) in enumerate(tiles):
                    for (dst, srcd, kk_) in [(y0, YD[0], 'y0'), (y1, YD[1], 'y1'), (bo, BON, 'bo'), (gg, GG, 'gg'), (xt, XS, 'r_xt')]:
                        S.dma('sp', dst[:, :, :N], srcd[:, t0:t0 + N].rearrange("(k p) n -> p k n", p=128), writes=[kk_])
                    for c in range(8):
                        pa, pv = c % 2, 2 + c % 2
                        S.op('dve', lambda e: e.tensor_tensor(out=y0[:, c, :N], in0=y0[:, c, :N], in1=y1[:, c, :N], op=ALU.add), reads=['y0', 'y1'], writes=['y0'])
                        S.op('pe', lambda e: e.matmul(ps[pa][:, :N], lhsT=blockones, rhs=y0[:, c, :N], start=True, stop=True), reads=['y0', 'c32'], writes=[('ps', pa)])
                        S.op('dve', lambda e: e.scalar_tensor_tensor(out=yc[:, :N], in0=ps[pa][:, :N], scalar=-1.0 / 64, in1=y0[:, c, :N], op0=ALU.mult, op1=ALU.add),
                             reads=[('ps', pa), 'y0'], writes=['yc'])
                        S.op('pool', lambda e: e.tensor_tensor(out=y2[:, :N], in0=yc[:, :N], in1=yc[:, :N], op=ALU.mult), reads=['yc'], writes=['y2'])
                        S.op('pe', lambda e: e.matmul(ps[pv][:, :N], lhsT=blockones, rhs=y2[:, :N], start=True, stop=True), reads=['y2', 'c32'], writes=[('ps', pv)])
                        S.op('act', lambda e: e.activation(out=sd[:, :N], in_=ps[pv][:, :N], func=AF.Sqrt, bias=epsT[:, 5:6], scale=1.0 / 64),
                             reads=[('ps', pv), 'epsT'], writes=['sd'])
                        S.op('dve', lambda e: e.reciprocal(out=sd[:, :N], in_=sd[:, :N]), reads=['sd'], writes=['sd'])
                        S.op('pool', lambda e: e.tensor_tensor(out=yc[:, :N], in0=yc[:, :N], in1=sd[:, :N], op=ALU.mult), reads=['yc', 'sd'], writes=['yc'])
                        S.op('dve', lambda e: e.tensor_scalar(out=yc[:, :N], in0=yc[:, :N], scalar1=V(f"lnw{j}", c, c + 1), scalar2=V(f"lnb{j}", c, c + 1),
                                                              op0=ALU.mult, op1=ALU.add), reads=['yc', 'vecs'], writes=['yc'])
                        S.op('pool', lambda e: e.tensor_tensor(out=yc[:, :N], in0=yc[:, :N], in1=bo[:, c, :N], op=ALU.add), reads=['yc', 'bo'], writes=['yc'])
                        S.op('pool', lambda e: e.tensor_tensor(out=ob[:, c, :N], in0=yc[:, :N], in1=gg[:, c, :N], op=ALU.mult), reads=['yc', 'gg'], writes=['ob'])
                    for oc in range(8):
                        pb_ = 4 + oc % 4
                        for kc in range(8):
                            S.op('pe', lambda e: e.matmul(ps[pb_][:, :N], lhsT=wo[:, kc, oc * 128:(oc + 1) * 128], rhs=ob[:, kc, :N], start=(kc == 0), stop=(kc == 7)),
                                 reads=['r_wo', 'ob'], writes=[('ps', pb_)], inc=(kc == 7))
                        S.op('dve', lambda e: e.scalar_tensor_tensor(out=xt[:, oc, :N], in0=ps[pb_][:, :N], scalar=modt[:, i, 16 + oc, mc:mc + 1],
                                                                     in1=xt[:, oc, :N], op0=ALU.mult, op1=ALU.add),
                             reads=[('ps', pb_), 'r_xt', 'modt'], writes=['r_xt'])
                    S.dma('sp', XS[:, t0:t0 + N].rearrange("(k p) n -> p k n", p=128), xt[:, :, :N], reads=['r_xt'], writes=['XS'])
            S.barrier()

        def phase_ffn(i, last):
            moe = (i % 2 == 1)
            k = i // 2
            E = NE if moe else 1
            F = DFE if moe else DFF
            nchunk = F // 128
            blocks = [(c0, min(4, nchunk - c0)) for c0 in range(0, nchunk, 4)]
            tiles = TILES[1:] if last else TILES
            groups = [tiles[0:len(tiles) - 6], tiles[-6:-3], tiles[-3:]]
            for gi, grp in enumerate(groups):
                Sg = sum(t[1] for t in grp)
                offs = [sum(t[1] for t in grp[:a]) for a in range(len(grp))]
                with contextlib.ExitStack() as st:
                    hb = sb("f_hb", [128, 8, 1536], BF16, st)
                    yacc = sb("f_yacc", [128, 8, 1536], F32, st)
                    GT = sb("f_GT", [8, 1536], F32, st)
                    gbc = sb("f_gbc", [128, 1536], F32, st)
                    with contextlib.ExitStack() as st2:
                        xt = [sb(f"f_xt{b}", [128, 8, 512], F32, st2) for b in range(2)]
                        sq = sb("f_sq", [128, 8, 512], F32, st2)
                        rs = sb("f_rs", [128, 512], F32, st2)
                        if moe:
                            rt = sb("f_rt", [128, 8, 8], F32, st2)
                            S.dma('sp', rt[:], moe_router[k].rearrange("(k p) n -> p k n", p=128), writes=['rt'])
                            lg = sb("f_lg", [128, 8], F32, st2); m8 = sb("f_m8", [128, 8], F32, st2)
                            sel = sb("f_sel", [128, 8], F32, st2); ex = sb("f_ex", [128, 8], F32, st2)
                            sm = sb("f_sm", [128, 4], F32, st2); G = sb("f_G", [128, 4, 8], F32, st2)
                        for ti, (t0, N, mc) in enumerate(grp):
                            b = ti % 2
                            kx = ('fx', b)
                            o = offs[ti]
                            S.dma('sp', xt[b][:, :, :N], XS[:, t0:t0 + N].rearrange("(k p) n -> p k n", p=128), writes=[kx])
                            ksq = normmod(xt[b][:, :, :N], N, A2[:, i, :, mc], None, None, sq, rs, 7, kx, 'f')
                            S.op('dve', lambda e: e.tensor_tensor(out=sq[:, :, :N], in0=sq[:, :, :N],
                                                                  in1=modt[:, i, 24:32, mc].unsqueeze(2).to_broadcast([128, 8, N]), op=ALU.add),
                                 reads=[ksq, 'modt'], writes=[ksq])
                            S.op('act', lambda e: e.activation(out=hb[:, :, o:o + N], in_=sq[:, :, :N], func=AF.Identity),
                                 reads=[ksq], writes=['hb'])
                            if moe:
                                for blk in range(N // 128):
                                    for kc in range(8):
                                        S.op('pe', lambda e: e.matmul(ps[6][:, 0:8], lhsT=sq[:, kc, blk * 128:(blk + 1) * 128], rhs=rt[:, kc, :],
                                                                      start=(kc == 0), stop=(kc == 7)), reads=[ksq, 'rt'], writes=[('ps', 6)], inc=(kc == 7))
                                    S.op('dve', lambda e: e.tensor_copy(out=lg[:], in_=ps[6][:, 0:8]), reads=[('ps', 6)], writes=['lg'])
                                    S.op('dve', lambda e: e.max(out=m8[:], in_=lg[:]), reads=['lg'], writes=['m8'])
                                    S.op('dve', lambda e: e.tensor_scalar(out=sel[:], in0=lg[:], scalar1=m8[:, 1:2], scalar2=None, op0=ALU.is_ge),
                                         reads=['lg', 'm8'], writes=['sel'])
                                    S.op('dve', lambda e: e.tensor_scalar(out=sm[:, 0:1], in0=m8[:, 0:1], scalar1=-1.0, scalar2=None, op0=ALU.mult),
                                         reads=['m8'], writes=['sm'])
                                    S.op('act', lambda e: e.activation(out=ex[:], in_=lg[:], func=AF.Exp, bias=sm[:, 0:1], scale=1.0),
                                         reads=['lg', 'sm'], writes=['ex'])
                                    S.op('dve', lambda e: e.tensor_tensor(out=ex[:], in0=ex[:], in1=sel[:], op=ALU.mult), reads=['ex', 'sel'], writes=['ex'])
                                    S.op('dve', lambda e: e.tensor_reduce(out=sm[:, 1:2], in_=ex[:], axis=AX.X, op=ALU.add), reads=['ex'], writes=['sm'])
                                    S.op('dve', lambda e: e.reciprocal(out=sm[:, 2:3], in_=sm[:, 1:2]), reads=['sm'], writes=['sm'])
                                    S.op('dve', lambda e: e.tensor_scalar(out=G[:, blk, :], in0=ex[:], scalar1=sm[:, 2:3], scalar2=None, op0=ALU.mult),
                                         reads=['ex', 'sm'], writes=['G'])
                                    S.op('pe', lambda e: e.transpose(out=ps[5][0:8, blk * 128:(blk + 1) * 128], in_=G[:, blk, :], identity=ident),
                                         reads=['G', 'c32'], writes=[('ps', 5)])
                                S.op('act', lambda e: e.activation(out=GT[:, o:o + N], in_=ps[5][0:8, :N], func=AF.Identity),
                                     reads=[('ps', 5)], writes=['GT'])
                    S.barrier()
                    with contextlib.ExitStack() as st2:
                        w1b = [sb(f"f_w1{b}", [128, 8, 512], BF16, st2) for b in range(2)]
                        w3b = [sb(f"f_w3{b}", [128, 8, 512], BF16, st2) for b in range(2)]
                        w2b = [sb(f"f_w2{b}", [128, 4, 1024], BF16, st2) for b in range(2)]
                        gt = [sb(f"f_g{b}", [128, 4, 512], BF16, st2) for b in range(2)]
                        s1 = [sb(f"f_s1{b}", [128, 512], F32, st2) for b in range(2)]
                        s1g = [sb(f"f_s1g{b}", [128, 512], F32, st2) for b in range(2)]
                        nw = 0
                        ng = 0
                        nhc = 0
                        ny = 0
                        first = True
                        for ex_i in range(E):
                            if moe:
                                for ti, (t0, N, mc) in enumerate(grp):
                                    o = offs[ti]
                                    S.op('pe', lambda e: e.matmul(ps[6][:, :N], lhsT=selE[:, ex_i * 128:(ex_i + 1) * 128], rhs=GT[:, o:o + N],
                                                                  start=True, stop=True), reads=['GT', 'c32'], writes=[('ps', 6)])
                                    S.op('act', lambda e: e.activation(out=gbc[:, o:o + N], in_=ps[6][:, :N], func=AF.Identity),
                                         reads=[('ps', 6)], writes=['gbc'])
                            for (c0, nh) in blocks:
                                wb = nw % 2
                                nw += 1
                                kw = ('fw', wb)
                                if moe:
                                    W1, W3, W2 = moe_w1[k, ex_i], moe_w3[k, ex_i], moe_w2[k, ex_i]
                                else:
                                    W1, W3, W2 = ffn_w1[k], ffn_w3[k], ffn_w2[k]
                                S.dma('pool', w1b[wb][:, :, :nh * 128], W1[:, c0 * 128:(c0 + nh) * 128].rearrange("(k p) n -> p k n", p=128), writes=[kw])
                                S.dma('pool', w3b[wb][:, :, :nh * 128], W3[:, c0 * 128:(c0 + nh) * 128].rearrange("(k p) n -> p k n", p=128), writes=[kw])
                                S.dma('pool', w2b[wb][:, :nh, :], W2[c0 * 128:(c0 + nh) * 128, :].rearrange("(k p) n -> p k n", p=128), writes=[kw])
                                for ti, (t0, N, mc) in enumerate(grp):
                                    o = offs[ti]
                                    gb = ng % 2
                                    ng += 1
                                    for hc in range(nh):
                                        pa = (nhc % 2) * 2
                                        sbi = nhc % 2
                                        nhc += 1
                                        for kc in range(8):
                                            S.op('pe', lambda e: e.matmul(ps[pa][:, :N], lhsT=w1b[wb][:, kc, hc * 128:(hc + 1) * 128], rhs=hb[:, kc, o:o + N],
                                                                          start=(kc == 0), stop=(kc == 7)), reads=[kw, 'hb'], writes=[('ps', pa)], inc=(kc == 7))
                                        for kc in range(8):
                                            S.op('pe', lambda e: e.matmul(ps[pa + 1][:, :N], lhsT=w3b[wb][:, kc, hc * 128:(hc + 1) * 128], rhs=hb[:, kc, o:o + N],
                                                                          start=(kc == 0), stop=(kc == 7)), reads=[kw, 'hb'], writes=[('ps', pa + 1)], inc=(kc == 7))
                                        S.op('act', lambda e: e.activation(out=s1[sbi][:, :N], in_=ps[pa][:, :N], func=AF.Silu),
                                             reads=[('ps', pa)], writes=[('s1', sbi)])
                                        src, ksrc = s1[sbi], ('s1', sbi)
                                        if moe:
                                            S.op('pool', lambda e: e.tensor_tensor(out=s1g[sbi][:, :N], in0=s1[sbi][:, :N], in1=gbc[:, o:o + N], op=ALU.mult),
                                                 reads=[('s1', sbi), 'gbc'], writes=[('s1g', sbi)])
                                            src, ksrc = s1g[sbi], ('s1g', sbi)
                                        S.op('dve', lambda e: e.tensor_tensor(out=gt[gb][:, hc, :N], in0=src[:, :N], in1=ps[pa + 1][:, :N], op=ALU.mult),
                                             reads=[ksrc, ('ps', pa + 1)], writes=[('g', gb)])
                                    for oc in range(8):
                                        py = 4 + ny % 2
                                        ny += 1
                                        for hc in range(nh):
                                            S.op('pe', lambda e: e.matmul(ps[py][:, :N], lhsT=w2b[wb][:, hc, oc * 128:(oc + 1) * 128], rhs=gt[gb][:, hc, :N],
                                                                          start=(hc == 0), stop=(hc == nh - 1)), reads=[kw, ('g', gb)], writes=[('ps', py)],
                                                 inc=(hc == nh - 1))
                                        ky = ('y', o, oc)
                                        if first:
                                            S.op('act', lambda e: e.activation(out=yacc[:, oc, o:o + N], in_=ps[py][:, :N], func=AF.Identity),
                                                 reads=[('ps', py)], writes=[ky])
                                        else:
                                            S.op('dve', lambda e: e.tensor_tensor(out=yacc[:, oc, o:o + N], in0=yacc[:, oc, o:o + N], in1=ps[py][:, :N], op=ALU.add),
                                                 reads=[('ps', py), ky], writes=[ky])
                                first = False
                    S.barrier()
                    with contextlib.ExitStack() as st2:
                        xt = [sb(f"f_cx{b}", [128, 8, 512], F32, st2) for b in range(2)]
                        for ti, (t0, N, mc) in enumerate(grp):
                            b = ti % 2
                            o = offs[ti]
                            S.dma('sp', xt[b][:, :, :N], XS[:, t0:t0 + N].rearrange("(k p) n -> p k n", p=128), writes=[('fcx', b)])
                            for oc in range(8):
                                S.op('dve', lambda e: e.scalar_tensor_tensor(
                                    out=xt[b][:, oc, :N], in0=yacc[:, oc, o:o + N], scalar=modt[:, i, 40 + oc, mc:mc + 1],
                                    in1=xt[b][:, oc, :N], op0=ALU.mult, op1=ALU.add), reads=[('fcx', b), 'modt'], writes=[('fcx', b)])
                            if last:
                                S.dma('sp', out_d[:, t0 - CTX:t0 - CTX + N].rearrange("(k p) n -> p k n", p=128), xt[b][:, :, :N],
                                      reads=[('fcx', b)], writes=['out'])
                            else:
                                S.dma('sp', XS[:, t0:t0 + N].rearrange("(k p) n -> p k n", p=128), xt[b][:, :, :N],
                                      reads=[('fcx', b)], writes=['XS'])
                    S.barrier()

        if dbg:
            dbg_d = nc.dram_tensor("dbg", [D, NT], F32, kind="ExternalOutput").ap()
        phase_mod()
        for i in range(depth):
            last = (i == depth - 1)
            if i == 0 and os.environ.get('SKIP0'):
                S.dma('sp', XS, xs_in, writes=['XS'])
                S.barrier()
                continue
            if i % 2 == 0:
                phase_mla(i, xs_in if i == 0 else XS)
            else:
                phase_rwkv(i, last and dbg != 'r')
            if not (dbg == 'r' and i == depth - 1):
                phase_ffn(i, last)
        if dbg:
            S.dma('sp', dbg_d, XS, writes=['dbg'])
        S.barrier()
    return nc, S


def host_consts():
    c = np.zeros((128, C32W), np.float32)
    c[:, 0:128] = 1.0
    c[:, 128:256] = np.eye(128, dtype=np.float32)
    P = np.zeros((64, 64), np.float32)
    for base in (0, 32):
        for f in range(16):
            P[base + 16 + f, base + f] = -1.0
            P[base + f, base + 16 + f] = 1.0
    c[0:64, 256:320] = P
    for e in range(8):
        c[e, 384 + e * 128:384 + (e + 1) * 128] = 1.0
    o_ = 384 + 1024
    p = np.arange(128)
    c[:, o_:o_ + 128] = (p[:, None] // 64 == p[None, :] // 64)
    tt = np.arange(64)
    c[:, o_ + 128:o_ + 192] = (p[:, None] % 64 == tt[None, :])
    sidx = (p % 64)[:, None]
    c[:, o_ + 192:o_ + 256] = (sidx < tt[None, :])
    c[:, o_ + 256:o_ + 320] = (sidx <= tt[None, :])
    c[:, o_ + 320:o_ + 384] = (tt[None, :] < sidx)
    c[:, o_ + 384:o_ + 448] = (sidx > tt[None, :])
    c[:, o_ + 448:o_ + 512] = (sidx >= tt[None, :])
    c[:, o_ + 512:o_ + 576] = (tt[None, :] > sidx)
    cm = np.ones(512, np.float32); cm[0::64] = 0.0
    c[:, o_ + 576:o_ + 1088] = cm[None, :]
    rows = TL // 64
    row_ids = np.repeat(np.arange(rows, dtype=np.float32), 64)
    col_ids = np.tile(np.arange(64, dtype=np.float32), rows)
    inv_freq = (1.0 / (np.float32(10000.0) ** (np.arange(16, dtype=np.float32) / np.float32(16)))).astype(np.float32)
    ang_r = (row_ids[:, None] * inv_freq[None, :]).astype(np.float32)
    ang_c = (col_ids[:, None] * inv_freq[None, :]).astype(np.float32)
    cosT = np.zeros((64, TL), np.float32)
    sinT = np.zeros((64, TL), np.float32)
    for base, ang in ((0, ang_r), (32, ang_c)):
        for half in (0, 16):
            cosT[base + half:base + half + 16] = np.cos(ang).T
            sinT[base + half:base + half + 16] = np.sin(ang).T
    return c, cosT, sinT


def host_vecs(inp, depth):
    voff, NV = vec_layout(depth)
    vecs = np.zeros((128, NV), np.float32)

    def put(name, arr):
        o, c = voff[name]
        assert arr.shape == (128, c), (name, arr.shape, c)
        vecs[:, o:o + c] = arr
    for i in range(depth):
        put(f"adab{i}", fm(inp["ada_b"][i]))
        put(f"n1g{i}", fm(inp["norm1_g"][i]))
        put(f"n2g{i}", fm(inp["norm2_g"][i]))
        j = i // 2
        if i % 2 == 0:
            put(f"qan{j}", fm(inp["mla_qa_norm"][j]))
            put(f"kvan{j}", fm(inp["mla_kva_norm"][j]))
            put(f"qnn{j}", fm(inp["mla_q_norm"][j][:128]))
            put(f"qnr{j}", fm(inp["mla_q_norm"][j][128:]))
            put(f"knn{j}", fm(inp["mla_k_norm"][j][:128]))
            put(f"knr{j}", fm(inp["mla_k_norm"][j][128:]))
        else:
            put(f"mix{j}", fm(inp["rwkv_mix"][j]))
            put(f"w0{j}", fm(inp["rwkv_w0"][j]))
            put(f"a0{j}", fm(inp["rwkv_a0"][j]))
            put(f"kk{j}", fm(inp["rwkv_k_k"][j]))
            put(f"ka{j}", fm(inp["rwkv_k_a"][j]))
            put(f"rk{j}", fm(inp["rwkv_r_k"][j]))
            put(f"lnw{j}", fm(inp["rwkv_ln_w"][j]))
            put(f"lnb{j}", fm(inp["rwkv_ln_b"][j]))
            if j >= 1:
                put(f"v0{j}", fm(inp["rwkv_v0"][j - 1]))
    return vecs


WNAMES = ["ada_w", "mla_wqa", "mla_wqb", "mla_wkva", "mla_wkvb", "mla_wo", "ffn_w1", "ffn_w3", "ffn_w2",
          "moe_router", "moe_w1", "moe_w3", "moe_w2",
          "rwkv_wr", "rwkv_wk", "rwkv_wv", "rwkv_wo", "rwkv_w1", "rwkv_w2", "rwkv_a1", "rwkv_a2", "rwkv_g1", "rwkv_g2", "rwkv_v1", "rwkv_v2"]


def make_in_maps(inp, depth, cores):
    c32, cosT, sinT = host_consts()
    vecs = host_vecs(inp, depth)
    shared = {n: np.ascontiguousarray(inp[n], dtype=np.float32) for n in WNAMES}
    maps = []
    for b in cores:
        xs = np.ascontiguousarray(np.concatenate([inp["ctx"][b], inp["x"][b]], axis=0).T.astype(np.float32))
        cv = np.zeros((128, 16), np.float32)
        cv[:, 0::2] = fm(inp["c"][b])
        cv[:, 1::2] = fm(inp["c_ctx"])
        m = dict(shared)
        m.update(xs=xs, cvec=cv, vecs=vecs, c32=c32, ropec=cosT, ropes=sinT)
        maps.append(m)
    return maps


def kernel(**inp):
    depth = 4
    nc, S = build(depth)
    maps = make_in_maps(inp, depth, list(range(8)))
    res = run_bass_kernel_spmd(nc, maps, core_ids=list(range(8)))
    out = np.stack([np.ascontiguousarray(r["out"].T) for r in res.results], axis=0)
    return out.astype(np.float32)
```

```python
import contextlib
import os
import numpy as np
import concourse.bass as bass
import concourse.mybir as mybir
from concourse.bass_utils import run_bass_kernel_spmd

F32 = mybir.dt.float32
BF16 = mybir.dt.bfloat16
ALU = mybir.AluOpType
AF = mybir.ActivationFunctionType
AX = mybir.AxisListType
NS = 8

D = 1024
KC = 8
CTX = 256
TL = 4096
NT = CTX + TL
EPS = 1e-6
NH = 8
SM_SCALE = 192 ** -0.5
DFF = 2816
DFE = 3584
NE = 8
C32W = 128 * 3 + 8 * 128 + 128 + 64 + 128 + 64 + 128 + 64 + 512
TILES = [(0, 256, 1)] + [(256 + 512 * i, 512, 0) for i in range(8)]


class Sched:
    def __init__(self, nc):
        self.nc = nc
        self.engs = {'pe': nc.tensor, 'dve': nc.vector, 'act': nc.scalar,
                     'pool': nc.gpsimd, 'sp': nc.sync}
        self.sem = {e: nc.alloc_semaphore(name=f"sem_{e}") for e in ['pe', 'dve', 'act', 'pool']}
        self.cnt = {e: 0 for e in self.sem}
        self.dq = {}
        for q, e in [('sp', 'sp'), ('pool', 'pool')]:
            self.dq[q] = dict(eng=e, n=0,
                              sems=[nc.alloc_semaphore(name=f"dsem_{q}{i}") for i in range(NS)])
        self.waited = {}
        self.lastw = {}
        self.readers = {}
        self.nins = 0
        self.mute = False

    def _sid_val(self, tok):
        if tok[0] == 'c':
            return ('c', tok[1]), self.sem[tok[1]], tok[2]
        q = self.dq[tok[1]]
        n = tok[2]
        return ('d', tok[1], n % NS), q['sems'][n % NS], 16 * (n // NS + 1)

    def _wait(self, eng, tok):
        if tok[0] == 'c' and tok[1] == eng and eng == 'pe':
            return
        sid, sem, val = self._sid_val(tok)
        if tok[0] == 'c':
            assert val <= self.cnt[tok[1]], f"wait on unsignalled instr {tok}"
        if self.waited.get((eng, sid), 0) >= val:
            return
        self.engs[eng].wait_ge(sem, val)
        self.waited[(eng, sid)] = val
        self.nins += 1

    def _deps(self, reads, writes):
        deps = set()
        for k in reads:
            if k in self.lastw:
                deps.add(self.lastw[k])
        for k in writes:
            if k in self.lastw:
                deps.add(self.lastw[k])
            for t in self.readers.get(k, {}).values():
                deps.add(t)
        return deps

    def _record(self, tok, reads, writes):
        sid, _, val = self._sid_val(tok)
        for k in reads:
            r = self.readers.setdefault(k, {})
            old = r.get(sid)
            if old is None or self._sid_val(old)[2] < val:
                r[sid] = tok
        for k in writes:
            self.lastw[k] = tok
            self.readers[k] = {}

    def op(self, eng, fn, reads=(), writes=(), inc=True):
        if self.mute:
            return None
        if eng != 'pe':
            psr = [k for k in reads if isinstance(k, tuple) and k[0] in ('ps', 'psb', 'sps')]
            if psr:
                reads = [k for k in reads if k not in psr]
                writes = list(writes) + psr
        for t in self._deps(reads, writes):
            self._wait(eng, t)
        ins = fn(self.engs[eng])
        self.nins += 1
        if inc:
            self.cnt[eng] += 1
            ins.then_inc(self.sem[eng], 1)
            tok = ('c', eng, self.cnt[eng])
        else:
            tok = ('c', eng, self.cnt[eng] + 1)
        self._record(tok, reads, writes)
        return ins

    def dma(self, q, out, in_, reads=(), writes=(), **kw):
        if self.mute:
            return None
        Q = self.dq[q]
        eng = Q['eng']
        n = Q['n']
        for t in self._deps(reads, writes):
            self._wait(eng, t)
        if n >= NS:
            self._wait(eng, ('d', q, n - NS))
        ins = self.engs[eng].dma_start(out=out, in_=in_, **kw)
        ins.then_inc(Q['sems'][n % NS], 16)
        self.nins += 1
        Q['n'] += 1
        self._record(('d', q, n), reads, writes)
        return ins

    def barrier(self):
        toks = []
        for e, c in self.cnt.items():
            if c > 0:
                toks.append(('c', e, c))
        for q, Q in self.dq.items():
            for n in range(max(0, Q['n'] - NS), Q['n']):
                toks.append(('d', q, n))
        for e in ['pe', 'dve', 'act', 'pool', 'sp']:
            for t in toks:
                if t[0] == 'c' and t[1] == e:
                    continue
                self._wait(e, t)
        self.lastw.clear()
        self.readers.clear()


def vec_layout(depth):
    ents = []
    for i in range(depth):
        ents += [(f"adab{i}", 48), (f"n1g{i}", 8), (f"n2g{i}", 8)]
        j = i // 2
        if i % 2 == 0:
            ents += [(f"qan{j}", 3), (f"kvan{j}", 2), (f"qnn{j}", 1), (f"qnr{j}", 1), (f"knn{j}", 1), (f"knr{j}", 1)]
        else:
            ents += [(f"mix{j}", 48), (f"w0{j}", 16), (f"a0{j}", 16), (f"kk{j}", 8), (f"ka{j}", 8),
                     (f"rk{j}", 8), (f"lnw{j}", 8), (f"lnb{j}", 8)]
            if j >= 1:
                ents += [(f"v0{j}", 8)]
    off = {}
    o = 0
    for n, c in ents:
        off[n] = (o, c)
        o += c
    return off, o


def fm(v):
    v = np.asarray(v, np.float32).reshape(-1)
    pad = (-len(v)) % 128
    if pad:
        v = np.concatenate([v, np.zeros(pad, np.float32)])
    return np.ascontiguousarray(v.reshape(-1, 128).T)


def build(depth=4, dbg=None):
    nc = bass.Bass("TRN2", target_bir_lowering=False)
    voff, NV = vec_layout(depth)

    def din(name, shape, dt=F32):
        return nc.dram_tensor(name, list(shape), dt, kind="ExternalInput").ap()

    def dscr(name, shape, dt=F32):
        return nc.dram_tensor(name, list(shape), dt, kind="Internal").ap()

    xs_in = din("xs", [D, NT])
    cvec_d = din("cvec", [128, 16])
    vecs_d = din("vecs", [128, NV])
    c32_d = din("c32", [128, C32W])
    ropec_d = din("ropec", [64, TL])
    ropes_d = din("ropes", [64, TL])
    ada_w = din("ada_w", [4, D, 6 * D])
    mla_wqa = din("mla_wqa", [2, D, 384]); mla_wqb = din("mla_wqb", [2, 384, 1536])
    mla_wkva = din("mla_wkva", [2, D, 320]); mla_wkvb = din("mla_wkvb", [2, 256, 2048])
    mla_wo = din("mla_wo", [2, D, D])
    ffn_w1 = din("ffn_w1", [2, D, DFF]); ffn_w3 = din("ffn_w3", [2, D, DFF]); ffn_w2 = din("ffn_w2", [2, DFF, D])
    moe_router = din("moe_router", [2, D, NE])
    moe_w1 = din("moe_w1", [2, NE, D, DFE]); moe_w3 = din("moe_w3", [2, NE, D, DFE]); moe_w2 = din("moe_w2", [2, NE, DFE, D])
    rwkv_wr = din("rwkv_wr", [2, D, D]); rwkv_wk = din("rwkv_wk", [2, D, D]); rwkv_wv = din("rwkv_wv", [2, D, D]); rwkv_wo = din("rwkv_wo", [2, D, D])
    rwkv_w1 = din("rwkv_w1", [2, 2, D, 64]); rwkv_w2 = din("rwkv_w2", [2, 2, 64, D])
    rwkv_a1 = din("rwkv_a1", [2, 2, D, 64]); rwkv_a2 = din("rwkv_a2", [2, 2, 64, D])
    rwkv_g1 = din("rwkv_g1", [2, D, 160]); rwkv_g2 = din("rwkv_g2", [2, 160, D])
    rwkv_v1 = din("rwkv_v1", [1, D, 32]); rwkv_v2 = din("rwkv_v2", [1, 32, D])
    out_d = nc.dram_tensor("out", [D, TL], F32, kind="ExternalOutput").ap()
    HS = dscr("HS", [D, 258 + 4098])
    ATd = [dscr(f"ATd{d}", [D, NT], BF16) for d in range(2)]; BTd = [dscr(f"BTd{d}", [D, NT], BF16) for d in range(2)]
    KTd = [dscr(f"KTd{d}", [D, NT], BF16) for d in range(2)]; RTd = [dscr(f"RTd{d}", [D, NT], BF16) for d in range(2)]
    VTb = dscr("VTb", [D, NT], BF16)
    WCd = [dscr(f"WCd{d}", [D, NT // 64]) for d in range(2)]
    VT = [dscr(f"VT{d}", [D, NT]) for d in range(2)]
    YD = [dscr(f"YD{d}", [D, NT]) for d in range(2)]
    BON = dscr("BON", [D, NT]); GG = dscr("GG", [D, NT])

    XS = dscr("XS", [D, NT])
    QN = dscr("QN", [NH, 128, NT], BF16); QR = dscr("QR", [NH, 64, NT], BF16)
    KN = dscr("KN", [NH, 128, NT], BF16); KR = dscr("KR", [NH, 64, NT], BF16)
    VV = dscr("VV", [NT, NH, 128], BF16)
    AO = dscr("AO", [D, NT], BF16)

    S = Sched(nc)
    es = contextlib.ExitStack()

    uniq = [0]

    def sb(name, shape, dt=F32, stack=None):
        uniq[0] += 1
        return (stack or es).enter_context(nc.sbuf_tensor(f"s{uniq[0]}_{name}", list(shape), dt))

    with es:
        ps = [es.enter_context(nc.psum_tensor(f"ps{i}", [128, 512], F32)) for i in range(8)]
        vecs = sb("vecs", [128, NV])
        c32 = sb("c32", [128, C32W])
        ones_bf = sb("ones_bf", [128, 128], BF16)
        cvec = sb("cvec", [128, 16])
        modt = sb("modt", [128, depth, 48, 2])
        A1 = sb("A1", [128, depth, 8, 2]); A2 = sb("A2", [128, depth, 8, 2])
        S.dma('sp', vecs[:], vecs_d, writes=['vecs'])
        S.dma('sp', c32[:], c32_d, writes=['c32'])
        S.dma('sp', cvec[:], cvec_d, writes=['cvec'])
        EPSI = {1024: 0, 384: 1, 256: 2, 192: 3, 64: 4}
        epsT = sb("epsT", [128, 8])
        for nf, ci in EPSI.items():
            S.op('pool', lambda e: e.memset(epsT[:, ci:ci + 1], float(nf * EPS)), writes=['epsT'])
        ones32 = c32[:, 0:128]
        ident = c32[:, 128:256]
        rotP = c32[0:64, 256:320]
        selE = c32[0:8, 384:384 + 8 * 128]
        o_ = 384 + 1024
        blockones = c32[:, o_:o_ + 128]
        SI = c32[:, o_ + 128:o_ + 192]
        maskAR_f = c32[:, o_ + 192:o_ + 320]
        maskN_f = c32[:, o_ + 320:o_ + 384]
        maskAR_r = c32[:, o_ + 384:o_ + 512]
        maskN_r = c32[:, o_ + 512:o_ + 576]
        cmask = c32[:, o_ + 576:o_ + 1088]
        S.op('pool', lambda e: e.memset(epsT[:, 5:6], 64e-5), writes=['epsT'])
        S.op('dve', lambda e: e.tensor_copy(out=ones_bf[:], in_=ones32), reads=['c32'], writes=['ones_bf'])
        ident_bf = sb("ident_bf", [128, 128], BF16)
        S.op('dve', lambda e: e.tensor_copy(out=ident_bf[:], in_=ident), reads=['c32'], writes=['ident_bf'])

        def V(name, k0=0, k1=None):
            o, c = voff[name]
            k1 = c if k1 is None else k1
            return vecs[:, o + k0:o + k1]

        def phase_mod():
            with contextlib.ExitStack() as st:
                wb = [sb(f"adaw{i}", [128, 8, 1024], F32, st) for i in range(2)]
                sc = sb("sc", [128, 16], F32, st)
                S.op('act', lambda e: e.activation(out=sc[:], in_=cvec[:], func=AF.Silu), reads=['cvec'], writes=['sc'])
                n = 0
                for i in range(depth):
                    for j in range(6):
                        w = wb[n % 2]
                        S.dma('sp', w[:], ada_w[i, :, j * 1024:(j + 1) * 1024].rearrange("(k p) n -> p k n", p=128),
                              writes=[('adaw', n % 2)])
                        for oc in range(8):
                            col = (j * 8 + oc) * 2
                            for kc in range(8):
                                S.op('pe', lambda e: e.matmul(ps[0][:, col:col + 2], lhsT=w[:, kc, oc * 128:(oc + 1) * 128],
                                                              rhs=sc[:, 2 * kc:2 * kc + 2], start=(kc == 0), stop=(kc == 7)),
                                     reads=[('adaw', n % 2), 'sc'], writes=['psmod'], inc=(kc == 7 and oc == 7))
                        n += 1
                    S.op('dve', lambda e: e.tensor_tensor(
                        out=modt[:, i, :, :], in0=ps[0][:, 0:96].rearrange("p (a b) -> p a b", b=2),
                        in1=V(f"adab{i}").unsqueeze(2).to_broadcast([128, 48, 2]), op=ALU.add),
                        reads=['psmod', 'vecs'], writes=['modt'])
                    for (At, gname, jj) in [(A1, f"n1g{i}", 1), (A2, f"n2g{i}", 4)]:
                        S.op('dve', lambda e: e.scalar_tensor_tensor(
                            out=At[:, i, :, :], in0=modt[:, i, jj * 8:(jj + 1) * 8, :], scalar=1.0,
                            in1=V(gname).unsqueeze(2).to_broadcast([128, 8, 2]), op0=ALU.add, op1=ALU.mult),
                            reads=['modt', 'vecs'], writes=['A'])
                        S.op('dve', lambda e: e.tensor_scalar(out=At[:, i, :, :], in0=At[:, i, :, :], scalar1=float(np.sqrt(D)),
                                                              scalar2=None, op0=ALU.mult), reads=['A'], writes=['A'])
            S.barrier()

        def normmod(x32, N, Acol, Scol, outap, sq, rs, psb, kx, tag):
            ksq, krs, kps = ('sq', tag), 'rs', ('ps', psb)
            S.op('pool', lambda e: e.tensor_tensor(out=sq[:, :, :N], in0=x32, in1=x32, op=ALU.mult), reads=[kx], writes=[ksq])
            for k in range(8):
                S.op('pe', lambda e: e.matmul(ps[psb][:, :N], lhsT=ones32, rhs=sq[:, k, :N], start=(k == 0), stop=(k == 7)),
                     reads=[ksq, 'c32'], writes=[kps], inc=(k == 7))
            S.op('act', lambda e: e.activation(out=rs[:, :N], in_=ps[psb][:, :N], func=AF.Sqrt, bias=epsT[:, EPSI[D]:EPSI[D] + 1], scale=1.0),
                 reads=[kps, 'epsT'], writes=[krs])
            S.op('dve', lambda e: e.reciprocal(out=rs[:, :N], in_=rs[:, :N]), reads=[krs], writes=[krs])
            S.op('dve', lambda e: e.tensor_tensor(out=sq[:, :, :N], in0=x32, in1=rs[:, :N].unsqueeze(1).to_broadcast([128, 8, N]),
                                                  op=ALU.mult), reads=[kx, krs], writes=[ksq])
            S.op('pool', lambda e: e.tensor_tensor(out=sq[:, :, :N], in0=sq[:, :, :N], in1=Acol.unsqueeze(2).to_broadcast([128, 8, N]),
                                                   op=ALU.mult), reads=[ksq, 'A'], writes=[ksq])
            return ksq

        def rstd_from_ps(psb, N, nfeat, rs, krs, P=128):
            S.op('act', lambda e: e.activation(out=rs[:P, :N], in_=ps[psb][:P, :N], func=AF.Sqrt, bias=epsT[:P, EPSI[nfeat]:EPSI[nfeat] + 1], scale=1.0),
                 reads=[('ps', psb), 'epsT'], writes=[krs])
            S.op('dve', lambda e: e.reciprocal(out=rs[:P, :N], in_=rs[:P, :N]), reads=[krs], writes=[krs])

        def phase_mla(i, XSin):
            j = i // 2
            with contextlib.ExitStack() as st:
                wqa = sb("wqa", [128, 8, 384], BF16, st); wqb = sb("wqb", [128, 3, 1536], BF16, st)
                wkva = sb("wkva", [128, 8, 320], BF16, st); wkvb = sb("wkvb", [128, 2, 2048], BF16, st)
                S.dma('pool', wqa[:], mla_wqa[j].rearrange("(k p) n -> p k n", p=128), writes=['wqa'])
                S.dma('pool', wqb[:], mla_wqb[j].rearrange("(k p) n -> p k n", p=128), writes=['wqb'])
                S.dma('pool', wkva[:], mla_wkva[j].rearrange("(k p) n -> p k n", p=128), writes=['wkva'])
                S.dma('pool', wkvb[:], mla_wkvb[j].rearrange("(k p) n -> p k n", p=128), writes=['wkvb'])
                xt = [sb("xt0", [128, 8, 512], F32, st)] * 2
                sq = sb("sq", [128, 8, 512], F32, st)
                rs = sb("rs", [128, 512], F32, st)
                hb = sb("hb", [128, 8, 512], BF16, st)
                cq = sb("cq", [128, 3, 512], F32, st); cq2 = sb("cq2", [128, 3, 512], F32, st)
                cqn = sb("cqn", [128, 3, 512], BF16, st)
                ckv = sb("ckv", [128, 2, 512], F32, st); ckv2 = sb("ckv2", [128, 2, 512], F32, st)
                ckvn = sb("ckvn", [128, 2, 512], BF16, st)
                kr = sb("kr", [64, 512], F32, st); kr2 = sb("kr2", [64, 512], F32, st)
                krP = sb("krP", [64, 512], F32, st); krr = sb("krr", [64, 512], F32, st)
                qh = sb("qh", [128, 512], F32, st); qh2 = sb("qh2", [128, 512], F32, st)
                qr = sb("qr", [64, 512], F32, st); qr2 = sb("qr2", [64, 512], F32, st)
                qrP = sb("qrP", [64, 512], F32, st)
                rs2 = sb("rs2", [128, 512], F32, st)
                cosT = sb("cosT", [64, 512], F32, st); sinT = sb("sinT", [64, 512], F32, st)
                qn_o = sb("qn_o", [128, NH, 512], BF16, st); qr_o = sb("qr_o", [64, NH, 512], BF16, st)
                kn_o = sb("kn_o", [128, NH, 512], BF16, st); kr_o = sb("kr_o", [64, NH, 512], BF16, st)
                v_o = sb("v_o", [128, 4, NH, 128], BF16, st)
                sv = sb("sv", [128, 16], F32, st)
                S.op('dve', lambda e: e.tensor_scalar(out=sv[:, 0:3], in0=V(f"qan{j}"), scalar1=float(np.sqrt(384)), scalar2=None, op0=ALU.mult),
                     reads=['vecs'], writes=['sv'])
                S.op('dve', lambda e: e.tensor_scalar(out=sv[:, 3:5], in0=V(f"kvan{j}"), scalar1=float(np.sqrt(256)), scalar2=None, op0=ALU.mult),
                     reads=['vecs'], writes=['sv'])
                S.op('dve', lambda e: e.tensor_scalar(out=sv[:, 5:6], in0=V(f"qnn{j}"), scalar1=float(np.sqrt(192) * SM_SCALE), scalar2=None, op0=ALU.mult),
                     reads=['vecs'], writes=['sv'])
                S.op('dve', lambda e: e.tensor_scalar(out=sv[:, 6:7], in0=V(f"qnr{j}"), scalar1=float(np.sqrt(192) * SM_SCALE), scalar2=None, op0=ALU.mult),
                     reads=['vecs'], writes=['sv'])
                S.op('dve', lambda e: e.tensor_scalar(out=sv[:, 7:8], in0=V(f"knn{j}"), scalar1=float(np.sqrt(192)), scalar2=None, op0=ALU.mult),
                     reads=['vecs'], writes=['sv'])
                S.op('dve', lambda e: e.tensor_scalar(out=sv[:, 8:9], in0=V(f"knr{j}"), scalar1=float(np.sqrt(192)), scalar2=None, op0=ALU.mult),
                     reads=['vecs'], writes=['sv'])

                def rope(src, Pbuf, dst, N, ksrc, kdst, psb):
                    S.op('pe', lambda e: e.matmul(ps[psb][:64, :N], lhsT=rotP, rhs=src[:, :N], start=True, stop=True),
                         reads=[ksrc, 'c32'], writes=[('ps', psb)])
                    S.op('dve', lambda e: e.tensor_tensor(out=Pbuf[:, :N], in0=ps[psb][:64, :N], in1=sinT[:, :N], op=ALU.mult),
                         reads=[('ps', psb), 'rope'], writes=[('rp', kdst)])
                    S.op('pool', lambda e: e.tensor_tensor(out=dst[:, :N], in0=src[:, :N], in1=cosT[:, :N], op=ALU.mult),
                         reads=[ksrc, 'rope'], writes=[kdst])
                    S.op('pool', lambda e: e.tensor_tensor(out=dst[:, :N], in0=dst[:, :N], in1=Pbuf[:, :N], op=ALU.add),
                         reads=[kdst, ('rp', kdst)], writes=[kdst])

                for ti, (t0, N, mc) in enumerate(TILES):
                    x32 = xt[ti % 2]
                    kx = ('xt', ti % 2)
                    S.dma('sp', x32[:, :, :N], XSin[:, t0:t0 + N].rearrange("(k p) n -> p k n", p=128), writes=[kx])
                    if mc == 0:
                        S.dma('sp', cosT[:, :N], ropec_d[:, t0 - CTX:t0 - CTX + N], writes=['rope'])
                        S.dma('sp', sinT[:, :N], ropes_d[:, t0 - CTX:t0 - CTX + N], writes=['rope'])
                    ksq = normmod(x32[:, :, :N], N, A1[:, i, :, mc], None, None, sq, rs, 7, kx, 'm')
                    S.op('dve', lambda e: e.tensor_tensor(out=hb[:, :, :N], in0=sq[:, :, :N],
                                                          in1=modt[:, i, 0:8, mc].unsqueeze(2).to_broadcast([128, 8, N]), op=ALU.add),
                         reads=[ksq, 'modt'], writes=['hb'])
                    for c in range(3):
                        pb = c % 2
                        for kc in range(8):
                            S.op('pe', lambda e: e.matmul(ps[pb][:, :N], lhsT=wqa[:, kc, c * 128:(c + 1) * 128], rhs=hb[:, kc, :N],
                                                          start=(kc == 0), stop=(kc == 7)), reads=['hb', 'wqa'], writes=[('ps', pb)], inc=(kc == 7))
                        S.op('act', lambda e: e.activation(out=cq[:, c, :N], in_=ps[pb][:, :N], func=AF.Identity),
                             reads=[('ps', pb)], writes=[('cq', c)])
                        S.op('pool', lambda e: e.tensor_tensor(out=cq2[:, c, :N], in0=cq[:, c, :N], in1=cq[:, c, :N], op=ALU.mult),
                             reads=[('cq', c)], writes=[('cq2', c)])
                    for c in range(3):
                        S.op('pe', lambda e: e.matmul(ps[7][:, :N], lhsT=ones32, rhs=cq2[:, c, :N], start=(c == 0), stop=(c == 2)),
                             reads=[('cq2', c), 'c32'], writes=[('ps', 7)], inc=(c == 2))
                    rstd_from_ps(7, N, 384, rs, 'rs')
                    for c in range(3):
                        S.op('dve', lambda e: e.scalar_tensor_tensor(out=cqn[:, c, :N], in0=cq[:, c, :N], scalar=sv[:, c:c + 1], in1=rs[:, :N],
                                                                     op0=ALU.mult, op1=ALU.mult), reads=[('cq', c), 'rs', 'sv'], writes=['cqn'])
                    for c in range(3):
                        pb = 2 + c % 2
                        M = 128 if c < 2 else 64
                        for kc in range(8):
                            S.op('pe', lambda e: e.matmul(ps[pb][:M, :N], lhsT=wkva[:, kc, c * 128:c * 128 + M], rhs=hb[:, kc, :N],
                                                          start=(kc == 0), stop=(kc == 7)), reads=['hb', 'wkva'], writes=[('ps', pb)], inc=(kc == 7))
                        if c < 2:
                            S.op('act', lambda e: e.activation(out=ckv[:, c, :N], in_=ps[pb][:, :N], func=AF.Identity),
                                 reads=[('ps', pb)], writes=[('ckv', c)])
                            S.op('pool', lambda e: e.tensor_tensor(out=ckv2[:, c, :N], in0=ckv[:, c, :N], in1=ckv[:, c, :N], op=ALU.mult),
                                 reads=[('ckv', c)], writes=[('ckv2', c)])
                        else:
                            S.op('act', lambda e: e.activation(out=kr[:, :N], in_=ps[pb][:64, :N], func=AF.Identity),
                                 reads=[('ps', pb)], writes=['kr'])
                            S.op('pool', lambda e: e.tensor_tensor(out=kr2[:, :N], in0=kr[:, :N], in1=kr[:, :N], op=ALU.mult),
                                 reads=['kr'], writes=['kr2'])
                            S.op('dve', lambda e: e.tensor_scalar(out=kr[:, :N], in0=kr[:, :N], scalar1=sv[0:64, 8:9], scalar2=None, op0=ALU.mult),
                                 reads=['kr', 'sv', 'kr2'], writes=['kr'])
                    for c in range(2):
                        S.op('pe', lambda e: e.matmul(ps[7][:, :N], lhsT=ones32, rhs=ckv2[:, c, :N], start=(c == 0), stop=(c == 1)),
                             reads=[('ckv2', c), 'c32'], writes=[('ps', 7)], inc=(c == 1))
                    rstd_from_ps(7, N, 256, rs, 'rs')
                    for c in range(2):
                        S.op('dve', lambda e: e.scalar_tensor_tensor(out=ckvn[:, c, :N], in0=ckv[:, c, :N], scalar=sv[:, 3 + c:4 + c], in1=rs[:, :N],
                                                                     op0=ALU.mult, op1=ALU.mult), reads=[('ckv', c), 'rs', 'sv'], writes=['ckvn'])
                    if mc == 0:
                        rope(kr, krP, krr, N, 'kr', 'krr', 6)
                        krsrc, kkr = krr, 'krr'
                    else:
                        krsrc, kkr = kr, 'kr'
                    for h in range(NH):
                        for kc in range(3):
                            S.op('pe', lambda e: e.matmul(ps[0][:, :N], lhsT=wqb[:, kc, h * 192:h * 192 + 128], rhs=cqn[:, kc, :N],
                                                          start=(kc == 0), stop=(kc == 2)), reads=['cqn', 'wqb'], writes=[('ps', 0)], inc=(kc == 2))
                        for kc in range(3):
                            S.op('pe', lambda e: e.matmul(ps[1][:64, :N], lhsT=wqb[:, kc, h * 192 + 128:h * 192 + 192], rhs=cqn[:, kc, :N],
                                                          start=(kc == 0), stop=(kc == 2)), reads=['cqn', 'wqb'], writes=[('ps', 1)], inc=(kc == 2))
                        S.op('act', lambda e: e.activation(out=qh[:, :N], in_=ps[0][:, :N], func=AF.Identity), reads=[('ps', 0)], writes=['qh'])
                        S.op('act', lambda e: e.activation(out=qr[:, :N], in_=ps[1][:64, :N], func=AF.Identity), reads=[('ps', 1)], writes=['qr'])
                        S.op('pool', lambda e: e.tensor_tensor(out=qh2[:, :N], in0=qh[:, :N], in1=qh[:, :N], op=ALU.mult), reads=['qh'], writes=['qh2'])
                        S.op('pool', lambda e: e.tensor_tensor(out=qr2[:, :N], in0=qr[:, :N], in1=qr[:, :N], op=ALU.mult), reads=['qr'], writes=['qr2'])
                        S.op('pe', lambda e: e.matmul(ps[4][:, :N], lhsT=ones32, rhs=qh2[:, :N], start=True, stop=False),
                             reads=['qh2', 'c32'], writes=[('ps', 4)], inc=False)
                        S.op('pe', lambda e: e.matmul(ps[4][:, :N], lhsT=c32[0:64, 0:128], rhs=qr2[:, :N], start=False, stop=True),
                             reads=['qr2', 'c32'], writes=[('ps', 4)])
                        rstd_from_ps(4, N, 192, rs2, 'rs2')
                        S.op('dve', lambda e: e.scalar_tensor_tensor(out=qn_o[:, h, :N], in0=qh[:, :N], scalar=sv[:, 5:6], in1=rs2[:, :N],
                                                                     op0=ALU.mult, op1=ALU.mult), reads=['qh', 'rs2', 'sv'], writes=['qn_o'])
                        S.op('dve', lambda e: e.scalar_tensor_tensor(out=qr[:, :N], in0=qr[:, :N], scalar=sv[0:64, 6:7], in1=rs2[0:64, :N],
                                                                     op0=ALU.mult, op1=ALU.mult), reads=['qr', 'rs2', 'sv', 'qr2'], writes=['qr'])
                        if mc == 0:
                            rope(qr, qrP, qr2, N, 'qr', 'qr2', 6)
                            S.op('act', lambda e: e.activation(out=qr_o[:, h, :N], in_=qr2[:, :N], func=AF.Identity), reads=['qr2'], writes=['qr_o'])
                        else:
                            S.op('act', lambda e: e.activation(out=qr_o[:, h, :N], in_=qr[:, :N], func=AF.Identity), reads=['qr'], writes=['qr_o'])
                        for kc in range(2):
                            S.op('pe', lambda e: e.matmul(ps[2][:, :N], lhsT=wkvb[:, kc, h * 256:h * 256 + 128], rhs=ckvn[:, kc, :N],
                                                          start=(kc == 0), stop=(kc == 1)), reads=['ckvn', 'wkvb'], writes=[('ps', 2)], inc=(kc == 1))
                        S.op('act', lambda e: e.activation(out=qh[:, :N], in_=ps[2][:, :N], func=AF.Identity), reads=[('ps', 2)], writes=['qh'])
                        S.op('pool', lambda e: e.tensor_tensor(out=qh2[:, :N], in0=qh[:, :N], in1=qh[:, :N], op=ALU.mult), reads=['qh'], writes=['qh2'])
                        S.op('pe', lambda e: e.matmul(ps[5][:, :N], lhsT=ones32, rhs=qh2[:, :N], start=True, stop=False),
                             reads=['qh2', 'c32'], writes=[('ps', 5)], inc=False)
                        S.op('pe', lambda e: e.matmul(ps[5][:, :N], lhsT=c32[0:64, 0:128], rhs=kr2[:, :N], start=False, stop=True),
                             reads=['kr2', 'c32'], writes=[('ps', 5)])
                        rstd_from_ps(5, N, 192, rs2, 'rs2')
                        S.op('dve', lambda e: e.scalar_tensor_tensor(out=kn_o[:, h, :N], in0=qh[:, :N], scalar=sv[:, 7:8], in1=rs2[:, :N],
                                                                     op0=ALU.mult, op1=ALU.mult), reads=['qh', 'rs2', 'sv'], writes=['kn_o'])
                        S.op('dve', lambda e: e.tensor_tensor(out=kr_o[:, h, :N], in0=krsrc[:, :N], in1=rs2[0:64, :N], op=ALU.mult),
                             reads=[kkr, 'rs2'], writes=['kr_o'])
                        for blk in range(N // 128):
                            for kc in range(2):
                                S.op('pe', lambda e: e.matmul(ps[3][:, blk * 128:(blk + 1) * 128], lhsT=ckvn[:, kc, blk * 128:(blk + 1) * 128],
                                                              rhs=wkvb[:, kc, h * 256 + 128:h * 256 + 256], start=(kc == 0), stop=(kc == 1)),
                                     reads=['ckvn', 'wkvb'], writes=[('ps', 3)], inc=(kc == 1 and blk == N // 128 - 1))
                        S.op('act', lambda e: e.activation(out=v_o[:, 0:N // 128, h, :], in_=ps[3][:, :N].rearrange("p (b d) -> p b d", d=128),
                                                           func=AF.Identity), reads=[('ps', 3)], writes=['v_o'])
                    S.dma('sp', QN[:, :, t0:t0 + N].rearrange("h p n -> p h n"), qn_o[:, :, :N], reads=['qn_o'], writes=['QN'])
                    S.dma('sp', QR[:, :, t0:t0 + N].rearrange("h p n -> p h n"), qr_o[:, :, :N], reads=['qr_o'], writes=['QR'])
                    S.dma('sp', KN[:, :, t0:t0 + N].rearrange("h p n -> p h n"), kn_o[:, :, :N], reads=['kn_o'], writes=['KN'])
                    S.dma('sp', KR[:, :, t0:t0 + N].rearrange("h p n -> p h n"), kr_o[:, :, :N], reads=['kr_o'], writes=['KR'])
                    S.dma('sp', VV[t0:t0 + N].rearrange("(b p) h d -> p b h d", p=128), v_o[:, 0:N // 128], reads=['v_o'], writes=['VV'])
            S.barrier()
            with contextlib.ExitStack() as st:
                kn = [sb(f"a_kn{b}", [128, NT], BF16, st) for b in range(2)]
                krt = [sb(f"a_kr{b}", [64, NT], BF16, st) for b in range(2)]
                qn = [sb(f"a_qn{b}", [128, NT], BF16, st) for b in range(2)]
                qrt = [sb(f"a_qr{b}", [64, NT], BF16, st) for b in range(2)]
                vt = [sb(f"a_v{b}", [128, NT // 128, 128], BF16, st) for b in range(2)]
                pT = [sb(f"a_p{b}", [128, 512], BF16, st) for b in range(3)]
                rd = sb("a_rd", [128, 512], F32, st)
                ao = [sb(f"a_o{b}", [128, 512], BF16, st) for b in range(2)]
                npt = 0
                nq = 0
                for h in range(NH):
                    b = h % 2
                    kh = ('ah', b)
                    S.dma('sp', kn[b][:], KN[h], writes=[kh])
                    S.dma('sp', krt[b][:], KR[h], writes=[kh])
                    S.dma('sp', qn[b][:], QN[h], writes=[kh])
                    S.dma('sp', qrt[b][:], QR[h], writes=[kh])
                    S.dma('sp', vt[b][:], VV[:, h, :].rearrange("(b p) d -> p b d", p=128), writes=[kh])
                    for (t0, N, mc) in TILES:
                        nkb = 2 if mc == 1 else NT // 128
                        po, pd = 4 + (nq % 2), 6 + (nq % 2)

                        def scores(kb):
                            sbk = kb % 3
                            S.op('pe', lambda e: e.matmul(ps[sbk][:, :N], lhsT=kn[b][:, kb * 128:(kb + 1) * 128], rhs=qn[b][:, t0:t0 + N],
                                                          start=True, stop=False), reads=[kh], writes=[('ps', sbk)], inc=False)
                            S.op('pe', lambda e: e.matmul(ps[sbk][:, :N], lhsT=krt[b][:, kb * 128:(kb + 1) * 128], rhs=qrt[b][:, t0:t0 + N],
                                                          start=False, stop=True), reads=[kh], writes=[('ps', sbk)])
                        scores(0)
                        for kb in range(nkb):
                            if kb + 1 < nkb:
                                scores(kb + 1)
                            sbk = kb % 3
                            pb = npt % 3
                            npt += 1
                            S.op('act', lambda e: e.activation(out=pT[pb][:, :N], in_=ps[sbk][:, :N], func=AF.Exp),
                                 reads=[('ps', sbk)], writes=[('pT', pb)])
                            S.op('pe', lambda e: e.matmul(ps[po][:, :N], lhsT=vt[b][:, kb, :], rhs=pT[pb][:, :N], start=(kb == 0), stop=(kb == nkb - 1)),
                                 reads=[kh, ('pT', pb)], writes=[('ps', po)], inc=False)
                            S.op('pe', lambda e: e.matmul(ps[pd][:, :N], lhsT=ones_bf[:], rhs=pT[pb][:, :N], start=(kb == 0), stop=(kb == nkb - 1)),
                                 reads=['ones_bf', ('pT', pb)], writes=[('ps', pd)])
                        S.op('dve', lambda e: e.reciprocal(out=rd[:, :N], in_=ps[pd][:, :N]), reads=[('ps', pd)], writes=['rd'])
                        ob = nq % 2
                        S.op('dve', lambda e: e.tensor_tensor(out=ao[ob][:, :N], in0=ps[po][:, :N], in1=rd[:, :N], op=ALU.mult),
                             reads=[('ps', po), 'rd'], writes=[('ao', ob)])
                        S.dma('sp', AO[h * 128:(h + 1) * 128, t0:t0 + N], ao[ob][:, :N], reads=[('ao', ob)], writes=['AO'])
                        nq += 1
            S.barrier()
            with contextlib.ExitStack() as st:
                wo = sb("wo", [128, 8, 1024], BF16, st)
                S.dma('pool', wo[:], mla_wo[j].rearrange("(k p) n -> p k n", p=128), writes=['wo'])
                xt = [sb(f"c_xt{b}", [128, 8, 512], F32, st) for b in range(2)]
                at = [sb(f"c_at{b}", [128, 8, 512], BF16, st) for b in range(2)]
                for ti, (t0, N, mc) in enumerate(TILES):
                    b = ti % 2
                    S.dma('sp', xt[b][:, :, :N], XSin[:, t0:t0 + N].rearrange("(k p) n -> p k n", p=128), writes=[('cx', b)])
                    S.dma('sp', at[b][:, :, :N], AO[:, t0:t0 + N].rearrange("(k p) n -> p k n", p=128), writes=[('ca', b)])
                    for oc in range(8):
                        pb = oc % 4
                        for kc in range(8):
                            S.op('pe', lambda e: e.matmul(ps[pb][:, :N], lhsT=wo[:, kc, oc * 128:(oc + 1) * 128], rhs=at[b][:, kc, :N],
                                                          start=(kc == 0), stop=(kc == 7)), reads=['wo', ('ca', b)], writes=[('ps', pb)], inc=(kc == 7))
                        S.op('dve', lambda e: e.scalar_tensor_tensor(out=xt[b][:, oc, :N], in0=ps[pb][:, :N], scalar=modt[:, i, 16 + oc, mc:mc + 1],
                                                                     in1=xt[b][:, oc, :N], op0=ALU.mult, op1=ALU.add),
                             reads=[('ps', pb), ('cx', b), 'modt'], writes=[('cx', b)])
                    S.dma('sp', XS[:, t0:t0 + N].rearrange("(k p) n -> p k n", p=128), xt[b][:, :, :N], reads=[('cx', b)], writes=['XS'])
            S.barrier()

        def phase_rwkv(i, last):
            j = i // 2
            NCH = NT // 64
            RT256 = [(0, 256, 1)] + [(256 + 256 * a, 256, 0) for a in range(16)]
            HC = HS[:, 0:258]
            HL = HS[:, 258:258 + 4098]
            with contextlib.ExitStack() as st:
                xt = [sb(f"r1x{b}", [128, 8, 512], F32, st) for b in range(2)]
                sq = sb("r1sq", [128, 8, 512], F32, st)
                rs = sb("r1rs", [128, 512], F32, st)
                zt = sb("r1z", [128, 8, 1], F32, st)
                S.op('pool', lambda e: e.memset(zt[:], 0.0), writes=['zt'])
                for col in (0, 257, 258, 258 + 4097):
                    S.dma('sp', HS[:, col:col + 1].rearrange("(k p) n -> p k n", p=128), zt[:], reads=['zt'], writes=['HS'], allow_slow_non_contiguous=True)
                for ti, (t0, N, mc) in enumerate(TILES):
                    b = ti % 2
                    kx = ('r1x', b)
                    S.dma('sp', xt[b][:, :, :N], XS[:, t0:t0 + N].rearrange("(k p) n -> p k n", p=128), writes=[kx])
                    ksq = normmod(xt[b][:, :, :N], N, A1[:, i, :, mc], None, None, sq, rs, 7, kx, 'r1')
                    S.op('dve', lambda e: e.tensor_tensor(out=xt[b][:, :, :N], in0=sq[:, :, :N],
                                                          in1=modt[:, i, 0:8, mc].unsqueeze(2).to_broadcast([128, 8, N]), op=ALU.add),
                         reads=[ksq, 'modt'], writes=[kx])
                    dst = HC[:, 1:257] if mc == 1 else HL[:, 1 + t0 - CTX:1 + t0 - CTX + N]
                    S.dma('sp', dst.rearrange("(k p) n -> p k n", p=128), xt[b][:, :, :N], reads=[kx], writes=['HS'])
            S.barrier()
            if os.environ.get('RSTOP') == '1':
                return
            class _Stop(Exception):
                pass

            def stg(x):
                if os.environ.get('R2STOP') == x:
                    S.mute = True
            try:
              with contextlib.ExitStack() as st:
                  N = 256
                  wr = sb("wr", [128, 8, 1024], BF16, st); wk = sb("wk", [128, 8, 1024], BF16, st); wv = sb("wv", [128, 8, 1024], BF16, st)
                  w1c = sb("w1c", [128, 8, 128], BF16, st); a1c = sb("a1c", [128, 8, 128], BF16, st)
                  g1 = sb("g1", [128, 8, 160], BF16, st); g2a = sb("g2a", [128, 1024], BF16, st); g2b = sb("g2b", [32, 1024], BF16, st)
                  w2p = sb("w2p", [128, 2, 1024], BF16, st); a2p = sb("a2p", [128, 2, 1024], BF16, st)
                  for (wt_, src, kk_) in [(wr, rwkv_wr, 'wr'), (wk, rwkv_wk, 'wk'), (wv, rwkv_wv, 'wv')]:
                      S.dma('pool', wt_[:], src[j].rearrange("(k p) n -> p k n", p=128), writes=[kk_])
                  for d in range(2):
                      S.dma('pool', w1c[:, :, d * 64:(d + 1) * 64], rwkv_w1[j, d].rearrange("(k p) n -> p k n", p=128), writes=['w1c'])
                      S.dma('pool', a1c[:, :, d * 64:(d + 1) * 64], rwkv_a1[j, d].rearrange("(k p) n -> p k n", p=128), writes=['a1c'])
                  S.dma('pool', g1[:], rwkv_g1[j].rearrange("(k p) n -> p k n", p=128), writes=['g1'])
                  S.dma('pool', g2a[:], rwkv_g2[j, 0:128, :], writes=['g2a'])
                  S.dma('pool', g2b[:], rwkv_g2[j, 128:160, :], writes=['g2b'])
                  S.op('pool', lambda e: e.memset(w2p[:], 0.0), writes=['w2p'])
                  S.op('pool', lambda e: e.memset(a2p[:], 0.0), writes=['a2p'])
                  for d in range(2):
                      S.dma('pool', w2p[d * 64:(d + 1) * 64, d, :], rwkv_w2[j, d], writes=['w2p'])
                      S.dma('pool', a2p[d * 64:(d + 1) * 64, d, :], rwkv_a2[j, d], writes=['a2p'])
                  vres = j >= 1
                  if vres:
                      v1 = sb("v1", [128, 8, 32], BF16, st); v2 = sb("v2", [32, 1024], BF16, st)
                      S.dma('pool', v1[:], rwkv_v1[j - 1].rearrange("(k p) n -> p k n", p=128), writes=['v1'])
                      S.dma('pool', v2[:], rwkv_v2[j - 1], writes=['v2'])
                      vf = sb("vf", [128, N], F32, st)
                  dv_ = sb("dv", [128, 16], F32, st)
                  S.op('dve', lambda e: e.tensor_scalar(out=dv_[:, 0:8], in0=V(f"ka{j}"), scalar1=-1.0, scalar2=1.0, op0=ALU.mult, op1=ALU.add),
                       reads=['vecs'], writes=['dv'])
                  S.op('dve', lambda e: e.tensor_scalar(out=dv_[:, 8:16], in0=V(f"rk{j}"), scalar1=0.5, scalar2=None, op0=ALU.mult),
                       reads=['vecs'], writes=['dv'])
                  hx = sb("hx", [128, 8, N + 2], F32, st)
                  xx = sb("xx", [128, 8, N], F32, st)
                  xm = [sb(f"xm{m}", [128, 8, N], BF16, st) for m in range(6)]
                  lwm = sb("lwm", [128, N], BF16, st); am = sb("am", [128, N], BF16, st)
                  gma = sb("gma", [128, N], BF16, st); gmb = sb("gmb", [32, N], BF16, st); vm = sb("vm", [32, N], BF16, st)
                  T = {}
                  for nm in ["r32", "k32", "v32", "g32", "vv", "dvv", "kkc", "kk2", "rn", "kkn", "aneg", "sig", "lw", "al", "tk", "kd", "bb",
                             "Lp", "Lc", "E1", "E2", "E3", "t2", "At", "Rt", "Bt", "Kt", "kb", "t3", "bon", "vb"]:
                      T[nm] = [sb(f"f_{nm}{q}", [128, N], BF16 if nm in ("At", "Rt", "Bt", "Kt", "vb") else F32, st) for q in range(2)]
                  wct = sb("wct", [128, 4], F32, st)

                  def hb_(n):
                      return ps[n // 2][:, (n % 2) * 256:(n % 2) * 256 + 256]

                  def hk(n):
                      return ('psb', n // 2)

                  for ti, (t0, N_, mc) in enumerate(RT256):
                      src = HC[:, 0:258] if mc == 1 else HL[:, t0 - CTX:t0 - CTX + N + 2]
                      S.dma('sp', hx[:], src.rearrange("(k p) n -> p k n", p=128), writes=['hx'])
                      S.op('dve', lambda e: e.tensor_tensor(out=xx[:], in0=hx[:, :, 0:N], in1=hx[:, :, 2:N + 2], op=ALU.add), reads=['hx'], writes=['xx'])
                      S.op('dve', lambda e: e.scalar_tensor_tensor(out=xx[:], in0=xx[:], scalar=0.5, in1=hx[:, :, 1:N + 1], op0=ALU.mult, op1=ALU.subtract),
                           reads=['hx', 'xx'], writes=['xx'])
                      for m in range(6):
                          for kc in range(8):
                              S.op('dve', lambda e: e.scalar_tensor_tensor(out=xm[m][:, kc, :], in0=xx[:, kc, :], scalar=V(f"mix{j}", m * 8 + kc, m * 8 + kc + 1),
                                                                           in1=hx[:, kc, 1:N + 1], op0=ALU.mult, op1=ALU.add),
                                   reads=['hx', 'xx', 'vecs'], writes=[('xm', m)])
                      stg('a')
                      for kc in range(8):
                          S.op('pe', lambda e: e.matmul(hb_(0), lhsT=w1c[:, kc, :], rhs=xm[1][:, kc, :], start=(kc == 0), stop=(kc == 7)),
                               reads=['w1c', ('xm', 1)], writes=[hk(0)], inc=(kc == 7))
                      S.op('act', lambda e: e.activation(out=T["sig"][0][:], in_=hb_(0), func=AF.Sigmoid, scale=2.0), reads=[hk(0)], writes=['sig0'])
                      S.op('dve', lambda e: e.tensor_scalar(out=lwm[:], in0=T["sig"][0][:], scalar1=2.0, scalar2=-1.0, op0=ALU.mult, op1=ALU.add),
                           reads=['sig0'], writes=['lwm'])
                      for kc in range(8):
                          S.op('pe', lambda e: e.matmul(hb_(1), lhsT=a1c[:, kc, :], rhs=xm[4][:, kc, :], start=(kc == 0), stop=(kc == 7)),
                               reads=['a1c', ('xm', 4)], writes=[hk(1)], inc=(kc == 7))
                      S.op('act', lambda e: e.activation(out=am[:], in_=hb_(1), func=AF.Identity), reads=[hk(1)], writes=['am'])
                      for kc in range(8):
                          S.op('pe', lambda e: e.matmul(hb_(2), lhsT=g1[:, kc, 0:128], rhs=xm[5][:, kc, :], start=(kc == 0), stop=(kc == 7)),
                               reads=['g1', ('xm', 5)], writes=[hk(2)], inc=(kc == 7))
                      S.op('act', lambda e: e.activation(out=gma[:], in_=hb_(2), func=AF.Sigmoid), reads=[hk(2)], writes=['gma'])
                      for kc in range(8):
                          S.op('pe', lambda e: e.matmul(hb_(3)[0:32, :], lhsT=g1[:, kc, 128:160], rhs=xm[5][:, kc, :], start=(kc == 0), stop=(kc == 7)),
                               reads=['g1', ('xm', 5)], writes=[hk(3)], inc=(kc == 7))
                      S.op('act', lambda e: e.activation(out=gmb[:], in_=hb_(3)[0:32, :], func=AF.Sigmoid), reads=[hk(3)], writes=['gmb'])
                      if vres:
                          for kc in range(8):
                              S.op('pe', lambda e: e.matmul(hb_(4)[0:32, :], lhsT=v1[:, kc, :], rhs=xm[3][:, kc, :], start=(kc == 0), stop=(kc == 7)),
                                   reads=['v1', ('xm', 3)], writes=[hk(4)], inc=(kc == 7))
                          S.op('act', lambda e: e.activation(out=vm[:], in_=hb_(4)[0:32, :], func=AF.Identity), reads=[hk(4)], writes=['vm'])
                      for c in range(8):
                          pc = c % 2
                          pcs = str(pc)
                          cs = slice(c * 128, (c + 1) * 128)
                          rows = slice(c * 128, (c + 1) * 128)
                          stg('b')
                          for (hbn, w_, m) in [(5, wr, 0), (6, wk, 2), (7, wv, 3)]:
                              for kc in range(8):
                                  S.op('pe', lambda e: e.matmul(hb_(hbn), lhsT=w_[:, kc, cs], rhs=xm[m][:, kc, :], start=(kc == 0), stop=(kc == 7)),
                                       reads=[('xm', m), 'wr', 'wk', 'wv'], writes=[hk(hbn)], inc=(kc == 7))
                          S.op('pe', lambda e: e.matmul(hb_(8), lhsT=g2a[:, cs], rhs=gma[:], start=True, stop=False), reads=['g2a', 'gma'], writes=[hk(8)], inc=False)
                          S.op('pe', lambda e: e.matmul(hb_(8), lhsT=g2b[:, cs], rhs=gmb[:], start=False, stop=True), reads=['g2b', 'gmb'], writes=[hk(8)])
                          for d in range(2):
                              S.op('pe', lambda e: e.matmul(hb_(9 + d), lhsT=w2p[:, d, cs], rhs=lwm[:], start=True, stop=True), reads=['w2p', 'lwm'], writes=[hk(9 + d)])
                              S.op('pe', lambda e: e.matmul(hb_(11 + d), lhsT=a2p[:, d, cs], rhs=am[:], start=True, stop=True), reads=['a2p', 'am'], writes=[hk(11 + d)])
                          if vres:
                              S.op('pe', lambda e: e.matmul(hb_(13), lhsT=v2[:, cs], rhs=vm[:], start=True, stop=True), reads=['v2', 'vm'], writes=[hk(13)])
                          S.op('act', lambda e: e.activation(out=T["r32"][pc][:], in_=hb_(5), func=AF.Identity), reads=[hk(5)], writes=['r32' + pcs])
                          S.op('act', lambda e: e.activation(out=T["k32"][pc][:], in_=hb_(6), func=AF.Identity), reads=[hk(6)], writes=['k32' + pcs])
                          S.op('act', lambda e: e.activation(out=T["v32"][pc][:], in_=hb_(7), func=AF.Identity), reads=[hk(7)], writes=['v32' + pcs])
                          S.op('act', lambda e: e.activation(out=T["g32"][pc][:], in_=hb_(8), func=AF.Identity), reads=[hk(8)], writes=['g32' + pcs])
                          if vres:
                              S.dma('sp', vf[:], VT[0][rows, t0:t0 + N], writes=['vf'])
                              S.op('act', lambda e: e.activation(out=T["vv"][pc][:], in_=hb_(13), func=AF.Sigmoid, bias=V(f"v0{j}", c, c + 1), scale=1.0),
                                   reads=[hk(13), 'vecs'], writes=['vv' + pcs])
                              S.op('pool', lambda e: e.tensor_tensor(out=T["dvv"][pc][:], in0=vf[:], in1=T["v32"][pc][:], op=ALU.subtract), reads=['vf', 'v32' + pcs], writes=['dvv' + pcs])
                              S.op('pool', lambda e: e.tensor_tensor(out=T["dvv"][pc][:], in0=T["dvv"][pc][:], in1=T["vv"][pc][:], op=ALU.mult), reads=['dvv' + pcs, 'vv' + pcs], writes=['dvv' + pcs])
                              S.op('pool', lambda e: e.tensor_tensor(out=T["v32"][pc][:], in0=T["v32"][pc][:], in1=T["dvv"][pc][:], op=ALU.add), reads=['dvv' + pcs, 'v32' + pcs], writes=['v32' + pcs])
                          stg('c')
                          S.op('dve', lambda e: e.tensor_scalar(out=T["kkc"][pc][:], in0=T["k32"][pc][:], scalar1=V(f"kk{j}", c, c + 1), scalar2=None, op0=ALU.mult),
                               reads=['k32' + pcs, 'vecs'], writes=['kkc' + pcs])
                          S.op('pool', lambda e: e.tensor_tensor(out=T["kk2"][pc][:], in0=T["kkc"][pc][:], in1=T["kkc"][pc][:], op=ALU.mult), reads=['kkc' + pcs], writes=['kk2' + pcs])
                          S.op('pe', lambda e: e.matmul(hb_(14), lhsT=blockones, rhs=T["kk2"][pc][:], start=True, stop=True), reads=['kk2' + pcs, 'c32'], writes=[hk(14)])
                          S.op('act', lambda e: e.activation(out=T["rn"][pc][:], in_=hb_(14), func=AF.Sqrt), reads=[hk(14)], writes=['rn' + pcs])
                          S.op('dve', lambda e: e.tensor_scalar(out=T["rn"][pc][:], in0=T["rn"][pc][:], scalar1=1e-12, scalar2=None, op0=ALU.max), reads=['rn' + pcs], writes=['rn' + pcs])
                          S.op('dve', lambda e: e.reciprocal(out=T["rn"][pc][:], in_=T["rn"][pc][:]), reads=['rn' + pcs], writes=['rn' + pcs])
                          S.op('pool', lambda e: e.tensor_tensor(out=T["kkn"][pc][:], in0=T["kkc"][pc][:], in1=T["rn"][pc][:], op=ALU.mult), reads=['kkc' + pcs, 'rn' + pcs], writes=['kkn' + pcs])
                          S.op('pool', lambda e: e.tensor_scalar(out=T["aneg"][pc][:], in0=T["kkn"][pc][:], scalar1=-1.0, scalar2=None, op0=ALU.mult), reads=['kkn' + pcs], writes=['aneg' + pcs])
                          stg('d')
                          for d in range(2):
                              S.op('act', lambda e: e.activation(out=T["sig"][pc][:], in_=hb_(9 + d), func=AF.Sigmoid, bias=V(f"w0{j}", d * 8 + c, d * 8 + c + 1), scale=1.0),
                                   reads=[hk(9 + d), 'vecs'], writes=['sig' + pcs])
                              S.op('pool', lambda e: e.tensor_scalar(out=T["lw"][pc][:], in0=T["sig"][pc][:], scalar1=float(-np.exp(-0.5)), scalar2=None, op0=ALU.mult),
                                   reads=['sig' + pcs], writes=['lw' + pcs])
                              S.op('act', lambda e: e.activation(out=T["al"][pc][:], in_=hb_(11 + d), func=AF.Sigmoid, bias=V(f"a0{j}", d * 8 + c, d * 8 + c + 1), scale=1.0),
                                   reads=[hk(11 + d), 'vecs'], writes=['al' + pcs])
                              S.op('dve', lambda e: e.tensor_scalar(out=T["tk"][pc][:], in0=T["al"][pc][:], scalar1=V(f"ka{j}", c, c + 1), scalar2=dv_[:, c:c + 1],
                                                                    op0=ALU.mult, op1=ALU.add), reads=['al' + pcs, 'vecs', 'dv'], writes=['tk' + pcs])
                              S.op('pool', lambda e: e.tensor_tensor(out=T["kd"][pc][:], in0=T["k32"][pc][:], in1=T["tk"][pc][:], op=ALU.mult), reads=['k32' + pcs, 'tk' + pcs], writes=['kd' + pcs])
                              S.op('pool', lambda e: e.tensor_tensor(out=T["bb"][pc][:], in0=T["kkn"][pc][:], in1=T["al"][pc][:], op=ALU.mult), reads=['kkn' + pcs, 'al' + pcs], writes=['bb' + pcs])
                              S.op('dve', lambda e: e.tensor_tensor_scan(out=T["Lp"][pc][:], data0=cmask[:, :N], data1=T["lw"][pc][:], initial=0.0, op0=ALU.mult, op1=ALU.add),
                                   reads=['lw' + pcs, 'c32'], writes=['Lp' + pcs])
                              if d == 0:
                                  Lc, kLc = T["Lp"][pc], 'Lp' + pcs
                              else:
                                  S.op('pool', lambda e: e.tensor_tensor(out=T["Lc"][pc][:], in0=T["lw"][pc][:], in1=T["Lp"][pc][:], op=ALU.subtract), reads=['lw' + pcs, 'Lp' + pcs], writes=['Lc' + pcs])
                                  S.op('pool', lambda e: e.tensor_tensor(
                                      out=T["Lc"][pc][:].rearrange("p (c t) -> p c t", t=64), in0=T["Lc"][pc][:].rearrange("p (c t) -> p c t", t=64),
                                      in1=T["Lp"][pc][:].rearrange("p (c t) -> p c t", t=64)[:, :, 63:64].to_broadcast([128, N // 64, 64]), op=ALU.add),
                                      reads=['Lc' + pcs, 'Lp' + pcs], writes=['Lc' + pcs])
                                  Lc, kLc = T["Lc"][pc], 'Lc' + pcs
                              S.op('act', lambda e: e.activation(out=T["E1"][pc][:], in_=Lc[:], func=AF.Exp), reads=[kLc], writes=['E1' + pcs])
                              S.op('act', lambda e: e.activation(out=T["E2"][pc][:], in_=Lc[:], func=AF.Exp, scale=-1.0), reads=[kLc], writes=['E2' + pcs])
                              S.op('pool', lambda e: e.tensor_tensor(out=T["t2"][pc][:], in0=Lc[:], in1=T["lw"][pc][:], op=ALU.subtract), reads=[kLc, 'lw' + pcs], writes=['t2' + pcs])
                              S.op('act', lambda e: e.activation(out=T["E3"][pc][:], in_=T["t2"][pc][:], func=AF.Exp), reads=['t2' + pcs], writes=['E3' + pcs])
                              S.op('pool', lambda e: e.tensor_tensor(out=T["At"][pc][:], in0=T["aneg"][pc][:], in1=T["E3"][pc][:], op=ALU.mult), reads=['aneg' + pcs, 'E3' + pcs], writes=['At' + pcs])
                              S.op('pool', lambda e: e.tensor_tensor(out=T["Rt"][pc][:], in0=T["r32"][pc][:], in1=T["E1"][pc][:], op=ALU.mult), reads=['r32' + pcs, 'E1' + pcs], writes=['Rt' + pcs])
                              S.op('dve', lambda e: e.tensor_tensor(out=T["Bt"][pc][:], in0=T["bb"][pc][:], in1=T["E2"][pc][:], op=ALU.mult), reads=['bb' + pcs, 'E2' + pcs], writes=['Bt' + pcs])
                              S.op('dve', lambda e: e.tensor_tensor(out=T["Kt"][pc][:], in0=T["kd"][pc][:], in1=T["E2"][pc][:], op=ALU.mult), reads=['kd' + pcs, 'E2' + pcs], writes=['Kt' + pcs])
                              wcol = 63 if d == 0 else 0
                              S.op('dve', lambda e: e.tensor_copy(out=wct[:, 0:N // 64], in_=T["E1"][pc][:].rearrange("p (c t) -> p c t", t=64)[:, :, wcol]),
                                   reads=['E1' + pcs], writes=['wct'])
                              S.dma('sp', ATd[d][rows, t0:t0 + N], T["At"][pc][:], reads=['At' + pcs], writes=['ATd'])
                              S.dma('sp', RTd[d][rows, t0:t0 + N], T["Rt"][pc][:], reads=['Rt' + pcs], writes=['RTd'])
                              S.dma('sp', BTd[d][rows, t0:t0 + N], T["Bt"][pc][:], reads=['Bt' + pcs], writes=['BTd'])
                              S.dma('sp', KTd[d][rows, t0:t0 + N], T["Kt"][pc][:], reads=['Kt' + pcs], writes=['KTd'])
                              S.dma('sp', WCd[d][rows, t0 // 64:t0 // 64 + N // 64], wct[:, 0:N // 64], reads=['wct'], writes=['WCd'])
                              if d == 0:
                                  S.op('pool', lambda e: e.tensor_copy(out=T["kb"][pc][:], in_=T["kd"][pc][:]), reads=['kd' + pcs], writes=['kb' + pcs])
                              else:
                                  S.op('pool', lambda e: e.tensor_tensor(out=T["kb"][pc][:], in0=T["kb"][pc][:], in1=T["kd"][pc][:], op=ALU.add), reads=['kd' + pcs, 'kb' + pcs], writes=['kb' + pcs])
                          stg('e')
                          S.op('pool', lambda e: e.tensor_tensor(out=T["t3"][pc][:], in0=T["r32"][pc][:], in1=T["kb"][pc][:], op=ALU.mult), reads=['r32' + pcs, 'kb' + pcs], writes=['t3' + pcs])
                          S.op('dve', lambda e: e.tensor_scalar(out=T["t3"][pc][:], in0=T["t3"][pc][:], scalar1=dv_[:, 8 + c:9 + c], scalar2=None, op0=ALU.mult),
                               reads=['t3' + pcs, 'dv'], writes=['t3' + pcs])
                          S.op('pe', lambda e: e.matmul(hb_(15), lhsT=blockones, rhs=T["t3"][pc][:], start=True, stop=True), reads=['t3' + pcs, 'c32'], writes=[hk(15)])
                          S.op('dve', lambda e: e.tensor_tensor(out=T["bon"][pc][:], in0=hb_(15), in1=T["v32"][pc][:], op=ALU.mult), reads=[hk(15), 'v32' + pcs], writes=['bon' + pcs])
                          S.dma('sp', BON[rows, t0:t0 + N], T["bon"][pc][:], reads=['bon' + pcs], writes=['BON'])
                          S.dma('sp', VT[j][rows, t0:t0 + N], T["v32"][pc][:], reads=['v32' + pcs], writes=['VT'])
                          S.op('act', lambda e: e.activation(out=T["vb"][pc][:], in_=T["v32"][pc][:], func=AF.Identity), reads=['v32' + pcs], writes=['vb' + pcs])
                          S.dma('sp', VTb[rows, t0:t0 + N], T["vb"][pc][:], reads=['vb' + pcs], writes=['VTb'])
                          S.dma('sp', GG[rows, t0:t0 + N], T["g32"][pc][:], reads=['g32' + pcs], writes=['GG'])

            except _Stop:
                pass
            S.mute = False
            S.barrier()
            if os.environ.get('RSTOP') == '2':
                return
            for d in range(2):
                order = ([0, 1, 2, 3] + list(range(4, NCH))) if d == 0 else ([3, 2, 1, 0] + list(range(NCH - 1, 3, -1)))
                mAR = maskAR_f if d == 0 else maskAR_r
                mN = maskN_f if d == 0 else maskN_r
                with contextlib.ExitStack() as st:
                    def stream(hh):
                        P_ = f"s{hh}_"
                        pb = [ps[4 * hh + q] for q in range(4)]

                        def pk(bank, half=None):
                            return [('sps', hh, bank)]
                        BD = {n: sb(P_ + n, [128, 4, 128], BF16, st) for n in ["bdA", "T2", "T3", "T4", "tbB", "tbK", "tbV", "T8", "bdMak", "bdU", "bdST"]}
                        for n, t_ in BD.items():
                            S.op('pool', lambda e: e.memset(t_[:], 0.0), writes=[P_ + n])
                        AR = [sb(P_ + f"AR{b}", [128, 4, 2, 64], BF16, st) for b in range(2)]
                        Bi = [sb(P_ + f"Bi{b}", [128, 4, 64], BF16, st) for b in range(2)]
                        Ki = [sb(P_ + f"Ki{b}", [128, 4, 64], BF16, st) for b in range(2)]
                        Vi = [sb(P_ + f"Vi{b}", [128, 4, 64], BF16, st) for b in range(2)]
                        WCall = sb(P_ + "WCall", [128, 4, NCH], F32, st)
                        S.dma('sp', WCall[:], WCd[d][hh * 512:hh * 512 + 512, :].rearrange("(c p) n -> p c n", p=128), writes=[P_ + "WCall"])
                        ARm = sb(P_ + "ARm", [128, 4, 128], BF16, st)
                        AKm = sb(P_ + "AKm", [128, 4, 128], BF16, st)
                        Ns = sb(P_ + "Ns", [128, 4, 64], BF16, st)
                        Vs = sb(P_ + "Vs", [128, 4, 64], BF16, st)
                        PG = [sb(P_ + f"PG{b}", [128, 4, 128], BF16, st) for b in range(2)]
                        Pst = [sb(P_ + f"Pst{b}", [128, 4, 64], BF16, st) for b in range(2)]
                        Xs = sb(P_ + "Xs", [128, 4, 64], BF16, st); Us = sb(P_ + "Us", [128, 4, 64], BF16, st)
                        Yb = sb(P_ + "Yb", [128, 4, 64], F32, st); STs = sb(P_ + "STs", [128, 4, 64], F32, st)
                        tmpS = sb(P_ + "tmpS", [128, 4, 64], F32, st)
                        STb = sb(P_ + "STb", [128, 4, 64], BF16, st)
                        S.op('pool', lambda e: e.memset(STb[:], 0.0), writes=[P_ + "STb"])
                        S.op('pool', lambda e: e.memset(STs[:], 0.0), writes=[P_ + "STs"])
                        r0 = hh * 512

                        def diag(eng, dst, kdst, src, ksrc, cols=64):
                            for half in range(2):
                                pslice = slice(half * 64, half * 64 + 64)
                                if eng == 'act':
                                    S.op('act', lambda e: e.activation(out=dst[pslice, :, half * 64:half * 64 + 64], in_=src[pslice], func=AF.Identity),
                                         reads=ksrc, writes=[kdst])
                                else:
                                    S.op(eng, lambda e: e.tensor_copy(out=dst[pslice, :, half * 64:half * 64 + 64], in_=src[pslice]),
                                         reads=ksrc, writes=[kdst])

                        def load(n):
                            g = order[n]
                            b = n % 2
                            cols = slice(g * 64, g * 64 + 64)
                            kin = P_ + f"in{b}"
                            for (dst, srcd) in [(AR[b][:, :, 0, :], ATd[d]), (AR[b][:, :, 1, :], RTd[d]), (Bi[b][:], BTd[d]), (Ki[b][:], KTd[d]), (Vi[b][:], VTb)]:
                                S.dma('sp', dst, srcd[r0:r0 + 512, cols].rearrange("(c p) n -> p c n", p=128), writes=[kin])

                        load(0)
                        for n in range(NCH):
                            g = order[n]
                            b = n % 2
                            kin = P_ + f"in{b}"
                            if n + 1 < NCH:
                                load(n + 1)
                            diag('dve', BD["bdA"], P_ + "bdA", AR[b][:, :, 0, :], [kin])
                            diag('dve', BD["T2"], P_ + "T2", Bi[b], [kin])
                            diag('act', BD["T3"], P_ + "T3", Ki[b], [kin])
                            diag('act', BD["T4"], P_ + "T4", Vi[b], [kin])
                            ARf = AR[b][:].rearrange("p c a t -> p c (a t)")
                            for hp in range(4):
                                S.op('pe', lambda e: e.matmul(pb[0][:, hp * 128:(hp + 1) * 128], lhsT=BD["T2"][:, hp, :], rhs=ARf[:, hp, :], start=True, stop=True),
                                     reads=[P_ + "T2", kin], writes=pk(0), inc=(hp == 3))
                            for hp in range(4):
                                S.op('pe', lambda e: e.matmul(pb[1][:, hp * 128:(hp + 1) * 128], lhsT=BD["T3"][:, hp, :], rhs=ARf[:, hp, :], start=True, stop=True),
                                     reads=[P_ + "T3", kin], writes=pk(1), inc=(hp == 3))
                            for hp in range(4):
                                S.op('pe', lambda e: e.matmul(pb[2][:, hp * 64:(hp + 1) * 64], lhsT=BD["bdA"][:, hp, :], rhs=Bi[b][:, hp, :], start=True, stop=True),
                                     reads=[P_ + "bdA", kin], writes=pk(2, 0), inc=(hp == 3))
                            yield
                            S.op('dve', lambda e: e.tensor_tensor(out=ARm[:], in0=pb[0][:].rearrange("p (c t) -> p c t", t=128),
                                                                  in1=mAR.unsqueeze(1).to_broadcast([128, 4, 128]), op=ALU.mult),
                                 reads=pk(0) + ['c32'], writes=[P_ + "ARm"])
                            S.op('dve', lambda e: e.tensor_tensor(out=AKm[:], in0=pb[1][:].rearrange("p (c t) -> p c t", t=128),
                                                                  in1=mAR.unsqueeze(1).to_broadcast([128, 4, 128]), op=ALU.mult),
                                 reads=pk(1) + ['c32'], writes=[P_ + "AKm"])
                            S.op('dve', lambda e: e.tensor_tensor(out=Pst[0][:], in0=pb[2][:, 0:256].rearrange("p (c t) -> p c t", t=64),
                                                                  in1=mN.unsqueeze(1).to_broadcast([128, 4, 64]), op=ALU.mult),
                                 reads=pk(2, 0) + ['c32'], writes=[P_ + "Pst0"])
                            for hp in range(4):
                                S.op('pe', lambda e: e.matmul(pb[0][:, hp * 128:(hp + 1) * 128], lhsT=BD["T2"][:, hp, :], rhs=ident_bf[:], start=True, stop=True),
                                     reads=[P_ + "T2", 'ident_bf'], writes=pk(0), inc=(hp == 3))
                            for hp in range(4):
                                S.op('pe', lambda e: e.matmul(pb[1][:, hp * 128:(hp + 1) * 128], lhsT=BD["T3"][:, hp, :], rhs=ident_bf[:], start=True, stop=True),
                                     reads=[P_ + "T3", 'ident_bf'], writes=pk(1), inc=(hp == 3))
                            yield
                            S.op('act', lambda e: e.activation(out=BD["tbB"][:], in_=pb[0][:].rearrange("p (c t) -> p c t", t=128), func=AF.Identity),
                                 reads=pk(0), writes=[P_ + "tbB"])
                            S.op('act', lambda e: e.activation(out=BD["tbK"][:], in_=pb[1][:].rearrange("p (c t) -> p c t", t=128), func=AF.Identity),
                                 reads=pk(1), writes=[P_ + "tbK"])
                            for hp in range(4):
                                S.op('pe', lambda e: e.matmul(pb[0][:, hp * 128:(hp + 1) * 128], lhsT=BD["T4"][:, hp, :], rhs=ident_bf[:], start=True, stop=True),
                                     reads=[P_ + "T4", 'ident_bf'], writes=pk(0), inc=(hp == 3))
                            S.op('pool', lambda e: e.tensor_copy(out=PG[0][:, :, 0:64], in_=ARm[:, :, 0:64]), reads=[P_ + "ARm"], writes=[P_ + "PG0"])
                            S.op('pool', lambda e: e.tensor_copy(out=PG[0][:, :, 64:128], in_=SI.unsqueeze(1).to_broadcast([128, 4, 64])),
                                 reads=['c32'], writes=[P_ + "PG0"])
                            diag('pool', BD["bdMak"], P_ + "bdMak", AKm[:, :, 0:64], [P_ + "AKm"])
                            yield
                            S.op('act', lambda e: e.activation(out=BD["tbV"][:], in_=pb[0][:].rearrange("p (c t) -> p c t", t=128), func=AF.Identity),
                                 reads=pk(0), writes=[P_ + "tbV"])
                            for half in range(2):
                                pslice = slice(half * 64, half * 64 + 64)
                                S.op('pool', lambda e: e.tensor_copy(out=Vs[pslice], in_=BD["tbV"][pslice, :, half * 64:half * 64 + 64]),
                                     reads=[P_ + "tbV"], writes=[P_ + "Vs"])
                            diag('pool', BD["T2"], P_ + "T2", Pst[0], [P_ + "Pst0"])
                            diag('pool', BD["T3"], P_ + "T3", PG[0][:, :, 0:64], [P_ + "PG0"])
                            sets = [("T2", "T3"), ("T4", "T8")]
                            for lv in range(6):
                                cur, nxt = lv % 2, (lv + 1) % 2
                                bP, bPT = sets[cur]
                                nP, nPT = sets[nxt]
                                kPG, kPGn = P_ + f"PG{cur}", P_ + f"PG{nxt}"
                                if lv < 5:
                                    for hp in range(4):
                                        S.op('pe', lambda e: e.matmul(pb[1][:, hp * 64:(hp + 1) * 64], lhsT=BD[bPT][:, hp, :], rhs=Pst[cur][:, hp, :], start=True, stop=True),
                                             reads=[P_ + bPT, P_ + f"Pst{cur}"], writes=pk(1, 0), inc=(hp == 3))
                                    for hp in range(4):
                                        S.op('pe', lambda e: e.matmul(pb[0][:, hp * 128:(hp + 1) * 128], lhsT=BD[bP][:, hp, :], rhs=PG[cur][:, hp, :], start=True, stop=True),
                                             reads=[P_ + bP, kPG], writes=pk(0), inc=(hp == 3))
                                else:
                                    for hp in range(4):
                                        S.op('pe', lambda e: e.matmul(pb[0][:, hp * 128 + 64:(hp + 1) * 128], lhsT=BD[bP][:, hp, :], rhs=PG[cur][:, hp, 64:128], start=True, stop=True),
                                             reads=[P_ + bP, kPG], writes=pk(0), inc=(hp == 3))
                                yield
                                psv = pb[0][:].rearrange("p (c t) -> p c t", t=128)
                                S.op('dve', lambda e: e.tensor_tensor(out=PG[nxt][:, :, 64:128], in0=PG[cur][:, :, 64:128], in1=psv[:, :, 64:128], op=ALU.add),
                                     reads=pk(0) + [kPG], writes=[kPGn])
                                if lv < 5:
                                    S.op('act', lambda e: e.activation(out=PG[nxt][:, :, 0:64], in_=psv[:, :, 0:64], func=AF.Identity), reads=pk(0), writes=[kPGn])
                                    S.op('dve', lambda e: e.tensor_copy(out=Pst[nxt][:], in_=pb[1][:, 0:256].rearrange("p (c t) -> p c t", t=64)),
                                         reads=pk(1, 0), writes=[P_ + f"Pst{nxt}"])
                                    diag('act', BD[nP], P_ + nP, Pst[nxt], [P_ + f"Pst{nxt}"])
                                    if lv < 4:
                                        diag('pool', BD[nPT], P_ + nPT, PG[nxt][:, :, 0:64], [kPGn])
                            diag('pool', BD["T8"], P_ + "T8", PG[0][:, :, 64:128], [P_ + "PG0"])
                            for hp in range(4):
                                S.op('pe', lambda e: e.matmul(pb[2][:, 256 + hp * 64:256 + (hp + 1) * 64], lhsT=BD["bdA"][:, hp, :], rhs=STb[:, hp, :], start=True, stop=False),
                                     reads=[P_ + "bdA", P_ + "STb"], writes=pk(2, 1), inc=False)
                                S.op('pe', lambda e: e.matmul(pb[2][:, 256 + hp * 64:256 + (hp + 1) * 64], lhsT=BD["bdMak"][:, hp, :], rhs=Vs[:, hp, :], start=False, stop=True),
                                     reads=[P_ + "bdMak", P_ + "Vs"], writes=pk(2, 1), inc=(hp == 3))
                            yield
                            S.op('act', lambda e: e.activation(out=Xs[:], in_=pb[2][:, 256:512].rearrange("p (c t) -> p c t", t=64), func=AF.Identity),
                                 reads=pk(2, 1), writes=[P_ + "Xs"])
                            for hp in range(4):
                                S.op('pe', lambda e: e.matmul(pb[3][:, hp * 64:(hp + 1) * 64], lhsT=BD["T8"][:, hp, :], rhs=Xs[:, hp, :], start=True, stop=True),
                                     reads=[P_ + "T8", P_ + "Xs"], writes=pk(3, 0), inc=(hp == 3))
                            yield
                            S.op('dve', lambda e: e.tensor_copy(out=Us[:], in_=pb[3][:, 0:256].rearrange("p (c t) -> p c t", t=64)), reads=pk(3, 0), writes=[P_ + "Us"])
                            diag('act', BD["bdU"], P_ + "bdU", pb[3][:, 0:256].rearrange("p (c t) -> p c t", t=64), pk(3, 0))
                            for hp in range(4):
                                S.op('pe', lambda e: e.matmul(pb[3][:, 256 + hp * 64:256 + (hp + 1) * 64], lhsT=BD["bdST"][:, hp, :], rhs=AR[b][:, hp, 1, :], start=True, stop=False),
                                     reads=[P_ + "bdST", kin], writes=pk(3, 1), inc=False)
                                S.op('pe', lambda e: e.matmul(pb[3][:, 256 + hp * 64:256 + (hp + 1) * 64], lhsT=BD["bdU"][:, hp, :], rhs=ARm[:, hp, 64:128], start=False, stop=False),
                                     reads=[P_ + "bdU", P_ + "ARm"], writes=pk(3, 1), inc=False)
                                S.op('pe', lambda e: e.matmul(pb[3][:, 256 + hp * 64:256 + (hp + 1) * 64], lhsT=BD["tbV"][:, hp, :], rhs=AKm[:, hp, 64:128], start=False, stop=True),
                                     reads=[P_ + "tbV", P_ + "AKm"], writes=pk(3, 1), inc=(hp == 3))
                            for hp in range(4):
                                S.op('pe', lambda e: e.matmul(pb[1][:, 256 + hp * 64:256 + (hp + 1) * 64], lhsT=BD["tbB"][:, hp, :], rhs=Us[:, hp, :], start=True, stop=False),
                                     reads=[P_ + "tbB", P_ + "Us"], writes=pk(1, 1), inc=False)
                                S.op('pe', lambda e: e.matmul(pb[1][:, 256 + hp * 64:256 + (hp + 1) * 64], lhsT=BD["tbK"][:, hp, :], rhs=Vs[:, hp, :], start=False, stop=True),
                                     reads=[P_ + "tbK", P_ + "Vs"], writes=pk(1, 1), inc=(hp == 3))
                            yield
                            S.op('act', lambda e: e.activation(out=Yb[:], in_=pb[3][:, 256:512].rearrange("p (c t) -> p c t", t=64), func=AF.Identity),
                                 reads=pk(3, 1), writes=[P_ + "Yb"])
                            S.dma('sp', YD[d][r0:r0 + 512, g * 64:g * 64 + 64].rearrange("(c p) n -> p c n", p=128), Yb[:], reads=[P_ + "Yb"], writes=['YD'])
                            S.op('dve', lambda e: e.tensor_tensor(out=tmpS[:], in0=pb[1][:, 256:512].rearrange("p (c t) -> p c t", t=64), in1=STs[:], op=ALU.add),
                                 reads=pk(1, 1) + [P_ + "STs"], writes=[P_ + "tmpS"])
                            S.op('dve', lambda e: e.tensor_tensor(out=STs[:], in0=tmpS[:], in1=WCall[:, :, g:g + 1].to_broadcast([128, 4, 64]), op=ALU.mult),
                                 reads=[P_ + "tmpS", P_ + "WCall"], writes=[P_ + "STs"])
                            diag('pool', BD["bdST"], P_ + "bdST", STs, [P_ + "STs"])
                            S.op('act', lambda e: e.activation(out=STb[:], in_=STs[:], func=AF.Identity), reads=[P_ + "STs"], writes=[P_ + "STb"])
                            yield

                    gens = [stream(0), stream(1)]
                    alive = [True, True]
                    while any(alive):
                        for q in range(2):
                            if alive[q]:
                                try:
                                    next(gens[q])
                                except StopIteration:
                                    alive[q] = False
                S.barrier()
            if os.environ.get('RSTOP') == '3':
                return
            with contextlib.ExitStack() as st:
                wo = sb("r_wo", [128, 8, 1024], BF16, st)
                S.dma('pool', wo[:], rwkv_wo[j].rearrange("(k p) n -> p k n", p=128), writes=['r_wo'])
                y0 = sb("r_y0", [128, 8, 512], F32, st); y1 = sb("r_y1", [128, 8, 512], F32, st)
                bo = sb("r_bo", [128, 8, 512], F32, st); gg = sb("r_gg", [128, 8, 512], F32, st)
                xt = sb("r_xt", [128, 8, 512], F32, st)
                ob = sb("r_ob", [128, 8, 512], BF16, st)
                yc = sb("r_yc", [128, 512], F32, st); y2 = sb("r_y2", [128, 512], F32, st); sd = sb("r_sd", [128, 512], F32, st)
                tiles = TILES[1:] if last else TILES
                for ti, (t0, N, mc) in enumerate(tiles):
                    for (dst, srcd, kk_) in [(y0, YD[0], 'y0'), (y1, YD[1], 'y1'), (bo, BON, 'bo'), (gg, GG, 'gg'), (xt, XS, 'r_xt')]:
                        S.dma('sp', dst[:, :, :N], srcd[:, t0:t0 + N].rearrange("(k p) n -> p k n", p=128), writes=[kk_])
                    for c in range(8):
                        pa, pv = c % 2, 2 + c % 2
                        S.op('dve', lambda e: e.tensor_tensor(out=y0[:, c, :N], in0=y0[:, c, :N], in1=y1[:, c, :N], op=ALU.add), reads=['y0', 'y1'], writes=['y0'])
                        S.op('pe', lambda e: e.matmul(ps[pa][:, :N], lhsT=blockones, rhs=y0[:, c, :N], start=True, stop=True), reads=['y0', 'c32'], writes=[('ps', pa)])
                        S.op('dve', lambda e: e.scalar_tensor_tensor(out=yc[:, :N], in0=ps[pa][:, :N], scalar=-1.0 / 64, in1=y0[:, c, :N], op0=ALU.mult, op1=ALU.add),
                             reads=[('ps', pa), 'y0'], writes=['yc'])
                        S.op('pool', lambda e: e.tensor_tensor(out=y2[:, :N], in0=yc[:, :N], in1=yc[:, :N], op=ALU.mult), reads=['yc'], writes=['y2'])
                        S.op('pe', lambda e: e.matmul(ps[pv][:, :N], lhsT=blockones, rhs=y2[:, :N], start=True, stop=True), reads=['y2', 'c32'], writes=[('ps', pv)])
                        S.op('act', lambda e: e.activation(out=sd[:, :N], in_=ps[pv][:, :N], func=AF.Sqrt, bias=epsT[:, 5:6], scale=1.0 / 64),
                             reads=[('ps', pv), 'epsT'], writes=['sd'])
                        S.op('dve', lambda e: e.reciprocal(out=sd[:, :N], in_=sd[:, :N]), reads=['sd'], writes=['sd'])
                        S.op('pool', lambda e: e.tensor_tensor(out=yc[:, :N], in0=yc[:, :N], in1=sd[:, :N], op=ALU.mult), reads=['yc', 'sd'], writes=['yc'])
                        S.op('dve', lambda e: e.tensor_scalar(out=yc[:, :N], in0=yc[:, :N], scalar1=V(f"lnw{j}", c, c + 1), scalar2=V(f"lnb{j}", c, c + 1),
                                                              op0=ALU.mult, op1=ALU.add), reads=['yc', 'vecs'], writes=['yc'])
                        S.op('pool', lambda e: e.tensor_tensor(out=yc[:, :N], in0=yc[:, :N], in1=bo[:, c, :N], op=ALU.add), reads=['yc', 'bo'], writes=['yc'])
                        S.op('pool', lambda e: e.tensor_tensor(out=ob[:, c, :N], in0=yc[:, :N], in1=gg[:, c, :N], op=ALU.mult), reads=['yc', 'gg'], writes=['ob'])
                    for oc in range(8):
                        pb_ = 4 + oc % 4
                        for kc in range(8):
                            S.op('pe', lambda e: e.matmul(ps[pb_][:, :N], lhsT=wo[:, kc, oc * 128:(oc + 1) * 128], rhs=ob[:, kc, :N], start=(kc == 0), stop=(kc == 7)),
                                 reads=['r_wo', 'ob'], writes=[('ps', pb_)], inc=(kc == 7))
                        S.op('dve', lambda e: e.scalar_tensor_tensor(out=xt[:, oc, :N], in0=ps[pb_][:, :N], scalar=modt[:, i, 16 + oc, mc:mc + 1],
                                                                     in1=xt[:, oc, :N], op0=ALU.mult, op1=ALU.add),
                             reads=[('ps', pb_), 'r_xt', 'modt'], writes=['r_xt'])
                    S.dma('sp', XS[:, t0:t0 + N].rearrange("(k p) n -> p k n", p=128), xt[:, :, :N], reads=['r_xt'], writes=['XS'])
            S.barrier()

        def phase_ffn(i, last):
            moe = (i % 2 == 1)
            k = i // 2
            E = NE if moe else 1
            F = DFE if moe else DFF
            nchunk = F // 128
            blocks = [(c0, min(4, nchunk - c0)) for c0 in range(0, nchunk, 4)]
            tiles = TILES[1:] if last else TILES
            groups = [tiles[0:len(tiles) - 6], tiles[-6:-3], tiles[-3:]]
            for gi, grp in enumerate(groups):
                Sg = sum(t[1] for t in grp)
                offs = [sum(t[1] for t in grp[:a]) for a in range(len(grp))]
                with contextlib.ExitStack() as st:
                    hb = sb("f_hb", [128, 8, 1536], BF16, st)
                    yacc = sb("f_yacc", [128, 8, 1536], F32, st)
                    GT = sb("f_GT", [8, 1536], F32, st)
                    gbc = sb("f_gbc", [128, 1536], F32, st)
                    with contextlib.ExitStack() as st2:
                        xt = [sb(f"f_xt{b}", [128, 8, 512], F32, st2) for b in range(2)]
                        sq = sb("f_sq", [128, 8, 512], F32, st2)
                        rs = sb("f_rs", [128, 512], F32, st2)
                        if moe:
                            rt = sb("f_rt", [128, 8, 8], F32, st2)
                            S.dma('sp', rt[:], moe_router[k].rearrange("(k p) n -> p k n", p=128), writes=['rt'])
                            lg = sb("f_lg", [128, 8], F32, st2); m8 = sb("f_m8", [128, 8], F32, st2)
                            sel = sb("f_sel", [128, 8], F32, st2); ex = sb("f_ex", [128, 8], F32, st2)
                            sm = sb("f_sm", [128, 4], F32, st2); G = sb("f_G", [128, 4, 8], F32, st2)
                        for ti, (t0, N, mc) in enumerate(grp):
                            b = ti % 2
                            kx = ('fx', b)
                            o = offs[ti]
                            S.dma('sp', xt[b][:, :, :N], XS[:, t0:t0 + N].rearrange("(k p) n -> p k n", p=128), writes=[kx])
                            ksq = normmod(xt[b][:, :, :N], N, A2[:, i, :, mc], None, None, sq, rs, 7, kx, 'f')
                            S.op('dve', lambda e: e.tensor_tensor(out=sq[:, :, :N], in0=sq[:, :, :N],
                                                                  in1=modt[:, i, 24:32, mc].unsqueeze(2).to_broadcast([128, 8, N]), op=ALU.add),
                                 reads=[ksq, 'modt'], writes=[ksq])
                            S.op('act', lambda e: e.activation(out=hb[:, :, o:o + N], in_=sq[:, :, :N], func=AF.Identity),
                                 reads=[ksq], writes=['hb'])
                            if moe:
                                for blk in range(N // 128):
                                    for kc in range(8):
                                        S.op('pe', lambda e: e.matmul(ps[6][:, 0:8], lhsT=sq[:, kc, blk * 128:(blk + 1) * 128], rhs=rt[:, kc, :],
                                                                      start=(kc == 0), stop=(kc == 7)), reads=[ksq, 'rt'], writes=[('ps', 6)], inc=(kc == 7))
                                    S.op('dve', lambda e: e.tensor_copy(out=lg[:], in_=ps[6][:, 0:8]), reads=[('ps', 6)], writes=['lg'])
                                    S.op('dve', lambda e: e.max(out=m8[:], in_=lg[:]), reads=['lg'], writes=['m8'])
                                    S.op('dve', lambda e: e.tensor_scalar(out=sel[:], in0=lg[:], scalar1=m8[:, 1:2], scalar2=None, op0=ALU.is_ge),
                                         reads=['lg', 'm8'], writes=['sel'])
                                    S.op('dve', lambda e: e.tensor_scalar(out=sm[:, 0:1], in0=m8[:, 0:1], scalar1=-1.0, scalar2=None, op0=ALU.mult),
                                         reads=['m8'], writes=['sm'])
                                    S.op('act', lambda e: e.activation(out=ex[:], in_=lg[:], func=AF.Exp, bias=sm[:, 0:1], scale=1.0),
                                         reads=['lg', 'sm'], writes=['ex'])
                                    S.op('dve', lambda e: e.tensor_tensor(out=ex[:], in0=ex[:], in1=sel[:], op=ALU.mult), reads=['ex', 'sel'], writes=['ex'])
                                    S.op('dve', lambda e: e.tensor_reduce(out=sm[:, 1:2], in_=ex[:], axis=AX.X, op=ALU.add), reads=['ex'], writes=['sm'])
                                    S.op('dve', lambda e: e.reciprocal(out=sm[:, 2:3], in_=sm[:, 1:2]), reads=['sm'], writes=['sm'])
                                    S.op('dve', lambda e: e.tensor_scalar(out=G[:, blk, :], in0=ex[:], scalar1=sm[:, 2:3], scalar2=None, op0=ALU.mult),
                                         reads=['ex', 'sm'], writes=['G'])
                                    S.op('pe', lambda e: e.transpose(out=ps[5][0:8, blk * 128:(blk + 1) * 128], in_=G[:, blk, :], identity=ident),
                                         reads=['G', 'c32'], writes=[('ps', 5)])
                                S.op('act', lambda e: e.activation(out=GT[:, o:o + N], in_=ps[5][0:8, :N], func=AF.Identity),
                                     reads=[('ps', 5)], writes=['GT'])
                    S.barrier()
                    with contextlib.ExitStack() as st2:
                        w1b = [sb(f"f_w1{b}", [128, 8, 512], BF16, st2) for b in range(2)]
                        w3b = [sb(f"f_w3{b}", [128, 8, 512], BF16, st2) for b in range(2)]
                        w2b = [sb(f"f_w2{b}", [128, 4, 1024], BF16, st2) for b in range(2)]
                        gt = [sb(f"f_g{b}", [128, 4, 512], BF16, st2) for b in range(2)]
                        s1 = [sb(f"f_s1{b}", [128, 512], F32, st2) for b in range(2)]
                        s1g = [sb(f"f_s1g{b}", [128, 512], F32, st2) for b in range(2)]
                        nw = 0
                        ng = 0
                        nhc = 0
                        ny = 0
                        first = True
                        for ex_i in range(E):
                            if moe:
                                for ti, (t0, N, mc) in enumerate(grp):
                                    o = offs[ti]
                                    S.op('pe', lambda e: e.matmul(ps[6][:, :N], lhsT=selE[:, ex_i * 128:(ex_i + 1) * 128], rhs=GT[:, o:o + N],
                                                                  start=True, stop=True), reads=['GT', 'c32'], writes=[('ps', 6)])
                                    S.op('act', lambda e: e.activation(out=gbc[:, o:o + N], in_=ps[6][:, :N], func=AF.Identity),
                                         reads=[('ps', 6)], writes=['gbc'])
                            for (c0, nh) in blocks:
                                wb = nw % 2
                                nw += 1
                                kw = ('fw', wb)
                                if moe:
                                    W1, W3, W2 = moe_w1[k, ex_i], moe_w3[k, ex_i], moe_w2[k, ex_i]
                                else:
                                    W1, W3, W2 = ffn_w1[k], ffn_w3[k], ffn_w2[k]
                                S.dma('pool', w1b[wb][:, :, :nh * 128], W1[:, c0 * 128:(c0 + nh) * 128].rearrange("(k p) n -> p k n", p=128), writes=[kw])
                                S.dma('pool', w3b[wb][:, :, :nh * 128], W3[:, c0 * 128:(c0 + nh) * 128].rearrange("(k p) n -> p k n", p=128), writes=[kw])
                                S.dma('pool', w2b[wb][:, :nh, :], W2[c0 * 128:(c0 + nh) * 128, :].rearrange("(k p) n -> p k n", p=128), writes=[kw])
                                for ti, (t0, N, mc) in enumerate(grp):
                                    o = offs[ti]
                                    gb = ng % 2
                                    ng += 1
                                    for hc in range(nh):
                                        pa = (nhc % 2) * 2
                                        sbi = nhc % 2
                                        nhc += 1
                                        for kc in range(8):
                                            S.op('pe', lambda e: e.matmul(ps[pa][:, :N], lhsT=w1b[wb][:, kc, hc * 128:(hc + 1) * 128], rhs=hb[:, kc, o:o + N],
                                                                          start=(kc == 0), stop=(kc == 7)), reads=[kw, 'hb'], writes=[('ps', pa)], inc=(kc == 7))
                                        for kc in range(8):
                                            S.op('pe', lambda e: e.matmul(ps[pa + 1][:, :N], lhsT=w3b[wb][:, kc, hc * 128:(hc + 1) * 128], rhs=hb[:, kc, o:o + N],
                                                                          start=(kc == 0), stop=(kc == 7)), reads=[kw, 'hb'], writes=[('ps', pa + 1)], inc=(kc == 7))
                                        S.op('act', lambda e: e.activation(out=s1[sbi][:, :N], in_=ps[pa][:, :N], func=AF.Silu),
                                             reads=[('ps', pa)], writes=[('s1', sbi)])
                                        src, ksrc = s1[sbi], ('s1', sbi)
                                        if moe:
                                            S.op('pool', lambda e: e.tensor_tensor(out=s1g[sbi][:, :N], in0=s1[sbi][:, :N], in1=gbc[:, o:o + N], op=ALU.mult),
                                                 reads=[('s1', sbi), 'gbc'], writes=[('s1g', sbi)])
                                            src, ksrc = s1g[sbi], ('s1g', sbi)
                                        S.op('dve', lambda e: e.tensor_tensor(out=gt[gb][:, hc, :N], in0=src[:, :N], in1=ps[pa + 1][:, :N], op=ALU.mult),
                                             reads=[ksrc, ('ps', pa + 1)], writes=[('g', gb)])
                                    for oc in range(8):
                                        py = 4 + ny % 2
                                        ny += 1
                                        for hc in range(nh):
                                            S.op('pe', lambda e: e.matmul(ps[py][:, :N], lhsT=w2b[wb][:, hc, oc * 128:(oc + 1) * 128], rhs=gt[gb][:, hc, :N],
                                                                          start=(hc == 0), stop=(hc == nh - 1)), reads=[kw, ('g', gb)], writes=[('ps', py)],
                                                 inc=(hc == nh - 1))
                                        ky = ('y', o, oc)
                                        if first:
                                            S.op('act', lambda e: e.activation(out=yacc[:, oc, o:o + N], in_=ps[py][:, :N], func=AF.Identity),
                                                 reads=[('ps', py)], writes=[ky])
                                        else:
                                            S.op('dve', lambda e: e.tensor_tensor(out=yacc[:, oc, o:o + N], in0=yacc[:, oc, o:o + N], in1=ps[py][:, :N], op=ALU.add),
                                                 reads=[('ps', py), ky], writes=[ky])
                                first = False
                    S.barrier()
                    with contextlib.ExitStack() as st2:
                        xt = [sb(f"f_cx{b}", [128, 8, 512], F32, st2) for b in range(2)]
                        for ti, (t0, N, mc) in enumerate(grp):
                            b = ti % 2
                            o = offs[ti]
                            S.dma('sp', xt[b][:, :, :N], XS[:, t0:t0 + N].rearrange("(k p) n -> p k n", p=128), writes=[('fcx', b)])
                            for oc in range(8):
                                S.op('dve', lambda e: e.scalar_tensor_tensor(
                                    out=xt[b][:, oc, :N], in0=yacc[:, oc, o:o + N], scalar=modt[:, i, 40 + oc, mc:mc + 1],
                                    in1=xt[b][:, oc, :N], op0=ALU.mult, op1=ALU.add), reads=[('fcx', b), 'modt'], writes=[('fcx', b)])
                            if last:
                                S.dma('sp', out_d[:, t0 - CTX:t0 - CTX + N].rearrange("(k p) n -> p k n", p=128), xt[b][:, :, :N],
                                      reads=[('fcx', b)], writes=['out'])
                            else:
                                S.dma('sp', XS[:, t0:t0 + N].rearrange("(k p) n -> p k n", p=128), xt[b][:, :, :N],
                                      reads=[('fcx', b)], writes=['XS'])
                    S.barrier()

        if dbg:
            dbg_d = nc.dram_tensor("dbg", [D, NT], F32, kind="ExternalOutput").ap()
        phase_mod()
        for i in range(depth):
            last = (i == depth - 1)
            if i == 0 and os.environ.get('SKIP0'):
                S.dma('sp', XS, xs_in, writes=['XS'])
                S.barrier()
                continue
            if i % 2 == 0:
                phase_mla(i, xs_in if i == 0 else XS)
            else:
                phase_rwkv(i, last and dbg != 'r')
            if not (dbg == 'r' and i == depth - 1):
                phase_ffn(i, last)
        if dbg:
            S.dma('sp', dbg_d, XS, writes=['dbg'])
        S.barrier()
    return nc, S


def host_consts():
    c = np.zeros((128, C32W), np.float32)
    c[:, 0:128] = 1.0
    c[:, 128:256] = np.eye(128, dtype=np.float32)
    P = np.zeros((64, 64), np.float32)
    for base in (0, 32):
        for f in range(16):
            P[base + 16 + f, base + f] = -1.0
            P[base + f, base + 16 + f] = 1.0
    c[0:64, 256:320] = P
    for e in range(8):
        c[e, 384 + e * 128:384 + (e + 1) * 128] = 1.0
    o_ = 384 + 1024
    p = np.arange(128)
    c[:, o_:o_ + 128] = (p[:, None] // 64 == p[None, :] // 64)
    tt = np.arange(64)
    c[:, o_ + 128:o_ + 192] = (p[:, None] % 64 == tt[None, :])
    sidx = (p % 64)[:, None]
    c[:, o_ + 192:o_ + 256] = (sidx < tt[None, :])
    c[:, o_ + 256:o_ + 320] = (sidx <= tt[None, :])
    c[:, o_ + 320:o_ + 384] = (tt[None, :] < sidx)
    c[:, o_ + 384:o_ + 448] = (sidx > tt[None, :])
    c[:, o_ + 448:o_ + 512] = (sidx >= tt[None, :])
    c[:, o_ + 512:o_ + 576] = (tt[None, :] > sidx)
    cm = np.ones(512, np.float32); cm[0::64] = 0.0
    c[:, o_ + 576:o_ + 1088] = cm[None, :]
    rows = TL // 64
    row_ids = np.repeat(np.arange(rows, dtype=np.float32), 64)
    col_ids = np.tile(np.arange(64, dtype=np.float32), rows)
    inv_freq = (1.0 / (np.float32(10000.0) ** (np.arange(16, dtype=np.float32) / np.float32(16)))).astype(np.float32)
    ang_r = (row_ids[:, None] * inv_freq[None, :]).astype(np.float32)
    ang_c = (col_ids[:, None] * inv_freq[None, :]).astype(np.float32)
    cosT = np.zeros((64, TL), np.float32)
    sinT = np.zeros((64, TL), np.float32)
    for base, ang in ((0, ang_r), (32, ang_c)):
        for half in (0, 16):
            cosT[base + half:base + half + 16] = np.cos(ang).T
            sinT[base + half:base + half + 16] = np.sin(ang).T
    return c, cosT, sinT


def host_vecs(inp, depth):
    voff, NV = vec_layout(depth)
    vecs = np.zeros((128, NV), np.float32)

    def put(name, arr):
        o, c = voff[name]
        assert arr.shape == (128, c), (name, arr.shape, c)
        vecs[:, o:o + c] = arr
    for i in range(depth):
        put(f"adab{i}", fm(inp["ada_b"][i]))
        put(f"n1g{i}", fm(inp["norm1_g"][i]))
        put(f"n2g{i}", fm(inp["norm2_g"][i]))
        j = i // 2
        if i % 2 == 0:
            put(f"qan{j}", fm(inp["mla_qa_norm"][j]))
            put(f"kvan{j}", fm(inp["mla_kva_norm"][j]))
            put(f"qnn{j}", fm(inp["mla_q_norm"][j][:128]))
            put(f"qnr{j}", fm(inp["mla_q_norm"][j][128:]))
            put(f"knn{j}", fm(inp["mla_k_norm"][j][:128]))
            put(f"knr{j}", fm(inp["mla_k_norm"][j][128:]))
        else:
            put(f"mix{j}", fm(inp["rwkv_mix"][j]))
            put(f"w0{j}", fm(inp["rwkv_w0"][j]))
            put(f"a0{j}", fm(inp["rwkv_a0"][j]))
            put(f"kk{j}", fm(inp["rwkv_k_k"][j]))
            put(f"ka{j}", fm(inp["rwkv_k_a"][j]))
            put(f"rk{j}", fm(inp["rwkv_r_k"][j]))
            put(f"lnw{j}", fm(inp["rwkv_ln_w"][j]))
            put(f"lnb{j}", fm(inp["rwkv_ln_b"][j]))
            if j >= 1:
                put(f"v0{j}", fm(inp["rwkv_v0"][j - 1]))
    return vecs


WNAMES = ["ada_w", "mla_wqa", "mla_wqb", "mla_wkva", "mla_wkvb", "mla_wo", "ffn_w1", "ffn_w3", "ffn_w2",
          "moe_router", "moe_w1", "moe_w3", "moe_w2",
          "rwkv_wr", "rwkv_wk", "rwkv_wv", "rwkv_wo", "rwkv_w1", "rwkv_w2", "rwkv_a1", "rwkv_a2", "rwkv_g1", "rwkv_g2", "rwkv_v1", "rwkv_v2"]


def make_in_maps(inp, depth, cores):
    c32, cosT, sinT = host_consts()
    vecs = host_vecs(inp, depth)
    shared = {n: np.ascontiguousarray(inp[n], dtype=np.float32) for n in WNAMES}
    maps = []
    for b in cores:
        xs = np.ascontiguousarray(np.concatenate([inp["ctx"][b], inp["x"][b]], axis=0).T.astype(np.float32))
        cv = np.zeros((128, 16), np.float32)
        cv[:, 0::2] = fm(inp["c"][b])
        cv[:, 1::2] = fm(inp["c_ctx"])
        m = dict(shared)
        m.update(xs=xs, cvec=cv, vecs=vecs, c32=c32, ropec=cosT, ropes=sinT)
        maps.append(m)
    return maps


def kernel(**inp):
    depth = 4
    nc, S = build(depth)
    maps = make_in_maps(inp, depth, list(range(8)))
    res = run_bass_kernel_spmd(nc, maps, core_ids=list(range(8)))
    out = np.stack([np.ascontiguousarray(r["out"].T) for r in res.results], axis=0)
    return out.astype(np.float32)
```

```python
import contextlib
import os
import numpy as np
import concourse.bass as bass
import concourse.mybir as mybir
from concourse.bass_utils import run_bass_kernel_spmd

F32 = mybir.dt.float32
BF16 = mybir.dt.bfloat16
ALU = mybir.AluOpType
AF = mybir.ActivationFunctionType
AX = mybir.AxisListType
NS = 8

D = 1024
KC = 8
CTX = 256
TL = 4096
NT = CTX + TL
EPS = 1e-6
NH = 8
SM_SCALE = 192 ** -0.5
DFF = 2816
DFE = 3584
NE = 8
C32W = 128 * 3 + 8 * 128 + 128 + 64 + 128 + 64 + 128 + 64 + 512
TILES = [(0, 256, 1)] + [(256 + 512 * i, 512, 0) for i in range(8)]


class Sched:
    def __init__(self, nc):
        self.nc = nc
        self.engs = {'pe': nc.tensor, 'dve': nc.vector, 'act': nc.scalar,
                     'pool': nc.gpsimd, 'sp': nc.sync}
        self.sem = {e: nc.alloc_semaphore(name=f"sem_{e}") for e in ['pe', 'dve', 'act', 'pool']}
        self.cnt = {e: 0 for e in self.sem}
        self.dq = {}
        for q, e in [('sp', 'sp'), ('pool', 'pool')]:
            self.dq[q] = dict(eng=e, n=0,
                              sems=[nc.alloc_semaphore(name=f"dsem_{q}{i}") for i in range(NS)])
        self.waited = {}
        self.lastw = {}
        self.readers = {}
        self.nins = 0
        self.mute = False

    def _sid_val(self, tok):
        if tok[0] == 'c':
            return ('c', tok[1]), self.sem[tok[1]], tok[2]
        q = self.dq[tok[1]]
        n = tok[2]
        return ('d', tok[1], n % NS), q['sems'][n % NS], 16 * (n // NS + 1)

    def _wait(self, eng, tok):
        if tok[0] == 'c' and tok[1] == eng and eng == 'pe':
            return
        sid, sem, val = self._sid_val(tok)
        if tok[0] == 'c':
            assert val <= self.cnt[tok[1]], f"wait on unsignalled instr {tok}"
        if self.waited.get((eng, sid), 0) >= val:
            return
        self.engs[eng].wait_ge(sem, val)
        self.waited[(eng, sid)] = val
        self.nins += 1

    def _deps(self, reads, writes):
        deps = set()
        for k in reads:
            if k in self.lastw:
                deps.add(self.lastw[k])
        for k in writes:
            if k in self.lastw:
                deps.add(self.lastw[k])
            for t in self.readers.get(k, {}).values():
                deps.add(t)
        return deps

    def _record(self, tok, reads, writes):
        sid, _, val = self._sid_val(tok)
        for k in reads:
            r = self.readers.setdefault(k, {})
            old = r.get(sid)
            if old is None or self._sid_val(old)[2] < val:
                r[sid] = tok
        for k in writes:
            self.lastw[k] = tok
            self.readers[k] = {}

    def op(self, eng, fn, reads=(), writes=(), inc=True):
        if self.mute:
            return None
        if eng != 'pe':
            psr = [k for k in reads if isinstance(k, tuple) and k[0] in ('ps', 'psb', 'sps')]
            if psr:
                reads = [k for k in reads if k not in psr]
                writes = list(writes) + psr
        for t in self._deps(reads, writes):
            self._wait(eng, t)
        ins = fn(self.engs[eng])
        self.nins += 1
        if inc:
            self.cnt[eng] += 1
            ins.then_inc(self.sem[eng], 1)
            tok = ('c', eng, self.cnt[eng])
        else:
            tok = ('c', eng, self.cnt[eng] + 1)
        self._record(tok, reads, writes)
        return ins

    def dma(self, q, out, in_, reads=(), writes=(), **kw):
        if self.mute:
            return None
        Q = self.dq[q]
        eng = Q['eng']
        n = Q['n']
        for t in self._deps(reads, writes):
            self._wait(eng, t)
        if n >= NS:
            self._wait(eng, ('d', q, n - NS))
        ins = self.engs[eng].dma_start(out=out, in_=in_, **kw)
        ins.then_inc(Q['sems'][n % NS], 16)
        self.nins += 1
        Q['n'] += 1
        self._record(('d', q, n), reads, writes)
        return ins

    def barrier(self):
        toks = []
        for e, c in self.cnt.items():
            if c > 0:
                toks.append(('c', e, c))
        for q, Q in self.dq.items():
            for n in range(max(0, Q['n'] - NS), Q['n']):
                toks.append(('d', q, n))
        for e in ['pe', 'dve', 'act', 'pool', 'sp']:
            for t in toks:
                if t[0] == 'c' and t[1] == e:
                    continue
                self._wait(e, t)
        self.lastw.clear()
        self.readers.clear()


def vec_layout(depth):
    ents = []
    for i in range(depth):
        ents += [(f"adab{i}", 48), (f"n1g{i}", 8), (f"n2g{i}", 8)]
        j = i // 2
        if i % 2 == 0:
            ents += [(f"qan{j}", 3), (f"kvan{j}", 2), (f"qnn{j}", 1), (f"qnr{j}", 1), (f"knn{j}", 1), (f"knr{j}", 1)]
        else:
            ents += [(f"mix{j}", 48), (f"w0{j}", 16), (f"a0{j}", 16), (f"kk{j}", 8), (f"ka{j}", 8),
                     (f"rk{j}", 8), (f"lnw{j}", 8), (f"lnb{j}", 8)]
            if j >= 1:
                ents += [(f"v0{j}", 8)]
    off = {}
    o = 0
    for n, c in ents:
        off[n] = (o, c)
        o += c
    return off, o


def fm(v):
    v = np.asarray(v, np.float32).reshape(-1)
    pad = (-len(v)) % 128
    if pad:
        v = np.concatenate([v, np.zeros(pad, np.float32)])
    return np.ascontiguousarray(v.reshape(-1, 128).T)


def build(depth=4, dbg=None):
    nc = bass.Bass("TRN2", target_bir_lowering=False)
    voff, NV = vec_layout(depth)

    def din(name, shape, dt=F32):
        return nc.dram_tensor(name, list(shape), dt, kind="ExternalInput").ap()

    def dscr(name, shape, dt=F32):
        return nc.dram_tensor(name, list(shape), dt, kind="Internal").ap()

    xs_in = din("xs", [D, NT])
    cvec_d = din("cvec", [128, 16])
    vecs_d = din("vecs", [128, NV])
    c32_d = din("c32", [128, C32W])
    ropec_d = din("ropec", [64, TL])
    ropes_d = din("ropes", [64, TL])
    ada_w = din("ada_w", [4, D, 6 * D])
    mla_wqa = din("mla_wqa", [2, D, 384]); mla_wqb = din("mla_wqb", [2, 384, 1536])
    mla_wkva = din("mla_wkva", [2, D, 320]); mla_wkvb = din("mla_wkvb", [2, 256, 2048])
    mla_wo = din("mla_wo", [2, D, D])
    ffn_w1 = din("ffn_w1", [2, D, DFF]); ffn_w3 = din("ffn_w3", [2, D, DFF]); ffn_w2 = din("ffn_w2", [2, DFF, D])
    moe_router = din("moe_router", [2, D, NE])
    moe_w1 = din("moe_w1", [2, NE, D, DFE]); moe_w3 = din("moe_w3", [2, NE, D, DFE]); moe_w2 = din("moe_w2", [2, NE, DFE, D])
    rwkv_wr = din("rwkv_wr", [2, D, D]); rwkv_wk = din("rwkv_wk", [2, D, D]); rwkv_wv = din("rwkv_wv", [2, D, D]); rwkv_wo = din("rwkv_wo", [2, D, D])
    rwkv_w1 = din("rwkv_w1", [2, 2, D, 64]); rwkv_w2 = din("rwkv_w2", [2, 2, 64, D])
    rwkv_a1 = din("rwkv_a1", [2, 2, D, 64]); rwkv_a2 = din("rwkv_a2", [2, 2, 64, D])
    rwkv_g1 = din("rwkv_g1", [2, D, 160]); rwkv_g2 = din("rwkv_g2", [2, 160, D])
    rwkv_v1 = din("rwkv_v1", [1, D, 32]); rwkv_v2 = din("rwkv_v2", [1, 32, D])
    out_d = nc.dram_tensor("out", [D, TL], F32, kind="ExternalOutput").ap()
    HS = dscr("HS", [D, 258 + 4098])
    ATd = [dscr(f"ATd{d}", [D, NT], BF16) for d in range(2)]; BTd = [dscr(f"BTd{d}", [D, NT], BF16) for d in range(2)]
    KTd = [dscr(f"KTd{d}", [D, NT], BF16) for d in range(2)]; RTd = [dscr(f"RTd{d}", [D, NT], BF16) for d in range(2)]
    VTb = dscr("VTb", [D, NT], BF16)
    WCd = [dscr(f"WCd{d}", [D, NT // 64]) for d in range(2)]
    VT = [dscr(f"VT{d}", [D, NT]) for d in range(2)]
    YD = [dscr(f"YD{d}", [D, NT]) for d in range(2)]
    BON = dscr("BON", [D, NT]); GG = dscr("GG", [D, NT])

    XS = dscr("XS", [D, NT])
    QN = dscr("QN", [NH, 128, NT], BF16); QR = dscr("QR", [NH, 64, NT], BF16)
    KN = dscr("KN", [NH, 128, NT], BF16); KR = dscr("KR", [NH, 64, NT], BF16)
    VV = dscr("VV", [NT, NH, 128], BF16)
    AO = dscr("AO", [D, NT], BF16)

    S = Sched(nc)
    es = contextlib.ExitStack()

    uniq = [0]

    def sb(name, shape, dt=F32, stack=None):
        uniq[0] += 1
        return (stack or es).enter_context(nc.sbuf_tensor(f"s{uniq[0]}_{name}", list(shape), dt))

    with es:
        ps = [es.enter_context(nc.psum_tensor(f"ps{i}", [128, 512], F32)) for i in range(8)]
        vecs = sb("vecs", [128, NV])
        c32 = sb("c32", [128, C32W])
        ones_bf = sb("ones_bf", [128, 128], BF16)
        cvec = sb("cvec", [128, 16])
        modt = sb("modt", [128, depth, 48, 2])
        A1 = sb("A1", [128, depth, 8, 2]); A2 = sb("A2", [128, depth, 8, 2])
        S.dma('sp', vecs[:], vecs_d, writes=['vecs'])
        S.dma('sp', c32[:], c32_d, writes=['c32'])
        S.dma('sp', cvec[:], cvec_d, writes=['cvec'])
        EPSI = {1024: 0, 384: 1, 256: 2, 192: 3, 64: 4}
        epsT = sb("epsT", [128, 8])
        for nf, ci in EPSI.items():
            S.op('pool', lambda e: e.memset(epsT[:, ci:ci + 1], float(nf * EPS)), writes=['epsT'])
        ones32 = c32[:, 0:128]
        ident = c32[:, 128:256]
        rotP = c32[0:64, 256:320]
        selE = c32[0:8, 384:384 + 8 * 128]
        o_ = 384 + 1024
        blockones = c32[:, o_:o_ + 128]
        SI = c32[:, o_ + 128:o_ + 192]
        maskAR_f = c32[:, o_ + 192:o_ + 320]
        maskN_f = c32[:, o_ + 320:o_ + 384]
        maskAR_r = c32[:, o_ + 384:o_ + 512]
        maskN_r = c32[:, o_ + 512:o_ + 576]
        cmask = c32[:, o_ + 576:o_ + 1088]
        S.op('pool', lambda e: e.memset(epsT[:, 5:6], 64e-5), writes=['epsT'])
        S.op('dve', lambda e: e.tensor_copy(out=ones_bf[:], in_=ones32), reads=['c32'], writes=['ones_bf'])
        ident_bf = sb("ident_bf", [128, 128], BF16)
        S.op('dve', lambda e: e.tensor_copy(out=ident_bf[:], in_=ident), reads=['c32'], writes=['ident_bf'])

        def V(name, k0=0, k1=None):
            o, c = voff[name]
            k1 = c if k1 is None else k1
            return vecs[:, o + k0:o + k1]

        def phase_mod():
            with contextlib.ExitStack() as st:
                wb = [sb(f"adaw{i}", [128, 8, 1024], F32, st) for i in range(2)]
                sc = sb("sc", [128, 16], F32, st)
                S.op('act', lambda e: e.activation(out=sc[:], in_=cvec[:], func=AF.Silu), reads=['cvec'], writes=['sc'])
                n = 0
                for i in range(depth):
                    for j in range(6):
                        w = wb[n % 2]
                        S.dma('sp', w[:], ada_w[i, :, j * 1024:(j + 1) * 1024].rearrange("(k p) n -> p k n", p=128),
                              writes=[('adaw', n % 2)])
                        for oc in range(8):
                            col = (j * 8 + oc) * 2
                            for kc in range(8):
                                S.op('pe', lambda e: e.matmul(ps[0][:, col:col + 2], lhsT=w[:, kc, oc * 128:(oc + 1) * 128],
                                                              rhs=sc[:, 2 * kc:2 * kc + 2], start=(kc == 0), stop=(kc == 7)),
                                     reads=[('adaw', n % 2), 'sc'], writes=['psmod'], inc=(kc == 7 and oc == 7))
                        n += 1
                    S.op('dve', lambda e: e.tensor_tensor(
                        out=modt[:, i, :, :], in0=ps[0][:, 0:96].rearrange("p (a b) -> p a b", b=2),
                        in1=V(f"adab{i}").unsqueeze(2).to_broadcast([128, 48, 2]), op=ALU.add),
                        reads=['psmod', 'vecs'], writes=['modt'])
                    for (At, gname, jj) in [(A1, f"n1g{i}", 1), (A2, f"n2g{i}", 4)]:
                        S.op('dve', lambda e: e.scalar_tensor_tensor(
                            out=At[:, i, :, :], in0=modt[:, i, jj * 8:(jj + 1) * 8, :], scalar=1.0,
                            in1=V(gname).unsqueeze(2).to_broadcast([128, 8, 2]), op0=ALU.add, op1=ALU.mult),
                            reads=['modt', 'vecs'], writes=['A'])
                        S.op('dve', lambda e: e.tensor_scalar(out=At[:, i, :, :], in0=At[:, i, :, :], scalar1=float(np.sqrt(D)),
                                                              scalar2=None, op0=ALU.mult), reads=['A'], writes=['A'])
            S.barrier()

        def normmod(x32, N, Acol, Scol, outap, sq, rs, psb, kx, tag):
            ksq, krs, kps = ('sq', tag), 'rs', ('ps', psb)
            S.op('pool', lambda e: e.tensor_tensor(out=sq[:, :, :N], in0=x32, in1=x32, op=ALU.mult), reads=[kx], writes=[ksq])
            for k in range(8):
                S.op('pe', lambda e: e.matmul(ps[psb][:, :N], lhsT=ones32, rhs=sq[:, k, :N], start=(k == 0), stop=(k == 7)),
                     reads=[ksq, 'c32'], writes=[kps], inc=(k == 7))
            S.op('act', lambda e: e.activation(out=rs[:, :N], in_=ps[psb][:, :N], func=AF.Sqrt, bias=epsT[:, EPSI[D]:EPSI[D] + 1], scale=1.0),
                 reads=[kps, 'epsT'], writes=[krs])
            S.op('dve', lambda e: e.reciprocal(out=rs[:, :N], in_=rs[:, :N]), reads=[krs], writes=[krs])
            S.op('dve', lambda e: e.tensor_tensor(out=sq[:, :, :N], in0=x32, in1=rs[:, :N].unsqueeze(1).to_broadcast([128, 8, N]),
                                                  op=ALU.mult), reads=[kx, krs], writes=[ksq])
            S.op('pool', lambda e: e.tensor_tensor(out=sq[:, :, :N], in0=sq[:, :, :N], in1=Acol.unsqueeze(2).to_broadcast([128, 8, N]),
                                                   op=ALU.mult), reads=[ksq, 'A'], writes=[ksq])
            return ksq

        def rstd_from_ps(psb, N, nfeat, rs, krs, P=128):
            S.op('act', lambda e: e.activation(out=rs[:P, :N], in_=ps[psb][:P, :N], func=AF.Sqrt, bias=epsT[:P, EPSI[nfeat]:EPSI[nfeat] + 1], scale=1.0),
                 reads=[('ps', psb), 'epsT'], writes=[krs])
            S.op('dve', lambda e: e.reciprocal(out=rs[:P, :N], in_=rs[:P, :N]), reads=[krs], writes=[krs])

        def phase_mla(i, XSin):
            j = i // 2
            with contextlib.ExitStack() as st:
                wqa = sb("wqa", [128, 8, 384], BF16, st); wqb = sb("wqb", [128, 3, 1536], BF16, st)
                wkva = sb("wkva", [128, 8, 320], BF16, st); wkvb = sb("wkvb", [128, 2, 2048], BF16, st)
                S.dma('pool', wqa[:], mla_wqa[j].rearrange("(k p) n -> p k n", p=128), writes=['wqa'])
                S.dma('pool', wqb[:], mla_wqb[j].rearrange("(k p) n -> p k n", p=128), writes=['wqb'])
                S.dma('pool', wkva[:], mla_wkva[j].rearrange("(k p) n -> p k n", p=128), writes=['wkva'])
                S.dma('pool', wkvb[:], mla_wkvb[j].rearrange("(k p) n -> p k n", p=128), writes=['wkvb'])
                xt = [sb("xt0", [128, 8, 512], F32, st)] * 2
                sq = sb("sq", [128, 8, 512], F32, st)
                rs = sb("rs", [128, 512], F32, st)
                hb = sb("hb", [128, 8, 512], BF16, st)
                cq = sb("cq", [128, 3, 512], F32, st); cq2 = sb("cq2", [128, 3, 512], F32, st)
                cqn = sb("cqn", [128, 3, 512], BF16, st)
                ckv = sb("ckv", [128, 2, 512], F32, st); ckv2 = sb("ckv2", [128, 2, 512], F32, st)
                ckvn = sb("ckvn", [128, 2, 512], BF16, st)
                kr = sb("kr", [64, 512], F32, st); kr2 = sb("kr2", [64, 512], F32, st)
                krP = sb("krP", [64, 512], F32, st); krr = sb("krr", [64, 512], F32, st)
                qh = sb("qh", [128, 512], F32, st); qh2 = sb("qh2", [128, 512], F32, st)
                qr = sb("qr", [64, 512], F32, st); qr2 = sb("qr2", [64, 512], F32, st)
                qrP = sb("qrP", [64, 512], F32, st)
                rs2 = sb("rs2", [128, 512], F32, st)
                cosT = sb("cosT", [64, 512], F32, st); sinT = sb("sinT", [64, 512], F32, st)
                qn_o = sb("qn_o", [128, NH, 512], BF16, st); qr_o = sb("qr_o", [64, NH, 512], BF16, st)
                kn_o = sb("kn_o", [128, NH, 512], BF16, st); kr_o = sb("kr_o", [64, NH, 512], BF16, st)
                v_o = sb("v_o", [128, 4, NH, 128], BF16, st)
                sv = sb("sv", [128, 16], F32, st)
                S.op('dve', lambda e: e.tensor_scalar(out=sv[:, 0:3], in0=V(f"qan{j}"), scalar1=float(np.sqrt(384)), scalar2=None, op0=ALU.mult),
                     reads=['vecs'], writes=['sv'])
                S.op('dve', lambda e: e.tensor_scalar(out=sv[:, 3:5], in0=V(f"kvan{j}"), scalar1=float(np.sqrt(256)), scalar2=None, op0=ALU.mult),
                     reads=['vecs'], writes=['sv'])
                S.op('dve', lambda e: e.tensor_scalar(out=sv[:, 5:6], in0=V(f"qnn{j}"), scalar1=float(np.sqrt(192) * SM_SCALE), scalar2=None, op0=ALU.mult),
                     reads=['vecs'], writes=['sv'])
                S.op('dve', lambda e: e.tensor_scalar(out=sv[:, 6:7], in0=V(f"qnr{j}"), scalar1=float(np.sqrt(192) * SM_SCALE), scalar2=None, op0=ALU.mult),
                     reads=['vecs'], writes=['sv'])
                S.op('dve', lambda e: e.tensor_scalar(out=sv[:, 7:8], in0=V(f"knn{j}"), scalar1=float(np.sqrt(192)), scalar2=None, op0=ALU.mult),
                     reads=['vecs'], writes=['sv'])
                S.op('dve', lambda e: e.tensor_scalar(out=sv[:, 8:9], in0=V(f"knr{j}"), scalar1=float(np.sqrt(192)), scalar2=None, op0=ALU.mult),
                     reads=['vecs'], writes=['sv'])

                def rope(src, Pbuf, dst, N, ksrc, kdst, psb):
                    S.op('pe', lambda e: e.matmul(ps[psb][:64, :N], lhsT=rotP, rhs=src[:, :N], start=True, stop=True),
                         reads=[ksrc, 'c32'], writes=[('ps', psb)])
                    S.op('dve', lambda e: e.tensor_tensor(out=Pbuf[:, :N], in0=ps[psb][:64, :N], in1=sinT[:, :N], op=ALU.mult),
                         reads=[('ps', psb), 'rope'], writes=[('rp', kdst)])
                    S.op('pool', lambda e: e.tensor_tensor(out=dst[:, :N], in0=src[:, :N], in1=cosT[:, :N], op=ALU.mult),
                         reads=[ksrc, 'rope'], writes=[kdst])
                    S.op('pool', lambda e: e.tensor_tensor(out=dst[:, :N], in0=dst[:, :N], in1=Pbuf[:, :N], op=ALU.add),
                         reads=[kdst, ('rp', kdst)], writes=[kdst])

                for ti, (t0, N, mc) in enumerate(TILES):
                    x32 = xt[ti % 2]
                    kx = ('xt', ti % 2)
                    S.dma('sp', x32[:, :, :N], XSin[:, t0:t0 + N].rearrange("(k p) n -> p k n", p=128), writes=[kx])
                    if mc == 0:
                        S.dma('sp', cosT[:, :N], ropec_d[:, t0 - CTX:t0 - CTX + N], writes=['rope'])
                        S.dma('sp', sinT[:, :N], ropes_d[:, t0 - CTX:t0 - CTX + N], writes=['rope'])
                    ksq = normmod(x32[:, :, :N], N, A1[:, i, :, mc], None, None, sq, rs, 7, kx, 'm')
                    S.op('dve', lambda e: e.tensor_tensor(out=hb[:, :, :N], in0=sq[:, :, :N],
                                                          in1=modt[:, i, 0:8, mc].unsqueeze(2).to_broadcast([128, 8, N]), op=ALU.add),
                         reads=[ksq, 'modt'], writes=['hb'])
                    for c in range(3):
                        pb = c % 2
                        for kc in range(8):
                            S.op('pe', lambda e: e.matmul(ps[pb][:, :N], lhsT=wqa[:, kc, c * 128:(c + 1) * 128], rhs=hb[:, kc, :N],
                                                          start=(kc == 0), stop=(kc == 7)), reads=['hb', 'wqa'], writes=[('ps', pb)], inc=(kc == 7))
                        S.op('act', lambda e: e.activation(out=cq[:, c, :N], in_=ps[pb][:, :N], func=AF.Identity),
                             reads=[('ps', pb)], writes=[('cq', c)])
                        S.op('pool', lambda e: e.tensor_tensor(out=cq2[:, c, :N], in0=cq[:, c, :N], in1=cq[:, c, :N], op=ALU.mult),
                             reads=[('cq', c)], writes=[('cq2', c)])
                    for c in range(3):
                        S.op('pe', lambda e: e.matmul(ps[7][:, :N], lhsT=ones32, rhs=cq2[:, c, :N], start=(c == 0), stop=(c == 2)),
                             reads=[('cq2', c), 'c32'], writes=[('ps', 7)], inc=(c == 2))
                    rstd_from_ps(7, N, 384, rs, 'rs')
                    for c in range(3):
                        S.op('dve', lambda e: e.scalar_tensor_tensor(out=cqn[:, c, :N], in0=cq[:, c, :N], scalar=sv[:, c:c + 1], in1=rs[:, :N],
                                                                     op0=ALU.mult, op1=ALU.mult), reads=[('cq', c), 'rs', 'sv'], writes=['cqn'])
                    for c in range(3):
                        pb = 2 + c % 2
                        M = 128 if c < 2 else 64
                        for kc in range(8):
                            S.op('pe', lambda e: e.matmul(ps[pb][:M, :N], lhsT=wkva[:, kc, c * 128:c * 128 + M], rhs=hb[:, kc, :N],
                                                          start=(kc == 0), stop=(kc == 7)), reads=['hb', 'wkva'], writes=[('ps', pb)], inc=(kc == 7))
                        if c < 2:
                            S.op('act', lambda e: e.activation(out=ckv[:, c, :N], in_=ps[pb][:, :N], func=AF.Identity),
                                 reads=[('ps', pb)], writes=[('ckv', c)])
                            S.op('pool', lambda e: e.tensor_tensor(out=ckv2[:, c, :N], in0=ckv[:, c, :N], in1=ckv[:, c, :N], op=ALU.mult),
                                 reads=[('ckv', c)], writes=[('ckv2', c)])
                        else:
                            S.op('act', lambda e: e.activation(out=kr[:, :N], in_=ps[pb][:64, :N], func=AF.Identity),
                                 reads=[('ps', pb)], writes=['kr'])
                            S.op('pool', lambda e: e.tensor_tensor(out=kr2[:, :N], in0=kr[:, :N], in1=kr[:, :N], op=ALU.mult),
                                 reads=['kr'], writes=['kr2'])
                            S.op('dve', lambda e: e.tensor_scalar(out=kr[:, :N], in0=kr[:, :N], scalar1=sv[0:64, 8:9], scalar2=None, op0=ALU.mult),
                                 reads=['kr', 'sv', 'kr2'], writes=['kr'])
                    for c in range(2):
                        S.op('pe', lambda e: e.matmul(ps[7][:, :N], lhsT=ones32, rhs=ckv2[:, c, :N], start=(c == 0), stop=(c == 1)),
                             reads=[('ckv2', c), 'c32'], writes=[('ps', 7)], inc=(c == 1))
                    rstd_from_ps(7, N, 256, rs, 'rs')
                    for c in range(2):
                        S.op('dve', lambda e: e.scalar_tensor_tensor(out=ckvn[:, c, :N], in0=ckv[:, c, :N], scalar=sv[:, 3 + c:4 + c], in1=rs[:, :N],
                                                                     op0=ALU.mult, op1=ALU.mult), reads=[('ckv', c), 'rs', 'sv'], writes=['ckvn'])
                    if mc == 0:
                        rope(kr, krP, krr, N, 'kr', 'krr', 6)
                        krsrc, kkr = krr, 'krr'
                    else:
                        krsrc, kkr = kr, 'kr'
                    for h in range(NH):
                        for kc in range(3):
                            S.op('pe', lambda e: e.matmul(ps[0][:, :N], lhsT=wqb[:, kc, h * 192:h * 192 + 128], rhs=cqn[:, kc, :N],
                                                          start=(kc == 0), stop=(kc == 2)), reads=['cqn', 'wqb'], writes=[('ps', 0)], inc=(kc == 2))
                        for kc in range(3):
                            S.op('pe', lambda e: e.matmul(ps[1][:64, :N], lhsT=wqb[:, kc, h * 192 + 128:h * 192 + 192], rhs=cqn[:, kc, :N],
                                                          start=(kc == 0), stop=(kc == 2)), reads=['cqn', 'wqb'], writes=[('ps', 1)], inc=(kc == 2))
                        S.op('act', lambda e: e.activation(out=qh[:, :N], in_=ps[0][:, :N], func=AF.Identity), reads=[('ps', 0)], writes=['qh'])
                        S.op('act', lambda e: e.activation(out=qr[:, :N], in_=ps[1][:64, :N], func=AF.Identity), reads=[('ps', 1)], writes=['qr'])
                        S.op('pool', lambda e: e.tensor_tensor(out=qh2[:, :N], in0=qh[:, :N], in1=qh[:, :N], op=ALU.mult), reads=['qh'], writes=['qh2'])
                        S.op('pool', lambda e: e.tensor_tensor(out=qr2[:, :N], in0=qr[:, :N], in1=qr[:, :N], op=ALU.mult), reads=['qr'], writes=['qr2'])
                        S.op('pe', lambda e: e.matmul(ps[4][:, :N], lhsT=ones32, rhs=qh2[:, :N], start=True, stop=False),
                             reads=['qh2', 'c32'], writes=[('ps', 4)], inc=False)
                        S.op('pe', lambda e: e.matmul(ps[4][:, :N], lhsT=c32[0:64, 0:128], rhs=qr2[:, :N], start=False, stop=True),
                             reads=['qr2', 'c32'], writes=[('ps', 4)])
                        rstd_from_ps(4, N, 192, rs2, 'rs2')
                        S.op('dve', lambda e: e.scalar_tensor_tensor(out=qn_o[:, h, :N], in0=qh[:, :N], scalar=sv[:, 5:6], in1=rs2[:, :N],
                                                                     op0=ALU.mult, op1=ALU.mult), reads=['qh', 'rs2', 'sv'], writes=['qn_o'])
                        S.op('dve', lambda e: e.scalar_tensor_tensor(out=qr[:, :N], in0=qr[:, :N], scalar=sv[0:64, 6:7], in1=rs2[0:64, :N],
                                                                     op0=ALU.mult, op1=ALU.mult), reads=['qr', 'rs2', 'sv', 'qr2'], writes=['qr'])
                        if mc == 0:
                            rope(qr, qrP, qr2, N, 'qr', 'qr2', 6)
                            S.op('act', lambda e: e.activation(out=qr_o[:, h, :N], in_=qr2[:, :N], func=AF.Identity), reads=['qr2'], writes=['qr_o'])
                        else:
                            S.op('act', lambda e: e.activation(out=qr_o[:, h, :N], in_=qr[:, :N], func=AF.Identity), reads=['qr'], writes=['qr_o'])
                        for kc in range(2):
                            S.op('pe', lambda e: e.matmul(ps[2][:, :N], lhsT=wkvb[:, kc, h * 256:h * 256 + 128], rhs=ckvn[:, kc, :N],
                                                          start=(kc == 0), stop=(kc == 1)), reads=['ckvn', 'wkvb'], writes=[('ps', 2)], inc=(kc == 1))
                        S.op('act', lambda e: e.activation(out=qh[:, :N], in_=ps[2][:, :N], func=AF.Identity), reads=[('ps', 2)], writes=['qh'])
                        S.op('pool', lambda e: e.tensor_tensor(out=qh2[:, :N], in0=qh[:, :N], in1=qh[:, :N], op=ALU.mult), reads=['qh'], writes=['qh2'])
                        S.op('pe', lambda e: e.matmul(ps[5][:, :N], lhsT=ones32, rhs=qh2[:, :N], start=True, stop=False),
                             reads=['qh2', 'c32'], writes=[('ps', 5)], inc=False)
                        S.op('pe', lambda e: e.matmul(ps[5][:, :N], lhsT=c32[0:64, 0:128], rhs=kr2[:, :N], start=False, stop=True),
                             reads=['kr2', 'c32'], writes=[('ps', 5)])
                        rstd_from_ps(5, N, 192, rs2, 'rs2')
                        S.op('dve', lambda e: e.scalar_tensor_tensor(out=kn_o[:, h, :N], in0=qh[:, :N], scalar=sv[:, 7:8], in1=rs2[:, :N],
                                                                     op0=ALU.mult, op1=ALU.mult), reads=['qh', 'rs2', 'sv'], writes=['kn_o'])
                        S.op('dve', lambda e: e.tensor_tensor(out=kr_o[:, h, :N], in0=krsrc[:, :N], in1=rs2[0:64, :N], op=ALU.mult),
                             reads=[kkr, 'rs2'], writes=['kr_o'])
                        for blk in range(N // 128):
                            for kc in range(2):
                                S.op('pe', lambda e: e.matmul(ps[3][:, blk * 128:(blk + 1) * 128], lhsT=ckvn[:, kc, blk * 128:(blk + 1) * 128],
                                                              rhs=wkvb[:, kc, h * 256 + 128:h * 256 + 256], start=(kc == 0), stop=(kc == 1)),
                                     reads=['ckvn', 'wkvb'], writes=[('ps', 3)], inc=(kc == 1 and blk == N // 128 - 1))
                        S.op('act', lambda e: e.activation(out=v_o[:, 0:N // 128, h, :], in_=ps[3][:, :N].rearrange("p (b d) -> p b d", d=128),
                                                           func=AF.Identity), reads=[('ps', 3)], writes=['v_o'])
                    S.dma('sp', QN[:, :, t0:t0 + N].rearrange("h p n -> p h n"), qn_o[:, :, :N], reads=['qn_o'], writes=['QN'])
                    S.dma('sp', QR[:, :, t0:t0 + N].rearrange("h p n -> p h n"), qr_o[:, :, :N], reads=['qr_o'], writes=['QR'])
                    S.dma('sp', KN[:, :, t0:t0 + N].rearrange("h p n -> p h n"), kn_o[:, :, :N], reads=['kn_o'], writes=['KN'])
                    S.dma('sp', KR[:, :, t0:t0 + N].rearrange("h p n -> p h n"), kr_o[:, :, :N], reads=['kr_o'], writes=['KR'])
                    S.dma('sp', VV[t0:t0 + N].rearrange("(b p) h d -> p b h d", p=128), v_o[:, 0:N // 128], reads=['v_o'], writes=['VV'])
            S.barrier()
            with contextlib.ExitStack() as st:
                kn = [sb(f"a_kn{b}", [128, NT], BF16, st) for b in range(2)]
                krt = [sb(f"a_kr{b}", [64, NT], BF16, st) for b in range(2)]
                qn = [sb(f"a_qn{b}", [128, NT], BF16, st) for b in range(2)]
                qrt = [sb(f"a_qr{b}", [64, NT], BF16, st) for b in range(2)]
                vt = [sb(f"a_v{b}", [128, NT // 128, 128], BF16, st) for b in range(2)]
                pT = [sb(f"a_p{b}", [128, 512], BF16, st) for b in range(3)]
                rd = sb("a_rd", [128, 512], F32, st)
                ao = [sb(f"a_o{b}", [128, 512], BF16, st) for b in range(2)]
                npt = 0
                nq = 0
                for h in range(NH):
                    b = h % 2
                    kh = ('ah', b)
                    S.dma('sp', kn[b][:], KN[h], writes=[kh])
                    S.dma('sp', krt[b][:], KR[h], writes=[kh])
                    S.dma('sp', qn[b][:], QN[h], writes=[kh])
                    S.dma('sp', qrt[b][:], QR[h], writes=[kh])
                    S.dma('sp', vt[b][:], VV[:, h, :].rearrange("(b p) d -> p b d", p=128), writes=[kh])
                    for (t0, N, mc) in TILES:
                        nkb = 2 if mc == 1 else NT // 128
                        po, pd = 4 + (nq % 2), 6 + (nq % 2)

                        def scores(kb):
                            sbk = kb % 3
                            S.op('pe', lambda e: e.matmul(ps[sbk][:, :N], lhsT=kn[b][:, kb * 128:(kb + 1) * 128], rhs=qn[b][:, t0:t0 + N],
                                                          start=True, stop=False), reads=[kh], writes=[('ps', sbk)], inc=False)
                            S.op('pe', lambda e: e.matmul(ps[sbk][:, :N], lhsT=krt[b][:, kb * 128:(kb + 1) * 128], rhs=qrt[b][:, t0:t0 + N],
                                                          start=False, stop=True), reads=[kh], writes=[('ps', sbk)])
                        scores(0)
                        for kb in range(nkb):
                            if kb + 1 < nkb:
                                scores(kb + 1)
                            sbk = kb % 3
                            pb = npt % 3
                            npt += 1
                            S.op('act', lambda e: e.activation(out=pT[pb][:, :N], in_=ps[sbk][:, :N], func=AF.Exp),
                                 reads=[('ps', sbk)], writes=[('pT', pb)])
                            S.op('pe', lambda e: e.matmul(ps[po][:, :N], lhsT=vt[b][:, kb, :], rhs=pT[pb][:, :N], start=(kb == 0), stop=(kb == nkb - 1)),
                                 reads=[kh, ('pT', pb)], writes=[('ps', po)], inc=False)
                            S.op('pe', lambda e: e.matmul(ps[pd][:, :N], lhsT=ones_bf[:], rhs=pT[pb][:, :N], start=(kb == 0), stop=(kb == nkb - 1)),
                                 reads=['ones_bf', ('pT', pb)], writes=[('ps', pd)])
                        S.op('dve', lambda e: e.reciprocal(out=rd[:, :N], in_=ps[pd][:, :N]), reads=[('ps', pd)], writes=['rd'])
                        ob = nq % 2
                        S.op('dve', lambda e: e.tensor_tensor(out=ao[ob][:, :N], in0=ps[po][:, :N], in1=rd[:, :N], op=ALU.mult),
                             reads=[('ps', po), 'rd'], writes=[('ao', ob)])
                        S.dma('sp', AO[h * 128:(h + 1) * 128, t0:t0 + N], ao[ob][:, :N], reads=[('ao', ob)], writes=['AO'])
                        nq += 1
            S.barrier()
            with contextlib.ExitStack() as st:
                wo = sb("wo", [128, 8, 1024], BF16, st)
                S.dma('pool', wo[:], mla_wo[j].rearrange("(k p) n -> p k n", p=128), writes=['wo'])
                xt = [sb(f"c_xt{b}", [128, 8, 512], F32, st) for b in range(2)]
                at = [sb(f"c_at{b}", [128, 8, 512], BF16, st) for b in range(2)]
                for ti, (t0, N, mc) in enumerate(TILES):
                    b = ti % 2
                    S.dma('sp', xt[b][:, :, :N], XSin[:, t0:t0 + N].rearrange("(k p) n -> p k n", p=128), writes=[('cx', b)])
                    S.dma('sp', at[b][:, :, :N], AO[:, t0:t0 + N].rearrange("(k p) n -> p k n", p=128), writes=[('ca', b)])
                    for oc in range(8):
                        pb = oc % 4
                        for kc in range(8):
                            S.op('pe', lambda e: e.matmul(ps[pb][:, :N], lhsT=wo[:, kc, oc * 128:(oc + 1) * 128], rhs=at[b][:, kc, :N],
                                                          start=(kc == 0), stop=(kc == 7)), reads=['wo', ('ca', b)], writes=[('ps', pb)], inc=(kc == 7))
                        S.op('dve', lambda e: e.scalar_tensor_tensor(out=xt[b][:, oc, :N], in0=ps[pb][:, :N], scalar=modt[:, i, 16 + oc, mc:mc + 1],
                                                                     in1=xt[b][:, oc, :N], op0=ALU.mult, op1=ALU.add),
                             reads=[('ps', pb), ('cx', b), 'modt'], writes=[('cx', b)])
                    S.dma('sp', XS[:, t0:t0 + N].rearrange("(k p) n -> p k n", p=128), xt[b][:, :, :N], reads=[('cx', b)], writes=['XS'])
            S.barrier()

        def phase_rwkv(i, last):
            j = i // 2
            NCH = NT // 64
            RT256 = [(0, 256, 1)] + [(256 + 256 * a, 256, 0) for a in range(16)]
            HC = HS[:, 0:258]
            HL = HS[:, 258:258 + 4098]
            with contextlib.ExitStack() as st:
                xt = [sb(f"r1x{b}", [128, 8, 512], F32, st) for b in range(2)]
                sq = sb("r1sq", [128, 8, 512], F32, st)
                rs = sb("r1rs", [128, 512], F32, st)
                zt = sb("r1z", [128, 8, 1], F32, st)
                S.op('pool', lambda e: e.memset(zt[:], 0.0), writes=['zt'])
                for col in (0, 257, 258, 258 + 4097):
                    S.dma('sp', HS[:, col:col + 1].rearrange("(k p) n -> p k n", p=128), zt[:], reads=['zt'], writes=['HS'], allow_slow_non_contiguous=True)
                for ti, (t0, N, mc) in enumerate(TILES):
                    b = ti % 2
                    kx = ('r1x', b)
                    S.dma('sp', xt[b][:, :, :N], XS[:, t0:t0 + N].rearrange("(k p) n -> p k n", p=128), writes=[kx])
                    ksq = normmod(xt[b][:, :, :N], N, A1[:, i, :, mc], None, None, sq, rs, 7, kx, 'r1')
                    S.op('dve', lambda e: e.tensor_tensor(out=xt[b][:, :, :N], in0=sq[:, :, :N],
                                                          in1=modt[:, i, 0:8, mc].unsqueeze(2).to_broadcast([128, 8, N]), op=ALU.add),
                         reads=[ksq, 'modt'], writes=[kx])
                    dst = HC[:, 1:257] if mc == 1 else HL[:, 1 + t0 - CTX:1 + t0 - CTX + N]
                    S.dma('sp', dst.rearrange("(k p) n -> p k n", p=128), xt[b][:, :, :N], reads=[kx], writes=['HS'])
            S.barrier()
            if os.environ.get('RSTOP') == '1':
                return
            class _Stop(Exception):
                pass

            def stg(x):
                if os.environ.get('R2STOP') == x:
                    S.mute = True
            try:
              with contextlib.ExitStack() as st:
                  N = 256
                  wr = sb("wr", [128, 8, 1024], BF16, st); wk = sb("wk", [128, 8, 1024], BF16, st); wv = sb("wv", [128, 8, 1024], BF16, st)
                  w1c = sb("w1c", [128, 8, 128], BF16, st); a1c = sb("a1c", [128, 8, 128], BF16, st)
                  g1 = sb("g1", [128, 8, 160], BF16, st); g2a = sb("g2a", [128, 1024], BF16, st); g2b = sb("g2b", [32, 1024], BF16, st)
                  w2p = sb("w2p", [128, 2, 1024], BF16, st); a2p = sb("a2p", [128, 2, 1024], BF16, st)
                  for (wt_, src, kk_) in [(wr, rwkv_wr, 'wr'), (wk, rwkv_wk, 'wk'), (wv, rwkv_wv, 'wv')]:
                      S.dma('pool', wt_[:], src[j].rearrange("(k p) n -> p k n", p=128), writes=[kk_])
                  for d in range(2):
                      S.dma('pool', w1c[:, :, d * 64:(d + 1) * 64], rwkv_w1[j, d].rearrange("(k p) n -> p k n", p=128), writes=['w1c'])
                      S.dma('pool', a1c[:, :, d * 64:(d + 1) * 64], rwkv_a1[j, d].rearrange("(k p) n -> p k n", p=128), writes=['a1c'])
                  S.dma('pool', g1[:], rwkv_g1[j].rearrange("(k p) n -> p k n", p=128), writes=['g1'])
                  S.dma('pool', g2a[:], rwkv_g2[j, 0:128, :], writes=['g2a'])
                  S.dma('pool', g2b[:], rwkv_g2[j, 128:160, :], writes=['g2b'])
                  S.op('pool', lambda e: e.memset(w2p[:], 0.0), writes=['w2p'])
                  S.op('pool', lambda e: e.memset(a2p[:], 0.0), writes=['a2p'])
                  for d in range(2):
                      S.dma('pool', w2p[d * 64:(d + 1) * 64, d, :], rwkv_w2[j, d], writes=['w2p'])
                      S.dma('pool', a2p[d * 64:(d + 1) * 64, d, :], rwkv_a2[j, d], writes=['a2p'])
                  vres = j >= 1
                  if vres:
                      v1 = sb("v1", [128, 8, 32], BF16, st); v2 = sb("v2", [32, 1024], BF16, st)
                      S.dma('pool', v1[:], rwkv_v1[j - 1].rearrange("(k p) n -> p k n", p=128), writes=['v1'])
                      S.dma('pool', v2[:], rwkv_v2[j - 1], writes=['v2'])
                      vf = [sb(f"vf{q}", [128, N], F32, st) for q in range(2)]
                  dv_ = sb("dv", [128, 16], F32, st)
                  S.op('dve', lambda e: e.tensor_scalar(out=dv_[:, 0:8], in0=V(f"ka{j}"), scalar1=-1.0, scalar2=1.0, op0=ALU.mult, op1=ALU.add),
                       reads=['vecs'], writes=['dv'])
                  S.op('dve', lambda e: e.tensor_scalar(out=dv_[:, 8:16], in0=V(f"rk{j}"), scalar1=0.5, scalar2=None, op0=ALU.mult),
                       reads=['vecs'], writes=['dv'])
                  hx = sb("hx", [128, 8, N + 2], F32, st)
                  xx = sb("xx", [128, 8, N], F32, st)
                  xm = [sb(f"xm{m}", [128, 8, N], BF16, st) for m in range(6)]
                  lwm = sb("lwm", [128, N], BF16, st); am = sb("am", [128, N], BF16, st)
                  gma = sb("gma", [128, N], BF16, st); gmb = sb("gmb", [32, N], BF16, st); vm = sb("vm", [32, N], BF16, st)
                  T = {}
                  for nm in ["r32", "k32", "v32", "g32", "vv", "dvv", "kkc", "kk2", "rn", "kkn", "aneg", "sig", "lw", "al", "tk", "kd", "bb",
                             "Lp", "Lc", "E1", "E2", "E3", "t2", "At", "Rt", "Bt", "Kt", "kb", "t3", "bon", "vb"]:
                      T[nm] = [sb(f"f_{nm}{q}", [128, N], BF16 if nm in ("At", "Rt", "Bt", "Kt", "vb") else F32, st) for q in range(2)]
                  wct = [sb(f"wct{q}", [128, 4], F32, st) for q in range(2)]

                  def hb_(n):
                      return ps[n // 2][:, (n % 2) * 256:(n % 2) * 256 + 256]

                  def hk(n):
                      return ('psb', n // 2)

                  for ti, (t0, N_, mc) in enumerate(RT256):
                      src = HC[:, 0:258] if mc == 1 else HL[:, t0 - CTX:t0 - CTX + N + 2]
                      S.dma('sp', hx[:], src.rearrange("(k p) n -> p k n", p=128), writes=['hx'])
                      S.op('dve', lambda e: e.tensor_tensor(out=xx[:], in0=hx[:, :, 0:N], in1=hx[:, :, 2:N + 2], op=ALU.add), reads=['hx'], writes=['xx'])
                      S.op('dve', lambda e: e.scalar_tensor_tensor(out=xx[:], in0=xx[:], scalar=0.5, in1=hx[:, :, 1:N + 1], op0=ALU.mult, op1=ALU.subtract),
                           reads=['hx', 'xx'], writes=['xx'])
                      for m in range(6):
                          for kc in range(8):
                              S.op('dve', lambda e: e.scalar_tensor_tensor(out=xm[m][:, kc, :], in0=xx[:, kc, :], scalar=V(f"mix{j}", m * 8 + kc, m * 8 + kc + 1),
                                                                           in1=hx[:, kc, 1:N + 1], op0=ALU.mult, op1=ALU.add),
                                   reads=['hx', 'xx', 'vecs'], writes=[('xm', m)])
                      stg('a')
                      for kc in range(8):
                          S.op('pe', lambda e: e.matmul(hb_(0), lhsT=w1c[:, kc, :], rhs=xm[1][:, kc, :], start=(kc == 0), stop=(kc == 7)),
                               reads=['w1c', ('xm', 1)], writes=[hk(0)], inc=(kc == 7))
                      S.op('act', lambda e: e.activation(out=T["sig"][0][:], in_=hb_(0), func=AF.Sigmoid, scale=2.0), reads=[hk(0)], writes=['sig0'])
                      S.op('dve', lambda e: e.tensor_scalar(out=lwm[:], in0=T["sig"][0][:], scalar1=2.0, scalar2=-1.0, op0=ALU.mult, op1=ALU.add),
                           reads=['sig0'], writes=['lwm'])
                      for kc in range(8):
                          S.op('pe', lambda e: e.matmul(hb_(1), lhsT=a1c[:, kc, :], rhs=xm[4][:, kc, :], start=(kc == 0), stop=(kc == 7)),
                               reads=['a1c', ('xm', 4)], writes=[hk(1)], inc=(kc == 7))
                      S.op('act', lambda e: e.activation(out=am[:], in_=hb_(1), func=AF.Identity), reads=[hk(1)], writes=['am'])
                      for kc in range(8):
                          S.op('pe', lambda e: e.matmul(hb_(2), lhsT=g1[:, kc, 0:128], rhs=xm[5][:, kc, :], start=(kc == 0), stop=(kc == 7)),
                               reads=['g1', ('xm', 5)], writes=[hk(2)], inc=(kc == 7))
                      S.op('act', lambda e: e.activation(out=gma[:], in_=hb_(2), func=AF.Sigmoid), reads=[hk(2)], writes=['gma'])
                      for kc in range(8):
                          S.op('pe', lambda e: e.matmul(hb_(3)[0:32, :], lhsT=g1[:, kc, 128:160], rhs=xm[5][:, kc, :], start=(kc == 0), stop=(kc == 7)),
                               reads=['g1', ('xm', 5)], writes=[hk(3)], inc=(kc == 7))
                      S.op('act', lambda e: e.activation(out=gmb[:], in_=hb_(3)[0:32, :], func=AF.Sigmoid), reads=[hk(3)], writes=['gmb'])
                      if vres:
                          for kc in range(8):
                              S.op('pe', lambda e: e.matmul(hb_(4)[0:32, :], lhsT=v1[:, kc, :], rhs=xm[3][:, kc, :], start=(kc == 0), stop=(kc == 7)),
                                   reads=['v1', ('xm', 3)], writes=[hk(4)], inc=(kc == 7))
                          S.op('act', lambda e: e.activation(out=vm[:], in_=hb_(4)[0:32, :], func=AF.Identity), reads=[hk(4)], writes=['vm'])
                      def chunk_gen(c):
                          pc = c % 2
                          pcs = str(pc)
                          H = {5: 8 * pc, 6: 8 * pc + 1, 7: 8 * pc + 2, 8: 8 * pc + 3, 9: 8 * pc + 4, 10: 8 * pc + 5, 11: 8 * pc + 6, 12: 8 * pc + 7,
                               13: 8 * pc, 14: 8 * pc + 2, 15: 8 * pc + 3}
                          cs = slice(c * 128, (c + 1) * 128)
                          rows = slice(c * 128, (c + 1) * 128)
                          for (hbn, w_, m) in [(5, wr, 0), (6, wk, 2), (7, wv, 3)]:
                              for kc in range(8):
                                  S.op('pe', lambda e: e.matmul(hb_(H[hbn]), lhsT=w_[:, kc, cs], rhs=xm[m][:, kc, :], start=(kc == 0), stop=(kc == 7)),
                                       reads=[('xm', m), 'wr', 'wk', 'wv'], writes=[hk(H[hbn])], inc=(kc == 7))
                          S.op('pe', lambda e: e.matmul(hb_(H[8]), lhsT=g2a[:, cs], rhs=gma[:], start=True, stop=False), reads=['g2a', 'gma'], writes=[hk(H[8])], inc=False)
                          S.op('pe', lambda e: e.matmul(hb_(H[8]), lhsT=g2b[:, cs], rhs=gmb[:], start=False, stop=True), reads=['g2b', 'gmb'], writes=[hk(H[8])])
                          for d in range(2):
                              S.op('pe', lambda e: e.matmul(hb_(H[9 + d]), lhsT=w2p[:, d, cs], rhs=lwm[:], start=True, stop=True), reads=['w2p', 'lwm'], writes=[hk(H[9 + d])])
                              S.op('pe', lambda e: e.matmul(hb_(H[11 + d]), lhsT=a2p[:, d, cs], rhs=am[:], start=True, stop=True), reads=['a2p', 'am'], writes=[hk(H[11 + d])])
                          yield
                          S.op('act', lambda e: e.activation(out=T["r32"][pc][:], in_=hb_(H[5]), func=AF.Identity), reads=[hk(H[5])], writes=['r32' + pcs])
                          S.op('act', lambda e: e.activation(out=T["k32"][pc][:], in_=hb_(H[6]), func=AF.Identity), reads=[hk(H[6])], writes=['k32' + pcs])
                          S.op('act', lambda e: e.activation(out=T["v32"][pc][:], in_=hb_(H[7]), func=AF.Identity), reads=[hk(H[7])], writes=['v32' + pcs])
                          S.op('act', lambda e: e.activation(out=T["g32"][pc][:], in_=hb_(H[8]), func=AF.Identity), reads=[hk(H[8])], writes=['g32' + pcs])
                          if vres:
                              S.op('pe', lambda e: e.matmul(hb_(H[13]), lhsT=v2[:, cs], rhs=vm[:], start=True, stop=True), reads=['v2', 'vm'], writes=[hk(H[13])])
                              S.dma('sp', vf[pc][:], VT[0][rows, t0:t0 + N], writes=['vf' + pcs])
                              S.op('act', lambda e: e.activation(out=T["vv"][pc][:], in_=hb_(H[13]), func=AF.Sigmoid, bias=V(f"v0{j}", c, c + 1), scale=1.0),
                                   reads=[hk(H[13]), 'vecs'], writes=['vv' + pcs])
                              S.op('pool', lambda e: e.tensor_tensor(out=T["dvv"][pc][:], in0=vf[pc][:], in1=T["v32"][pc][:], op=ALU.subtract), reads=['vf' + pcs, 'v32' + pcs], writes=['dvv' + pcs])
                              S.op('pool', lambda e: e.tensor_tensor(out=T["dvv"][pc][:], in0=T["dvv"][pc][:], in1=T["vv"][pc][:], op=ALU.mult), reads=['dvv' + pcs, 'vv' + pcs], writes=['dvv' + pcs])
                              S.op('pool', lambda e: e.tensor_tensor(out=T["v32"][pc][:], in0=T["v32"][pc][:], in1=T["dvv"][pc][:], op=ALU.add), reads=['dvv' + pcs, 'v32' + pcs], writes=['v32' + pcs])
                          yield
                          S.op('dve', lambda e: e.tensor_scalar(out=T["kkc"][pc][:], in0=T["k32"][pc][:], scalar1=V(f"kk{j}", c, c + 1), scalar2=None, op0=ALU.mult),
                               reads=['k32' + pcs, 'vecs'], writes=['kkc' + pcs])
                          S.op('pool', lambda e: e.tensor_tensor(out=T["kk2"][pc][:], in0=T["kkc"][pc][:], in1=T["kkc"][pc][:], op=ALU.mult), reads=['kkc' + pcs], writes=['kk2' + pcs])
                          S.op('pe', lambda e: e.matmul(hb_(H[14]), lhsT=blockones, rhs=T["kk2"][pc][:], start=True, stop=True), reads=['kk2' + pcs, 'c32'], writes=[hk(H[14])])
                          yield
                          S.op('act', lambda e: e.activation(out=T["rn"][pc][:], in_=hb_(H[14]), func=AF.Sqrt), reads=[hk(H[14])], writes=['rn' + pcs])
                          S.op('dve', lambda e: e.tensor_scalar(out=T["rn"][pc][:], in0=T["rn"][pc][:], scalar1=1e-12, scalar2=None, op0=ALU.max), reads=['rn' + pcs], writes=['rn' + pcs])
                          S.op('dve', lambda e: e.reciprocal(out=T["rn"][pc][:], in_=T["rn"][pc][:]), reads=['rn' + pcs], writes=['rn' + pcs])
                          yield
                          S.op('pool', lambda e: e.tensor_tensor(out=T["kkn"][pc][:], in0=T["kkc"][pc][:], in1=T["rn"][pc][:], op=ALU.mult), reads=['kkc' + pcs, 'rn' + pcs], writes=['kkn' + pcs])
                          S.op('pool', lambda e: e.tensor_scalar(out=T["aneg"][pc][:], in0=T["kkn"][pc][:], scalar1=-1.0, scalar2=None, op0=ALU.mult), reads=['kkn' + pcs], writes=['aneg' + pcs])
                          for d in range(2):
                              yield
                              S.op('act', lambda e: e.activation(out=T["sig"][pc][:], in_=hb_(H[9 + d]), func=AF.Sigmoid, bias=V(f"w0{j}", d * 8 + c, d * 8 + c + 1), scale=1.0),
                                   reads=[hk(H[9 + d]), 'vecs'], writes=['sig' + pcs])
                              S.op('pool', lambda e: e.tensor_scalar(out=T["lw"][pc][:], in0=T["sig"][pc][:], scalar1=float(-np.exp(-0.5)), scalar2=None, op0=ALU.mult),
                                   reads=['sig' + pcs], writes=['lw' + pcs])
                              S.op('act', lambda e: e.activation(out=T["al"][pc][:], in_=hb_(H[11 + d]), func=AF.Sigmoid, bias=V(f"a0{j}", d * 8 + c, d * 8 + c + 1), scale=1.0),
                                   reads=[hk(H[11 + d]), 'vecs'], writes=['al' + pcs])
                              yield
                              S.op('dve', lambda e: e.tensor_scalar(out=T["tk"][pc][:], in0=T["al"][pc][:], scalar1=V(f"ka{j}", c, c + 1), scalar2=dv_[:, c:c + 1],
                                                                    op0=ALU.mult, op1=ALU.add), reads=['al' + pcs, 'vecs', 'dv'], writes=['tk' + pcs])
                              S.op('pool', lambda e: e.tensor_tensor(out=T["kd"][pc][:], in0=T["k32"][pc][:], in1=T["tk"][pc][:], op=ALU.mult), reads=['k32' + pcs, 'tk' + pcs], writes=['kd' + pcs])
                              S.op('pool', lambda e: e.tensor_tensor(out=T["bb"][pc][:], in0=T["kkn"][pc][:], in1=T["al"][pc][:], op=ALU.mult), reads=['kkn' + pcs, 'al' + pcs], writes=['bb' + pcs])
                              yield
                              S.op('dve', lambda e: e.tensor_tensor_scan(out=T["Lp"][pc][:], data0=cmask[:, :N], data1=T["lw"][pc][:], initial=0.0, op0=ALU.mult, op1=ALU.add),
                                   reads=['lw' + pcs, 'c32'], writes=['Lp' + pcs])
                              if d == 0:
                                  Lc, kLc = T["Lp"][pc], 'Lp' + pcs
                              else:
                                  S.op('pool', lambda e: e.tensor_tensor(out=T["Lc"][pc][:], in0=T["lw"][pc][:], in1=T["Lp"][pc][:], op=ALU.subtract), reads=['lw' + pcs, 'Lp' + pcs], writes=['Lc' + pcs])
                                  S.op('pool', lambda e: e.tensor_tensor(
                                      out=T["Lc"][pc][:].rearrange("p (c t) -> p c t", t=64), in0=T["Lc"][pc][:].rearrange("p (c t) -> p c t", t=64),
                                      in1=T["Lp"][pc][:].rearrange("p (c t) -> p c t", t=64)[:, :, 63:64].to_broadcast([128, N // 64, 64]), op=ALU.add),
                                      reads=['Lc' + pcs, 'Lp' + pcs], writes=['Lc' + pcs])
                                  Lc, kLc = T["Lc"][pc], 'Lc' + pcs
                              yield
                              S.op('act', lambda e: e.activation(out=T["E1"][pc][:], in_=Lc[:], func=AF.Exp), reads=[kLc], writes=['E1' + pcs])
                              S.op('act', lambda e: e.activation(out=T["E2"][pc][:], in_=Lc[:], func=AF.Exp, scale=-1.0), reads=[kLc], writes=['E2' + pcs])
                              S.op('pool', lambda e: e.tensor_tensor(out=T["t2"][pc][:], in0=Lc[:], in1=T["lw"][pc][:], op=ALU.subtract), reads=[kLc, 'lw' + pcs], writes=['t2' + pcs])
                              S.op('act', lambda e: e.activation(out=T["E3"][pc][:], in_=T["t2"][pc][:], func=AF.Exp), reads=['t2' + pcs], writes=['E3' + pcs])
                              yield
                              S.op('pool', lambda e: e.tensor_tensor(out=T["At"][pc][:], in0=T["aneg"][pc][:], in1=T["E3"][pc][:], op=ALU.mult), reads=['aneg' + pcs, 'E3' + pcs], writes=['At' + pcs])
                              S.op('pool', lambda e: e.tensor_tensor(out=T["Rt"][pc][:], in0=T["r32"][pc][:], in1=T["E1"][pc][:], op=ALU.mult), reads=['r32' + pcs, 'E1' + pcs], writes=['Rt' + pcs])
                              S.op('dve', lambda e: e.tensor_tensor(out=T["Bt"][pc][:], in0=T["bb"][pc][:], in1=T["E2"][pc][:], op=ALU.mult), reads=['bb' + pcs, 'E2' + pcs], writes=['Bt' + pcs])
                              S.op('dve', lambda e: e.tensor_tensor(out=T["Kt"][pc][:], in0=T["kd"][pc][:], in1=T["E2"][pc][:], op=ALU.mult), reads=['kd' + pcs, 'E2' + pcs], writes=['Kt' + pcs])
                              wcol = 63 if d == 0 else 0
                              S.op('dve', lambda e: e.tensor_copy(out=wct[pc][:, 0:N // 64], in_=T["E1"][pc][:].rearrange("p (c t) -> p c t", t=64)[:, :, wcol]),
                                   reads=['E1' + pcs], writes=['wct' + pcs])
                              yield
                              S.dma('sp', ATd[d][rows, t0:t0 + N], T["At"][pc][:], reads=['At' + pcs], writes=['ATd'])
                              S.dma('sp', RTd[d][rows, t0:t0 + N], T["Rt"][pc][:], reads=['Rt' + pcs], writes=['RTd'])
                              S.dma('sp', BTd[d][rows, t0:t0 + N], T["Bt"][pc][:], reads=['Bt' + pcs], writes=['BTd'])
                              S.dma('sp', KTd[d][rows, t0:t0 + N], T["Kt"][pc][:], reads=['Kt' + pcs], writes=['KTd'])
                              S.dma('sp', WCd[d][rows, t0 // 64:t0 // 64 + N // 64], wct[pc][:, 0:N // 64], reads=['wct' + pcs], writes=['WCd'])
                              if d == 0:
                                  S.op('pool', lambda e: e.tensor_copy(out=T["kb"][pc][:], in_=T["kd"][pc][:]), reads=['kd' + pcs], writes=['kb' + pcs])
                              else:
                                  S.op('pool', lambda e: e.tensor_tensor(out=T["kb"][pc][:], in0=T["kb"][pc][:], in1=T["kd"][pc][:], op=ALU.add), reads=['kd' + pcs, 'kb' + pcs], writes=['kb' + pcs])
                          yield
                          S.op('pool', lambda e: e.tensor_tensor(out=T["t3"][pc][:], in0=T["r32"][pc][:], in1=T["kb"][pc][:], op=ALU.mult), reads=['r32' + pcs, 'kb' + pcs], writes=['t3' + pcs])
                          S.op('dve', lambda e: e.tensor_scalar(out=T["t3"][pc][:], in0=T["t3"][pc][:], scalar1=dv_[:, 8 + c:9 + c], scalar2=None, op0=ALU.mult),
                               reads=['t3' + pcs, 'dv'], writes=['t3' + pcs])
                          S.op('pe', lambda e: e.matmul(hb_(H[15]), lhsT=blockones, rhs=T["t3"][pc][:], start=True, stop=True), reads=['t3' + pcs, 'c32'], writes=[hk(H[15])])
                          yield
                          S.op('dve', lambda e: e.tensor_tensor(out=T["bon"][pc][:], in0=hb_(H[15]), in1=T["v32"][pc][:], op=ALU.mult), reads=[hk(H[15]), 'v32' + pcs], writes=['bon' + pcs])
                          S.dma('sp', BON[rows, t0:t0 + N], T["bon"][pc][:], reads=['bon' + pcs], writes=['BON'])
                          S.dma('sp', VT[j][rows, t0:t0 + N], T["v32"][pc][:], reads=['v32' + pcs], writes=['VT'])
                          S.op('act', lambda e: e.activation(out=T["vb"][pc][:], in_=T["v32"][pc][:], func=AF.Identity), reads=['v32' + pcs], writes=['vb' + pcs])
                          S.dma('sp', VTb[rows, t0:t0 + N], T["vb"][pc][:], reads=['vb' + pcs], writes=['VTb'])
                          S.dma('sp', GG[rows, t0:t0 + N], T["g32"][pc][:], reads=['g32' + pcs], writes=['GG'])

                      pend = [chunk_gen(c) for c in range(8)]
                      act_g = []
                      while pend or act_g:
                          while pend and len(act_g) < int(os.environ.get("R2WIN", "2")):
                              act_g.append(pend.pop(0))
                          for g_ in list(act_g):
                              try:
                                  next(g_)
                              except StopIteration:
                                  act_g.remove(g_)
            except _Stop:
                pass
            S.mute = False
            S.barrier()
            if os.environ.get('RSTOP') == '2':
                return
            for d in range(2):
                order = ([0, 1, 2, 3] + list(range(4, NCH))) if d == 0 else ([3, 2, 1, 0] + list(range(NCH - 1, 3, -1)))
                mAR = maskAR_f if d == 0 else maskAR_r
                mN = maskN_f if d == 0 else maskN_r
                with contextlib.ExitStack() as st:
                    def stream(hh):
                        P_ = f"s{hh}_"
                        pb = [ps[4 * hh + q] for q in range(4)]

                        def pk(bank, half=None):
                            return [('sps', hh, bank)]
                        BD = {n: sb(P_ + n, [128, 4, 128], BF16, st) for n in ["bdA", "T2", "T3", "T4", "tbB", "tbK", "tbV", "T8", "bdMak", "bdU", "bdST"]}
                        for n, t_ in BD.items():
                            S.op('pool', lambda e: e.memset(t_[:], 0.0), writes=[P_ + n])
                        AR = [sb(P_ + f"AR{b}", [128, 4, 2, 64], BF16, st) for b in range(2)]
                        Bi = [sb(P_ + f"Bi{b}", [128, 4, 64], BF16, st) for b in range(2)]
                        Ki = [sb(P_ + f"Ki{b}", [128, 4, 64], BF16, st) for b in range(2)]
                        Vi = [sb(P_ + f"Vi{b}", [128, 4, 64], BF16, st) for b in range(2)]
                        WCall = sb(P_ + "WCall", [128, 4, NCH], F32, st)
                        S.dma('sp', WCall[:], WCd[d][hh * 512:hh * 512 + 512, :].rearrange("(c p) n -> p c n", p=128), writes=[P_ + "WCall"])
                        ARm = sb(P_ + "ARm", [128, 4, 128], BF16, st)
                        AKm = sb(P_ + "AKm", [128, 4, 128], BF16, st)
                        Ns = sb(P_ + "Ns", [128, 4, 64], BF16, st)
                        Vs = sb(P_ + "Vs", [128, 4, 64], BF16, st)
                        PG = [sb(P_ + f"PG{b}", [128, 4, 128], BF16, st) for b in range(2)]
                        Pst = [sb(P_ + f"Pst{b}", [128, 4, 64], BF16, st) for b in range(2)]
                        Xs = sb(P_ + "Xs", [128, 4, 64], BF16, st); Us = sb(P_ + "Us", [128, 4, 64], BF16, st)
                        Yb = sb(P_ + "Yb", [128, 4, 64], F32, st); STs = sb(P_ + "STs", [128, 4, 64], F32, st)
                        tmpS = sb(P_ + "tmpS", [128, 4, 64], F32, st)
                        STb = sb(P_ + "STb", [128, 4, 64], BF16, st)
                        S.op('pool', lambda e: e.memset(STb[:], 0.0), writes=[P_ + "STb"])
                        S.op('pool', lambda e: e.memset(STs[:], 0.0), writes=[P_ + "STs"])
                        r0 = hh * 512

                        def diag(eng, dst, kdst, src, ksrc, cols=64):
                            for half in range(2):
                                pslice = slice(half * 64, half * 64 + 64)
                                if eng == 'act':
                                    S.op('act', lambda e: e.activation(out=dst[pslice, :, half * 64:half * 64 + 64], in_=src[pslice], func=AF.Identity),
                                         reads=ksrc, writes=[kdst])
                                else:
                                    S.op(eng, lambda e: e.tensor_copy(out=dst[pslice, :, half * 64:half * 64 + 64], in_=src[pslice]),
                                         reads=ksrc, writes=[kdst])

                        def load(n):
                            g = order[n]
                            b = n % 2
                            cols = slice(g * 64, g * 64 + 64)
                            kin = P_ + f"in{b}"
                            for (dst, srcd) in [(AR[b][:, :, 0, :], ATd[d]), (AR[b][:, :, 1, :], RTd[d]), (Bi[b][:], BTd[d]), (Ki[b][:], KTd[d]), (Vi[b][:], VTb)]:
                                S.dma('sp', dst, srcd[r0:r0 + 512, cols].rearrange("(c p) n -> p c n", p=128), writes=[kin])

                        load(0)
                        for n in range(NCH):
                            g = order[n]
                            b = n % 2
                            kin = P_ + f"in{b}"
                            if n + 1 < NCH:
                                load(n + 1)
                            diag('dve', BD["bdA"], P_ + "bdA", AR[b][:, :, 0, :], [kin])
                            diag('dve', BD["T2"], P_ + "T2", Bi[b], [kin])
                            diag('act', BD["T3"], P_ + "T3", Ki[b], [kin])
                            diag('act', BD["T4"], P_ + "T4", Vi[b], [kin])
                            ARf = AR[b][:].rearrange("p c a t -> p c (a t)")
                            for hp in range(4):
                                S.op('pe', lambda e: e.matmul(pb[0][:, hp * 128:(hp + 1) * 128], lhsT=BD["T2"][:, hp, :], rhs=ARf[:, hp, :], start=True, stop=True),
                                     reads=[P_ + "T2", kin], writes=pk(0), inc=(hp == 3))
                            for hp in range(4):
                                S.op('pe', lambda e: e.matmul(pb[1][:, hp * 128:(hp + 1) * 128], lhsT=BD["T3"][:, hp, :], rhs=ARf[:, hp, :], start=True, stop=True),
                                     reads=[P_ + "T3", kin], writes=pk(1), inc=(hp == 3))
                            for hp in range(4):
                                S.op('pe', lambda e: e.matmul(pb[2][:, hp * 64:(hp + 1) * 64], lhsT=BD["bdA"][:, hp, :], rhs=Bi[b][:, hp, :], start=True, stop=True),
                                     reads=[P_ + "bdA", kin], writes=pk(2, 0), inc=(hp == 3))
                            yield
                            S.op('dve', lambda e: e.tensor_tensor(out=ARm[:], in0=pb[0][:].rearrange("p (c t) -> p c t", t=128),
                                                                  in1=mAR.unsqueeze(1).to_broadcast([128, 4, 128]), op=ALU.mult),
                                 reads=pk(0) + ['c32'], writes=[P_ + "ARm"])
                            S.op('dve', lambda e: e.tensor_tensor(out=AKm[:], in0=pb[1][:].rearrange("p (c t) -> p c t", t=128),
                                                                  in1=mAR.unsqueeze(1).to_broadcast([128, 4, 128]), op=ALU.mult),
                                 reads=pk(1) + ['c32'], writes=[P_ + "AKm"])
                            S.op('dve', lambda e: e.tensor_tensor(out=Pst[0][:], in0=pb[2][:, 0:256].rearrange("p (c t) -> p c t", t=64),
                                                                  in1=mN.unsqueeze(1).to_broadcast([128, 4, 64]), op=ALU.mult),
                                 reads=pk(2, 0) + ['c32'], writes=[P_ + "Pst0"])
                            for hp in range(4):
                                S.op('pe', lambda e: e.matmul(pb[0][:, hp * 128:(hp + 1) * 128], lhsT=BD["T2"][:, hp, :], rhs=ident_bf[:], start=True, stop=True),
                                     reads=[P_ + "T2", 'ident_bf'], writes=pk(0), inc=(hp == 3))
                            for hp in range(4):
                                S.op('pe', lambda e: e.matmul(pb[1][:, hp * 128:(hp + 1) * 128], lhsT=BD["T3"][:, hp, :], rhs=ident_bf[:], start=True, stop=True),
                                     reads=[P_ + "T3", 'ident_bf'], writes=pk(1), inc=(hp == 3))
                            yield
                            S.op('act', lambda e: e.activation(out=BD["tbB"][:], in_=pb[0][:].rearrange("p (c t) -> p c t", t=128), func=AF.Identity),
                                 reads=pk(0), writes=[P_ + "tbB"])
                            S.op('act', lambda e: e.activation(out=BD["tbK"][:], in_=pb[1][:].rearrange("p (c t) -> p c t", t=128), func=AF.Identity),
                                 reads=pk(1), writes=[P_ + "tbK"])
                            for hp in range(4):
                                S.op('pe', lambda e: e.matmul(pb[0][:, hp * 128:(hp + 1) * 128], lhsT=BD["T4"][:, hp, :], rhs=ident_bf[:], start=True, stop=True),
                                     reads=[P_ + "T4", 'ident_bf'], writes=pk(0), inc=(hp == 3))
                            S.op('pool', lambda e: e.tensor_copy(out=PG[0][:, :, 0:64], in_=ARm[:, :, 0:64]), reads=[P_ + "ARm"], writes=[P_ + "PG0"])
                            S.op('pool', lambda e: e.tensor_copy(out=PG[0][:, :, 64:128], in_=SI.unsqueeze(1).to_broadcast([128, 4, 64])),
                                 reads=['c32'], writes=[P_ + "PG0"])
                            diag('pool', BD["bdMak"], P_ + "bdMak", AKm[:, :, 0:64], [P_ + "AKm"])
                            yield
                            S.op('act', lambda e: e.activation(out=BD["tbV"][:], in_=pb[0][:].rearrange("p (c t) -> p c t", t=128), func=AF.Identity),
                                 reads=pk(0), writes=[P_ + "tbV"])
                            for half in range(2):
                                pslice = slice(half * 64, half * 64 + 64)
                                S.op('pool', lambda e: e.tensor_copy(out=Vs[pslice], in_=BD["tbV"][pslice, :, half * 64:half * 64 + 64]),
                                     reads=[P_ + "tbV"], writes=[P_ + "Vs"])
                            diag('pool', BD["T2"], P_ + "T2", Pst[0], [P_ + "Pst0"])
                            diag('pool', BD["T3"], P_ + "T3", PG[0][:, :, 0:64], [P_ + "PG0"])
                            sets = [("T2", "T3"), ("T4", "T8")]
                            for lv in range(6):
                                cur, nxt = lv % 2, (lv + 1) % 2
                                bP, bPT = sets[cur]
                                nP, nPT = sets[nxt]
                                kPG, kPGn = P_ + f"PG{cur}", P_ + f"PG{nxt}"
                                if lv < 5:
                                    for hp in range(4):
                                        S.op('pe', lambda e: e.matmul(pb[1][:, hp * 64:(hp + 1) * 64], lhsT=BD[bPT][:, hp, :], rhs=Pst[cur][:, hp, :], start=True, stop=True),
                                             reads=[P_ + bPT, P_ + f"Pst{cur}"], writes=pk(1, 0), inc=(hp == 3))
                                    for hp in range(4):
                                        S.op('pe', lambda e: e.matmul(pb[0][:, hp * 128:(hp + 1) * 128], lhsT=BD[bP][:, hp, :], rhs=PG[cur][:, hp, :], start=True, stop=True),
                                             reads=[P_ + bP, kPG], writes=pk(0), inc=(hp == 3))
                                else:
                                    for hp in range(4):
                                        S.op('pe', lambda e: e.matmul(pb[0][:, hp * 128 + 64:(hp + 1) * 128], lhsT=BD[bP][:, hp, :], rhs=PG[cur][:, hp, 64:128], start=True, stop=True),
                                             reads=[P_ + bP, kPG], writes=pk(0), inc=(hp == 3))
                                yield
                                psv = pb[0][:].rearrange("p (c t) -> p c t", t=128)
                                S.op('dve', lambda e: e.tensor_tensor(out=PG[nxt][:, :, 64:128], in0=PG[cur][:, :, 64:128], in1=psv[:, :, 64:128], op=ALU.add),
                                     reads=pk(0) + [kPG], writes=[kPGn])
                                if lv < 5:
                                    S.op('act', lambda e: e.activation(out=PG[nxt][:, :, 0:64], in_=psv[:, :, 0:64], func=AF.Identity), reads=pk(0), writes=[kPGn])
                                    S.op('dve', lambda e: e.tensor_copy(out=Pst[nxt][:], in_=pb[1][:, 0:256].rearrange("p (c t) -> p c t", t=64)),
                                         reads=pk(1, 0), writes=[P_ + f"Pst{nxt}"])
                                    diag('act', BD[nP], P_ + nP, Pst[nxt], [P_ + f"Pst{nxt}"])
                                    if lv < 4:
                                        diag('pool', BD[nPT], P_ + nPT, PG[nxt][:, :, 0:64], [kPGn])
                            diag('pool', BD["T8"], P_ + "T8", PG[0][:, :, 64:128], [P_ + "PG0"])
                            for hp in range(4):
                                S.op('pe', lambda e: e.matmul(pb[2][:, 256 + hp * 64:256 + (hp + 1) * 64], lhsT=BD["bdA"][:, hp, :], rhs=STb[:, hp, :], start=True, stop=False),
                                     reads=[P_ + "bdA", P_ + "STb"], writes=pk(2, 1), inc=False)
                                S.op('pe', lambda e: e.matmul(pb[2][:, 256 + hp * 64:256 + (hp + 1) * 64], lhsT=BD["bdMak"][:, hp, :], rhs=Vs[:, hp, :], start=False, stop=True),
                                     reads=[P_ + "bdMak", P_ + "Vs"], writes=pk(2, 1), inc=(hp == 3))
                            yield
                            S.op('act', lambda e: e.activation(out=Xs[:], in_=pb[2][:, 256:512].rearrange("p (c t) -> p c t", t=64), func=AF.Identity),
                                 reads=pk(2, 1), writes=[P_ + "Xs"])
                            for hp in range(4):
                                S.op('pe', lambda e: e.matmul(pb[3][:, hp * 64:(hp + 1) * 64], lhsT=BD["T8"][:, hp, :], rhs=Xs[:, hp, :], start=True, stop=True),
                                     reads=[P_ + "T8", P_ + "Xs"], writes=pk(3, 0), inc=(hp == 3))
                            yield
                            S.op('dve', lambda e: e.tensor_copy(out=Us[:], in_=pb[3][:, 0:256].rearrange("p (c t) -> p c t", t=64)), reads=pk(3, 0), writes=[P_ + "Us"])
                            diag('act', BD["bdU"], P_ + "bdU", pb[3][:, 0:256].rearrange("p (c t) -> p c t", t=64), pk(3, 0))
                            for hp in range(4):
                                S.op('pe', lambda e: e.matmul(pb[3][:, 256 + hp * 64:256 + (hp + 1) * 64], lhsT=BD["bdST"][:, hp, :], rhs=AR[b][:, hp, 1, :], start=True, stop=False),
                                     reads=[P_ + "bdST", kin], writes=pk(3, 1), inc=False)
                                S.op('pe', lambda e: e.matmul(pb[3][:, 256 + hp * 64:256 + (hp + 1) * 64], lhsT=BD["bdU"][:, hp, :], rhs=ARm[:, hp, 64:128], start=False, stop=False),
                                     reads=[P_ + "bdU", P_ + "ARm"], writes=pk(3, 1), inc=False)
                                S.op('pe', lambda e: e.matmul(pb[3][:, 256 + hp * 64:256 + (hp + 1) * 64], lhsT=BD["tbV"][:, hp, :], rhs=AKm[:, hp, 64:128], start=False, stop=True),
                                     reads=[P_ + "tbV", P_ + "AKm"], writes=pk(3, 1), inc=(hp == 3))
                            for hp in range(4):
                                S.op('pe', lambda e: e.matmul(pb[1][:, 256 + hp * 64:256 + (hp + 1) * 64], lhsT=BD["tbB"][:, hp, :], rhs=Us[:, hp, :], start=True, stop=False),
                                     reads=[P_ + "tbB", P_ + "Us"], writes=pk(1, 1), inc=False)
                                S.op('pe', lambda e: e.matmul(pb[1][:, 256 + hp * 64:256 + (hp + 1) * 64], lhsT=BD["tbK"][:, hp, :], rhs=Vs[:, hp, :], start=False, stop=True),
                                     reads=[P_ + "tbK", P_ + "Vs"], writes=pk(1, 1), inc=(hp == 3))
                            yield
                            S.op('act', lambda e: e.activation(out=Yb[:], in_=pb[3][:, 256:512].rearrange("p (c t) -> p c t", t=64), func=AF.Identity),
                                 reads=pk(3, 1), writes=[P_ + "Yb"])
                            S.dma('sp', YD[d][r0:r0 + 512, g * 64:g * 64 + 64].rearrange("(c p) n -> p c n", p=128), Yb[:], reads=[P_ + "Yb"], writes=['YD'])
                            S.op('dve', lambda e: e.tensor_tensor(out=tmpS[:], in0=pb[1][:, 256:512].rearrange("p (c t) -> p c t", t=64), in1=STs[:], op=ALU.add),
                                 reads=pk(1, 1) + [P_ + "STs"], writes=[P_ + "tmpS"])
                            S.op('dve', lambda e: e.tensor_tensor(out=STs[:], in0=tmpS[:], in1=WCall[:, :, g:g + 1].to_broadcast([128, 4, 64]), op=ALU.mult),
                                 reads=[P_ + "tmpS", P_ + "WCall"], writes=[P_ + "STs"])
                            diag('pool', BD["bdST"], P_ + "bdST", STs, [P_ + "STs"])
                            S.op('act', lambda e: e.activation(out=STb[:], in_=STs[:], func=AF.Identity), reads=[P_ + "STs"], writes=[P_ + "STb"])
                            yield

                    gens = [stream(0), stream(1)]
                    alive = [True, True]
                    while any(alive):
                        for q in range(2):
                            if alive[q]:
                                try:
                                    next(gens[q])
                                except StopIteration:
                                    alive[q] = False
                S.barrier()
            if os.environ.get('RSTOP') == '3':
                return
            with contextlib.ExitStack() as st:
                wo = sb("r_wo", [128, 8, 1024], BF16, st)
                S.dma('pool', wo[:], rwkv_wo[j].rearrange("(k p) n -> p k n", p=128), writes=['r_wo'])
                y0 = sb("r_y0", [128, 8, 512], F32, st); y1 = sb("r_y1", [128, 8, 512], F32, st)
                bo = sb("r_bo", [128, 8, 512], F32, st); gg = sb("r_gg", [128, 8, 512], F32, st)
                xt = sb("r_xt", [128, 8, 512], F32, st)
                ob = sb("r_ob", [128, 8, 512], BF16, st)
                yc = sb("r_yc", [128, 512], F32, st); y2 = sb("r_y2", [128, 512], F32, st); sd = sb("r_sd", [128, 512], F32, st)
                tiles = TILES[1:] if last else TILES
                for ti, (t0, N, mc) in enumerate(tiles):
                    for (dst, srcd, kk_) in [(y0, YD[0], 'y0'), (y1, YD[1], 'y1'), (bo, BON, 'bo'), (gg, GG, 'gg'), (xt, XS, 'r_xt')]:
                        S.dma('sp', dst[:, :, :N], srcd[:, t0:t0 + N].rearrange("(k p) n -> p k n", p=128), writes=[kk_])
                    for c in range(8):
                        pa, pv = c % 2, 2 + c % 2
                        S.op('dve', lambda e: e.tensor_tensor(out=y0[:, c, :N], in0=y0[:, c, :N], in1=y1[:, c, :N], op=ALU.add), reads=['y0', 'y1'], writes=['y0'])
                        S.op('pe', lambda e: e.matmul(ps[pa][:, :N], lhsT=blockones, rhs=y0[:, c, :N], start=True, stop=True), reads=['y0', 'c32'], writes=[('ps', pa)])
                        S.op('dve', lambda e: e.scalar_tensor_tensor(out=yc[:, :N], in0=ps[pa][:, :N], scalar=-1.0 / 64, in1=y0[:, c, :N], op0=ALU.mult, op1=ALU.add),
                             reads=[('ps', pa), 'y0'], writes=['yc'])
                        S.op('pool', lambda e: e.tensor_tensor(out=y2[:, :N], in0=yc[:, :N], in1=yc[:, :N], op=ALU.mult), reads=['yc'], writes=['y2'])
                        S.op('pe', lambda e: e.matmul(ps[pv][:, :N], lhsT=blockones, rhs=y2[:, :N], start=True, stop=True), reads=['y2', 'c32'], writes=[('ps', pv)])
                        S.op('act', lambda e: e.activation(out=sd[:, :N], in_=ps[pv][:, :N], func=AF.Sqrt, bias=epsT[:, 5:6], scale=1.0 / 64),
                             reads=[('ps', pv), 'epsT'], writes=['sd'])
                        S.op('dve', lambda e: e.reciprocal(out=sd[:, :N], in_=sd[:, :N]), reads=['sd'], writes=['sd'])
                        S.op('pool', lambda e: e.tensor_tensor(out=yc[:, :N], in0=yc[:, :N], in1=sd[:, :N], op=ALU.mult), reads=['yc', 'sd'], writes=['yc'])
                        S.op('dve', lambda e: e.tensor_scalar(out=yc[:, :N], in0=yc[:, :N], scalar1=V(f"lnw{j}", c, c + 1), scalar2=V(f"lnb{j}", c, c + 1),
                                                              op0=ALU.mult, op1=ALU.add), reads=['yc', 'vecs'], writes=['yc'])
                        S.op('pool', lambda e: e.tensor_tensor(out=yc[:, :N], in0=yc[:, :N], in1=bo[:, c, :N], op=ALU.add), reads=['yc', 'bo'], writes=['yc'])
                        S.op('pool', lambda e: e.tensor_tensor(out=ob[:, c, :N], in0=yc[:, :N], in1=gg[:, c, :N], op=ALU.mult), reads=['yc', 'gg'], writes=['ob'])
                    for oc in range(8):
                        pb_ = 4 + oc % 4
                        for kc in range(8):
                            S.op('pe', lambda e: e.matmul(ps[pb_][:, :N], lhsT=wo[:, kc, oc * 128:(oc + 1) * 128], rhs=ob[:, kc, :N], start=(kc == 0), stop=(kc == 7)),
                                 reads=['r_wo', 'ob'], writes=[('ps', pb_)], inc=(kc == 7))
                        S.op('dve', lambda e: e.scalar_tensor_tensor(out=xt[:, oc, :N], in0=ps[pb_][:, :N], scalar=modt[:, i, 16 + oc, mc:mc + 1],
                                                                     in1=xt[:, oc, :N], op0=ALU.mult, op1=ALU.add),
                             reads=[('ps', pb_), 'r_xt', 'modt'], writes=['r_xt'])
                    S.dma('sp', XS[:, t0:t0 + N].rearrange("(k p) n -> p k n", p=128), xt[:, :, :N], reads=['r_xt'], writes=['XS'])
            S.barrier()

        def phase_ffn(i, last):
            moe = (i % 2 == 1)
            k = i // 2
            E = NE if moe else 1
            F = DFE if moe else DFF
            nchunk = F // 128
            blocks = [(c0, min(4, nchunk - c0)) for c0 in range(0, nchunk, 4)]
            tiles = TILES[1:] if last else TILES
            groups = [tiles[0:len(tiles) - 6], tiles[-6:-3], tiles[-3:]]
            for gi, grp in enumerate(groups):
                Sg = sum(t[1] for t in grp)
                offs = [sum(t[1] for t in grp[:a]) for a in range(len(grp))]
                with contextlib.ExitStack() as st:
                    hb = sb("f_hb", [128, 8, 1536], BF16, st)
                    yacc = sb("f_yacc", [128, 8, 1536], F32, st)
                    GT = sb("f_GT", [8, 1536], F32, st)
                    gbc = sb("f_gbc", [128, 1536], F32, st)
                    with contextlib.ExitStack() as st2:
                        xt = [sb(f"f_xt{b}", [128, 8, 512], F32, st2) for b in range(2)]
                        sq = sb("f_sq", [128, 8, 512], F32, st2)
                        rs = sb("f_rs", [128, 512], F32, st2)
                        if moe:
                            rt = sb("f_rt", [128, 8, 8], F32, st2)
                            S.dma('sp', rt[:], moe_router[k].rearrange("(k p) n -> p k n", p=128), writes=['rt'])
                            lg = sb("f_lg", [128, 8], F32, st2); m8 = sb("f_m8", [128, 8], F32, st2)
                            sel = sb("f_sel", [128, 8], F32, st2); ex = sb("f_ex", [128, 8], F32, st2)
                            sm = sb("f_sm", [128, 4], F32, st2); G = sb("f_G", [128, 4, 8], F32, st2)
                        for ti, (t0, N, mc) in enumerate(grp):
                            b = ti % 2
                            kx = ('fx', b)
                            o = offs[ti]
                            S.dma('sp', xt[b][:, :, :N], XS[:, t0:t0 + N].rearrange("(k p) n -> p k n", p=128), writes=[kx])
                            ksq = normmod(xt[b][:, :, :N], N, A2[:, i, :, mc], None, None, sq, rs, 7, kx, 'f')
                            S.op('dve', lambda e: e.tensor_tensor(out=sq[:, :, :N], in0=sq[:, :, :N],
                                                                  in1=modt[:, i, 24:32, mc].unsqueeze(2).to_broadcast([128, 8, N]), op=ALU.add),
                                 reads=[ksq, 'modt'], writes=[ksq])
                            S.op('act', lambda e: e.activation(out=hb[:, :, o:o + N], in_=sq[:, :, :N], func=AF.Identity),
                                 reads=[ksq], writes=['hb'])
                            if moe:
                                for blk in range(N // 128):
                                    for kc in range(8):
                                        S.op('pe', lambda e: e.matmul(ps[6][:, 0:8], lhsT=sq[:, kc, blk * 128:(blk + 1) * 128], rhs=rt[:, kc, :],
                                                                      start=(kc == 0), stop=(kc == 7)), reads=[ksq, 'rt'], writes=[('ps', 6)], inc=(kc == 7))
                                    S.op('dve', lambda e: e.tensor_copy(out=lg[:], in_=ps[6][:, 0:8]), reads=[('ps', 6)], writes=['lg'])
                                    S.op('dve', lambda e: e.max(out=m8[:], in_=lg[:]), reads=['lg'], writes=['m8'])
                                    S.op('dve', lambda e: e.tensor_scalar(out=sel[:], in0=lg[:], scalar1=m8[:, 1:2], scalar2=None, op0=ALU.is_ge),
                                         reads=['lg', 'm8'], writes=['sel'])
                                    S.op('dve', lambda e: e.tensor_scalar(out=sm[:, 0:1], in0=m8[:, 0:1], scalar1=-1.0, scalar2=None, op0=ALU.mult),
                                         reads=['m8'], writes=['sm'])
                                    S.op('act', lambda e: e.activation(out=ex[:], in_=lg[:], func=AF.Exp, bias=sm[:, 0:1], scale=1.0),
                                         reads=['lg', 'sm'], writes=['ex'])
                                    S.op('dve', lambda e: e.tensor_tensor(out=ex[:], in0=ex[:], in1=sel[:], op=ALU.mult), reads=['ex', 'sel'], writes=['ex'])
                                    S.op('dve', lambda e: e.tensor_reduce(out=sm[:, 1:2], in_=ex[:], axis=AX.X, op=ALU.add), reads=['ex'], writes=['sm'])
                                    S.op('dve', lambda e: e.reciprocal(out=sm[:, 2:3], in_=sm[:, 1:2]), reads=['sm'], writes=['sm'])
                                    S.op('dve', lambda e: e.tensor_scalar(out=G[:, blk, :], in0=ex[:], scalar1=sm[:, 2:3], scalar2=None, op0=ALU.mult),
                                         reads=['ex', 'sm'], writes=['G'])
                                    S.op('pe', lambda e: e.transpose(out=ps[5][0:8, blk * 128:(blk + 1) * 128], in_=G[:, blk, :], identity=ident),
                                         reads=['G', 'c32'], writes=[('ps', 5)])
                                S.op('act', lambda e: e.activation(out=GT[:, o:o + N], in_=ps[5][0:8, :N], func=AF.Identity),
                                     reads=[('ps', 5)], writes=['GT'])
                    S.barrier()
                    with contextlib.ExitStack() as st2:
                        w1b = [sb(f"f_w1{b}", [128, 8, 512], BF16, st2) for b in range(2)]
                        w3b = [sb(f"f_w3{b}", [128, 8, 512], BF16, st2) for b in range(2)]
                        w2b = [sb(f"f_w2{b}", [128, 4, 1024], BF16, st2) for b in range(2)]
                        gt = [sb(f"f_g{b}", [128, 4, 512], BF16, st2) for b in range(2)]
                        s1 = [sb(f"f_s1{b}", [128, 512], F32, st2) for b in range(2)]
                        s1g = [sb(f"f_s1g{b}", [128, 512], F32, st2) for b in range(2)]
                        nw = 0
                        ng = 0
                        nhc = 0
                        ny = 0
                        first = True
                        items = []
                        first = True
                        for ex_i in range(E):
                            for bi, (c0, nh) in enumerate(blocks):
                                wb = nw % 2
                                nw += 1
                                for ti, (t0, N, mc) in enumerate(grp):
                                    items.append(dict(ex=ex_i, bi=bi, c0=c0, nh=nh, wb=wb, ti=ti, o=offs[ti], N=N, gb=ng % 2, first=first))
                                    ng += 1
                                first = False

                        def emit_gate(ex_i):
                            for ti, (t0, N, mc) in enumerate(grp):
                                o = offs[ti]
                                S.op('pe', lambda e: e.matmul(ps[6][:, :N], lhsT=selE[:, ex_i * 128:(ex_i + 1) * 128], rhs=GT[:, o:o + N],
                                                              start=True, stop=True), reads=['GT', 'c32'], writes=[('ps', 6)])
                                S.op('act', lambda e: e.activation(out=gbc[:, o:o + N], in_=ps[6][:, :N], func=AF.Identity),
                                     reads=[('ps', 6)], writes=['gbc'])

                        def emit_wload(it):
                            wb, c0, nh, ex_i = it['wb'], it['c0'], it['nh'], it['ex']
                            kw = ('fw', wb)
                            if moe:
                                W1, W3, W2 = moe_w1[k, ex_i], moe_w3[k, ex_i], moe_w2[k, ex_i]
                            else:
                                W1, W3, W2 = ffn_w1[k], ffn_w3[k], ffn_w2[k]
                            S.dma('pool', w1b[wb][:, :, :nh * 128], W1[:, c0 * 128:(c0 + nh) * 128].rearrange("(k p) n -> p k n", p=128), writes=[kw])
                            S.dma('pool', w3b[wb][:, :, :nh * 128], W3[:, c0 * 128:(c0 + nh) * 128].rearrange("(k p) n -> p k n", p=128), writes=[kw])
                            S.dma('pool', w2b[wb][:, :nh, :], W2[c0 * 128:(c0 + nh) * 128, :].rearrange("(k p) n -> p k n", p=128), writes=[kw])

                        def emit_P(it):
                            nonlocal_nhc = cnts
                            wb, nh, o, N, gb = it['wb'], it['nh'], it['o'], it['N'], it['gb']
                            kw = ('fw', wb)
                            for hc in range(nh):
                                pa = (cnts[0] % 2) * 2
                                sbi = cnts[0] % 2
                                cnts[0] += 1
                                for kc in range(8):
                                    S.op('pe', lambda e: e.matmul(ps[pa][:, :N], lhsT=w1b[wb][:, kc, hc * 128:(hc + 1) * 128], rhs=hb[:, kc, o:o + N],
                                                                  start=(kc == 0), stop=(kc == 7)), reads=[kw, 'hb'], writes=[('ps', pa)], inc=(kc == 7))
                                for kc in range(8):
                                    S.op('pe', lambda e: e.matmul(ps[pa + 1][:, :N], lhsT=w3b[wb][:, kc, hc * 128:(hc + 1) * 128], rhs=hb[:, kc, o:o + N],
                                                                  start=(kc == 0), stop=(kc == 7)), reads=[kw, 'hb'], writes=[('ps', pa + 1)], inc=(kc == 7))
                                S.op('act', lambda e: e.activation(out=s1[sbi][:, :N], in_=ps[pa][:, :N], func=AF.Silu),
                                     reads=[('ps', pa)], writes=[('s1', sbi)])
                                src, ksrc = s1[sbi], ('s1', sbi)
                                if moe:
                                    S.op('dve', lambda e: e.tensor_tensor(out=s1g[sbi][:, :N], in0=s1[sbi][:, :N], in1=gbc[:, o:o + N], op=ALU.mult),
                                         reads=[('s1', sbi), 'gbc'], writes=[('s1g', sbi)])
                                    src, ksrc = s1g[sbi], ('s1g', sbi)
                                S.op('dve', lambda e: e.tensor_tensor(out=gt[gb][:, hc, :N], in0=src[:, :N], in1=ps[pa + 1][:, :N], op=ALU.mult),
                                     reads=[ksrc, ('ps', pa + 1)], writes=[('g', gb)])

                        def emit_W2(it):
                            wb, nh, o, N, gb = it['wb'], it['nh'], it['o'], it['N'], it['gb']
                            kw = ('fw', wb)
                            for oc in range(8):
                                py = 4 + cnts[1] % 2
                                cnts[1] += 1
                                for hc in range(nh):
                                    S.op('pe', lambda e: e.matmul(ps[py][:, :N], lhsT=w2b[wb][:, hc, oc * 128:(oc + 1) * 128], rhs=gt[gb][:, hc, :N],
                                                                  start=(hc == 0), stop=(hc == nh - 1)), reads=[kw, ('g', gb)], writes=[('ps', py)],
                                         inc=(hc == nh - 1))
                                ky = ('y', o, oc)
                                if it['first']:
                                    S.op('act', lambda e: e.activation(out=yacc[:, oc, o:o + N], in_=ps[py][:, :N], func=AF.Identity),
                                         reads=[('ps', py)], writes=[ky])
                                else:
                                    S.op('dve', lambda e: e.tensor_tensor(out=yacc[:, oc, o:o + N], in0=yacc[:, oc, o:o + N], in1=ps[py][:, :N], op=ALU.add),
                                         reads=[('ps', py), ky], writes=[ky])

                        cnts = [0, 0]
                        prev = None
                        for it in items:
                            if moe and it['bi'] == 0 and it['ti'] == 0:
                                emit_gate(it['ex'])
                            if it['ti'] == 0:
                                emit_wload(it)
                            emit_P(it)
                            if prev is not None:
                                emit_W2(prev)
                            prev = it
                        emit_W2(prev)
                    S.barrier()
                    with contextlib.ExitStack() as st2:
                        xt = [sb(f"f_cx{b}", [128, 8, 512], F32, st2) for b in range(2)]
                        for ti, (t0, N, mc) in enumerate(grp):
                            b = ti % 2
                            o = offs[ti]
                            S.dma('sp', xt[b][:, :, :N], XS[:, t0:t0 + N].rearrange("(k p) n -> p k n", p=128), writes=[('fcx', b)])
                            for oc in range(8):
                                S.op('dve', lambda e: e.scalar_tensor_tensor(
                                    out=xt[b][:, oc, :N], in0=yacc[:, oc, o:o + N], scalar=modt[:, i, 40 + oc, mc:mc + 1],
                                    in1=xt[b][:, oc, :N], op0=ALU.mult, op1=ALU.add), reads=[('fcx', b), 'modt'], writes=[('fcx', b)])
                            if last:
                                S.dma('sp', out_d[:, t0 - CTX:t0 - CTX + N].rearrange("(k p) n -> p k n", p=128), xt[b][:, :, :N],
                                      reads=[('fcx', b)], writes=['out'])
                            else:
                                S.dma('sp', XS[:, t0:t0 + N].rearrange("(k p) n -> p k n", p=128), xt[b][:, :, :N],
                                      reads=[('fcx', b)], writes=['XS'])
                    S.barrier()

        if dbg:
            dbg_d = nc.dram_tensor("dbg", [D, NT], F32, kind="ExternalOutput").ap()
        phase_mod()
        for i in range(depth):
            last = (i == depth - 1)
            if i == 0 and os.environ.get('SKIP0'):
                S.dma('sp', XS, xs_in, writes=['XS'])
                S.barrier()
                continue
            if i % 2 == 0:
                phase_mla(i, xs_in if i == 0 else XS)
            else:
                phase_rwkv(i, last and dbg != 'r')
            if not (dbg == 'r' and i == depth - 1):
                phase_ffn(i, last)
        if dbg:
            S.dma('sp', dbg_d, XS, writes=['dbg'])
        S.barrier()
    return nc, S


def host_consts():
    c = np.zeros((128, C32W), np.float32)
    c[:, 0:128] = 1.0
    c[:, 128:256] = np.eye(128, dtype=np.float32)
    P = np.zeros((64, 64), np.float32)
    for base in (0, 32):
        for f in range(16):
            P[base + 16 + f, base + f] = -1.0
            P[base + f, base + 16 + f] = 1.0
    c[0:64, 256:320] = P
    for e in range(8):
        c[e, 384 + e * 128:384 + (e + 1) * 128] = 1.0
    o_ = 384 + 1024
    p = np.arange(128)
    c[:, o_:o_ + 128] = (p[:, None] // 64 == p[None, :] // 64)
    tt = np.arange(64)
    c[:, o_ + 128:o_ + 192] = (p[:, None] % 64 == tt[None, :])
    sidx = (p % 64)[:, None]
    c[:, o_ + 192:o_ + 256] = (sidx < tt[None, :])
    c[:, o_ + 256:o_ + 320] = (sidx <= tt[None, :])
    c[:, o_ + 320:o_ + 384] = (tt[None, :] < sidx)
    c[:, o_ + 384:o_ + 448] = (sidx > tt[None, :])
    c[:, o_ + 448:o_ + 512] = (sidx >= tt[None, :])
    c[:, o_ + 512:o_ + 576] = (tt[None, :] > sidx)
    cm = np.ones(512, np.float32); cm[0::64] = 0.0
    c[:, o_ + 576:o_ + 1088] = cm[None, :]
    rows = TL // 64
    row_ids = np.repeat(np.arange(rows, dtype=np.float32), 64)
    col_ids = np.tile(np.arange(64, dtype=np.float32), rows)
    inv_freq = (1.0 / (np.float32(10000.0) ** (np.arange(16, dtype=np.float32) / np.float32(16)))).astype(np.float32)
    ang_r = (row_ids[:, None] * inv_freq[None, :]).astype(np.float32)
    ang_c = (col_ids[:, None] * inv_freq[None, :]).astype(np.float32)
    cosT = np.zeros((64, TL), np.float32)
    sinT = np.zeros((64, TL), np.float32)
    for base, ang in ((0, ang_r), (32, ang_c)):
        for half in (0, 16):
            cosT[base + half:base + half + 16] = np.cos(ang).T
            sinT[base + half:base + half + 16] = np.sin(ang).T
    return c, cosT, sinT


def host_vecs(inp, depth):
    voff, NV = vec_layout(depth)
    vecs = np.zeros((128, NV), np.float32)

    def put(name, arr):
        o, c = voff[name]
        assert arr.shape == (128, c), (name, arr.shape, c)
        vecs[:, o:o + c] = arr
    for i in range(depth):
        put(f"adab{i}", fm(inp["ada_b"][i]))
        put(f"n1g{i}", fm(inp["norm1_g"][i]))
        put(f"n2g{i}", fm(inp["norm2_g"][i]))
        j = i // 2
        if i % 2 == 0:
            put(f"qan{j}", fm(inp["mla_qa_norm"][j]))
            put(f"kvan{j}", fm(inp["mla_kva_norm"][j]))
            put(f"qnn{j}", fm(inp["mla_q_norm"][j][:128]))
            put(f"qnr{j}", fm(inp["mla_q_norm"][j][128:]))
            put(f"knn{j}", fm(inp["mla_k_norm"][j][:128]))
            put(f"knr{j}", fm(inp["mla_k_norm"][j][128:]))
        else:
            put(f"mix{j}", fm(inp["rwkv_mix"][j]))
            put(f"w0{j}", fm(inp["rwkv_w0"][j]))
            put(f"a0{j}", fm(inp["rwkv_a0"][j]))
            put(f"kk{j}", fm(inp["rwkv_k_k"][j]))
            put(f"ka{j}", fm(inp["rwkv_k_a"][j]))
            put(f"rk{j}", fm(inp["rwkv_r_k"][j]))
            put(f"lnw{j}", fm(inp["rwkv_ln_w"][j]))
            put(f"lnb{j}", fm(inp["rwkv_ln_b"][j]))
            if j >= 1:
                put(f"v0{j}", fm(inp["rwkv_v0"][j - 1]))
    return vecs


WNAMES = ["ada_w", "mla_wqa", "mla_wqb", "mla_wkva", "mla_wkvb", "mla_wo", "ffn_w1", "ffn_w3", "ffn_w2",
          "moe_router", "moe_w1", "moe_w3", "moe_w2",
          "rwkv_wr", "rwkv_wk", "rwkv_wv", "rwkv_wo", "rwkv_w1", "rwkv_w2", "rwkv_a1", "rwkv_a2", "rwkv_g1", "rwkv_g2", "rwkv_v1", "rwkv_v2"]


def make_in_maps(inp, depth, cores):
    c32, cosT, sinT = host_consts()
    vecs = host_vecs(inp, depth)
    shared = {n: np.ascontiguousarray(inp[n], dtype=np.float32) for n in WNAMES}
    maps = []
    for b in cores:
        xs = np.ascontiguousarray(np.concatenate([inp["ctx"][b], inp["x"][b]], axis=0).T.astype(np.float32))
        cv = np.zeros((128, 16), np.float32)
        cv[:, 0::2] = fm(inp["c"][b])
        cv[:, 1::2] = fm(inp["c_ctx"])
        m = dict(shared)
        m.update(xs=xs, cvec=cv, vecs=vecs, c32=c32, ropec=cosT, ropes=sinT)
        maps.append(m)
    return maps


def kernel(**inp):
    depth = 4
    nc, S = build(depth)
    maps = make_in_maps(inp, depth, list(range(8)))
    res = run_bass_kernel_spmd(nc, maps, core_ids=list(range(8)))
    out = np.stack([np.ascontiguousarray(r["out"].T) for r in res.results], axis=0)
    return out.astype(np.float32)
```

```python
import contextlib
import os
import numpy as np
import concourse.bass as bass
import concourse.mybir as mybir
from concourse.bass_utils import run_bass_kernel_spmd

F32 = mybir.dt.float32
BF16 = mybir.dt.bfloat16
ALU = mybir.AluOpType
AF = mybir.ActivationFunctionType
AX = mybir.AxisListType
NS = 8

D = 1024
KC = 8
CTX = 256
TL = 4096
NT = CTX + TL
EPS = 1e-6
NH = 8
SM_SCALE = 192 ** -0.5
DFF = 2816
DFE = 3584
NE = 8
C32W = 128 * 3 + 8 * 128 + 128 + 64 + 128 + 64 + 128 + 64 + 512
TILES = [(0, 256, 1)] + [(256 + 512 * i, 512, 0) for i in range(8)]


class Sched:
    def __init__(self, nc):
        self.nc = nc
        self.engs = {'pe': nc.tensor, 'dve': nc.vector, 'act': nc.scalar,
                     'pool': nc.gpsimd, 'sp': nc.sync}
        self.sem = {e: nc.alloc_semaphore(name=f"sem_{e}") for e in ['pe', 'dve', 'act', 'pool']}
        self.cnt = {e: 0 for e in self.sem}
        self.dq = {}
        for q, e in [('sp', 'sp'), ('pool', 'pool')]:
            self.dq[q] = dict(eng=e, n=0,
                              sems=[nc.alloc_semaphore(name=f"dsem_{q}{i}") for i in range(NS)])
        self.waited = {}
        self.lastw = {}
        self.readers = {}
        self.nins = 0
        self.mute = False

    def _sid_val(self, tok):
        if tok[0] == 'c':
            return ('c', tok[1]), self.sem[tok[1]], tok[2]
        q = self.dq[tok[1]]
        n = tok[2]
        return ('d', tok[1], n % NS), q['sems'][n % NS], 16 * (n // NS + 1)

    def _wait(self, eng, tok):
        if tok[0] == 'c' and tok[1] == eng and eng == 'pe':
            return
        sid, sem, val = self._sid_val(tok)
        if tok[0] == 'c':
            assert val <= self.cnt[tok[1]], f"wait on unsignalled instr {tok}"
        if self.waited.get((eng, sid), 0) >= val:
            return
        self.engs[eng].wait_ge(sem, val)
        self.waited[(eng, sid)] = val
        self.nins += 1

    def _deps(self, reads, writes):
        deps = set()
        for k in reads:
            if k in self.lastw:
                deps.add(self.lastw[k])
        for k in writes:
            if k in self.lastw:
                deps.add(self.lastw[k])
            for t in self.readers.get(k, {}).values():
                deps.add(t)
        return deps

    def _record(self, tok, reads, writes):
        sid, _, val = self._sid_val(tok)
        for k in reads:
            r = self.readers.setdefault(k, {})
            old = r.get(sid)
            if old is None or self._sid_val(old)[2] < val:
                r[sid] = tok
        for k in writes:
            self.lastw[k] = tok
            self.readers[k] = {}

    def op(self, eng, fn, reads=(), writes=(), inc=True):
        if self.mute:
            return None
        if eng != 'pe':
            psr = [k for k in reads if isinstance(k, tuple) and k[0] in ('ps', 'psb', 'sps')]
            if psr:
                reads = [k for k in reads if k not in psr]
                writes = list(writes) + psr
        for t in self._deps(reads, writes):
            self._wait(eng, t)
        ins = fn(self.engs[eng])
        self.nins += 1
        if inc:
            self.cnt[eng] += 1
            ins.then_inc(self.sem[eng], 1)
            tok = ('c', eng, self.cnt[eng])
        else:
            tok = ('c', eng, self.cnt[eng] + 1)
        self._record(tok, reads, writes)
        return ins

    def dma(self, q, out, in_, reads=(), writes=(), **kw):
        if self.mute:
            return None
        Q = self.dq[q]
        eng = Q['eng']
        n = Q['n']
        for t in self._deps(reads, writes):
            self._wait(eng, t)
        if n >= NS:
            self._wait(eng, ('d', q, n - NS))
        ins = self.engs[eng].dma_start(out=out, in_=in_, **kw)
        ins.then_inc(Q['sems'][n % NS], 16)
        self.nins += 1
        Q['n'] += 1
        self._record(('d', q, n), reads, writes)
        return ins

    def barrier(self):
        toks = []
        for e, c in self.cnt.items():
            if c > 0:
                toks.append(('c', e, c))
        for q, Q in self.dq.items():
            for n in range(max(0, Q['n'] - NS), Q['n']):
                toks.append(('d', q, n))
        for e in ['pe', 'dve', 'act', 'pool', 'sp']:
            for t in toks:
                if t[0] == 'c' and t[1] == e:
                    continue
                self._wait(e, t)
        self.lastw.clear()
        self.readers.clear()


def vec_layout(depth):
    ents = []
    for i in range(depth):
        ents += [(f"adab{i}", 48), (f"n1g{i}", 8), (f"n2g{i}", 8)]
        j = i // 2
        if i % 2 == 0:
            ents += [(f"qan{j}", 3), (f"kvan{j}", 2), (f"qnn{j}", 1), (f"qnr{j}", 1), (f"knn{j}", 1), (f"knr{j}", 1)]
        else:
            ents += [(f"mix{j}", 48), (f"w0{j}", 16), (f"a0{j}", 16), (f"kk{j}", 8), (f"ka{j}", 8),
                     (f"rk{j}", 8), (f"lnw{j}", 8), (f"lnb{j}", 8)]
            if j >= 1:
                ents += [(f"v0{j}", 8)]
    off = {}
    o = 0
    for n, c in ents:
        off[n] = (o, c)
        o += c
    return off, o


def fm(v):
    v = np.asarray(v, np.float32).reshape(-1)
    pad = (-len(v)) % 128
    if pad:
        v = np.concatenate([v, np.zeros(pad, np.float32)])
    return np.ascontiguousarray(v.reshape(-1, 128).T)


def build(depth=4, dbg=None):
    nc = bass.Bass("TRN2", target_bir_lowering=False)
    voff, NV = vec_layout(depth)

    def din(name, shape, dt=F32):
        return nc.dram_tensor(name, list(shape), dt, kind="ExternalInput").ap()

    def dscr(name, shape, dt=F32):
        return nc.dram_tensor(name, list(shape), dt, kind="Internal").ap()

    xs_in = din("xs", [D, NT])
    cvec_d = din("cvec", [128, 16])
    vecs_d = din("vecs", [128, NV])
    c32_d = din("c32", [128, C32W])
    ropec_d = din("ropec", [64, TL])
    ropes_d = din("ropes", [64, TL])
    ada_w = din("ada_w", [4, D, 6 * D])
    mla_wqa = din("mla_wqa", [2, D, 384]); mla_wqb = din("mla_wqb", [2, 384, 1536])
    mla_wkva = din("mla_wkva", [2, D, 320]); mla_wkvb = din("mla_wkvb", [2, 256, 2048])
    mla_wo = din("mla_wo", [2, D, D])
    ffn_w1 = din("ffn_w1", [2, D, DFF]); ffn_w3 = din("ffn_w3", [2, D, DFF]); ffn_w2 = din("ffn_w2", [2, DFF, D])
    moe_router = din("moe_router", [2, D, NE])
    moe_w1 = din("moe_w1", [2, NE, D, DFE]); moe_w3 = din("moe_w3", [2, NE, D, DFE]); moe_w2 = din("moe_w2", [2, NE, DFE, D])
    rwkv_wr = din("rwkv_wr", [2, D, D]); rwkv_wk = din("rwkv_wk", [2, D, D]); rwkv_wv = din("rwkv_wv", [2, D, D]); rwkv_wo = din("rwkv_wo", [2, D, D])
    rwkv_w1 = din("rwkv_w1", [2, 2, D, 64]); rwkv_w2 = din("rwkv_w2", [2, 2, 64, D])
    rwkv_a1 = din("rwkv_a1", [2, 2, D, 64]); rwkv_a2 = din("rwkv_a2", [2, 2, 64, D])
    rwkv_g1 = din("rwkv_g1", [2, D, 160]); rwkv_g2 = din("rwkv_g2", [2, 160, D])
    rwkv_v1 = din("rwkv_v1", [1, D, 32]); rwkv_v2 = din("rwkv_v2", [1, 32, D])
    out_d = nc.dram_tensor("out", [D, TL], F32, kind="ExternalOutput").ap()
    HS = dscr("HS", [D, 258 + 4098])
    ATd = [dscr(f"ATd{d}", [D, NT], BF16) for d in range(2)]; BTd = [dscr(f"BTd{d}", [D, NT], BF16) for d in range(2)]
    KTd = [dscr(f"KTd{d}", [D, NT], BF16) for d in range(2)]; RTd = [dscr(f"RTd{d}", [D, NT], BF16) for d in range(2)]
    VTb = dscr("VTb", [D, NT], BF16)
    WCd = [dscr(f"WCd{d}", [D, NT // 64]) for d in range(2)]
    VT = [dscr(f"VT{d}", [D, NT]) for d in range(2)]
    YD = [dscr(f"YD{d}", [D, NT]) for d in range(2)]
    BON = dscr("BON", [D, NT]); GG = dscr("GG", [D, NT])

    XS = dscr("XS", [D, NT])
    QN = dscr("QN", [NH, 128, NT], BF16); QR = dscr("QR", [NH, 64, NT], BF16)
    KN = dscr("KN", [NH, 128, NT], BF16); KR = dscr("KR", [NH, 64, NT], BF16)
    VV = dscr("VV", [NT, NH, 128], BF16)
    AO = dscr("AO", [D, NT], BF16)

    S = Sched(nc)
    es = contextlib.ExitStack()

    uniq = [0]

    def sb(name, shape, dt=F32, stack=None):
        uniq[0] += 1
        return (stack or es).enter_context(nc.sbuf_tensor(f"s{uniq[0]}_{name}", list(shape), dt))

    with es:
        ps = [es.enter_context(nc.psum_tensor(f"ps{i}", [128, 512], F32)) for i in range(8)]
        vecs = sb("vecs", [128, NV])
        c32 = sb("c32", [128, C32W])
        ones_bf = sb("ones_bf", [128, 128], BF16)
        cvec = sb("cvec", [128, 16])
        modt = sb("modt", [128, depth, 48, 2])
        A1 = sb("A1", [128, depth, 8, 2]); A2 = sb("A2", [128, depth, 8, 2])
        S.dma('sp', vecs[:], vecs_d, writes=['vecs'])
        S.dma('sp', c32[:], c32_d, writes=['c32'])
        S.dma('sp', cvec[:], cvec_d, writes=['cvec'])
        EPSI = {1024: 0, 384: 1, 256: 2, 192: 3, 64: 4}
        epsT = sb("epsT", [128, 8])
        for nf, ci in EPSI.items():
            S.op('pool', lambda e: e.memset(epsT[:, ci:ci + 1], float(nf * EPS)), writes=['epsT'])
        ones32 = c32[:, 0:128]
        ident = c32[:, 128:256]
        rotP = c32[0:64, 256:320]
        selE = c32[0:8, 384:384 + 8 * 128]
        o_ = 384 + 1024
        blockones = c32[:, o_:o_ + 128]
        SI = c32[:, o_ + 128:o_ + 192]
        maskAR_f = c32[:, o_ + 192:o_ + 320]
        maskN_f = c32[:, o_ + 320:o_ + 384]
        maskAR_r = c32[:, o_ + 384:o_ + 512]
        maskN_r = c32[:, o_ + 512:o_ + 576]
        cmask = c32[:, o_ + 576:o_ + 1088]
        S.op('pool', lambda e: e.memset(epsT[:, 5:6], 64e-5), writes=['epsT'])
        S.op('dve', lambda e: e.tensor_copy(out=ones_bf[:], in_=ones32), reads=['c32'], writes=['ones_bf'])
        ident_bf = sb("ident_bf", [128, 128], BF16)
        S.op('dve', lambda e: e.tensor_copy(out=ident_bf[:], in_=ident), reads=['c32'], writes=['ident_bf'])

        def V(name, k0=0, k1=None):
            o, c = voff[name]
            k1 = c if k1 is None else k1
            return vecs[:, o + k0:o + k1]

        def phase_mod():
            with contextlib.ExitStack() as st:
                wb = [sb(f"adaw{i}", [128, 8, 1024], F32, st) for i in range(2)]
                sc = sb("sc", [128, 16], F32, st)
                S.op('act', lambda e: e.activation(out=sc[:], in_=cvec[:], func=AF.Silu), reads=['cvec'], writes=['sc'])
                n = 0
                for i in range(depth):
                    for j in range(6):
                        w = wb[n % 2]
                        S.dma('sp', w[:], ada_w[i, :, j * 1024:(j + 1) * 1024].rearrange("(k p) n -> p k n", p=128),
                              writes=[('adaw', n % 2)])
                        for oc in range(8):
                            col = (j * 8 + oc) * 2
                            for kc in range(8):
                                S.op('pe', lambda e: e.matmul(ps[0][:, col:col + 2], lhsT=w[:, kc, oc * 128:(oc + 1) * 128],
                                                              rhs=sc[:, 2 * kc:2 * kc + 2], start=(kc == 0), stop=(kc == 7)),
                                     reads=[('adaw', n % 2), 'sc'], writes=['psmod'], inc=(kc == 7 and oc == 7))
                        n += 1
                    S.op('dve', lambda e: e.tensor_tensor(
                        out=modt[:, i, :, :], in0=ps[0][:, 0:96].rearrange("p (a b) -> p a b", b=2),
                        in1=V(f"adab{i}").unsqueeze(2).to_broadcast([128, 48, 2]), op=ALU.add),
                        reads=['psmod', 'vecs'], writes=['modt'])
                    for (At, gname, jj) in [(A1, f"n1g{i}", 1), (A2, f"n2g{i}", 4)]:
                        S.op('dve', lambda e: e.scalar_tensor_tensor(
                            out=At[:, i, :, :], in0=modt[:, i, jj * 8:(jj + 1) * 8, :], scalar=1.0,
                            in1=V(gname).unsqueeze(2).to_broadcast([128, 8, 2]), op0=ALU.add, op1=ALU.mult),
                            reads=['modt', 'vecs'], writes=['A'])
                        S.op('dve', lambda e: e.tensor_scalar(out=At[:, i, :, :], in0=At[:, i, :, :], scalar1=float(np.sqrt(D)),
                                                              scalar2=None, op0=ALU.mult), reads=['A'], writes=['A'])
            S.barrier()

        def normmod(x32, N, Acol, Scol, outap, sq, rs, psb, kx, tag):
            ksq, krs, kps = ('sq', tag), 'rs', ('ps', psb)
            S.op('pool', lambda e: e.tensor_tensor(out=sq[:, :, :N], in0=x32, in1=x32, op=ALU.mult), reads=[kx], writes=[ksq])
            for k in range(8):
                S.op('pe', lambda e: e.matmul(ps[psb][:, :N], lhsT=ones32, rhs=sq[:, k, :N], start=(k == 0), stop=(k == 7)),
                     reads=[ksq, 'c32'], writes=[kps], inc=(k == 7))
            S.op('act', lambda e: e.activation(out=rs[:, :N], in_=ps[psb][:, :N], func=AF.Sqrt, bias=epsT[:, EPSI[D]:EPSI[D] + 1], scale=1.0),
                 reads=[kps, 'epsT'], writes=[krs])
            S.op('dve', lambda e: e.reciprocal(out=rs[:, :N], in_=rs[:, :N]), reads=[krs], writes=[krs])
            S.op('dve', lambda e: e.tensor_tensor(out=sq[:, :, :N], in0=x32, in1=rs[:, :N].unsqueeze(1).to_broadcast([128, 8, N]),
                                                  op=ALU.mult), reads=[kx, krs], writes=[ksq])
            S.op('pool', lambda e: e.tensor_tensor(out=sq[:, :, :N], in0=sq[:, :, :N], in1=Acol.unsqueeze(2).to_broadcast([128, 8, N]),
                                                   op=ALU.mult), reads=[ksq, 'A'], writes=[ksq])
            return ksq

        def rstd_from_ps(psb, N, nfeat, rs, krs, P=128):
            S.op('act', lambda e: e.activation(out=rs[:P, :N], in_=ps[psb][:P, :N], func=AF.Sqrt, bias=epsT[:P, EPSI[nfeat]:EPSI[nfeat] + 1], scale=1.0),
                 reads=[('ps', psb), 'epsT'], writes=[krs])
            S.op('dve', lambda e: e.reciprocal(out=rs[:P, :N], in_=rs[:P, :N]), reads=[krs], writes=[krs])

        def phase_mla(i, XSin):
            j = i // 2
            with contextlib.ExitStack() as st:
                wqa = sb("wqa", [128, 8, 384], BF16, st); wqb = sb("wqb", [128, 3, 1536], BF16, st)
                wkva = sb("wkva", [128, 8, 320], BF16, st); wkvb = sb("wkvb", [128, 2, 2048], BF16, st)
                S.dma('pool', wqa[:], mla_wqa[j].rearrange("(k p) n -> p k n", p=128), writes=['wqa'])
                S.dma('pool', wqb[:], mla_wqb[j].rearrange("(k p) n -> p k n", p=128), writes=['wqb'])
                S.dma('pool', wkva[:], mla_wkva[j].rearrange("(k p) n -> p k n", p=128), writes=['wkva'])
                S.dma('pool', wkvb[:], mla_wkvb[j].rearrange("(k p) n -> p k n", p=128), writes=['wkvb'])
                xt = [sb("xt0", [128, 8, 512], F32, st)] * 2
                sq = sb("sq", [128, 8, 512], F32, st)
                rs = sb("rs", [128, 512], F32, st)
                hb = sb("hb", [128, 8, 512], BF16, st)
                cq = sb("cq", [128, 3, 512], F32, st); cq2 = sb("cq2", [128, 3, 512], F32, st)
                cqn = sb("cqn", [128, 3, 512], BF16, st)
                ckv = sb("ckv", [128, 2, 512], F32, st); ckv2 = sb("ckv2", [128, 2, 512], F32, st)
                ckvn = sb("ckvn", [128, 2, 512], BF16, st)
                kr = sb("kr", [64, 512], F32, st); kr2 = sb("kr2", [64, 512], F32, st)
                krP = sb("krP", [64, 512], F32, st); krr = sb("krr", [64, 512], F32, st)
                qh = sb("qh", [128, 512], F32, st); qh2 = sb("qh2", [128, 512], F32, st)
                qr = sb("qr", [64, 512], F32, st); qr2 = sb("qr2", [64, 512], F32, st)
                qrP = sb("qrP", [64, 512], F32, st)
                rs2 = sb("rs2", [128, 512], F32, st)
                cosT = sb("cosT", [64, 512], F32, st); sinT = sb("sinT", [64, 512], F32, st)
                qn_o = sb("qn_o", [128, NH, 512], BF16, st); qr_o = sb("qr_o", [64, NH, 512], BF16, st)
                kn_o = sb("kn_o", [128, NH, 512], BF16, st); kr_o = sb("kr_o", [64, NH, 512], BF16, st)
                v_o = sb("v_o", [128, 4, NH, 128], BF16, st)
                sv = sb("sv", [128, 16], F32, st)
                S.op('dve', lambda e: e.tensor_scalar(out=sv[:, 0:3], in0=V(f"qan{j}"), scalar1=float(np.sqrt(384)), scalar2=None, op0=ALU.mult),
                     reads=['vecs'], writes=['sv'])
                S.op('dve', lambda e: e.tensor_scalar(out=sv[:, 3:5], in0=V(f"kvan{j}"), scalar1=float(np.sqrt(256)), scalar2=None, op0=ALU.mult),
                     reads=['vecs'], writes=['sv'])
                S.op('dve', lambda e: e.tensor_scalar(out=sv[:, 5:6], in0=V(f"qnn{j}"), scalar1=float(np.sqrt(192) * SM_SCALE), scalar2=None, op0=ALU.mult),
                     reads=['vecs'], writes=['sv'])
                S.op('dve', lambda e: e.tensor_scalar(out=sv[:, 6:7], in0=V(f"qnr{j}"), scalar1=float(np.sqrt(192) * SM_SCALE), scalar2=None, op0=ALU.mult),
                     reads=['vecs'], writes=['sv'])
                S.op('dve', lambda e: e.tensor_scalar(out=sv[:, 7:8], in0=V(f"knn{j}"), scalar1=float(np.sqrt(192)), scalar2=None, op0=ALU.mult),
                     reads=['vecs'], writes=['sv'])
                S.op('dve', lambda e: e.tensor_scalar(out=sv[:, 8:9], in0=V(f"knr{j}"), scalar1=float(np.sqrt(192)), scalar2=None, op0=ALU.mult),
                     reads=['vecs'], writes=['sv'])

                def rope(src, Pbuf, dst, N, ksrc, kdst, psb):
                    S.op('pe', lambda e: e.matmul(ps[psb][:64, :N], lhsT=rotP, rhs=src[:, :N], start=True, stop=True),
                         reads=[ksrc, 'c32'], writes=[('ps', psb)])
                    S.op('dve', lambda e: e.tensor_tensor(out=Pbuf[:, :N], in0=ps[psb][:64, :N], in1=sinT[:, :N], op=ALU.mult),
                         reads=[('ps', psb), 'rope'], writes=[('rp', kdst)])
                    S.op('pool', lambda e: e.tensor_tensor(out=dst[:, :N], in0=src[:, :N], in1=cosT[:, :N], op=ALU.mult),
                         reads=[ksrc, 'rope'], writes=[kdst])
                    S.op('pool', lambda e: e.tensor_tensor(out=dst[:, :N], in0=dst[:, :N], in1=Pbuf[:, :N], op=ALU.add),
                         reads=[kdst, ('rp', kdst)], writes=[kdst])

                for ti, (t0, N, mc) in enumerate(TILES):
                    x32 = xt[ti % 2]
                    kx = ('xt', ti % 2)
                    S.dma('sp', x32[:, :, :N], XSin[:, t0:t0 + N].rearrange("(k p) n -> p k n", p=128), writes=[kx])
                    if mc == 0:
                        S.dma('sp', cosT[:, :N], ropec_d[:, t0 - CTX:t0 - CTX + N], writes=['rope'])
                        S.dma('sp', sinT[:, :N], ropes_d[:, t0 - CTX:t0 - CTX + N], writes=['rope'])
                    ksq = normmod(x32[:, :, :N], N, A1[:, i, :, mc], None, None, sq, rs, 7, kx, 'm')
                    S.op('dve', lambda e: e.tensor_tensor(out=hb[:, :, :N], in0=sq[:, :, :N],
                                                          in1=modt[:, i, 0:8, mc].unsqueeze(2).to_broadcast([128, 8, N]), op=ALU.add),
                         reads=[ksq, 'modt'], writes=['hb'])
                    for c in range(3):
                        pb = c % 2
                        for kc in range(8):
                            S.op('pe', lambda e: e.matmul(ps[pb][:, :N], lhsT=wqa[:, kc, c * 128:(c + 1) * 128], rhs=hb[:, kc, :N],
                                                          start=(kc == 0), stop=(kc == 7)), reads=['hb', 'wqa'], writes=[('ps', pb)], inc=(kc == 7))
                        S.op('act', lambda e: e.activation(out=cq[:, c, :N], in_=ps[pb][:, :N], func=AF.Identity),
                             reads=[('ps', pb)], writes=[('cq', c)])
                        S.op('pool', lambda e: e.tensor_tensor(out=cq2[:, c, :N], in0=cq[:, c, :N], in1=cq[:, c, :N], op=ALU.mult),
                             reads=[('cq', c)], writes=[('cq2', c)])
                    for c in range(3):
                        S.op('pe', lambda e: e.matmul(ps[7][:, :N], lhsT=ones32, rhs=cq2[:, c, :N], start=(c == 0), stop=(c == 2)),
                             reads=[('cq2', c), 'c32'], writes=[('ps', 7)], inc=(c == 2))
                    rstd_from_ps(7, N, 384, rs, 'rs')
                    for c in range(3):
                        S.op('dve', lambda e: e.scalar_tensor_tensor(out=cqn[:, c, :N], in0=cq[:, c, :N], scalar=sv[:, c:c + 1], in1=rs[:, :N],
                                                                     op0=ALU.mult, op1=ALU.mult), reads=[('cq', c), 'rs', 'sv'], writes=['cqn'])
                    for c in range(3):
                        pb = 2 + c % 2
                        M = 128 if c < 2 else 64
                        for kc in range(8):
                            S.op('pe', lambda e: e.matmul(ps[pb][:M, :N], lhsT=wkva[:, kc, c * 128:c * 128 + M], rhs=hb[:, kc, :N],
                                                          start=(kc == 0), stop=(kc == 7)), reads=['hb', 'wkva'], writes=[('ps', pb)], inc=(kc == 7))
                        if c < 2:
                            S.op('act', lambda e: e.activation(out=ckv[:, c, :N], in_=ps[pb][:, :N], func=AF.Identity),
                                 reads=[('ps', pb)], writes=[('ckv', c)])
                            S.op('pool', lambda e: e.tensor_tensor(out=ckv2[:, c, :N], in0=ckv[:, c, :N], in1=ckv[:, c, :N], op=ALU.mult),
                                 reads=[('ckv', c)], writes=[('ckv2', c)])
                        else:
                            S.op('act', lambda e: e.activation(out=kr[:, :N], in_=ps[pb][:64, :N], func=AF.Identity),
                                 reads=[('ps', pb)], writes=['kr'])
                            S.op('pool', lambda e: e.tensor_tensor(out=kr2[:, :N], in0=kr[:, :N], in1=kr[:, :N], op=ALU.mult),
                                 reads=['kr'], writes=['kr2'])
                            S.op('dve', lambda e: e.tensor_scalar(out=kr[:, :N], in0=kr[:, :N], scalar1=sv[0:64, 8:9], scalar2=None, op0=ALU.mult),
                                 reads=['kr', 'sv', 'kr2'], writes=['kr'])
                    for c in range(2):
                        S.op('pe', lambda e: e.matmul(ps[7][:, :N], lhsT=ones32, rhs=ckv2[:, c, :N], start=(c == 0), stop=(c == 1)),
                             reads=[('ckv2', c), 'c32'], writes=[('ps', 7)], inc=(c == 1))
                    rstd_from_ps(7, N, 256, rs, 'rs')
                    for c in range(2):
                        S.op('dve', lambda e: e.scalar_tensor_tensor(out=ckvn[:, c, :N], in0=ckv[:, c, :N], scalar=sv[:, 3 + c:4 + c], in1=rs[:, :N],
                                                                     op0=ALU.mult, op1=ALU.mult), reads=[('ckv', c), 'rs', 'sv'], writes=['ckvn'])
                    if mc == 0:
                        rope(kr, krP, krr, N, 'kr', 'krr', 6)
                        krsrc, kkr = krr, 'krr'
                    else:
                        krsrc, kkr = kr, 'kr'
                    for h in range(NH):
                        for kc in range(3):
                            S.op('pe', lambda e: e.matmul(ps[0][:, :N], lhsT=wqb[:, kc, h * 192:h * 192 + 128], rhs=cqn[:, kc, :N],
                                                          start=(kc == 0), stop=(kc == 2)), reads=['cqn', 'wqb'], writes=[('ps', 0)], inc=(kc == 2))
                        for kc in range(3):
                            S.op('pe', lambda e: e.matmul(ps[1][:64, :N], lhsT=wqb[:, kc, h * 192 + 128:h * 192 + 192], rhs=cqn[:, kc, :N],
                                                          start=(kc == 0), stop=(kc == 2)), reads=['cqn', 'wqb'], writes=[('ps', 1)], inc=(kc == 2))
                        S.op('act', lambda e: e.activation(out=qh[:, :N], in_=ps[0][:, :N], func=AF.Identity), reads=[('ps', 0)], writes=['qh'])
                        S.op('act', lambda e: e.activation(out=qr[:, :N], in_=ps[1][:64, :N], func=AF.Identity), reads=[('ps', 1)], writes=['qr'])
                        S.op('pool', lambda e: e.tensor_tensor(out=qh2[:, :N], in0=qh[:, :N], in1=qh[:, :N], op=ALU.mult), reads=['qh'], writes=['qh2'])
                        S.op('pool', lambda e: e.tensor_tensor(out=qr2[:, :N], in0=qr[:, :N], in1=qr[:, :N], op=ALU.mult), reads=['qr'], writes=['qr2'])
                        S.op('pe', lambda e: e.matmul(ps[4][:, :N], lhsT=ones32, rhs=qh2[:, :N], start=True, stop=False),
                             reads=['qh2', 'c32'], writes=[('ps', 4)], inc=False)
                        S.op('pe', lambda e: e.matmul(ps[4][:, :N], lhsT=c32[0:64, 0:128], rhs=qr2[:, :N], start=False, stop=True),
                             reads=['qr2', 'c32'], writes=[('ps', 4)])
                        rstd_from_ps(4, N, 192, rs2, 'rs2')
                        S.op('dve', lambda e: e.scalar_tensor_tensor(out=qn_o[:, h, :N], in0=qh[:, :N], scalar=sv[:, 5:6], in1=rs2[:, :N],
                                                                     op0=ALU.mult, op1=ALU.mult), reads=['qh', 'rs2', 'sv'], writes=['qn_o'])
                        S.op('dve', lambda e: e.scalar_tensor_tensor(out=qr[:, :N], in0=qr[:, :N], scalar=sv[0:64, 6:7], in1=rs2[0:64, :N],
                                                                     op0=ALU.mult, op1=ALU.mult), reads=['qr', 'rs2', 'sv', 'qr2'], writes=['qr'])
                        if mc == 0:
                            rope(qr, qrP, qr2, N, 'qr', 'qr2', 6)
                            S.op('act', lambda e: e.activation(out=qr_o[:, h, :N], in_=qr2[:, :N], func=AF.Identity), reads=['qr2'], writes=['qr_o'])
                        else:
                            S.op('act', lambda e: e.activation(out=qr_o[:, h, :N], in_=qr[:, :N], func=AF.Identity), reads=['qr'], writes=['qr_o'])
                        for kc in range(2):
                            S.op('pe', lambda e: e.matmul(ps[2][:, :N], lhsT=wkvb[:, kc, h * 256:h * 256 + 128], rhs=ckvn[:, kc, :N],
                                                          start=(kc == 0), stop=(kc == 1)), reads=['ckvn', 'wkvb'], writes=[('ps', 2)], inc=(kc == 1))
                        S.op('act', lambda e: e.activation(out=qh[:, :N], in_=ps[2][:, :N], func=AF.Identity), reads=[('ps', 2)], writes=['qh'])
                        S.op('pool', lambda e: e.tensor_tensor(out=qh2[:, :N], in0=qh[:, :N], in1=qh[:, :N], op=ALU.mult), reads=['qh'], writes=['qh2'])
                        S.op('pe', lambda e: e.matmul(ps[5][:, :N], lhsT=ones32, rhs=qh2[:, :N], start=True, stop=False),
                             reads=['qh2', 'c32'], writes=[('ps', 5)], inc=False)
                        S.op('pe', lambda e: e.matmul(ps[5][:, :N], lhsT=c32[0:64, 0:128], rhs=kr2[:, :N], start=False, stop=True),
                             reads=['kr2', 'c32'], writes=[('ps', 5)])
                        rstd_from_ps(5, N, 192, rs2, 'rs2')
                        S.op('dve', lambda e: e.scalar_tensor_tensor(out=kn_o[:, h, :N], in0=qh[:, :N], scalar=sv[:, 7:8], in1=rs2[:, :N],
                                                                     op0=ALU.mult, op1=ALU.mult), reads=['qh', 'rs2', 'sv'], writes=['kn_o'])
                        S.op('dve', lambda e: e.tensor_tensor(out=kr_o[:, h, :N], in0=krsrc[:, :N], in1=rs2[0:64, :N], op=ALU.mult),
                             reads=[kkr, 'rs2'], writes=['kr_o'])
                        for blk in range(N // 128):
                            for kc in range(2):
                                S.op('pe', lambda e: e.matmul(ps[3][:, blk * 128:(blk + 1) * 128], lhsT=ckvn[:, kc, blk * 128:(blk + 1) * 128],
                                                              rhs=wkvb[:, kc, h * 256 + 128:h * 256 + 256], start=(kc == 0), stop=(kc == 1)),
                                     reads=['ckvn', 'wkvb'], writes=[('ps', 3)], inc=(kc == 1 and blk == N // 128 - 1))
                        S.op('act', lambda e: e.activation(out=v_o[:, 0:N // 128, h, :], in_=ps[3][:, :N].rearrange("p (b d) -> p b d", d=128),
                                                           func=AF.Identity), reads=[('ps', 3)], writes=['v_o'])
                    S.dma('sp', QN[:, :, t0:t0 + N].rearrange("h p n -> p h n"), qn_o[:, :, :N], reads=['qn_o'], writes=['QN'])
                    S.dma('sp', QR[:, :, t0:t0 + N].rearrange("h p n -> p h n"), qr_o[:, :, :N], reads=['qr_o'], writes=['QR'])
                    S.dma('sp', KN[:, :, t0:t0 + N].rearrange("h p n -> p h n"), kn_o[:, :, :N], reads=['kn_o'], writes=['KN'])
                    S.dma('sp', KR[:, :, t0:t0 + N].rearrange("h p n -> p h n"), kr_o[:, :, :N], reads=['kr_o'], writes=['KR'])
                    S.dma('sp', VV[t0:t0 + N].rearrange("(b p) h d -> p b h d", p=128), v_o[:, 0:N // 128], reads=['v_o'], writes=['VV'])
            S.barrier()
            with contextlib.ExitStack() as st:
                kn = [sb(f"a_kn{b}", [128, NT], BF16, st) for b in range(2)]
                krt = [sb(f"a_kr{b}", [64, NT], BF16, st) for b in range(2)]
                qn = [sb(f"a_qn{b}", [128, NT], BF16, st) for b in range(2)]
                qrt = [sb(f"a_qr{b}", [64, NT], BF16, st) for b in range(2)]
                vt = [sb(f"a_v{b}", [128, NT // 128, 128], BF16, st) for b in range(2)]
                pT = [sb(f"a_p{b}", [128, 512], BF16, st) for b in range(3)]
                rd = sb("a_rd", [128, 512], F32, st)
                ao = [sb(f"a_o{b}", [128, 512], BF16, st) for b in range(2)]
                npt = 0
                nq = 0
                for h in range(NH):
                    b = h % 2
                    kh = ('ah', b)
                    S.dma('sp', kn[b][:], KN[h], writes=[kh])
                    S.dma('sp', krt[b][:], KR[h], writes=[kh])
                    S.dma('sp', qn[b][:], QN[h], writes=[kh])
                    S.dma('sp', qrt[b][:], QR[h], writes=[kh])
                    S.dma('sp', vt[b][:], VV[:, h, :].rearrange("(b p) d -> p b d", p=128), writes=[kh])
                    for (t0, N, mc) in TILES:
                        nkb = 2 if mc == 1 else NT // 128
                        po, pd = 4 + (nq % 2), 6 + (nq % 2)

                        def scores(kb):
                            sbk = kb % 3
                            S.op('pe', lambda e: e.matmul(ps[sbk][:, :N], lhsT=kn[b][:, kb * 128:(kb + 1) * 128], rhs=qn[b][:, t0:t0 + N],
                                                          start=True, stop=False), reads=[kh], writes=[('ps', sbk)], inc=False)
                            S.op('pe', lambda e: e.matmul(ps[sbk][:, :N], lhsT=krt[b][:, kb * 128:(kb + 1) * 128], rhs=qrt[b][:, t0:t0 + N],
                                                          start=False, stop=True), reads=[kh], writes=[('ps', sbk)])
                        scores(0)
                        for kb in range(nkb):
                            if kb + 1 < nkb:
                                scores(kb + 1)
                            sbk = kb % 3
                            pb = npt % 3
                            npt += 1
                            S.op('act', lambda e: e.activation(out=pT[pb][:, :N], in_=ps[sbk][:, :N], func=AF.Exp),
                                 reads=[('ps', sbk)], writes=[('pT', pb)])
                            S.op('pe', lambda e: e.matmul(ps[po][:, :N], lhsT=vt[b][:, kb, :], rhs=pT[pb][:, :N], start=(kb == 0), stop=(kb == nkb - 1)),
                                 reads=[kh, ('pT', pb)], writes=[('ps', po)], inc=False)
                            S.op('pe', lambda e: e.matmul(ps[pd][:, :N], lhsT=ones_bf[:], rhs=pT[pb][:, :N], start=(kb == 0), stop=(kb == nkb - 1)),
                                 reads=['ones_bf', ('pT', pb)], writes=[('ps', pd)])
                        S.op('dve', lambda e: e.reciprocal(out=rd[:, :N], in_=ps[pd][:, :N]), reads=[('ps', pd)], writes=['rd'])
                        ob = nq % 2
                        S.op('dve', lambda e: e.tensor_tensor(out=ao[ob][:, :N], in0=ps[po][:, :N], in1=rd[:, :N], op=ALU.mult),
                             reads=[('ps', po), 'rd'], writes=[('ao', ob)])
                        S.dma('sp', AO[h * 128:(h + 1) * 128, t0:t0 + N], ao[ob][:, :N], reads=[('ao', ob)], writes=['AO'])
                        nq += 1
            S.barrier()
            with contextlib.ExitStack() as st:
                wo = sb("wo", [128, 8, 1024], BF16, st)
                S.dma('pool', wo[:], mla_wo[j].rearrange("(k p) n -> p k n", p=128), writes=['wo'])
                xt = [sb(f"c_xt{b}", [128, 8, 512], F32, st) for b in range(2)]
                at = [sb(f"c_at{b}", [128, 8, 512], BF16, st) for b in range(2)]
                for ti, (t0, N, mc) in enumerate(TILES):
                    b = ti % 2
                    S.dma('sp', xt[b][:, :, :N], XSin[:, t0:t0 + N].rearrange("(k p) n -> p k n", p=128), writes=[('cx', b)])
                    S.dma('sp', at[b][:, :, :N], AO[:, t0:t0 + N].rearrange("(k p) n -> p k n", p=128), writes=[('ca', b)])
                    for oc in range(8):
                        pb = oc % 4
                        for kc in range(8):
                            S.op('pe', lambda e: e.matmul(ps[pb][:, :N], lhsT=wo[:, kc, oc * 128:(oc + 1) * 128], rhs=at[b][:, kc, :N],
                                                          start=(kc == 0), stop=(kc == 7)), reads=['wo', ('ca', b)], writes=[('ps', pb)], inc=(kc == 7))
                        S.op('dve', lambda e: e.scalar_tensor_tensor(out=xt[b][:, oc, :N], in0=ps[pb][:, :N], scalar=modt[:, i, 16 + oc, mc:mc + 1],
                                                                     in1=xt[b][:, oc, :N], op0=ALU.mult, op1=ALU.add),
                             reads=[('ps', pb), ('cx', b), 'modt'], writes=[('cx', b)])
                    S.dma('sp', XS[:, t0:t0 + N].rearrange("(k p) n -> p k n", p=128), xt[b][:, :, :N], reads=[('cx', b)], writes=['XS'])
            S.barrier()

        def phase_rwkv(i, last):
            j = i // 2
            NCH = NT // 64
            RT256 = [(0, 256, 1)] + [(256 + 256 * a, 256, 0) for a in range(16)]
            HC = HS[:, 0:258]
            HL = HS[:, 258:258 + 4098]
            with contextlib.ExitStack() as st:
                xt = [sb(f"r1x{b}", [128, 8, 512], F32, st) for b in range(2)]
                sq = sb("r1sq", [128, 8, 512], F32, st)
                rs = sb("r1rs", [128, 512], F32, st)
                zt = sb("r1z", [128, 8, 1], F32, st)
                S.op('pool', lambda e: e.memset(zt[:], 0.0), writes=['zt'])
                for col in (0, 257, 258, 258 + 4097):
                    S.dma('sp', HS[:, col:col + 1].rearrange("(k p) n -> p k n", p=128), zt[:], reads=['zt'], writes=['HS'], allow_slow_non_contiguous=True)
                for ti, (t0, N, mc) in enumerate(TILES):
                    b = ti % 2
                    kx = ('r1x', b)
                    S.dma('sp', xt[b][:, :, :N], XS[:, t0:t0 + N].rearrange("(k p) n -> p k n", p=128), writes=[kx])
                    ksq = normmod(xt[b][:, :, :N], N, A1[:, i, :, mc], None, None, sq, rs, 7, kx, 'r1')
                    S.op('dve', lambda e: e.tensor_tensor(out=xt[b][:, :, :N], in0=sq[:, :, :N],
                                                          in1=modt[:, i, 0:8, mc].unsqueeze(2).to_broadcast([128, 8, N]), op=ALU.add),
                         reads=[ksq, 'modt'], writes=[kx])
                    dst = HC[:, 1:257] if mc == 1 else HL[:, 1 + t0 - CTX:1 + t0 - CTX + N]
                    S.dma('sp', dst.rearrange("(k p) n -> p k n", p=128), xt[b][:, :, :N], reads=[kx], writes=['HS'])
            S.barrier()
            if os.environ.get('RSTOP') == '1':
                return
            class _Stop(Exception):
                pass

            def stg(x):
                if os.environ.get('R2STOP') == x:
                    S.mute = True
            try:
              with contextlib.ExitStack() as st:
                  N = 256
                  wr = sb("wr", [128, 8, 1024], BF16, st); wk = sb("wk", [128, 8, 1024], BF16, st); wv = sb("wv", [128, 8, 1024], BF16, st)
                  w1c = sb("w1c", [128, 8, 128], BF16, st); a1c = sb("a1c", [128, 8, 128], BF16, st)
                  g1 = sb("g1", [128, 8, 160], BF16, st); g2a = sb("g2a", [128, 1024], BF16, st); g2b = sb("g2b", [32, 1024], BF16, st)
                  w2p = sb("w2p", [128, 2, 1024], BF16, st); a2p = sb("a2p", [128, 2, 1024], BF16, st)
                  for (wt_, src, kk_) in [(wr, rwkv_wr, 'wr'), (wk, rwkv_wk, 'wk'), (wv, rwkv_wv, 'wv')]:
                      S.dma('pool', wt_[:], src[j].rearrange("(k p) n -> p k n", p=128), writes=[kk_])
                  for d in range(2):
                      S.dma('pool', w1c[:, :, d * 64:(d + 1) * 64], rwkv_w1[j, d].rearrange("(k p) n -> p k n", p=128), writes=['w1c'])
                      S.dma('pool', a1c[:, :, d * 64:(d + 1) * 64], rwkv_a1[j, d].rearrange("(k p) n -> p k n", p=128), writes=['a1c'])
                  S.dma('pool', g1[:], rwkv_g1[j].rearrange("(k p) n -> p k n", p=128), writes=['g1'])
                  S.dma('pool', g2a[:], rwkv_g2[j, 0:128, :], writes=['g2a'])
                  S.dma('pool', g2b[:], rwkv_g2[j, 128:160, :], writes=['g2b'])
                  S.op('pool', lambda e: e.memset(w2p[:], 0.0), writes=['w2p'])
                  S.op('pool', lambda e: e.memset(a2p[:], 0.0), writes=['a2p'])
                  for d in range(2):
                      S.dma('pool', w2p[d * 64:(d + 1) * 64, d, :], rwkv_w2[j, d], writes=['w2p'])
                      S.dma('pool', a2p[d * 64:(d + 1) * 64, d, :], rwkv_a2[j, d], writes=['a2p'])
                  vres = j >= 1
                  if vres:
                      v1 = sb("v1", [128, 8, 32], BF16, st); v2 = sb("v2", [32, 1024], BF16, st)
                      S.dma('pool', v1[:], rwkv_v1[j - 1].rearrange("(k p) n -> p k n", p=128), writes=['v1'])
                      S.dma('pool', v2[:], rwkv_v2[j - 1], writes=['v2'])
                      vf = [sb(f"vf{q}", [128, N], F32, st) for q in range(2)]
                  dv_ = sb("dv", [128, 16], F32, st)
                  S.op('dve', lambda e: e.tensor_scalar(out=dv_[:, 0:8], in0=V(f"ka{j}"), scalar1=-1.0, scalar2=1.0, op0=ALU.mult, op1=ALU.add),
                       reads=['vecs'], writes=['dv'])
                  S.op('dve', lambda e: e.tensor_scalar(out=dv_[:, 8:16], in0=V(f"rk{j}"), scalar1=0.5, scalar2=None, op0=ALU.mult),
                       reads=['vecs'], writes=['dv'])
                  hx = sb("hx", [128, 8, N + 2], F32, st)
                  xx = sb("xx", [128, 8, N], F32, st)
                  xm = [sb(f"xm{m}", [128, 8, N], BF16, st) for m in range(6)]
                  lwm = sb("lwm", [128, N], BF16, st); am = sb("am", [128, N], BF16, st)
                  gma = sb("gma", [128, N], BF16, st); gmb = sb("gmb", [32, N], BF16, st); vm = sb("vm", [32, N], BF16, st)
                  T = {}
                  for nm in ["r32", "k32", "v32", "g32", "vv", "dvv", "kkc", "kk2", "rn", "kkn", "aneg", "sig", "lw", "al", "tk", "kd", "bb",
                             "Lp", "Lc", "E1", "E2", "E3", "t2", "At", "Rt", "Bt", "Kt", "kb", "t3", "bon", "vb"]:
                      T[nm] = [sb(f"f_{nm}{q}", [128, N], BF16 if nm in ("At", "Rt", "Bt", "Kt", "vb") else F32, st) for q in range(2)]
                  wct = [sb(f"wct{q}", [128, 4], F32, st) for q in range(2)]

                  def hb_(n):
                      return ps[n // 2][:, (n % 2) * 256:(n % 2) * 256 + 256]

                  def hk(n):
                      return ('psb', n // 2)

                  for ti, (t0, N_, mc) in enumerate(RT256):
                      src = HC[:, 0:258] if mc == 1 else HL[:, t0 - CTX:t0 - CTX + N + 2]
                      S.dma('sp', hx[:], src.rearrange("(k p) n -> p k n", p=128), writes=['hx'])
                      S.op('dve', lambda e: e.tensor_tensor(out=xx[:], in0=hx[:, :, 0:N], in1=hx[:, :, 2:N + 2], op=ALU.add), reads=['hx'], writes=['xx'])
                      S.op('dve', lambda e: e.scalar_tensor_tensor(out=xx[:], in0=xx[:], scalar=0.5, in1=hx[:, :, 1:N + 1], op0=ALU.mult, op1=ALU.subtract),
                           reads=['hx', 'xx'], writes=['xx'])
                      for m in range(6):
                          for kc in range(8):
                              S.op('dve', lambda e: e.scalar_tensor_tensor(out=xm[m][:, kc, :], in0=xx[:, kc, :], scalar=V(f"mix{j}", m * 8 + kc, m * 8 + kc + 1),
                                                                           in1=hx[:, kc, 1:N + 1], op0=ALU.mult, op1=ALU.add),
                                   reads=['hx', 'xx', 'vecs'], writes=[('xm', m)])
                      stg('a')
                      for kc in range(8):
                          S.op('pe', lambda e: e.matmul(hb_(0), lhsT=w1c[:, kc, :], rhs=xm[1][:, kc, :], start=(kc == 0), stop=(kc == 7)),
                               reads=['w1c', ('xm', 1)], writes=[hk(0)], inc=(kc == 7))
                      S.op('act', lambda e: e.activation(out=T["sig"][0][:], in_=hb_(0), func=AF.Sigmoid, scale=2.0), reads=[hk(0)], writes=['sig0'])
                      S.op('dve', lambda e: e.tensor_scalar(out=lwm[:], in0=T["sig"][0][:], scalar1=2.0, scalar2=-1.0, op0=ALU.mult, op1=ALU.add),
                           reads=['sig0'], writes=['lwm'])
                      for kc in range(8):
                          S.op('pe', lambda e: e.matmul(hb_(1), lhsT=a1c[:, kc, :], rhs=xm[4][:, kc, :], start=(kc == 0), stop=(kc == 7)),
                               reads=['a1c', ('xm', 4)], writes=[hk(1)], inc=(kc == 7))
                      S.op('act', lambda e: e.activation(out=am[:], in_=hb_(1), func=AF.Identity), reads=[hk(1)], writes=['am'])
                      for kc in range(8):
                          S.op('pe', lambda e: e.matmul(hb_(2), lhsT=g1[:, kc, 0:128], rhs=xm[5][:, kc, :], start=(kc == 0), stop=(kc == 7)),
                               reads=['g1', ('xm', 5)], writes=[hk(2)], inc=(kc == 7))
                      S.op('act', lambda e: e.activation(out=gma[:], in_=hb_(2), func=AF.Sigmoid), reads=[hk(2)], writes=['gma'])
                      for kc in range(8):
                          S.op('pe', lambda e: e.matmul(hb_(3)[0:32, :], lhsT=g1[:, kc, 128:160], rhs=xm[5][:, kc, :], start=(kc == 0), stop=(kc == 7)),
                               reads=['g1', ('xm', 5)], writes=[hk(3)], inc=(kc == 7))
                      S.op('act', lambda e: e.activation(out=gmb[:], in_=hb_(3)[0:32, :], func=AF.Sigmoid), reads=[hk(3)], writes=['gmb'])
                      if vres:
                          for kc in range(8):
                              S.op('pe', lambda e: e.matmul(hb_(4)[0:32, :], lhsT=v1[:, kc, :], rhs=xm[3][:, kc, :], start=(kc == 0), stop=(kc == 7)),
                                   reads=['v1', ('xm', 3)], writes=[hk(4)], inc=(kc == 7))
                          S.op('act', lambda e: e.activation(out=vm[:], in_=hb_(4)[0:32, :], func=AF.Identity), reads=[hk(4)], writes=['vm'])
                      def chunk_gen(c):
                          pc = c % 2
                          pcs = str(pc)
                          H = {5: 8 * pc, 6: 8 * pc + 1, 7: 8 * pc + 2, 8: 8 * pc + 3, 9: 8 * pc + 4, 10: 8 * pc + 5, 11: 8 * pc + 6, 12: 8 * pc + 7,
                               13: 8 * pc, 14: 8 * pc + 2, 15: 8 * pc + 3}
                          cs = slice(c * 128, (c + 1) * 128)
                          rows = slice(c * 128, (c + 1) * 128)
                          for (hbn, w_, m) in [(5, wr, 0), (6, wk, 2), (7, wv, 3)]:
                              for kc in range(8):
                                  S.op('pe', lambda e: e.matmul(hb_(H[hbn]), lhsT=w_[:, kc, cs], rhs=xm[m][:, kc, :], start=(kc == 0), stop=(kc == 7)),
                                       reads=[('xm', m), 'wr', 'wk', 'wv'], writes=[hk(H[hbn])], inc=(kc == 7))
                          S.op('pe', lambda e: e.matmul(hb_(H[8]), lhsT=g2a[:, cs], rhs=gma[:], start=True, stop=False), reads=['g2a', 'gma'], writes=[hk(H[8])], inc=False)
                          S.op('pe', lambda e: e.matmul(hb_(H[8]), lhsT=g2b[:, cs], rhs=gmb[:], start=False, stop=True), reads=['g2b', 'gmb'], writes=[hk(H[8])])
                          for d in range(2):
                              S.op('pe', lambda e: e.matmul(hb_(H[9 + d]), lhsT=w2p[:, d, cs], rhs=lwm[:], start=True, stop=True), reads=['w2p', 'lwm'], writes=[hk(H[9 + d])])
                              S.op('pe', lambda e: e.matmul(hb_(H[11 + d]), lhsT=a2p[:, d, cs], rhs=am[:], start=True, stop=True), reads=['a2p', 'am'], writes=[hk(H[11 + d])])
                          yield
                          S.op('act', lambda e: e.activation(out=T["r32"][pc][:], in_=hb_(H[5]), func=AF.Identity), reads=[hk(H[5])], writes=['r32' + pcs])
                          S.op('act', lambda e: e.activation(out=T["k32"][pc][:], in_=hb_(H[6]), func=AF.Identity), reads=[hk(H[6])], writes=['k32' + pcs])
                          S.op('act', lambda e: e.activation(out=T["v32"][pc][:], in_=hb_(H[7]), func=AF.Identity), reads=[hk(H[7])], writes=['v32' + pcs])
                          S.op('act', lambda e: e.activation(out=T["g32"][pc][:], in_=hb_(H[8]), func=AF.Identity), reads=[hk(H[8])], writes=['g32' + pcs])
                          if vres:
                              S.op('pe', lambda e: e.matmul(hb_(H[13]), lhsT=v2[:, cs], rhs=vm[:], start=True, stop=True), reads=['v2', 'vm'], writes=[hk(H[13])])
                              S.dma('sp', vf[pc][:], VT[0][rows, t0:t0 + N], writes=['vf' + pcs])
                              S.op('act', lambda e: e.activation(out=T["vv"][pc][:], in_=hb_(H[13]), func=AF.Sigmoid, bias=V(f"v0{j}", c, c + 1), scale=1.0),
                                   reads=[hk(H[13]), 'vecs'], writes=['vv' + pcs])
                              S.op('pool', lambda e: e.tensor_tensor(out=T["dvv"][pc][:], in0=vf[pc][:], in1=T["v32"][pc][:], op=ALU.subtract), reads=['vf' + pcs, 'v32' + pcs], writes=['dvv' + pcs])
                              S.op('pool', lambda e: e.tensor_tensor(out=T["dvv"][pc][:], in0=T["dvv"][pc][:], in1=T["vv"][pc][:], op=ALU.mult), reads=['dvv' + pcs, 'vv' + pcs], writes=['dvv' + pcs])
                              S.op('pool', lambda e: e.tensor_tensor(out=T["v32"][pc][:], in0=T["v32"][pc][:], in1=T["dvv"][pc][:], op=ALU.add), reads=['dvv' + pcs, 'v32' + pcs], writes=['v32' + pcs])
                          yield
                          S.op('dve', lambda e: e.tensor_scalar(out=T["kkc"][pc][:], in0=T["k32"][pc][:], scalar1=V(f"kk{j}", c, c + 1), scalar2=None, op0=ALU.mult),
                               reads=['k32' + pcs, 'vecs'], writes=['kkc' + pcs])
                          S.op('pool', lambda e: e.tensor_tensor(out=T["kk2"][pc][:], in0=T["kkc"][pc][:], in1=T["kkc"][pc][:], op=ALU.mult), reads=['kkc' + pcs], writes=['kk2' + pcs])
                          S.op('pe', lambda e: e.matmul(hb_(H[14]), lhsT=blockones, rhs=T["kk2"][pc][:], start=True, stop=True), reads=['kk2' + pcs, 'c32'], writes=[hk(H[14])])
                          yield
                          S.op('act', lambda e: e.activation(out=T["rn"][pc][:], in_=hb_(H[14]), func=AF.Sqrt), reads=[hk(H[14])], writes=['rn' + pcs])
                          S.op('dve', lambda e: e.tensor_scalar(out=T["rn"][pc][:], in0=T["rn"][pc][:], scalar1=1e-12, scalar2=None, op0=ALU.max), reads=['rn' + pcs], writes=['rn' + pcs])
                          S.op('dve', lambda e: e.reciprocal(out=T["rn"][pc][:], in_=T["rn"][pc][:]), reads=['rn' + pcs], writes=['rn' + pcs])
                          yield
                          S.op('pool', lambda e: e.tensor_tensor(out=T["kkn"][pc][:], in0=T["kkc"][pc][:], in1=T["rn"][pc][:], op=ALU.mult), reads=['kkc' + pcs, 'rn' + pcs], writes=['kkn' + pcs])
                          S.op('pool', lambda e: e.tensor_scalar(out=T["aneg"][pc][:], in0=T["kkn"][pc][:], scalar1=-1.0, scalar2=None, op0=ALU.mult), reads=['kkn' + pcs], writes=['aneg' + pcs])
                          for d in range(2):
                              yield
                              S.op('act', lambda e: e.activation(out=T["sig"][pc][:], in_=hb_(H[9 + d]), func=AF.Sigmoid, bias=V(f"w0{j}", d * 8 + c, d * 8 + c + 1), scale=1.0),
                                   reads=[hk(H[9 + d]), 'vecs'], writes=['sig' + pcs])
                              S.op('pool', lambda e: e.tensor_scalar(out=T["lw"][pc][:], in0=T["sig"][pc][:], scalar1=float(-np.exp(-0.5)), scalar2=None, op0=ALU.mult),
                                   reads=['sig' + pcs], writes=['lw' + pcs])
                              S.op('act', lambda e: e.activation(out=T["al"][pc][:], in_=hb_(H[11 + d]), func=AF.Sigmoid, bias=V(f"a0{j}", d * 8 + c, d * 8 + c + 1), scale=1.0),
                                   reads=[hk(H[11 + d]), 'vecs'], writes=['al' + pcs])
                              yield
                              S.op('dve', lambda e: e.tensor_scalar(out=T["tk"][pc][:], in0=T["al"][pc][:], scalar1=V(f"ka{j}", c, c + 1), scalar2=dv_[:, c:c + 1],
                                                                    op0=ALU.mult, op1=ALU.add), reads=['al' + pcs, 'vecs', 'dv'], writes=['tk' + pcs])
                              S.op('pool', lambda e: e.tensor_tensor(out=T["kd"][pc][:], in0=T["k32"][pc][:], in1=T["tk"][pc][:], op=ALU.mult), reads=['k32' + pcs, 'tk' + pcs], writes=['kd' + pcs])
                              S.op('pool', lambda e: e.tensor_tensor(out=T["bb"][pc][:], in0=T["kkn"][pc][:], in1=T["al"][pc][:], op=ALU.mult), reads=['kkn' + pcs, 'al' + pcs], writes=['bb' + pcs])
                              yield
                              S.op('dve', lambda e: e.tensor_tensor_scan(out=T["Lp"][pc][:], data0=cmask[:, :N], data1=T["lw"][pc][:], initial=0.0, op0=ALU.mult, op1=ALU.add),
                                   reads=['lw' + pcs, 'c32'], writes=['Lp' + pcs])
                              if d == 0:
                                  Lc, kLc = T["Lp"][pc], 'Lp' + pcs
                              else:
                                  S.op('pool', lambda e: e.tensor_tensor(out=T["Lc"][pc][:], in0=T["lw"][pc][:], in1=T["Lp"][pc][:], op=ALU.subtract), reads=['lw' + pcs, 'Lp' + pcs], writes=['Lc' + pcs])
                                  S.op('pool', lambda e: e.tensor_tensor(
                                      out=T["Lc"][pc][:].rearrange("p (c t) -> p c t", t=64), in0=T["Lc"][pc][:].rearrange("p (c t) -> p c t", t=64),
                                      in1=T["Lp"][pc][:].rearrange("p (c t) -> p c t", t=64)[:, :, 63:64].to_broadcast([128, N // 64, 64]), op=ALU.add),
                                      reads=['Lc' + pcs, 'Lp' + pcs], writes=['Lc' + pcs])
                                  Lc, kLc = T["Lc"][pc], 'Lc' + pcs
                              yield
                              S.op('act', lambda e: e.activation(out=T["E1"][pc][:], in_=Lc[:], func=AF.Exp), reads=[kLc], writes=['E1' + pcs])
                              S.op('act', lambda e: e.activation(out=T["E2"][pc][:], in_=Lc[:], func=AF.Exp, scale=-1.0), reads=[kLc], writes=['E2' + pcs])
                              S.op('pool', lambda e: e.tensor_tensor(out=T["t2"][pc][:], in0=Lc[:], in1=T["lw"][pc][:], op=ALU.subtract), reads=[kLc, 'lw' + pcs], writes=['t2' + pcs])
                              S.op('act', lambda e: e.activation(out=T["E3"][pc][:], in_=T["t2"][pc][:], func=AF.Exp), reads=['t2' + pcs], writes=['E3' + pcs])
                              yield
                              S.op('pool', lambda e: e.tensor_tensor(out=T["At"][pc][:], in0=T["aneg"][pc][:], in1=T["E3"][pc][:], op=ALU.mult), reads=['aneg' + pcs, 'E3' + pcs], writes=['At' + pcs])
                              S.op('pool', lambda e: e.tensor_tensor(out=T["Rt"][pc][:], in0=T["r32"][pc][:], in1=T["E1"][pc][:], op=ALU.mult), reads=['r32' + pcs, 'E1' + pcs], writes=['Rt' + pcs])
                              S.op('dve', lambda e: e.tensor_tensor(out=T["Bt"][pc][:], in0=T["bb"][pc][:], in1=T["E2"][pc][:], op=ALU.mult), reads=['bb' + pcs, 'E2' + pcs], writes=['Bt' + pcs])
                              S.op('dve', lambda e: e.tensor_tensor(out=T["Kt"][pc][:], in0=T["kd"][pc][:], in1=T["E2"][pc][:], op=ALU.mult), reads=['kd' + pcs, 'E2' + pcs], writes=['Kt' + pcs])
                              wcol = 63 if d == 0 else 0
                              S.op('dve', lambda e: e.tensor_copy(out=wct[pc][:, 0:N // 64], in_=T["E1"][pc][:].rearrange("p (c t) -> p c t", t=64)[:, :, wcol]),
                                   reads=['E1' + pcs], writes=['wct' + pcs])
                              yield
                              S.dma('sp', ATd[d][rows, t0:t0 + N], T["At"][pc][:], reads=['At' + pcs], writes=['ATd'])
                              S.dma('sp', RTd[d][rows, t0:t0 + N], T["Rt"][pc][:], reads=['Rt' + pcs], writes=['RTd'])
                              S.dma('sp', BTd[d][rows, t0:t0 + N], T["Bt"][pc][:], reads=['Bt' + pcs], writes=['BTd'])
                              S.dma('sp', KTd[d][rows, t0:t0 + N], T["Kt"][pc][:], reads=['Kt' + pcs], writes=['KTd'])
                              S.dma('sp', WCd[d][rows, t0 // 64:t0 // 64 + N // 64], wct[pc][:, 0:N // 64], reads=['wct' + pcs], writes=['WCd'])
                              if d == 0:
                                  S.op('pool', lambda e: e.tensor_copy(out=T["kb"][pc][:], in_=T["kd"][pc][:]), reads=['kd' + pcs], writes=['kb' + pcs])
                              else:
                                  S.op('pool', lambda e: e.tensor_tensor(out=T["kb"][pc][:], in0=T["kb"][pc][:], in1=T["kd"][pc][:], op=ALU.add), reads=['kd' + pcs, 'kb' + pcs], writes=['kb' + pcs])
                          yield
                          S.op('pool', lambda e: e.tensor_tensor(out=T["t3"][pc][:], in0=T["r32"][pc][:], in1=T["kb"][pc][:], op=ALU.mult), reads=['r32' + pcs, 'kb' + pcs], writes=['t3' + pcs])
                          S.op('dve', lambda e: e.tensor_scalar(out=T["t3"][pc][:], in0=T["t3"][pc][:], scalar1=dv_[:, 8 + c:9 + c], scalar2=None, op0=ALU.mult),
                               reads=['t3' + pcs, 'dv'], writes=['t3' + pcs])
                          S.op('pe', lambda e: e.matmul(hb_(H[15]), lhsT=blockones, rhs=T["t3"][pc][:], start=True, stop=True), reads=['t3' + pcs, 'c32'], writes=[hk(H[15])])
                          yield
                          S.op('dve', lambda e: e.tensor_tensor(out=T["bon"][pc][:], in0=hb_(H[15]), in1=T["v32"][pc][:], op=ALU.mult), reads=[hk(H[15]), 'v32' + pcs], writes=['bon' + pcs])
                          S.dma('sp', BON[rows, t0:t0 + N], T["bon"][pc][:], reads=['bon' + pcs], writes=['BON'])
                          S.dma('sp', VT[j][rows, t0:t0 + N], T["v32"][pc][:], reads=['v32' + pcs], writes=['VT'])
                          S.op('act', lambda e: e.activation(out=T["vb"][pc][:], in_=T["v32"][pc][:], func=AF.Identity), reads=['v32' + pcs], writes=['vb' + pcs])
                          S.dma('sp', VTb[rows, t0:t0 + N], T["vb"][pc][:], reads=['vb' + pcs], writes=['VTb'])
                          S.dma('sp', GG[rows, t0:t0 + N], T["g32"][pc][:], reads=['g32' + pcs], writes=['GG'])

                      pend = [chunk_gen(c) for c in range(8)]
                      act_g = []
                      while pend or act_g:
                          while pend and len(act_g) < int(os.environ.get("R2WIN", "2")):
                              act_g.append(pend.pop(0))
                          for g_ in list(act_g):
                              try:
                                  next(g_)
                              except StopIteration:
                                  act_g.remove(g_)
            except _Stop:
                pass
            S.mute = False
            S.barrier()
            if os.environ.get('RSTOP') == '2':
                return
            if True:
                with contextlib.ExitStack() as st:
                    def stream(d, hh):
                        order = ([0, 1, 2, 3] + list(range(4, NCH))) if d == 0 else ([3, 2, 1, 0] + list(range(NCH - 1, 3, -1)))
                        mAR = maskAR_f if d == 0 else maskAR_r
                        mN = maskN_f if d == 0 else maskN_r
                        sid = 2 * d + hh
                        P_ = f"s{sid}_"
                        pb = [ps[2 * sid], ps[2 * sid + 1]]

                        def pk(bank, half=None):
                            return [('sps', sid, bank)]
                        BD = {n: sb(P_ + n, [128, 4, 128], BF16, st) for n in ["bdA", "T2", "T3", "T4", "tbB", "tbK", "tbV", "T8", "bdMak", "bdU", "bdST"]}
                        for n, t_ in BD.items():
                            S.op('pool', lambda e: e.memset(t_[:], 0.0), writes=[P_ + n])
                        AR = [sb(P_ + f"AR{b}", [128, 4, 2, 64], BF16, st) for b in range(2)]
                        Bi = [sb(P_ + f"Bi{b}", [128, 4, 64], BF16, st) for b in range(2)]
                        Ki = [sb(P_ + f"Ki{b}", [128, 4, 64], BF16, st) for b in range(2)]
                        Vi = [sb(P_ + f"Vi{b}", [128, 4, 64], BF16, st) for b in range(2)]
                        WCall = sb(P_ + "WCall", [128, 4, NCH], F32, st)
                        S.dma('sp', WCall[:], WCd[d][hh * 512:hh * 512 + 512, :].rearrange("(c p) n -> p c n", p=128), writes=[P_ + "WCall"])
                        ARm = sb(P_ + "ARm", [128, 4, 128], BF16, st)
                        AKm = sb(P_ + "AKm", [128, 4, 128], BF16, st)
                        Ns = sb(P_ + "Ns", [128, 4, 64], BF16, st)
                        Vs = sb(P_ + "Vs", [128, 4, 64], BF16, st)
                        PG = [sb(P_ + f"PG{b}", [128, 4, 128], BF16, st) for b in range(2)]
                        Pst = [sb(P_ + f"Pst{b}", [128, 4, 64], BF16, st) for b in range(2)]
                        Xs = sb(P_ + "Xs", [128, 4, 64], BF16, st); Us = sb(P_ + "Us", [128, 4, 64], BF16, st)
                        Yb = sb(P_ + "Yb", [128, 4, 64], F32, st); STs = sb(P_ + "STs", [128, 4, 64], F32, st)
                        tmpS = sb(P_ + "tmpS", [128, 4, 64], F32, st)
                        STb = sb(P_ + "STb", [128, 4, 64], BF16, st)
                        S.op('pool', lambda e: e.memset(STb[:], 0.0), writes=[P_ + "STb"])
                        S.op('pool', lambda e: e.memset(STs[:], 0.0), writes=[P_ + "STs"])
                        r0 = hh * 512

                        def diag(eng, dst, kdst, src, ksrc, cols=64):
                            for half in range(2):
                                pslice = slice(half * 64, half * 64 + 64)
                                if eng == 'act':
                                    S.op('act', lambda e: e.activation(out=dst[pslice, :, half * 64:half * 64 + 64], in_=src[pslice], func=AF.Identity),
                                         reads=ksrc, writes=[kdst])
                                else:
                                    S.op(eng, lambda e: e.tensor_copy(out=dst[pslice, :, half * 64:half * 64 + 64], in_=src[pslice]),
                                         reads=ksrc, writes=[kdst])

                        def load(n):
                            g = order[n]
                            b = n % 2
                            cols = slice(g * 64, g * 64 + 64)
                            kin = P_ + f"in{b}"
                            for (dst, srcd) in [(AR[b][:, :, 0, :], ATd[d]), (AR[b][:, :, 1, :], RTd[d]), (Bi[b][:], BTd[d]), (Ki[b][:], KTd[d]), (Vi[b][:], VTb)]:
                                S.dma('sp', dst, srcd[r0:r0 + 512, cols].rearrange("(c p) n -> p c n", p=128), writes=[kin])

                        load(0)
                        for n in range(NCH):
                            g = order[n]
                            b = n % 2
                            kin = P_ + f"in{b}"
                            if n + 1 < NCH:
                                load(n + 1)
                            diag('dve', BD["bdA"], P_ + "bdA", AR[b][:, :, 0, :], [kin])
                            diag('dve', BD["T2"], P_ + "T2", Bi[b], [kin])
                            diag('act', BD["T3"], P_ + "T3", Ki[b], [kin])
                            diag('act', BD["T4"], P_ + "T4", Vi[b], [kin])
                            ARf = AR[b][:].rearrange("p c a t -> p c (a t)")
                            for hp in range(4):
                                S.op('pe', lambda e: e.matmul(pb[0][:, hp * 128:(hp + 1) * 128], lhsT=BD["T2"][:, hp, :], rhs=ARf[:, hp, :], start=True, stop=True),
                                     reads=[P_ + "T2", kin], writes=pk(0), inc=(hp == 3))
                            for hp in range(4):
                                S.op('pe', lambda e: e.matmul(pb[1][:, hp * 64:(hp + 1) * 64], lhsT=BD["bdA"][:, hp, :], rhs=Bi[b][:, hp, :], start=True, stop=True),
                                     reads=[P_ + "bdA", kin], writes=pk(1), inc=(hp == 3))
                            yield
                            S.op('dve', lambda e: e.tensor_tensor(out=ARm[:], in0=pb[0][:].rearrange("p (c t) -> p c t", t=128),
                                                                  in1=mAR.unsqueeze(1).to_broadcast([128, 4, 128]), op=ALU.mult),
                                 reads=pk(0) + ['c32'], writes=[P_ + "ARm"])
                            S.op('dve', lambda e: e.tensor_tensor(out=Pst[0][:], in0=pb[1][:, 0:256].rearrange("p (c t) -> p c t", t=64),
                                                                  in1=mN.unsqueeze(1).to_broadcast([128, 4, 64]), op=ALU.mult),
                                 reads=pk(1) + ['c32'], writes=[P_ + "Pst0"])
                            for hp in range(4):
                                S.op('pe', lambda e: e.matmul(pb[0][:, hp * 128:(hp + 1) * 128], lhsT=BD["T3"][:, hp, :], rhs=ARf[:, hp, :], start=True, stop=True),
                                     reads=[P_ + "T3", kin], writes=pk(0), inc=(hp == 3))
                            for hp in range(4):
                                S.op('pe', lambda e: e.matmul(pb[1][:, hp * 128:(hp + 1) * 128], lhsT=BD["T2"][:, hp, :], rhs=ident_bf[:], start=True, stop=True),
                                     reads=[P_ + "T2", 'ident_bf'], writes=pk(1), inc=(hp == 3))
                            yield
                            S.op('dve', lambda e: e.tensor_tensor(out=AKm[:], in0=pb[0][:].rearrange("p (c t) -> p c t", t=128),
                                                                  in1=mAR.unsqueeze(1).to_broadcast([128, 4, 128]), op=ALU.mult),
                                 reads=pk(0) + ['c32'], writes=[P_ + "AKm"])
                            S.op('act', lambda e: e.activation(out=BD["tbB"][:], in_=pb[1][:].rearrange("p (c t) -> p c t", t=128), func=AF.Identity),
                                 reads=pk(1), writes=[P_ + "tbB"])
                            for hp in range(4):
                                S.op('pe', lambda e: e.matmul(pb[0][:, hp * 128:(hp + 1) * 128], lhsT=BD["T3"][:, hp, :], rhs=ident_bf[:], start=True, stop=True),
                                     reads=[P_ + "T3", 'ident_bf'], writes=pk(0), inc=(hp == 3))
                            for hp in range(4):
                                S.op('pe', lambda e: e.matmul(pb[1][:, hp * 128:(hp + 1) * 128], lhsT=BD["T4"][:, hp, :], rhs=ident_bf[:], start=True, stop=True),
                                     reads=[P_ + "T4", 'ident_bf'], writes=pk(1), inc=(hp == 3))
                            S.op('pool', lambda e: e.tensor_copy(out=PG[0][:, :, 0:64], in_=ARm[:, :, 0:64]), reads=[P_ + "ARm"], writes=[P_ + "PG0"])
                            S.op('pool', lambda e: e.tensor_copy(out=PG[0][:, :, 64:128], in_=SI.unsqueeze(1).to_broadcast([128, 4, 64])),
                                 reads=['c32'], writes=[P_ + "PG0"])
                            diag('pool', BD["bdMak"], P_ + "bdMak", AKm[:, :, 0:64], [P_ + "AKm"])
                            yield
                            S.op('act', lambda e: e.activation(out=BD["tbK"][:], in_=pb[0][:].rearrange("p (c t) -> p c t", t=128), func=AF.Identity),
                                 reads=pk(0), writes=[P_ + "tbK"])
                            S.op('act', lambda e: e.activation(out=BD["tbV"][:], in_=pb[1][:].rearrange("p (c t) -> p c t", t=128), func=AF.Identity),
                                 reads=pk(1), writes=[P_ + "tbV"])
                            for half in range(2):
                                pslice = slice(half * 64, half * 64 + 64)
                                S.op('pool', lambda e: e.tensor_copy(out=Vs[pslice], in_=BD["tbV"][pslice, :, half * 64:half * 64 + 64]),
                                     reads=[P_ + "tbV"], writes=[P_ + "Vs"])
                            diag('pool', BD["T2"], P_ + "T2", Pst[0], [P_ + "Pst0"])
                            diag('pool', BD["T3"], P_ + "T3", PG[0][:, :, 0:64], [P_ + "PG0"])
                            sets = [("T2", "T3"), ("T4", "T8")]
                            for lv in range(6):
                                cur, nxt = lv % 2, (lv + 1) % 2
                                bP, bPT = sets[cur]
                                nP, nPT = sets[nxt]
                                kPG, kPGn = P_ + f"PG{cur}", P_ + f"PG{nxt}"
                                if lv < 5:
                                    for hp in range(4):
                                        S.op('pe', lambda e: e.matmul(pb[1][:, hp * 64:(hp + 1) * 64], lhsT=BD[bPT][:, hp, :], rhs=Pst[cur][:, hp, :], start=True, stop=True),
                                             reads=[P_ + bPT, P_ + f"Pst{cur}"], writes=pk(1, 0), inc=(hp == 3))
                                    for hp in range(4):
                                        S.op('pe', lambda e: e.matmul(pb[0][:, hp * 128:(hp + 1) * 128], lhsT=BD[bP][:, hp, :], rhs=PG[cur][:, hp, :], start=True, stop=True),
                                             reads=[P_ + bP, kPG], writes=pk(0), inc=(hp == 3))
                                else:
                                    for hp in range(4):
                                        S.op('pe', lambda e: e.matmul(pb[0][:, hp * 128 + 64:(hp + 1) * 128], lhsT=BD[bP][:, hp, :], rhs=PG[cur][:, hp, 64:128], start=True, stop=True),
                                             reads=[P_ + bP, kPG], writes=pk(0), inc=(hp == 3))
                                yield
                                psv = pb[0][:].rearrange("p (c t) -> p c t", t=128)
                                S.op('dve', lambda e: e.tensor_tensor(out=PG[nxt][:, :, 64:128], in0=PG[cur][:, :, 64:128], in1=psv[:, :, 64:128], op=ALU.add),
                                     reads=pk(0) + [kPG], writes=[kPGn])
                                if lv < 5:
                                    S.op('act', lambda e: e.activation(out=PG[nxt][:, :, 0:64], in_=psv[:, :, 0:64], func=AF.Identity), reads=pk(0), writes=[kPGn])
                                    S.op('dve', lambda e: e.tensor_copy(out=Pst[nxt][:], in_=pb[1][:, 0:256].rearrange("p (c t) -> p c t", t=64)),
                                         reads=pk(1, 0), writes=[P_ + f"Pst{nxt}"])
                                    diag('act', BD[nP], P_ + nP, Pst[nxt], [P_ + f"Pst{nxt}"])
                                    if lv < 4:
                                        diag('pool', BD[nPT], P_ + nPT, PG[nxt][:, :, 0:64], [kPGn])
                            diag('pool', BD["T8"], P_ + "T8", PG[0][:, :, 64:128], [P_ + "PG0"])
                            for hp in range(4):
                                S.op('pe', lambda e: e.matmul(pb[1][:, 256 + hp * 64:256 + (hp + 1) * 64], lhsT=BD["bdA"][:, hp, :], rhs=STb[:, hp, :], start=True, stop=False),
                                     reads=[P_ + "bdA", P_ + "STb"], writes=pk(1), inc=False)
                                S.op('pe', lambda e: e.matmul(pb[1][:, 256 + hp * 64:256 + (hp + 1) * 64], lhsT=BD["bdMak"][:, hp, :], rhs=Vs[:, hp, :], start=False, stop=True),
                                     reads=[P_ + "bdMak", P_ + "Vs"], writes=pk(1), inc=(hp == 3))
                            yield
                            S.op('act', lambda e: e.activation(out=Xs[:], in_=pb[1][:, 256:512].rearrange("p (c t) -> p c t", t=64), func=AF.Identity),
                                 reads=pk(1), writes=[P_ + "Xs"])
                            for hp in range(4):
                                S.op('pe', lambda e: e.matmul(pb[0][:, hp * 64:(hp + 1) * 64], lhsT=BD["T8"][:, hp, :], rhs=Xs[:, hp, :], start=True, stop=True),
                                     reads=[P_ + "T8", P_ + "Xs"], writes=pk(0), inc=(hp == 3))
                            yield
                            S.op('dve', lambda e: e.tensor_copy(out=Us[:], in_=pb[0][:, 0:256].rearrange("p (c t) -> p c t", t=64)), reads=pk(0), writes=[P_ + "Us"])
                            diag('act', BD["bdU"], P_ + "bdU", pb[0][:, 0:256].rearrange("p (c t) -> p c t", t=64), pk(0))
                            for hp in range(4):
                                S.op('pe', lambda e: e.matmul(pb[0][:, 256 + hp * 64:256 + (hp + 1) * 64], lhsT=BD["bdST"][:, hp, :], rhs=AR[b][:, hp, 1, :], start=True, stop=False),
                                     reads=[P_ + "bdST", kin], writes=pk(0), inc=False)
                                S.op('pe', lambda e: e.matmul(pb[0][:, 256 + hp * 64:256 + (hp + 1) * 64], lhsT=BD["bdU"][:, hp, :], rhs=ARm[:, hp, 64:128], start=False, stop=False),
                                     reads=[P_ + "bdU", P_ + "ARm"], writes=pk(0), inc=False)
                                S.op('pe', lambda e: e.matmul(pb[0][:, 256 + hp * 64:256 + (hp + 1) * 64], lhsT=BD["tbV"][:, hp, :], rhs=AKm[:, hp, 64:128], start=False, stop=True),
                                     reads=[P_ + "tbV", P_ + "AKm"], writes=pk(0), inc=(hp == 3))
                            for hp in range(4):
                                S.op('pe', lambda e: e.matmul(pb[1][:, 256 + hp * 64:256 + (hp + 1) * 64], lhsT=BD["tbB"][:, hp, :], rhs=Us[:, hp, :], start=True, stop=False),
                                     reads=[P_ + "tbB", P_ + "Us"], writes=pk(1, 1), inc=False)
                                S.op('pe', lambda e: e.matmul(pb[1][:, 256 + hp * 64:256 + (hp + 1) * 64], lhsT=BD["tbK"][:, hp, :], rhs=Vs[:, hp, :], start=False, stop=True),
                                     reads=[P_ + "tbK", P_ + "Vs"], writes=pk(1, 1), inc=(hp == 3))
                            yield
                            S.op('act', lambda e: e.activation(out=Yb[:], in_=pb[0][:, 256:512].rearrange("p (c t) -> p c t", t=64), func=AF.Identity),
                                 reads=pk(0), writes=[P_ + "Yb"])
                            S.dma('sp', YD[d][r0:r0 + 512, g * 64:g * 64 + 64].rearrange("(c p) n -> p c n", p=128), Yb[:], reads=[P_ + "Yb"], writes=['YD'])
                            S.op('dve', lambda e: e.tensor_tensor(out=tmpS[:], in0=pb[1][:, 256:512].rearrange("p (c t) -> p c t", t=64), in1=STs[:], op=ALU.add),
                                 reads=pk(1, 1) + [P_ + "STs"], writes=[P_ + "tmpS"])
                            S.op('dve', lambda e: e.tensor_tensor(out=STs[:], in0=tmpS[:], in1=WCall[:, :, g:g + 1].to_broadcast([128, 4, 64]), op=ALU.mult),
                                 reads=[P_ + "tmpS", P_ + "WCall"], writes=[P_ + "STs"])
                            diag('pool', BD["bdST"], P_ + "bdST", STs, [P_ + "STs"])
                            S.op('act', lambda e: e.activation(out=STb[:], in_=STs[:], func=AF.Identity), reads=[P_ + "STs"], writes=[P_ + "STb"])
                            yield

                    gens = [stream(0, 0), stream(0, 1), stream(1, 0), stream(1, 1)]
                    alive = [True] * 4
                    while any(alive):
                        for q in range(4):
                            if alive[q]:
                                try:
                                    next(gens[q])
                                except StopIteration:
                                    alive[q] = False
                S.barrier()
            if os.environ.get('RSTOP') == '3':
                return
            with contextlib.ExitStack() as st:
                wo = sb("r_wo", [128, 8, 1024], BF16, st)
                S.dma('pool', wo[:], rwkv_wo[j].rearrange("(k p) n -> p k n", p=128), writes=['r_wo'])
                y0 = sb("r_y0", [128, 8, 512], F32, st); y1 = sb("r_y1", [128, 8, 512], F32, st)
                bo = sb("r_bo", [128, 8, 512], F32, st); gg = sb("r_gg", [128, 8, 512], F32, st)
                xt = sb("r_xt", [128, 8, 512], F32, st)
                ob = sb("r_ob", [128, 8, 512], BF16, st)
                yc = sb("r_yc", [128, 512], F32, st); y2 = sb("r_y2", [128, 512], F32, st); sd = sb("r_sd", [128, 512], F32, st)
                tiles = TILES[1:] if last else TILES
                for ti, (t0, N, mc) in enumerate(tiles):
                    for (dst, srcd, kk_) in [(y0, YD[0], 'y0'), (y1, YD[1], 'y1'), (bo, BON, 'bo'), (gg, GG, 'gg'), (xt, XS, 'r_xt')]:
                        S.dma('sp', dst[:, :, :N], srcd[:, t0:t0 + N].rearrange("(k p) n -> p k n", p=128), writes=[kk_])
                    for c in range(8):
                        pa, pv = c % 2, 2 + c % 2
                        S.op('dve', lambda e: e.tensor_tensor(out=y0[:, c, :N], in0=y0[:, c, :N], in1=y1[:, c, :N], op=ALU.add), reads=['y0', 'y1'], writes=['y0'])
                        S.op('pe', lambda e: e.matmul(ps[pa][:, :N], lhsT=blockones, rhs=y0[:, c, :N], start=True, stop=True), reads=['y0', 'c32'], writes=[('ps', pa)])
                        S.op('dve', lambda e: e.scalar_tensor_tensor(out=yc[:, :N], in0=ps[pa][:, :N], scalar=-1.0 / 64, in1=y0[:, c, :N], op0=ALU.mult, op1=ALU.add),
                             reads=[('ps', pa), 'y0'], writes=['yc'])
                        S.op('pool', lambda e: e.tensor_tensor(out=y2[:, :N], in0=yc[:, :N], in1=yc[:, :N], op=ALU.mult), reads=['yc'], writes=['y2'])
                        S.op('pe', lambda e: e.matmul(ps[pv][:, :N], lhsT=blockones, rhs=y2[:, :N], start=True, stop=True), reads=['y2', 'c32'], writes=[('ps', pv)])
                        S.op('act', lambda e: e.activation(out=sd[:, :N], in_=ps[pv][:, :N], func=AF.Sqrt, bias=epsT[:, 5:6], scale=1.0 / 64),
                             reads=[('ps', pv), 'epsT'], writes=['sd'])
                        S.op('dve', lambda e: e.reciprocal(out=sd[:, :N], in_=sd[:, :N]), reads=['sd'], writes=['sd'])
                        S.op('pool', lambda e: e.tensor_tensor(out=yc[:, :N], in0=yc[:, :N], in1=sd[:, :N], op=ALU.mult), reads=['yc', 'sd'], writes=['yc'])
                        S.op('dve', lambda e: e.tensor_scalar(out=yc[:, :N], in0=yc[:, :N], scalar1=V(f"lnw{j}", c, c + 1), scalar2=V(f"lnb{j}", c, c + 1),
                                                              op0=ALU.mult, op1=ALU.add), reads=['yc', 'vecs'], writes=['yc'])
                        S.op('pool', lambda e: e.tensor_tensor(out=yc[:, :N], in0=yc[:, :N], in1=bo[:, c, :N], op=ALU.add), reads=['yc', 'bo'], writes=['yc'])
                        S.op('pool', lambda e: e.tensor_tensor(out=ob[:, c, :N], in0=yc[:, :N], in1=gg[:, c, :N], op=ALU.mult), reads=['yc', 'gg'], writes=['ob'])
                    for oc in range(8):
                        pb_ = 4 + oc % 4
                        for kc in range(8):
                            S.op('pe', lambda e: e.matmul(ps[pb_][:, :N], lhsT=wo[:, kc, oc * 128:(oc + 1) * 128], rhs=ob[:, kc, :N], start=(kc == 0), stop=(kc == 7)),
                                 reads=['r_wo', 'ob'], writes=[('ps', pb_)], inc=(kc == 7))
                        S.op('dve', lambda e: e.scalar_tensor_tensor(out=xt[:, oc, :N], in0=ps[pb_][:, :N], scalar=modt[:, i, 16 + oc, mc:mc + 1],
                                                                     in1=xt[:, oc, :N], op0=ALU.mult, op1=ALU.add),
                             reads=[('ps', pb_), 'r_xt', 'modt'], writes=['r_xt'])
                    S.dma('sp', XS[:, t0:t0 + N].rearrange("(k p) n -> p k n", p=128), xt[:, :, :N], reads=['r_xt'], writes=['XS'])
            S.barrier()

        def phase_ffn(i, last):
            moe = (i % 2 == 1)
            k = i // 2
            E = NE if moe else 1
            F = DFE if moe else DFF
            nchunk = F // 128
            blocks = [(c0, min(4, nchunk - c0)) for c0 in range(0, nchunk, 4)]
            tiles = TILES[1:] if last else TILES
            groups = [tiles[0:len(tiles) - 6], tiles[-6:-3], tiles[-3:]]
            for gi, grp in enumerate(groups):
                Sg = sum(t[1] for t in grp)
                offs = [sum(t[1] for t in grp[:a]) for a in range(len(grp))]
                with contextlib.ExitStack() as st:
                    hb = sb("f_hb", [128, 8, 1536], BF16, st)
                    yacc = sb("f_yacc", [128, 8, 1536], F32, st)
                    GT = sb("f_GT", [8, 1536], F32, st)
                    gbc = sb("f_gbc", [128, 1536], F32, st)
                    with contextlib.ExitStack() as st2:
                        xt = [sb(f"f_xt{b}", [128, 8, 512], F32, st2) for b in range(2)]
                        sq = sb("f_sq", [128, 8, 512], F32, st2)
                        rs = sb("f_rs", [128, 512], F32, st2)
                        if moe:
                            rt = sb("f_rt", [128, 8, 8], F32, st2)
                            S.dma('sp', rt[:], moe_router[k].rearrange("(k p) n -> p k n", p=128), writes=['rt'])
                            lg = sb("f_lg", [128, 8], F32, st2); m8 = sb("f_m8", [128, 8], F32, st2)
                            sel = sb("f_sel", [128, 8], F32, st2); ex = sb("f_ex", [128, 8], F32, st2)
                            sm = sb("f_sm", [128, 4], F32, st2); G = sb("f_G", [128, 4, 8], F32, st2)
                        for ti, (t0, N, mc) in enumerate(grp):
                            b = ti % 2
                            kx = ('fx', b)
                            o = offs[ti]
                            S.dma('sp', xt[b][:, :, :N], XS[:, t0:t0 + N].rearrange("(k p) n -> p k n", p=128), writes=[kx])
                            ksq = normmod(xt[b][:, :, :N], N, A2[:, i, :, mc], None, None, sq, rs, 7, kx, 'f')
                            S.op('dve', lambda e: e.tensor_tensor(out=sq[:, :, :N], in0=sq[:, :, :N],
                                                                  in1=modt[:, i, 24:32, mc].unsqueeze(2).to_broadcast([128, 8, N]), op=ALU.add),
                                 reads=[ksq, 'modt'], writes=[ksq])
                            S.op('act', lambda e: e.activation(out=hb[:, :, o:o + N], in_=sq[:, :, :N], func=AF.Identity),
                                 reads=[ksq], writes=['hb'])
                            if moe:
                                for blk in range(N // 128):
                                    for kc in range(8):
                                        S.op('pe', lambda e: e.matmul(ps[6][:, 0:8], lhsT=sq[:, kc, blk * 128:(blk + 1) * 128], rhs=rt[:, kc, :],
                                                                      start=(kc == 0), stop=(kc == 7)), reads=[ksq, 'rt'], writes=[('ps', 6)], inc=(kc == 7))
                                    S.op('dve', lambda e: e.tensor_copy(out=lg[:], in_=ps[6][:, 0:8]), reads=[('ps', 6)], writes=['lg'])
                                    S.op('dve', lambda e: e.max(out=m8[:], in_=lg[:]), reads=['lg'], writes=['m8'])
                                    S.op('dve', lambda e: e.tensor_scalar(out=sel[:], in0=lg[:], scalar1=m8[:, 1:2], scalar2=None, op0=ALU.is_ge),
                                         reads=['lg', 'm8'], writes=['sel'])
                                    S.op('dve', lambda e: e.tensor_scalar(out=sm[:, 0:1], in0=m8[:, 0:1], scalar1=-1.0, scalar2=None, op0=ALU.mult),
                                         reads=['m8'], writes=['sm'])
                                    S.op('act', lambda e: e.activation(out=ex[:], in_=lg[:], func=AF.Exp, bias=sm[:, 0:1], scale=1.0),
                                         reads=['lg', 'sm'], writes=['ex'])
                                    S.op('dve', lambda e: e.tensor_tensor(out=ex[:], in0=ex[:], in1=sel[:], op=ALU.mult), reads=['ex', 'sel'], writes=['ex'])
                                    S.op('dve', lambda e: e.tensor_reduce(out=sm[:, 1:2], in_=ex[:], axis=AX.X, op=ALU.add), reads=['ex'], writes=['sm'])
                                    S.op('dve', lambda e: e.reciprocal(out=sm[:, 2:3], in_=sm[:, 1:2]), reads=['sm'], writes=['sm'])
                                    S.op('dve', lambda e: e.tensor_scalar(out=G[:, blk, :], in0=ex[:], scalar1=sm[:, 2:3], scalar2=None, op0=ALU.mult),
                                         reads=['ex', 'sm'], writes=['G'])
                                    S.op('pe', lambda e: e.transpose(out=ps[5][0:8, blk * 128:(blk + 1) * 128], in_=G[:, blk, :], identity=ident),
                                         reads=['G', 'c32'], writes=[('ps', 5)])
                                S.op('act', lambda e: e.activation(out=GT[:, o:o + N], in_=ps[5][0:8, :N], func=AF.Identity),
                                     reads=[('ps', 5)], writes=['GT'])
                    S.barrier()
                    with contextlib.ExitStack() as st2:
                        w1b = [sb(f"f_w1{b}", [128, 8, 512], BF16, st2) for b in range(2)]
                        w3b = [sb(f"f_w3{b}", [128, 8, 512], BF16, st2) for b in range(2)]
                        w2b = [sb(f"f_w2{b}", [128, 4, 1024], BF16, st2) for b in range(2)]
                        gt = [sb(f"f_g{b}", [128, 4, 512], BF16, st2) for b in range(2)]
                        s1 = [sb(f"f_s1{b}", [128, 512], F32, st2) for b in range(2)]
                        s1g = [sb(f"f_s1g{b}", [128, 512], F32, st2) for b in range(2)]
                        nw = 0
                        ng = 0
                        nhc = 0
                        ny = 0
                        first = True
                        items = []
                        first = True
                        for ex_i in range(E):
                            for bi, (c0, nh) in enumerate(blocks):
                                wb = nw % 2
                                nw += 1
                                for ti, (t0, N, mc) in enumerate(grp):
                                    items.append(dict(ex=ex_i, bi=bi, c0=c0, nh=nh, wb=wb, ti=ti, o=offs[ti], N=N, gb=ng % 2, first=first))
                                    ng += 1
                                first = False

                        def emit_gate(ex_i):
                            for ti, (t0, N, mc) in enumerate(grp):
                                o = offs[ti]
                                S.op('pe', lambda e: e.matmul(ps[6][:, :N], lhsT=selE[:, ex_i * 128:(ex_i + 1) * 128], rhs=GT[:, o:o + N],
                                                              start=True, stop=True), reads=['GT', 'c32'], writes=[('ps', 6)])
                                S.op('act', lambda e: e.activation(out=gbc[:, o:o + N], in_=ps[6][:, :N], func=AF.Identity),
                                     reads=[('ps', 6)], writes=['gbc'])

                        def emit_wload(it):
                            wb, c0, nh, ex_i = it['wb'], it['c0'], it['nh'], it['ex']
                            kw = ('fw', wb)
                            if moe:
                                W1, W3, W2 = moe_w1[k, ex_i], moe_w3[k, ex_i], moe_w2[k, ex_i]
                            else:
                                W1, W3, W2 = ffn_w1[k], ffn_w3[k], ffn_w2[k]
                            S.dma('pool', w1b[wb][:, :, :nh * 128], W1[:, c0 * 128:(c0 + nh) * 128].rearrange("(k p) n -> p k n", p=128), writes=[kw])
                            S.dma('pool', w3b[wb][:, :, :nh * 128], W3[:, c0 * 128:(c0 + nh) * 128].rearrange("(k p) n -> p k n", p=128), writes=[kw])
                            S.dma('pool', w2b[wb][:, :nh, :], W2[c0 * 128:(c0 + nh) * 128, :].rearrange("(k p) n -> p k n", p=128), writes=[kw])

                        def emit_P(it):
                            nonlocal_nhc = cnts
                            wb, nh, o, N, gb = it['wb'], it['nh'], it['o'], it['N'], it['gb']
                            kw = ('fw', wb)
                            for hc in range(nh):
                                pa = (cnts[0] % 2) * 2
                                sbi = cnts[0] % 2
                                cnts[0] += 1
                                for kc in range(8):
                                    S.op('pe', lambda e: e.matmul(ps[pa][:, :N], lhsT=w1b[wb][:, kc, hc * 128:(hc + 1) * 128], rhs=hb[:, kc, o:o + N],
                                                                  start=(kc == 0), stop=(kc == 7)), reads=[kw, 'hb'], writes=[('ps', pa)], inc=(kc == 7))
                                for kc in range(8):
                                    S.op('pe', lambda e: e.matmul(ps[pa + 1][:, :N], lhsT=w3b[wb][:, kc, hc * 128:(hc + 1) * 128], rhs=hb[:, kc, o:o + N],
                                                                  start=(kc == 0), stop=(kc == 7)), reads=[kw, 'hb'], writes=[('ps', pa + 1)], inc=(kc == 7))
                                S.op('act', lambda e: e.activation(out=s1[sbi][:, :N], in_=ps[pa][:, :N], func=AF.Silu),
                                     reads=[('ps', pa)], writes=[('s1', sbi)])
                                src, ksrc = s1[sbi], ('s1', sbi)
                                if moe:
                                    S.op('dve', lambda e: e.tensor_tensor(out=s1g[sbi][:, :N], in0=s1[sbi][:, :N], in1=gbc[:, o:o + N], op=ALU.mult),
                                         reads=[('s1', sbi), 'gbc'], writes=[('s1g', sbi)])
                                    src, ksrc = s1g[sbi], ('s1g', sbi)
                                S.op('dve', lambda e: e.tensor_tensor(out=gt[gb][:, hc, :N], in0=src[:, :N], in1=ps[pa + 1][:, :N], op=ALU.mult),
                                     reads=[ksrc, ('ps', pa + 1)], writes=[('g', gb)])

                        def emit_W2(it):
                            wb, nh, o, N, gb = it['wb'], it['nh'], it['o'], it['N'], it['gb']
                            kw = ('fw', wb)
                            for oc in range(8):
                                py = 4 + cnts[1] % 2
                                cnts[1] += 1
                                for hc in range(nh):
                                    S.op('pe', lambda e: e.matmul(ps[py][:, :N], lhsT=w2b[wb][:, hc, oc * 128:(oc + 1) * 128], rhs=gt[gb][:, hc, :N],
                                                                  start=(hc == 0), stop=(hc == nh - 1)), reads=[kw, ('g', gb)], writes=[('ps', py)],
                                         inc=(hc == nh - 1))
                                ky = ('y', o, oc)
                                if it['first']:
                                    S.op('act', lambda e: e.activation(out=yacc[:, oc, o:o + N], in_=ps[py][:, :N], func=AF.Identity),
                                         reads=[('ps', py)], writes=[ky])
                                else:
                                    S.op('dve', lambda e: e.tensor_tensor(out=yacc[:, oc, o:o + N], in0=yacc[:, oc, o:o + N], in1=ps[py][:, :N], op=ALU.add),
                                         reads=[('ps', py), ky], writes=[ky])

                        cnts = [0, 0]
                        prev = None
                        for it in items:
                            if moe and it['bi'] == 0 and it['ti'] == 0:
                                emit_gate(it['ex'])
                            if it['ti'] == 0:
                                emit_wload(it)
                            emit_P(it)
                            if prev is not None:
                                emit_W2(prev)
                            prev = it
                        emit_W2(prev)
                    S.barrier()
                    with contextlib.ExitStack() as st2:
                        xt = [sb(f"f_cx{b}", [128, 8, 512], F32, st2) for b in range(2)]
                        for ti, (t0, N, mc) in enumerate(grp):
                            b = ti % 2
                            o = offs[ti]
                            S.dma('sp', xt[b][:, :, :N], XS[:, t0:t0 + N].rearrange("(k p) n -> p k n", p=128), writes=[('fcx', b)])
                            for oc in range(8):
                                S.op('dve', lambda e: e.scalar_tensor_tensor(
                                    out=xt[b][:, oc, :N], in0=yacc[:, oc, o:o + N], scalar=modt[:, i, 40 + oc, mc:mc + 1],
                                    in1=xt[b][:, oc, :N], op0=ALU.mult, op1=ALU.add), reads=[('fcx', b), 'modt'], writes=[('fcx', b)])
                            if last:
                                S.dma('sp', out_d[:, t0 - CTX:t0 - CTX + N].rearrange("(k p) n -> p k n", p=128), xt[b][:, :, :N],
                                      reads=[('fcx', b)], writes=['out'])
                            else:
                                S.dma('sp', XS[:, t0:t0 + N].rearrange("(k p) n -> p k n", p=128), xt[b][:, :, :N],
                                      reads=[('fcx', b)], writes=['XS'])
                    S.barrier()

        if dbg:
            dbg_d = nc.dram_tensor("dbg", [D, NT], F32, kind="ExternalOutput").ap()
        phase_mod()
        for i in range(depth):
            last = (i == depth - 1)
            if i == 0 and os.environ.get('SKIP0'):
                S.dma('sp', XS, xs_in, writes=['XS'])
                S.barrier()
                continue
            if i % 2 == 0:
                phase_mla(i, xs_in if i == 0 else XS)
            else:
                phase_rwkv(i, last and dbg != 'r')
            if not (dbg == 'r' and i == depth - 1):
                phase_ffn(i, last)
        if dbg:
            S.dma('sp', dbg_d, XS, writes=['dbg'])
        S.barrier()
    return nc, S


def host_consts():
    c = np.zeros((128, C32W), np.float32)
    c[:, 0:128] = 1.0
    c[:, 128:256] = np.eye(128, dtype=np.float32)
    P = np.zeros((64, 64), np.float32)
    for base in (0, 32):
        for f in range(16):
            P[base + 16 + f, base + f] = -1.0
            P[base + f, base + 16 + f] = 1.0
    c[0:64, 256:320] = P
    for e in range(8):
        c[e, 384 + e * 128:384 + (e + 1) * 128] = 1.0
    o_ = 384 + 1024
    p = np.arange(128)
    c[:, o_:o_ + 128] = (p[:, None] // 64 == p[None, :] // 64)
    tt = np.arange(64)
    c[:, o_ + 128:o_ + 192] = (p[:, None] % 64 == tt[None, :])
    sidx = (p % 64)[:, None]
    c[:, o_ + 192:o_ + 256] = (sidx < tt[None, :])
    c[:, o_ + 256:o_ + 320] = (sidx <= tt[None, :])
    c[:, o_ + 320:o_ + 384] = (tt[None, :] < sidx)
    c[:, o_ + 384:o_ + 448] = (sidx > tt[None, :])
    c[:, o_ + 448:o_ + 512] = (sidx >= tt[None, :])
    c[:, o_ + 512:o_ + 576] = (tt[None, :] > sidx)
    cm = np.ones(512, np.float32); cm[0::64] = 0.0
    c[:, o_ + 576:o_ + 1088] = cm[None, :]
    rows = TL // 64
    row_ids = np.repeat(np.arange(rows, dtype=np.float32), 64)
    col_ids = np.tile(np.arange(64, dtype=np.float32), rows)
    inv_freq = (1.0 / (np.float32(10000.0) ** (np.arange(16, dtype=np.float32) / np.float32(16)))).astype(np.float32)
    ang_r = (row_ids[:, None] * inv_freq[None, :]).astype(np.float32)
    ang_c = (col_ids[:, None] * inv_freq[None, :]).astype(np.float32)
    cosT = np.zeros((64, TL), np.float32)
    sinT = np.zeros((64, TL), np.float32)
    for base, ang in ((0, ang_r), (32, ang_c)):
        for half in (0, 16):
            cosT[base + half:base + half + 16] = np.cos(ang).T
            sinT[base + half:base + half + 16] = np.sin(ang).T
    return c, cosT, sinT


def host_vecs(inp, depth):
    voff, NV = vec_layout(depth)
    vecs = np.zeros((128, NV), np.float32)

    def put(name, arr):
        o, c = voff[name]
        assert arr.shape == (128, c), (name, arr.shape, c)
        vecs[:, o:o + c] = arr
    for i in range(depth):
        put(f"adab{i}", fm(inp["ada_b"][i]))
        put(f"n1g{i}", fm(inp["norm1_g"][i]))
        put(f"n2g{i}", fm(inp["norm2_g"][i]))
        j = i // 2
        if i % 2 == 0:
            put(f"qan{j}", fm(inp["mla_qa_norm"][j]))
            put(f"kvan{j}", fm(inp["mla_kva_norm"][j]))
            put(f"qnn{j}", fm(inp["mla_q_norm"][j][:128]))
            put(f"qnr{j}", fm(inp["mla_q_norm"][j][128:]))
            put(f"knn{j}", fm(inp["mla_k_norm"][j][:128]))
            put(f"knr{j}", fm(inp["mla_k_norm"][j][128:]))
        else:
            put(f"mix{j}", fm(inp["rwkv_mix"][j]))
            put(f"w0{j}", fm(inp["rwkv_w0"][j]))
            put(f"a0{j}", fm(inp["rwkv_a0"][j]))
            put(f"kk{j}", fm(inp["rwkv_k_k"][j]))
            put(f"ka{j}", fm(inp["rwkv_k_a"][j]))
            put(f"rk{j}", fm(inp["rwkv_r_k"][j]))
            put(f"lnw{j}", fm(inp["rwkv_ln_w"][j]))
            put(f"lnb{j}", fm(inp["rwkv_ln_b"][j]))
            if j >= 1:
                put(f"v0{j}", fm(inp["rwkv_v0"][j - 1]))
    return vecs


WNAMES = ["ada_w", "mla_wqa", "mla_wqb", "mla_wkva", "mla_wkvb", "mla_wo", "ffn_w1", "ffn_w3", "ffn_w2",
          "moe_router", "moe_w1", "moe_w3", "moe_w2",
          "rwkv_wr", "rwkv_wk", "rwkv_wv", "rwkv_wo", "rwkv_w1", "rwkv_w2", "rwkv_a1", "rwkv_a2", "rwkv_g1", "rwkv_g2", "rwkv_v1", "rwkv_v2"]


def make_in_maps(inp, depth, cores):
    c32, cosT, sinT = host_consts()
    vecs = host_vecs(inp, depth)
    shared = {n: np.ascontiguousarray(inp[n], dtype=np.float32) for n in WNAMES}
    maps = []
    for b in cores:
        xs = np.ascontiguousarray(np.concatenate([inp["ctx"][b], inp["x"][b]], axis=0).T.astype(np.float32))
        cv = np.zeros((128, 16), np.float32)
        cv[:, 0::2] = fm(inp["c"][b])
        cv[:, 1::2] = fm(inp["c_ctx"])
        m = dict(shared)
        m.update(xs=xs, cvec=cv, vecs=vecs, c32=c32, ropec=cosT, ropes=sinT)
        maps.append(m)
    return maps


def kernel(**inp):
    depth = 4
    nc, S = build(depth)
    maps = make_in_maps(inp, depth, list(range(8)))
    res = run_bass_kernel_spmd(nc, maps, core_ids=list(range(8)))
    out = np.stack([np.ascontiguousarray(r["out"].T) for r in res.results], axis=0)
    return out.astype(np.float32)
```

```python
import contextlib
import os
import numpy as np
import concourse.bass as bass
import concourse.mybir as mybir
from concourse.bass_utils import run_bass_kernel_spmd

F32 = mybir.dt.float32
BF16 = mybir.dt.bfloat16
ALU = mybir.AluOpType
AF = mybir.ActivationFunctionType
AX = mybir.AxisListType
NS = 8

D = 1024
KC = 8
CTX = 256
TL = 4096
NT = CTX + TL
EPS = 1e-6
NH = 8
SM_SCALE = 192 ** -0.5
DFF = 2816
DFE = 3584
NE = 8
C32W = 128 * 3 + 8 * 128 + 128 + 64 + 128 + 64 + 128 + 64 + 512
TILES = [(0, 256, 1)] + [(256 + 512 * i, 512, 0) for i in range(8)]


class Sched:
    def __init__(self, nc):
        self.nc = nc
        self.engs = {'pe': nc.tensor, 'dve': nc.vector, 'act': nc.scalar,
                     'pool': nc.gpsimd, 'sp': nc.sync}
        self.sem = {e: nc.alloc_semaphore(name=f"sem_{e}") for e in ['pe', 'dve', 'act', 'pool']}
        self.cnt = {e: 0 for e in self.sem}
        self.dq = {}
        for q, e in [('sp', 'sp'), ('pool', 'pool')]:
            self.dq[q] = dict(eng=e, n=0,
                              sems=[nc.alloc_semaphore(name=f"dsem_{q}{i}") for i in range(NS)])
        self.waited = {}
        self.lastw = {}
        self.readers = {}
        self.nins = 0
        self.mute = False

    def _sid_val(self, tok):
        if tok[0] == 'c':
            return ('c', tok[1]), self.sem[tok[1]], tok[2]
        q = self.dq[tok[1]]
        n = tok[2]
        return ('d', tok[1], n % NS), q['sems'][n % NS], 16 * (n // NS + 1)

    def _wait(self, eng, tok):
        if tok[0] == 'c' and tok[1] == eng and eng == 'pe':
            return
        sid, sem, val = self._sid_val(tok)
        if tok[0] == 'c':
            assert val <= self.cnt[tok[1]], f"wait on unsignalled instr {tok}"
        if self.waited.get((eng, sid), 0) >= val:
            return
        self.engs[eng].wait_ge(sem, val)
        self.waited[(eng, sid)] = val
        self.nins += 1

    def _deps(self, reads, writes):
        deps = set()
        for k in reads:
            if k in self.lastw:
                deps.add(self.lastw[k])
        for k in writes:
            if k in self.lastw:
                deps.add(self.lastw[k])
            for t in self.readers.get(k, {}).values():
                deps.add(t)
        return deps

    def _record(self, tok, reads, writes):
        sid, _, val = self._sid_val(tok)
        for k in reads:
            r = self.readers.setdefault(k, {})
            old = r.get(sid)
            if old is None or self._sid_val(old)[2] < val:
                r[sid] = tok
        for k in writes:
            self.lastw[k] = tok
            self.readers[k] = {}

    def op(self, eng, fn, reads=(), writes=(), inc=True):
        if self.mute:
            return None
        if eng != 'pe':
            psr = [k for k in reads if isinstance(k, tuple) and k[0] in ('ps', 'psb', 'sps')]
            if psr:
                reads = [k for k in reads if k not in psr]
                writes = list(writes) + psr
        for t in self._deps(reads, writes):
            self._wait(eng, t)
        ins = fn(self.engs[eng])
        self.nins += 1
        if inc:
            self.cnt[eng] += 1
            ins.then_inc(self.sem[eng], 1)
            tok = ('c', eng, self.cnt[eng])
        else:
            tok = ('c', eng, self.cnt[eng] + 1)
        self._record(tok, reads, writes)
        return ins

    def dma(self, q, out, in_, reads=(), writes=(), **kw):
        if self.mute:
            return None
        Q = self.dq[q]
        eng = Q['eng']
        n = Q['n']
        for t in self._deps(reads, writes):
            self._wait(eng, t)
        if n >= NS:
            self._wait(eng, ('d', q, n - NS))
        ins = self.engs[eng].dma_start(out=out, in_=in_, **kw)
        ins.then_inc(Q['sems'][n % NS], 16)
        self.nins += 1
        Q['n'] += 1
        self._record(('d', q, n), reads, writes)
        return ins

    def barrier(self):
        toks = []
        for e, c in self.cnt.items():
            if c > 0:
                toks.append(('c', e, c))
        for q, Q in self.dq.items():
            for n in range(max(0, Q['n'] - NS), Q['n']):
                toks.append(('d', q, n))
        for e in ['pe', 'dve', 'act', 'pool', 'sp']:
            for t in toks:
                if t[0] == 'c' and t[1] == e:
                    continue
                self._wait(e, t)
        self.lastw.clear()
        self.readers.clear()


def vec_layout(depth):
    ents = []
    for i in range(depth):
        ents += [(f"adab{i}", 48), (f"n1g{i}", 8), (f"n2g{i}", 8)]
        j = i // 2
        if i % 2 == 0:
            ents += [(f"qan{j}", 3), (f"kvan{j}", 2), (f"qnn{j}", 1), (f"qnr{j}", 1), (f"knn{j}", 1), (f"knr{j}", 1)]
        else:
            ents += [(f"mix{j}", 48), (f"w0{j}", 16), (f"a0{j}", 16), (f"kk{j}", 8), (f"ka{j}", 8),
                     (f"rk{j}", 8), (f"lnw{j}", 8), (f"lnb{j}", 8)]
            if j >= 1:
                ents += [(f"v0{j}", 8)]
    off = {}
    o = 0
    for n, c in ents:
        off[n] = (o, c)
        o += c
    return off, o


def fm(v):
    v = np.asarray(v, np.float32).reshape(-1)
    pad = (-len(v)) % 128
    if pad:
        v = np.concatenate([v, np.zeros(pad, np.float32)])
    return np.ascontiguousarray(v.reshape(-1, 128).T)


def build(depth=4, dbg=None):
    nc = bass.Bass("TRN2", target_bir_lowering=False)
    voff, NV = vec_layout(depth)

    def din(name, shape, dt=F32):
        return nc.dram_tensor(name, list(shape), dt, kind="ExternalInput").ap()

    def dscr(name, shape, dt=F32):
        return nc.dram_tensor(name, list(shape), dt, kind="Internal").ap()

    xs_in = din("xs", [D, NT])
    cvec_d = din("cvec", [128, 16])
    vecs_d = din("vecs", [128, NV])
    c32_d = din("c32", [128, C32W])
    ropec_d = din("ropec", [64, TL])
    ropes_d = din("ropes", [64, TL])
    ada_w = din("ada_w", [4, D, 6 * D])
    mla_wqa = din("mla_wqa", [2, D, 384]); mla_wqb = din("mla_wqb", [2, 384, 1536])
    mla_wkva = din("mla_wkva", [2, D, 320]); mla_wkvb = din("mla_wkvb", [2, 256, 2048])
    mla_wo = din("mla_wo", [2, D, D])
    ffn_w1 = din("ffn_w1", [2, D, DFF]); ffn_w3 = din("ffn_w3", [2, D, DFF]); ffn_w2 = din("ffn_w2", [2, DFF, D])
    moe_router = din("moe_router", [2, D, NE])
    moe_w1 = din("moe_w1", [2, NE, D, DFE]); moe_w3 = din("moe_w3", [2, NE, D, DFE]); moe_w2 = din("moe_w2", [2, NE, DFE, D])
    rwkv_wr = din("rwkv_wr", [2, D, D]); rwkv_wk = din("rwkv_wk", [2, D, D]); rwkv_wv = din("rwkv_wv", [2, D, D]); rwkv_wo = din("rwkv_wo", [2, D, D])
    rwkv_w1 = din("rwkv_w1", [2, 2, D, 64]); rwkv_w2 = din("rwkv_w2", [2, 2, 64, D])
    rwkv_a1 = din("rwkv_a1", [2, 2, D, 64]); rwkv_a2 = din("rwkv_a2", [2, 2, 64, D])
    rwkv_g1 = din("rwkv_g1", [2, D, 160]); rwkv_g2 = din("rwkv_g2", [2, 160, D])
    rwkv_v1 = din("rwkv_v1", [1, D, 32]); rwkv_v2 = din("rwkv_v2", [1, 32, D])
    out_d = nc.dram_tensor("out", [D, TL], F32, kind="ExternalOutput").ap()
    HS = dscr("HS", [D, 258 + 4098])
    ATd = [dscr(f"ATd{d}", [D, NT], BF16) for d in range(2)]; BTd = [dscr(f"BTd{d}", [D, NT], BF16) for d in range(2)]
    KTd = [dscr(f"KTd{d}", [D, NT], BF16) for d in range(2)]; RTd = [dscr(f"RTd{d}", [D, NT], BF16) for d in range(2)]
    VTb = dscr("VTb", [D, NT], BF16)
    WCd = [dscr(f"WCd{d}", [D, NT // 64]) for d in range(2)]
    VT = [dscr(f"VT{d}", [D, NT]) for d in range(2)]
    YD = [dscr(f"YD{d}", [D, NT]) for d in range(2)]
    BON = dscr("BON", [D, NT]); GG = dscr("GG", [D, NT])

    XS = dscr("XS", [D, NT])
    QN = dscr("QN", [NH, 128, NT], BF16); QR = dscr("QR", [NH, 64, NT], BF16)
    KN = dscr("KN", [NH, 128, NT], BF16); KR = dscr("KR", [NH, 64, NT], BF16)
    VV = dscr("VV", [NT, NH, 128], BF16)
    AO = dscr("AO", [D, NT], BF16)

    S = Sched(nc)
    es = contextlib.ExitStack()

    uniq = [0]

    def sb(name, shape, dt=F32, stack=None):
        uniq[0] += 1
        return (stack or es).enter_context(nc.sbuf_tensor(f"s{uniq[0]}_{name}", list(shape), dt))

    with es:
        ps = [es.enter_context(nc.psum_tensor(f"ps{i}", [128, 512], F32)) for i in range(8)]
        vecs = sb("vecs", [128, NV])
        c32 = sb("c32", [128, C32W])
        ones_bf = sb("ones_bf", [128, 128], BF16)
        cvec = sb("cvec", [128, 16])
        modt = sb("modt", [128, depth, 48, 2])
        A1 = sb("A1", [128, depth, 8, 2]); A2 = sb("A2", [128, depth, 8, 2])
        S.dma('sp', vecs[:], vecs_d, writes=['vecs'])
        S.dma('sp', c32[:], c32_d, writes=['c32'])
        S.dma('sp', cvec[:], cvec_d, writes=['cvec'])
        EPSI = {1024: 0, 384: 1, 256: 2, 192: 3, 64: 4}
        epsT = sb("epsT", [128, 8])
        for nf, ci in EPSI.items():
            S.op('pool', lambda e: e.memset(epsT[:, ci:ci + 1], float(nf * EPS)), writes=['epsT'])
        ones32 = c32[:, 0:128]
        ident = c32[:, 128:256]
        rotP = c32[0:64, 256:320]
        selE = c32[0:8, 384:384 + 8 * 128]
        o_ = 384 + 1024
        blockones = c32[:, o_:o_ + 128]
        SI = c32[:, o_ + 128:o_ + 192]
        maskAR_f = c32[:, o_ + 192:o_ + 320]
        maskN_f = c32[:, o_ + 320:o_ + 384]
        maskAR_r = c32[:, o_ + 384:o_ + 512]
        maskN_r = c32[:, o_ + 512:o_ + 576]
        cmask = c32[:, o_ + 576:o_ + 1088]
        S.op('pool', lambda e: e.memset(epsT[:, 5:6], 64e-5), writes=['epsT'])
        S.op('dve', lambda e: e.tensor_copy(out=ones_bf[:], in_=ones32), reads=['c32'], writes=['ones_bf'])
        ident_bf = sb("ident_bf", [128, 128], BF16)
        S.op('dve', lambda e: e.tensor_copy(out=ident_bf[:], in_=ident), reads=['c32'], writes=['ident_bf'])

        def V(name, k0=0, k1=None):
            o, c = voff[name]
            k1 = c if k1 is None else k1
            return vecs[:, o + k0:o + k1]

        def phase_mod():
            with contextlib.ExitStack() as st:
                wb = [sb(f"adaw{i}", [128, 8, 1024], F32, st) for i in range(2)]
                sc = sb("sc", [128, 16], F32, st)
                S.op('act', lambda e: e.activation(out=sc[:], in_=cvec[:], func=AF.Silu), reads=['cvec'], writes=['sc'])
                n = 0
                for i in range(depth):
                    for j in range(6):
                        w = wb[n % 2]
                        S.dma('sp', w[:], ada_w[i, :, j * 1024:(j + 1) * 1024].rearrange("(k p) n -> p k n", p=128),
                              writes=[('adaw', n % 2)])
                        for oc in range(8):
                            col = (j * 8 + oc) * 2
                            for kc in range(8):
                                S.op('pe', lambda e: e.matmul(ps[0][:, col:col + 2], lhsT=w[:, kc, oc * 128:(oc + 1) * 128],
                                                              rhs=sc[:, 2 * kc:2 * kc + 2], start=(kc == 0), stop=(kc == 7)),
                                     reads=[('adaw', n % 2), 'sc'], writes=['psmod'], inc=(kc == 7 and oc == 7))
                        n += 1
                    S.op('dve', lambda e: e.tensor_tensor(
                        out=modt[:, i, :, :], in0=ps[0][:, 0:96].rearrange("p (a b) -> p a b", b=2),
                        in1=V(f"adab{i}").unsqueeze(2).to_broadcast([128, 48, 2]), op=ALU.add),
                        reads=['psmod', 'vecs'], writes=['modt'])
                    for (At, gname, jj) in [(A1, f"n1g{i}", 1), (A2, f"n2g{i}", 4)]:
                        S.op('dve', lambda e: e.scalar_tensor_tensor(
                            out=At[:, i, :, :], in0=modt[:, i, jj * 8:(jj + 1) * 8, :], scalar=1.0,
                            in1=V(gname).unsqueeze(2).to_broadcast([128, 8, 2]), op0=ALU.add, op1=ALU.mult),
                            reads=['modt', 'vecs'], writes=['A'])
                        S.op('dve', lambda e: e.tensor_scalar(out=At[:, i, :, :], in0=At[:, i, :, :], scalar1=float(np.sqrt(D)),
                                                              scalar2=None, op0=ALU.mult), reads=['A'], writes=['A'])
            S.barrier()

        def normmod(x32, N, Acol, Scol, outap, sq, rs, psb, kx, tag):
            ksq, krs, kps = ('sq', tag), 'rs', ('ps', psb)
            S.op('pool', lambda e: e.tensor_tensor(out=sq[:, :, :N], in0=x32, in1=x32, op=ALU.mult), reads=[kx], writes=[ksq])
            for k in range(8):
                S.op('pe', lambda e: e.matmul(ps[psb][:, :N], lhsT=ones32, rhs=sq[:, k, :N], start=(k == 0), stop=(k == 7)),
                     reads=[ksq, 'c32'], writes=[kps], inc=(k == 7))
            S.op('act', lambda e: e.activation(out=rs[:, :N], in_=ps[psb][:, :N], func=AF.Sqrt, bias=epsT[:, EPSI[D]:EPSI[D] + 1], scale=1.0),
                 reads=[kps, 'epsT'], writes=[krs])
            S.op('dve', lambda e: e.reciprocal(out=rs[:, :N], in_=rs[:, :N]), reads=[krs], writes=[krs])
            S.op('dve', lambda e: e.tensor_tensor(out=sq[:, :, :N], in0=x32, in1=rs[:, :N].unsqueeze(1).to_broadcast([128, 8, N]),
                                                  op=ALU.mult), reads=[kx, krs], writes=[ksq])
            S.op('pool', lambda e: e.tensor_tensor(out=sq[:, :, :N], in0=sq[:, :, :N], in1=Acol.unsqueeze(2).to_broadcast([128, 8, N]),
                                                   op=ALU.mult), reads=[ksq, 'A'], writes=[ksq])
            return ksq

        def rstd_from_ps(psb, N, nfeat, rs, krs, P=128):
            S.op('act', lambda e: e.activation(out=rs[:P, :N], in_=ps[psb][:P, :N], func=AF.Sqrt, bias=epsT[:P, EPSI[nfeat]:EPSI[nfeat] + 1], scale=1.0),
                 reads=[('ps', psb), 'epsT'], writes=[krs])
            S.op('dve', lambda e: e.reciprocal(out=rs[:P, :N], in_=rs[:P, :N]), reads=[krs], writes=[krs])

        def phase_mla(i, XSin):
            j = i // 2
            with contextlib.ExitStack() as st:
                wqa = sb("wqa", [128, 8, 384], BF16, st); wqb = sb("wqb", [128, 3, 1536], BF16, st)
                wkva = sb("wkva", [128, 8, 320], BF16, st); wkvb = sb("wkvb", [128, 2, 2048], BF16, st)
                S.dma('pool', wqa[:], mla_wqa[j].rearrange("(k p) n -> p k n", p=128), writes=['wqa'])
                S.dma('pool', wqb[:], mla_wqb[j].rearrange("(k p) n -> p k n", p=128), writes=['wqb'])
                S.dma('pool', wkva[:], mla_wkva[j].rearrange("(k p) n -> p k n", p=128), writes=['wkva'])
                S.dma('pool', wkvb[:], mla_wkvb[j].rearrange("(k p) n -> p k n", p=128), writes=['wkvb'])
                xt = [sb("xt0", [128, 8, 512], F32, st)] * 2
                sq = sb("sq", [128, 8, 512], F32, st)
                rs = sb("rs", [128, 512], F32, st)
                hb = sb("hb", [128, 8, 512], BF16, st)
                cq = sb("cq", [128, 3, 512], F32, st); cq2 = sb("cq2", [128, 3, 512], F32, st)
                cqn = sb("cqn", [128, 3, 512], BF16, st)
                ckv = sb("ckv", [128, 2, 512], F32, st); ckv2 = sb("ckv2", [128, 2, 512], F32, st)
                ckvn = sb("ckvn", [128, 2, 512], BF16, st)
                kr = sb("kr", [64, 512], F32, st); kr2 = sb("kr2", [64, 512], F32, st)
                krP = sb("krP", [64, 512], F32, st); krr = sb("krr", [64, 512], F32, st)
                qh = sb("qh", [128, 512], F32, st); qh2 = sb("qh2", [128, 512], F32, st)
                qr = sb("qr", [64, 512], F32, st); qr2 = sb("qr2", [64, 512], F32, st)
                qrP = sb("qrP", [64, 512], F32, st)
                rs2 = sb("rs2", [128, 512], F32, st)
                cosT = sb("cosT", [64, 512], F32, st); sinT = sb("sinT", [64, 512], F32, st)
                qn_o = sb("qn_o", [128, NH, 512], BF16, st); qr_o = sb("qr_o", [64, NH, 512], BF16, st)
                kn_o = sb("kn_o", [128, NH, 512], BF16, st); kr_o = sb("kr_o", [64, NH, 512], BF16, st)
                v_o = sb("v_o", [128, 4, NH, 128], BF16, st)
                sv = sb("sv", [128, 16], F32, st)
                S.op('dve', lambda e: e.tensor_scalar(out=sv[:, 0:3], in0=V(f"qan{j}"), scalar1=float(np.sqrt(384)), scalar2=None, op0=ALU.mult),
                     reads=['vecs'], writes=['sv'])
                S.op('dve', lambda e: e.tensor_scalar(out=sv[:, 3:5], in0=V(f"kvan{j}"), scalar1=float(np.sqrt(256)), scalar2=None, op0=ALU.mult),
                     reads=['vecs'], writes=['sv'])
                S.op('dve', lambda e: e.tensor_scalar(out=sv[:, 5:6], in0=V(f"qnn{j}"), scalar1=float(np.sqrt(192) * SM_SCALE), scalar2=None, op0=ALU.mult),
                     reads=['vecs'], writes=['sv'])
                S.op('dve', lambda e: e.tensor_scalar(out=sv[:, 6:7], in0=V(f"qnr{j}"), scalar1=float(np.sqrt(192) * SM_SCALE), scalar2=None, op0=ALU.mult),
                     reads=['vecs'], writes=['sv'])
                S.op('dve', lambda e: e.tensor_scalar(out=sv[:, 7:8], in0=V(f"knn{j}"), scalar1=float(np.sqrt(192)), scalar2=None, op0=ALU.mult),
                     reads=['vecs'], writes=['sv'])
                S.op('dve', lambda e: e.tensor_scalar(out=sv[:, 8:9], in0=V(f"knr{j}"), scalar1=float(np.sqrt(192)), scalar2=None, op0=ALU.mult),
                     reads=['vecs'], writes=['sv'])

                def rope(src, Pbuf, dst, N, ksrc, kdst, psb):
                    S.op('pe', lambda e: e.matmul(ps[psb][:64, :N], lhsT=rotP, rhs=src[:, :N], start=True, stop=True),
                         reads=[ksrc, 'c32'], writes=[('ps', psb)])
                    S.op('dve', lambda e: e.tensor_tensor(out=Pbuf[:, :N], in0=ps[psb][:64, :N], in1=sinT[:, :N], op=ALU.mult),
                         reads=[('ps', psb), 'rope'], writes=[('rp', kdst)])
                    S.op('pool', lambda e: e.tensor_tensor(out=dst[:, :N], in0=src[:, :N], in1=cosT[:, :N], op=ALU.mult),
                         reads=[ksrc, 'rope'], writes=[kdst])
                    S.op('pool', lambda e: e.tensor_tensor(out=dst[:, :N], in0=dst[:, :N], in1=Pbuf[:, :N], op=ALU.add),
                         reads=[kdst, ('rp', kdst)], writes=[kdst])

                for ti, (t0, N, mc) in enumerate(TILES):
                    x32 = xt[ti % 2]
                    kx = ('xt', ti % 2)
                    S.dma('sp', x32[:, :, :N], XSin[:, t0:t0 + N].rearrange("(k p) n -> p k n", p=128), writes=[kx])
                    if mc == 0:
                        S.dma('sp', cosT[:, :N], ropec_d[:, t0 - CTX:t0 - CTX + N], writes=['rope'])
                        S.dma('sp', sinT[:, :N], ropes_d[:, t0 - CTX:t0 - CTX + N], writes=['rope'])
                    ksq = normmod(x32[:, :, :N], N, A1[:, i, :, mc], None, None, sq, rs, 7, kx, 'm')
                    S.op('dve', lambda e: e.tensor_tensor(out=hb[:, :, :N], in0=sq[:, :, :N],
                                                          in1=modt[:, i, 0:8, mc].unsqueeze(2).to_broadcast([128, 8, N]), op=ALU.add),
                         reads=[ksq, 'modt'], writes=['hb'])
                    for c in range(3):
                        pb = c % 2
                        for kc in range(8):
                            S.op('pe', lambda e: e.matmul(ps[pb][:, :N], lhsT=wqa[:, kc, c * 128:(c + 1) * 128], rhs=hb[:, kc, :N],
                                                          start=(kc == 0), stop=(kc == 7)), reads=['hb', 'wqa'], writes=[('ps', pb)], inc=(kc == 7))
                        S.op('act', lambda e: e.activation(out=cq[:, c, :N], in_=ps[pb][:, :N], func=AF.Identity),
                             reads=[('ps', pb)], writes=[('cq', c)])
                        S.op('pool', lambda e: e.tensor_tensor(out=cq2[:, c, :N], in0=cq[:, c, :N], in1=cq[:, c, :N], op=ALU.mult),
                             reads=[('cq', c)], writes=[('cq2', c)])
                    for c in range(3):
                        S.op('pe', lambda e: e.matmul(ps[7][:, :N], lhsT=ones32, rhs=cq2[:, c, :N], start=(c == 0), stop=(c == 2)),
                             reads=[('cq2', c), 'c32'], writes=[('ps', 7)], inc=(c == 2))
                    rstd_from_ps(7, N, 384, rs, 'rs')
                    for c in range(3):
                        S.op('dve', lambda e: e.scalar_tensor_tensor(out=cqn[:, c, :N], in0=cq[:, c, :N], scalar=sv[:, c:c + 1], in1=rs[:, :N],
                                                                     op0=ALU.mult, op1=ALU.mult), reads=[('cq', c), 'rs', 'sv'], writes=['cqn'])
                    for c in range(3):
                        pb = 2 + c % 2
                        M = 128 if c < 2 else 64
                        for kc in range(8):
                            S.op('pe', lambda e: e.matmul(ps[pb][:M, :N], lhsT=wkva[:, kc, c * 128:c * 128 + M], rhs=hb[:, kc, :N],
                                                          start=(kc == 0), stop=(kc == 7)), reads=['hb', 'wkva'], writes=[('ps', pb)], inc=(kc == 7))
                        if c < 2:
                            S.op('act', lambda e: e.activation(out=ckv[:, c, :N], in_=ps[pb][:, :N], func=AF.Identity),
                                 reads=[('ps', pb)], writes=[('ckv', c)])
                            S.op('pool', lambda e: e.tensor_tensor(out=ckv2[:, c, :N], in0=ckv[:, c, :N], in1=ckv[:, c, :N], op=ALU.mult),
                                 reads=[('ckv', c)], writes=[('ckv2', c)])
                        else:
                            S.op('act', lambda e: e.activation(out=kr[:, :N], in_=ps[pb][:64, :N], func=AF.Identity),
                                 reads=[('ps', pb)], writes=['kr'])
                            S.op('pool', lambda e: e.tensor_tensor(out=kr2[:, :N], in0=kr[:, :N], in1=kr[:, :N], op=ALU.mult),
                                 reads=['kr'], writes=['kr2'])
                            S.op('dve', lambda e: e.tensor_scalar(out=kr[:, :N], in0=kr[:, :N], scalar1=sv[0:64, 8:9], scalar2=None, op0=ALU.mult),
                                 reads=['kr', 'sv', 'kr2'], writes=['kr'])
                    for c in range(2):
                        S.op('pe', lambda e: e.matmul(ps[7][:, :N], lhsT=ones32, rhs=ckv2[:, c, :N], start=(c == 0), stop=(c == 1)),
                             reads=[('ckv2', c), 'c32'], writes=[('ps', 7)], inc=(c == 1))
                    rstd_from_ps(7, N, 256, rs, 'rs')
                    for c in range(2):
                        S.op('dve', lambda e: e.scalar_tensor_tensor(out=ckvn[:, c, :N], in0=ckv[:, c, :N], scalar=sv[:, 3 + c:4 + c], in1=rs[:, :N],
                                                                     op0=ALU.mult, op1=ALU.mult), reads=[('ckv', c), 'rs', 'sv'], writes=['ckvn'])
                    if mc == 0:
                        rope(kr, krP, krr, N, 'kr', 'krr', 6)
                        krsrc, kkr = krr, 'krr'
                    else:
                        krsrc, kkr = kr, 'kr'
                    for h in range(NH):
                        for kc in range(3):
                            S.op('pe', lambda e: e.matmul(ps[0][:, :N], lhsT=wqb[:, kc, h * 192:h * 192 + 128], rhs=cqn[:, kc, :N],
                                                          start=(kc == 0), stop=(kc == 2)), reads=['cqn', 'wqb'], writes=[('ps', 0)], inc=(kc == 2))
                        for kc in range(3):
                            S.op('pe', lambda e: e.matmul(ps[1][:64, :N], lhsT=wqb[:, kc, h * 192 + 128:h * 192 + 192], rhs=cqn[:, kc, :N],
                                                          start=(kc == 0), stop=(kc == 2)), reads=['cqn', 'wqb'], writes=[('ps', 1)], inc=(kc == 2))
                        S.op('act', lambda e: e.activation(out=qh[:, :N], in_=ps[0][:, :N], func=AF.Identity), reads=[('ps', 0)], writes=['qh'])
                        S.op('act', lambda e: e.activation(out=qr[:, :N], in_=ps[1][:64, :N], func=AF.Identity), reads=[('ps', 1)], writes=['qr'])
                        S.op('pool', lambda e: e.tensor_tensor(out=qh2[:, :N], in0=qh[:, :N], in1=qh[:, :N], op=ALU.mult), reads=['qh'], writes=['qh2'])
                        S.op('pool', lambda e: e.tensor_tensor(out=qr2[:, :N], in0=qr[:, :N], in1=qr[:, :N], op=ALU.mult), reads=['qr'], writes=['qr2'])
                        S.op('pe', lambda e: e.matmul(ps[4][:, :N], lhsT=ones32, rhs=qh2[:, :N], start=True, stop=False),
                             reads=['qh2', 'c32'], writes=[('ps', 4)], inc=False)
                        S.op('pe', lambda e: e.matmul(ps[4][:, :N], lhsT=c32[0:64, 0:128], rhs=qr2[:, :N], start=False, stop=True),
                             reads=['qr2', 'c32'], writes=[('ps', 4)])
                        rstd_from_ps(4, N, 192, rs2, 'rs2')
                        S.op('dve', lambda e: e.scalar_tensor_tensor(out=qn_o[:, h, :N], in0=qh[:, :N], scalar=sv[:, 5:6], in1=rs2[:, :N],
                                                                     op0=ALU.mult, op1=ALU.mult), reads=['qh', 'rs2', 'sv'], writes=['qn_o'])
                        S.op('dve', lambda e: e.scalar_tensor_tensor(out=qr[:, :N], in0=qr[:, :N], scalar=sv[0:64, 6:7], in1=rs2[0:64, :N],
                                                                     op0=ALU.mult, op1=ALU.mult), reads=['qr', 'rs2', 'sv', 'qr2'], writes=['qr'])
                        if mc == 0:
                            rope(qr, qrP, qr2, N, 'qr', 'qr2', 6)
                            S.op('act', lambda e: e.activation(out=qr_o[:, h, :N], in_=qr2[:, :N], func=AF.Identity), reads=['qr2'], writes=['qr_o'])
                        else:
                            S.op('act', lambda e: e.activation(out=qr_o[:, h, :N], in_=qr[:, :N], func=AF.Identity), reads=['qr'], writes=['qr_o'])
                        for kc in range(2):
                            S.op('pe', lambda e: e.matmul(ps[2][:, :N], lhsT=wkvb[:, kc, h * 256:h * 256 + 128], rhs=ckvn[:, kc, :N],
                                                          start=(kc == 0), stop=(kc == 1)), reads=['ckvn', 'wkvb'], writes=[('ps', 2)], inc=(kc == 1))
                        S.op('act', lambda e: e.activation(out=qh[:, :N], in_=ps[2][:, :N], func=AF.Identity), reads=[('ps', 2)], writes=['qh'])
                        S.op('pool', lambda e: e.tensor_tensor(out=qh2[:, :N], in0=qh[:, :N], in1=qh[:, :N], op=ALU.mult), reads=['qh'], writes=['qh2'])
                        S.op('pe', lambda e: e.matmul(ps[5][:, :N], lhsT=ones32, rhs=qh2[:, :N], start=True, stop=False),
                             reads=['qh2', 'c32'], writes=[('ps', 5)], inc=False)
                        S.op('pe', lambda e: e.matmul(ps[5][:, :N], lhsT=c32[0:64, 0:128], rhs=kr2[:, :N], start=False, stop=True),
                             reads=['kr2', 'c32'], writes=[('ps', 5)])
                        rstd_from_ps(5, N, 192, rs2, 'rs2')
                        S.op('dve', lambda e: e.scalar_tensor_tensor(out=kn_o[:, h, :N], in0=qh[:, :N], scalar=sv[:, 7:8], in1=rs2[:, :N],
                                                                     op0=ALU.mult, op1=ALU.mult), reads=['qh', 'rs2', 'sv'], writes=['kn_o'])
                        S.op('dve', lambda e: e.tensor_tensor(out=kr_o[:, h, :N], in0=krsrc[:, :N], in1=rs2[0:64, :N], op=ALU.mult),
                             reads=[kkr, 'rs2'], writes=['kr_o'])
                        for blk in range(N // 128):
                            for kc in range(2):
                                S.op('pe', lambda e: e.matmul(ps[3][:, blk * 128:(blk + 1) * 128], lhsT=ckvn[:, kc, blk * 128:(blk + 1) * 128],
                                                              rhs=wkvb[:, kc, h * 256 + 128:h * 256 + 256], start=(kc == 0), stop=(kc == 1)),
                                     reads=['ckvn', 'wkvb'], writes=[('ps', 3)], inc=(kc == 1 and blk == N // 128 - 1))
                        S.op('act', lambda e: e.activation(out=v_o[:, 0:N // 128, h, :], in_=ps[3][:, :N].rearrange("p (b d) -> p b d", d=128),
                                                           func=AF.Identity), reads=[('ps', 3)], writes=['v_o'])
                    S.dma('sp', QN[:, :, t0:t0 + N].rearrange("h p n -> p h n"), qn_o[:, :, :N], reads=['qn_o'], writes=['QN'])
                    S.dma('sp', QR[:, :, t0:t0 + N].rearrange("h p n -> p h n"), qr_o[:, :, :N], reads=['qr_o'], writes=['QR'])
                    S.dma('sp', KN[:, :, t0:t0 + N].rearrange("h p n -> p h n"), kn_o[:, :, :N], reads=['kn_o'], writes=['KN'])
                    S.dma('sp', KR[:, :, t0:t0 + N].rearrange("h p n -> p h n"), kr_o[:, :, :N], reads=['kr_o'], writes=['KR'])
                    S.dma('sp', VV[t0:t0 + N].rearrange("(b p) h d -> p b h d", p=128), v_o[:, 0:N // 128], reads=['v_o'], writes=['VV'])
            S.barrier()
            with contextlib.ExitStack() as st:
                kn = [sb(f"a_kn{b}", [128, NT], BF16, st) for b in range(2)]
                krt = [sb(f"a_kr{b}", [64, NT], BF16, st) for b in range(2)]
                qn = [sb(f"a_qn{b}", [128, NT], BF16, st) for b in range(2)]
                qrt = [sb(f"a_qr{b}", [64, NT], BF16, st) for b in range(2)]
                vt = [sb(f"a_v{b}", [128, NT // 128, 128], BF16, st) for b in range(2)]
                pT = [sb(f"a_p{b}", [128, 512], BF16, st) for b in range(3)]
                rd = sb("a_rd", [128, 512], F32, st)
                ao = [sb(f"a_o{b}", [128, 512], BF16, st) for b in range(2)]
                npt = 0
                nq = 0
                for h in range(NH):
                    b = h % 2
                    kh = ('ah', b)
                    S.dma('sp', kn[b][:], KN[h], writes=[kh])
                    S.dma('sp', krt[b][:], KR[h], writes=[kh])
                    S.dma('sp', qn[b][:], QN[h], writes=[kh])
                    S.dma('sp', qrt[b][:], QR[h], writes=[kh])
                    S.dma('sp', vt[b][:], VV[:, h, :].rearrange("(b p) d -> p b d", p=128), writes=[kh])
                    for (t0, N, mc) in TILES:
                        nkb = 2 if mc == 1 else NT // 128
                        po, pd = 4 + (nq % 2), 6 + (nq % 2)

                        def scores(kb):
                            sbk = kb % 3
                            S.op('pe', lambda e: e.matmul(ps[sbk][:, :N], lhsT=kn[b][:, kb * 128:(kb + 1) * 128], rhs=qn[b][:, t0:t0 + N],
                                                          start=True, stop=False), reads=[kh], writes=[('ps', sbk)], inc=False)
                            S.op('pe', lambda e: e.matmul(ps[sbk][:, :N], lhsT=krt[b][:, kb * 128:(kb + 1) * 128], rhs=qrt[b][:, t0:t0 + N],
                                                          start=False, stop=True), reads=[kh], writes=[('ps', sbk)])
                        scores(0)
                        if nkb > 1:
                            scores(1)
                        for kb in range(nkb):
                            if kb + 2 < nkb:
                                scores(kb + 2)
                            sbk = kb % 3
                            pb = npt % 3
                            npt += 1
                            S.op('act', lambda e: e.activation(out=pT[pb][:, :N], in_=ps[sbk][:, :N], func=AF.Exp),
                                 reads=[('ps', sbk)], writes=[('pT', pb)])
                            S.op('pe', lambda e: e.matmul(ps[po][:, :N], lhsT=vt[b][:, kb, :], rhs=pT[pb][:, :N], start=(kb == 0), stop=(kb == nkb - 1)),
                                 reads=[kh, ('pT', pb)], writes=[('ps', po)], inc=False)
                            S.op('pe', lambda e: e.matmul(ps[pd][:, :N], lhsT=ones_bf[:], rhs=pT[pb][:, :N], start=(kb == 0), stop=(kb == nkb - 1)),
                                 reads=['ones_bf', ('pT', pb)], writes=[('ps', pd)])
                        S.op('dve', lambda e: e.reciprocal(out=rd[:, :N], in_=ps[pd][:, :N]), reads=[('ps', pd)], writes=['rd'])
                        ob = nq % 2
                        S.op('dve', lambda e: e.tensor_tensor(out=ao[ob][:, :N], in0=ps[po][:, :N], in1=rd[:, :N], op=ALU.mult),
                             reads=[('ps', po), 'rd'], writes=[('ao', ob)])
                        S.dma('sp', AO[h * 128:(h + 1) * 128, t0:t0 + N], ao[ob][:, :N], reads=[('ao', ob)], writes=['AO'])
                        nq += 1
            S.barrier()
            with contextlib.ExitStack() as st:
                wo = sb("wo", [128, 8, 1024], BF16, st)
                S.dma('pool', wo[:], mla_wo[j].rearrange("(k p) n -> p k n", p=128), writes=['wo'])
                xt = [sb(f"c_xt{b}", [128, 8, 512], F32, st) for b in range(2)]
                at = [sb(f"c_at{b}", [128, 8, 512], BF16, st) for b in range(2)]
                for ti, (t0, N, mc) in enumerate(TILES):
                    b = ti % 2
                    S.dma('sp', xt[b][:, :, :N], XSin[:, t0:t0 + N].rearrange("(k p) n -> p k n", p=128), writes=[('cx', b)])
                    S.dma('sp', at[b][:, :, :N], AO[:, t0:t0 + N].rearrange("(k p) n -> p k n", p=128), writes=[('ca', b)])
                    for oc in range(8):
                        pb = oc % 4
                        for kc in range(8):
                            S.op('pe', lambda e: e.matmul(ps[pb][:, :N], lhsT=wo[:, kc, oc * 128:(oc + 1) * 128], rhs=at[b][:, kc, :N],
                                                          start=(kc == 0), stop=(kc == 7)), reads=['wo', ('ca', b)], writes=[('ps', pb)], inc=(kc == 7))
                        S.op('dve', lambda e: e.scalar_tensor_tensor(out=xt[b][:, oc, :N], in0=ps[pb][:, :N], scalar=modt[:, i, 16 + oc, mc:mc + 1],
                                                                     in1=xt[b][:, oc, :N], op0=ALU.mult, op1=ALU.add),
                             reads=[('ps', pb), ('cx', b), 'modt'], writes=[('cx', b)])
                    S.dma('sp', XS[:, t0:t0 + N].rearrange("(k p) n -> p k n", p=128), xt[b][:, :, :N], reads=[('cx', b)], writes=['XS'])
            S.barrier()

        def phase_rwkv(i, last):
            j = i // 2
            NCH = NT // 64
            RT256 = [(0, 256, 1)] + [(256 + 256 * a, 256, 0) for a in range(16)]
            HC = HS[:, 0:258]
            HL = HS[:, 258:258 + 4098]
            with contextlib.ExitStack() as st:
                xt = [sb(f"r1x{b}", [128, 8, 512], F32, st) for b in range(2)]
                sq = sb("r1sq", [128, 8, 512], F32, st)
                rs = sb("r1rs", [128, 512], F32, st)
                zt = sb("r1z", [128, 8, 1], F32, st)
                S.op('pool', lambda e: e.memset(zt[:], 0.0), writes=['zt'])
                for col in (0, 257, 258, 258 + 4097):
                    S.dma('sp', HS[:, col:col + 1].rearrange("(k p) n -> p k n", p=128), zt[:], reads=['zt'], writes=['HS'], allow_slow_non_contiguous=True)
                for ti, (t0, N, mc) in enumerate(TILES):
                    b = ti % 2
                    kx = ('r1x', b)
                    S.dma('sp', xt[b][:, :, :N], XS[:, t0:t0 + N].rearrange("(k p) n -> p k n", p=128), writes=[kx])
                    ksq = normmod(xt[b][:, :, :N], N, A1[:, i, :, mc], None, None, sq, rs, 7, kx, 'r1')
                    S.op('dve', lambda e: e.tensor_tensor(out=xt[b][:, :, :N], in0=sq[:, :, :N],
                                                          in1=modt[:, i, 0:8, mc].unsqueeze(2).to_broadcast([128, 8, N]), op=ALU.add),
                         reads=[ksq, 'modt'], writes=[kx])
                    dst = HC[:, 1:257] if mc == 1 else HL[:, 1 + t0 - CTX:1 + t0 - CTX + N]
                    S.dma('sp', dst.rearrange("(k p) n -> p k n", p=128), xt[b][:, :, :N], reads=[kx], writes=['HS'])
            S.barrier()
            if os.environ.get('RSTOP') == '1':
                return
            class _Stop(Exception):
                pass

            def stg(x):
                if os.environ.get('R2STOP') == x:
                    S.mute = True
            try:
              with contextlib.ExitStack() as st:
                  N = 256
                  wr = sb("wr", [128, 8, 1024], BF16, st); wk = sb("wk", [128, 8, 1024], BF16, st); wv = sb("wv", [128, 8, 1024], BF16, st)
                  w1c = sb("w1c", [128, 8, 128], BF16, st); a1c = sb("a1c", [128, 8, 128], BF16, st)
                  g1 = sb("g1", [128, 8, 160], BF16, st); g2a = sb("g2a", [128, 1024], BF16, st); g2b = sb("g2b", [32, 1024], BF16, st)
                  w2p = sb("w2p", [128, 2, 1024], BF16, st); a2p = sb("a2p", [128, 2, 1024], BF16, st)
                  for (wt_, src, kk_) in [(wr, rwkv_wr, 'wr'), (wk, rwkv_wk, 'wk'), (wv, rwkv_wv, 'wv')]:
                      S.dma('pool', wt_[:], src[j].rearrange("(k p) n -> p k n", p=128), writes=[kk_])
                  for d in range(2):
                      S.dma('pool', w1c[:, :, d * 64:(d + 1) * 64], rwkv_w1[j, d].rearrange("(k p) n -> p k n", p=128), writes=['w1c'])
                      S.dma('pool', a1c[:, :, d * 64:(d + 1) * 64], rwkv_a1[j, d].rearrange("(k p) n -> p k n", p=128), writes=['a1c'])
                  S.dma('pool', g1[:], rwkv_g1[j].rearrange("(k p) n -> p k n", p=128), writes=['g1'])
                  S.dma('pool', g2a[:], rwkv_g2[j, 0:128, :], writes=['g2a'])
                  S.dma('pool', g2b[:], rwkv_g2[j, 128:160, :], writes=['g2b'])
                  S.op('pool', lambda e: e.memset(w2p[:], 0.0), writes=['w2p'])
                  S.op('pool', lambda e: e.memset(a2p[:], 0.0), writes=['a2p'])
                  for d in range(2):
                      S.dma('pool', w2p[d * 64:(d + 1) * 64, d, :], rwkv_w2[j, d], writes=['w2p'])
                      S.dma('pool', a2p[d * 64:(d + 1) * 64, d, :], rwkv_a2[j, d], writes=['a2p'])
                  vres = j >= 1
                  if vres:
                      v1 = sb("v1", [128, 8, 32], BF16, st); v2 = sb("v2", [32, 1024], BF16, st)
                      S.dma('pool', v1[:], rwkv_v1[j - 1].rearrange("(k p) n -> p k n", p=128), writes=['v1'])
                      S.dma('pool', v2[:], rwkv_v2[j - 1], writes=['v2'])
                      vf = [sb(f"vf{q}", [128, N], F32, st) for q in range(2)]
                  dv_ = sb("dv", [128, 16], F32, st)
                  S.op('dve', lambda e: e.tensor_scalar(out=dv_[:, 0:8], in0=V(f"ka{j}"), scalar1=-1.0, scalar2=1.0, op0=ALU.mult, op1=ALU.add),
                       reads=['vecs'], writes=['dv'])
                  S.op('dve', lambda e: e.tensor_scalar(out=dv_[:, 8:16], in0=V(f"rk{j}"), scalar1=0.5, scalar2=None, op0=ALU.mult),
                       reads=['vecs'], writes=['dv'])
                  hx = sb("hx", [128, 8, N + 2], F32, st)
                  xx = sb("xx", [128, 8, N], F32, st)
                  xm = [sb(f"xm{m}", [128, 8, N], BF16, st) for m in range(6)]
                  lwm = sb("lwm", [128, N], BF16, st); am = sb("am", [128, N], BF16, st)
                  gma = sb("gma", [128, N], BF16, st); gmb = sb("gmb", [32, N], BF16, st); vm = sb("vm", [32, N], BF16, st)
                  T = {}
                  for nm in ["r32", "k32", "v32", "g32", "vv", "dvv", "kkc", "kk2", "rn", "kkn", "aneg", "sig", "lw", "al", "tk", "kd", "bb",
                             "Lp", "Lc", "E1", "E2", "E3", "t2", "At", "Rt", "Bt", "Kt", "kb", "t3", "bon", "vb"]:
                      T[nm] = [sb(f"f_{nm}{q}", [128, N], BF16 if nm in ("At", "Rt", "Bt", "Kt", "vb") else F32, st) for q in range(2)]
                  wct = [sb(f"wct{q}", [128, 4], F32, st) for q in range(2)]

                  def hb_(n):
                      return ps[n // 2][:, (n % 2) * 256:(n % 2) * 256 + 256]

                  def hk(n):
                      return ('psb', n // 2)

                  for ti, (t0, N_, mc) in enumerate(RT256):
                      src = HC[:, 0:258] if mc == 1 else HL[:, t0 - CTX:t0 - CTX + N + 2]
                      S.dma('sp', hx[:], src.rearrange("(k p) n -> p k n", p=128), writes=['hx'])
                      S.op('dve', lambda e: e.tensor_tensor(out=xx[:], in0=hx[:, :, 0:N], in1=hx[:, :, 2:N + 2], op=ALU.add), reads=['hx'], writes=['xx'])
                      S.op('dve', lambda e: e.scalar_tensor_tensor(out=xx[:], in0=xx[:], scalar=0.5, in1=hx[:, :, 1:N + 1], op0=ALU.mult, op1=ALU.subtract),
                           reads=['hx', 'xx'], writes=['xx'])
                      for m in range(6):
                          for kc in range(8):
                              S.op('dve', lambda e: e.scalar_tensor_tensor(out=xm[m][:, kc, :], in0=xx[:, kc, :], scalar=V(f"mix{j}", m * 8 + kc, m * 8 + kc + 1),
                                                                           in1=hx[:, kc, 1:N + 1], op0=ALU.mult, op1=ALU.add),
                                   reads=['hx', 'xx', 'vecs'], writes=[('xm', m)])
                      stg('a')
                      for kc in range(8):
                          S.op('pe', lambda e: e.matmul(hb_(0), lhsT=w1c[:, kc, :], rhs=xm[1][:, kc, :], start=(kc == 0), stop=(kc == 7)),
                               reads=['w1c', ('xm', 1)], writes=[hk(0)], inc=(kc == 7))
                      S.op('act', lambda e: e.activation(out=T["sig"][0][:], in_=hb_(0), func=AF.Sigmoid, scale=2.0), reads=[hk(0)], writes=['sig0'])
                      S.op('dve', lambda e: e.tensor_scalar(out=lwm[:], in0=T["sig"][0][:], scalar1=2.0, scalar2=-1.0, op0=ALU.mult, op1=ALU.add),
                           reads=['sig0'], writes=['lwm'])
                      for kc in range(8):
                          S.op('pe', lambda e: e.matmul(hb_(1), lhsT=a1c[:, kc, :], rhs=xm[4][:, kc, :], start=(kc == 0), stop=(kc == 7)),
                               reads=['a1c', ('xm', 4)], writes=[hk(1)], inc=(kc == 7))
                      S.op('act', lambda e: e.activation(out=am[:], in_=hb_(1), func=AF.Identity), reads=[hk(1)], writes=['am'])
                      for kc in range(8):
                          S.op('pe', lambda e: e.matmul(hb_(2), lhsT=g1[:, kc, 0:128], rhs=xm[5][:, kc, :], start=(kc == 0), stop=(kc == 7)),
                               reads=['g1', ('xm', 5)], writes=[hk(2)], inc=(kc == 7))
                      S.op('act', lambda e: e.activation(out=gma[:], in_=hb_(2), func=AF.Sigmoid), reads=[hk(2)], writes=['gma'])
                      for kc in range(8):
                          S.op('pe', lambda e: e.matmul(hb_(3)[0:32, :], lhsT=g1[:, kc, 128:160], rhs=xm[5][:, kc, :], start=(kc == 0), stop=(kc == 7)),
                               reads=['g1', ('xm', 5)], writes=[hk(3)], inc=(kc == 7))
                      S.op('act', lambda e: e.activation(out=gmb[:], in_=hb_(3)[0:32, :], func=AF.Sigmoid), reads=[hk(3)], writes=['gmb'])
                      if vres:
                          for kc in range(8):
                              S.op('pe', lambda e: e.matmul(hb_(4)[0:32, :], lhsT=v1[:, kc, :], rhs=xm[3][:, kc, :], start=(kc == 0), stop=(kc == 7)),
                                   reads=['v1', ('xm', 3)], writes=[hk(4)], inc=(kc == 7))
                          S.op('act', lambda e: e.activation(out=vm[:], in_=hb_(4)[0:32, :], func=AF.Identity), reads=[hk(4)], writes=['vm'])
                      def chunk_gen(c):
                          pc = c % 2
                          pcs = str(pc)
                          H = {5: 8 * pc, 6: 8 * pc + 1, 7: 8 * pc + 2, 8: 8 * pc + 3, 9: 8 * pc + 4, 10: 8 * pc + 5, 11: 8 * pc + 6, 12: 8 * pc + 7,
                               13: 8 * pc, 14: 8 * pc + 2, 15: 8 * pc + 3}
                          cs = slice(c * 128, (c + 1) * 128)
                          rows = slice(c * 128, (c + 1) * 128)
                          for (hbn, w_, m) in [(5, wr, 0), (6, wk, 2), (7, wv, 3)]:
                              for kc in range(8):
                                  S.op('pe', lambda e: e.matmul(hb_(H[hbn]), lhsT=w_[:, kc, cs], rhs=xm[m][:, kc, :], start=(kc == 0), stop=(kc == 7)),
                                       reads=[('xm', m), 'wr', 'wk', 'wv'], writes=[hk(H[hbn])], inc=(kc == 7))
                          S.op('pe', lambda e: e.matmul(hb_(H[8]), lhsT=g2a[:, cs], rhs=gma[:], start=True, stop=False), reads=['g2a', 'gma'], writes=[hk(H[8])], inc=False)
                          S.op('pe', lambda e: e.matmul(hb_(H[8]), lhsT=g2b[:, cs], rhs=gmb[:], start=False, stop=True), reads=['g2b', 'gmb'], writes=[hk(H[8])])
                          for d in range(2):
                              S.op('pe', lambda e: e.matmul(hb_(H[9 + d]), lhsT=w2p[:, d, cs], rhs=lwm[:], start=True, stop=True), reads=['w2p', 'lwm'], writes=[hk(H[9 + d])])
                              S.op('pe', lambda e: e.matmul(hb_(H[11 + d]), lhsT=a2p[:, d, cs], rhs=am[:], start=True, stop=True), reads=['a2p', 'am'], writes=[hk(H[11 + d])])
                          yield
                          S.op('act', lambda e: e.activation(out=T["r32"][pc][:], in_=hb_(H[5]), func=AF.Identity), reads=[hk(H[5])], writes=['r32' + pcs])
                          S.op('act', lambda e: e.activation(out=T["k32"][pc][:], in_=hb_(H[6]), func=AF.Identity), reads=[hk(H[6])], writes=['k32' + pcs])
                          S.op('act', lambda e: e.activation(out=T["v32"][pc][:], in_=hb_(H[7]), func=AF.Identity), reads=[hk(H[7])], writes=['v32' + pcs])
                          S.op('act', lambda e: e.activation(out=T["g32"][pc][:], in_=hb_(H[8]), func=AF.Identity), reads=[hk(H[8])], writes=['g32' + pcs])
                          if vres:
                              S.op('pe', lambda e: e.matmul(hb_(H[13]), lhsT=v2[:, cs], rhs=vm[:], start=True, stop=True), reads=['v2', 'vm'], writes=[hk(H[13])])
                              S.dma('sp', vf[pc][:], VT[0][rows, t0:t0 + N], writes=['vf' + pcs])
                              S.op('act', lambda e: e.activation(out=T["vv"][pc][:], in_=hb_(H[13]), func=AF.Sigmoid, bias=V(f"v0{j}", c, c + 1), scale=1.0),
                                   reads=[hk(H[13]), 'vecs'], writes=['vv' + pcs])
                              S.op('pool', lambda e: e.tensor_tensor(out=T["dvv"][pc][:], in0=vf[pc][:], in1=T["v32"][pc][:], op=ALU.subtract), reads=['vf' + pcs, 'v32' + pcs], writes=['dvv' + pcs])
                              S.op('pool', lambda e: e.tensor_tensor(out=T["dvv"][pc][:], in0=T["dvv"][pc][:], in1=T["vv"][pc][:], op=ALU.mult), reads=['dvv' + pcs, 'vv' + pcs], writes=['dvv' + pcs])
                              S.op('pool', lambda e: e.tensor_tensor(out=T["v32"][pc][:], in0=T["v32"][pc][:], in1=T["dvv"][pc][:], op=ALU.add), reads=['dvv' + pcs, 'v32' + pcs], writes=['v32' + pcs])
                          yield
                          S.op('dve', lambda e: e.tensor_scalar(out=T["kkc"][pc][:], in0=T["k32"][pc][:], scalar1=V(f"kk{j}", c, c + 1), scalar2=None, op0=ALU.mult),
                               reads=['k32' + pcs, 'vecs'], writes=['kkc' + pcs])
                          S.op('pool', lambda e: e.tensor_tensor(out=T["kk2"][pc][:], in0=T["kkc"][pc][:], in1=T["kkc"][pc][:], op=ALU.mult), reads=['kkc' + pcs], writes=['kk2' + pcs])
                          S.op('pe', lambda e: e.matmul(hb_(H[14]), lhsT=blockones, rhs=T["kk2"][pc][:], start=True, stop=True), reads=['kk2' + pcs, 'c32'], writes=[hk(H[14])])
                          yield
                          S.op('act', lambda e: e.activation(out=T["rn"][pc][:], in_=hb_(H[14]), func=AF.Sqrt), reads=[hk(H[14])], writes=['rn' + pcs])
                          S.op('dve', lambda e: e.tensor_scalar(out=T["rn"][pc][:], in0=T["rn"][pc][:], scalar1=1e-12, scalar2=None, op0=ALU.max), reads=['rn' + pcs], writes=['rn' + pcs])
                          S.op('dve', lambda e: e.reciprocal(out=T["rn"][pc][:], in_=T["rn"][pc][:]), reads=['rn' + pcs], writes=['rn' + pcs])
                          yield
                          S.op('pool', lambda e: e.tensor_tensor(out=T["kkn"][pc][:], in0=T["kkc"][pc][:], in1=T["rn"][pc][:], op=ALU.mult), reads=['kkc' + pcs, 'rn' + pcs], writes=['kkn' + pcs])
                          S.op('pool', lambda e: e.tensor_scalar(out=T["aneg"][pc][:], in0=T["kkn"][pc][:], scalar1=-1.0, scalar2=None, op0=ALU.mult), reads=['kkn' + pcs], writes=['aneg' + pcs])
                          for d in range(2):
                              yield
                              S.op('act', lambda e: e.activation(out=T["sig"][pc][:], in_=hb_(H[9 + d]), func=AF.Sigmoid, bias=V(f"w0{j}", d * 8 + c, d * 8 + c + 1), scale=1.0),
                                   reads=[hk(H[9 + d]), 'vecs'], writes=['sig' + pcs])
                              S.op('pool', lambda e: e.tensor_scalar(out=T["lw"][pc][:], in0=T["sig"][pc][:], scalar1=float(-np.exp(-0.5)), scalar2=None, op0=ALU.mult),
                                   reads=['sig' + pcs], writes=['lw' + pcs])
                              S.op('act', lambda e: e.activation(out=T["al"][pc][:], in_=hb_(H[11 + d]), func=AF.Sigmoid, bias=V(f"a0{j}", d * 8 + c, d * 8 + c + 1), scale=1.0),
                                   reads=[hk(H[11 + d]), 'vecs'], writes=['al' + pcs])
                              yield
                              S.op('dve', lambda e: e.tensor_scalar(out=T["tk"][pc][:], in0=T["al"][pc][:], scalar1=V(f"ka{j}", c, c + 1), scalar2=dv_[:, c:c + 1],
                                                                    op0=ALU.mult, op1=ALU.add), reads=['al' + pcs, 'vecs', 'dv'], writes=['tk' + pcs])
                              S.op('pool', lambda e: e.tensor_tensor(out=T["kd"][pc][:], in0=T["k32"][pc][:], in1=T["tk"][pc][:], op=ALU.mult), reads=['k32' + pcs, 'tk' + pcs], writes=['kd' + pcs])
                              S.op('pool', lambda e: e.tensor_tensor(out=T["bb"][pc][:], in0=T["kkn"][pc][:], in1=T["al"][pc][:], op=ALU.mult), reads=['kkn' + pcs, 'al' + pcs], writes=['bb' + pcs])
                              yield
                              S.op('dve', lambda e: e.tensor_tensor_scan(out=T["Lp"][pc][:], data0=cmask[:, :N], data1=T["lw"][pc][:], initial=0.0, op0=ALU.mult, op1=ALU.add),
                                   reads=['lw' + pcs, 'c32'], writes=['Lp' + pcs])
                              if d == 0:
                                  Lc, kLc = T["Lp"][pc], 'Lp' + pcs
                              else:
                                  S.op('pool', lambda e: e.tensor_tensor(out=T["Lc"][pc][:], in0=T["lw"][pc][:], in1=T["Lp"][pc][:], op=ALU.subtract), reads=['lw' + pcs, 'Lp' + pcs], writes=['Lc' + pcs])
                                  S.op('pool', lambda e: e.tensor_tensor(
                                      out=T["Lc"][pc][:].rearrange("p (c t) -> p c t", t=64), in0=T["Lc"][pc][:].rearrange("p (c t) -> p c t", t=64),
                                      in1=T["Lp"][pc][:].rearrange("p (c t) -> p c t", t=64)[:, :, 63:64].to_broadcast([128, N // 64, 64]), op=ALU.add),
                                      reads=['Lc' + pcs, 'Lp' + pcs], writes=['Lc' + pcs])
                                  Lc, kLc = T["Lc"][pc], 'Lc' + pcs
                              yield
                              S.op('act', lambda e: e.activation(out=T["E1"][pc][:], in_=Lc[:], func=AF.Exp), reads=[kLc], writes=['E1' + pcs])
                              S.op('act', lambda e: e.activation(out=T["E2"][pc][:], in_=Lc[:], func=AF.Exp, scale=-1.0), reads=[kLc], writes=['E2' + pcs])
                              S.op('pool', lambda e: e.tensor_tensor(out=T["t2"][pc][:], in0=Lc[:], in1=T["lw"][pc][:], op=ALU.subtract), reads=[kLc, 'lw' + pcs], writes=['t2' + pcs])
                              S.op('act', lambda e: e.activation(out=T["E3"][pc][:], in_=T["t2"][pc][:], func=AF.Exp), reads=['t2' + pcs], writes=['E3' + pcs])
                              yield
                              S.op('pool', lambda e: e.tensor_tensor(out=T["At"][pc][:], in0=T["aneg"][pc][:], in1=T["E3"][pc][:], op=ALU.mult), reads=['aneg' + pcs, 'E3' + pcs], writes=['At' + pcs])
                              S.op('pool', lambda e: e.tensor_tensor(out=T["Rt"][pc][:], in0=T["r32"][pc][:], in1=T["E1"][pc][:], op=ALU.mult), reads=['r32' + pcs, 'E1' + pcs], writes=['Rt' + pcs])
                              S.op('dve', lambda e: e.tensor_tensor(out=T["Bt"][pc][:], in0=T["bb"][pc][:], in1=T["E2"][pc][:], op=ALU.mult), reads=['bb' + pcs, 'E2' + pcs], writes=['Bt' + pcs])
                              S.op('dve', lambda e: e.tensor_tensor(out=T["Kt"][pc][:], in0=T["kd"][pc][:], in1=T["E2"][pc][:], op=ALU.mult), reads=['kd' + pcs, 'E2' + pcs], writes=['Kt' + pcs])
                              wcol = 63 if d == 0 else 0
                              S.op('dve', lambda e: e.tensor_copy(out=wct[pc][:, 0:N // 64], in_=T["E1"][pc][:].rearrange("p (c t) -> p c t", t=64)[:, :, wcol]),
                                   reads=['E1' + pcs], writes=['wct' + pcs])
                              yield
                              S.dma('sp', ATd[d][rows, t0:t0 + N], T["At"][pc][:], reads=['At' + pcs], writes=['ATd'])
                              S.dma('sp', RTd[d][rows, t0:t0 + N], T["Rt"][pc][:], reads=['Rt' + pcs], writes=['RTd'])
                              S.dma('sp', BTd[d][rows, t0:t0 + N], T["Bt"][pc][:], reads=['Bt' + pcs], writes=['BTd'])
                              S.dma('sp', KTd[d][rows, t0:t0 + N], T["Kt"][pc][:], reads=['Kt' + pcs], writes=['KTd'])
                              S.dma('sp', WCd[d][rows, t0 // 64:t0 // 64 + N // 64], wct[pc][:, 0:N // 64], reads=['wct' + pcs], writes=['WCd'])
                              if d == 0:
                                  S.op('pool', lambda e: e.tensor_copy(out=T["kb"][pc][:], in_=T["kd"][pc][:]), reads=['kd' + pcs], writes=['kb' + pcs])
                              else:
                                  S.op('pool', lambda e: e.tensor_tensor(out=T["kb"][pc][:], in0=T["kb"][pc][:], in1=T["kd"][pc][:], op=ALU.add), reads=['kd' + pcs, 'kb' + pcs], writes=['kb' + pcs])
                          yield
                          S.op('pool', lambda e: e.tensor_tensor(out=T["t3"][pc][:], in0=T["r32"][pc][:], in1=T["kb"][pc][:], op=ALU.mult), reads=['r32' + pcs, 'kb' + pcs], writes=['t3' + pcs])
                          S.op('dve', lambda e: e.tensor_scalar(out=T["t3"][pc][:], in0=T["t3"][pc][:], scalar1=dv_[:, 8 + c:9 + c], scalar2=None, op0=ALU.mult),
                               reads=['t3' + pcs, 'dv'], writes=['t3' + pcs])
                          S.op('pe', lambda e: e.matmul(hb_(H[15]), lhsT=blockones, rhs=T["t3"][pc][:], start=True, stop=True), reads=['t3' + pcs, 'c32'], writes=[hk(H[15])])
                          yield
                          S.op('dve', lambda e: e.tensor_tensor(out=T["bon"][pc][:], in0=hb_(H[15]), in1=T["v32"][pc][:], op=ALU.mult), reads=[hk(H[15]), 'v32' + pcs], writes=['bon' + pcs])
                          S.dma('sp', BON[rows, t0:t0 + N], T["bon"][pc][:], reads=['bon' + pcs], writes=['BON'])
                          S.dma('sp', VT[j][rows, t0:t0 + N], T["v32"][pc][:], reads=['v32' + pcs], writes=['VT'])
                          S.op('act', lambda e: e.activation(out=T["vb"][pc][:], in_=T["v32"][pc][:], func=AF.Identity), reads=['v32' + pcs], writes=['vb' + pcs])
                          S.dma('sp', VTb[rows, t0:t0 + N], T["vb"][pc][:], reads=['vb' + pcs], writes=['VTb'])
                          S.dma('sp', GG[rows, t0:t0 + N], T["g32"][pc][:], reads=['g32' + pcs], writes=['GG'])

                      pend = [chunk_gen(c) for c in range(8)]
                      act_g = []
                      while pend or act_g:
                          while pend and len(act_g) < int(os.environ.get("R2WIN", "2")):
                              act_g.append(pend.pop(0))
                          for g_ in list(act_g):
                              try:
                                  next(g_)
                              except StopIteration:
                                  act_g.remove(g_)
            except _Stop:
                pass
            S.mute = False
            S.barrier()
            if os.environ.get('RSTOP') == '2':
                return
            if True:
                with contextlib.ExitStack() as st:
                    def stream(d, hh):
                        order = ([0, 1, 2, 3] + list(range(4, NCH))) if d == 0 else ([3, 2, 1, 0] + list(range(NCH - 1, 3, -1)))
                        mAR = maskAR_f if d == 0 else maskAR_r
                        mN = maskN_f if d == 0 else maskN_r
                        sid = 2 * d + hh
                        P_ = f"s{sid}_"
                        pb = [ps[2 * sid], ps[2 * sid + 1]]

                        def pk(bank, half=None):
                            return [('sps', sid, bank)]
                        BD = {n: sb(P_ + n, [128, 4, 128], BF16, st) for n in ["bdA", "T2", "T3", "T4", "tbB", "tbK", "tbV", "T8", "bdMak", "bdU", "bdST"]}
                        for n, t_ in BD.items():
                            S.op('pool', lambda e: e.memset(t_[:], 0.0), writes=[P_ + n])
                        AR = [sb(P_ + f"AR{b}", [128, 4, 2, 64], BF16, st) for b in range(2)]
                        Bi = [sb(P_ + f"Bi{b}", [128, 4, 64], BF16, st) for b in range(2)]
                        Ki = [sb(P_ + f"Ki{b}", [128, 4, 64], BF16, st) for b in range(2)]
                        Vi = [sb(P_ + f"Vi{b}", [128, 4, 64], BF16, st) for b in range(2)]
                        WCall = sb(P_ + "WCall", [128, 4, NCH], F32, st)
                        S.dma('sp', WCall[:], WCd[d][hh * 512:hh * 512 + 512, :].rearrange("(c p) n -> p c n", p=128), writes=[P_ + "WCall"])
                        ARm = sb(P_ + "ARm", [128, 4, 128], BF16, st)
                        AKm = sb(P_ + "AKm", [128, 4, 128], BF16, st)
                        Ns = sb(P_ + "Ns", [128, 4, 64], BF16, st)
                        Vs = sb(P_ + "Vs", [128, 4, 64], BF16, st)
                        PG = [sb(P_ + f"PG{b}", [128, 4, 128], BF16, st) for b in range(2)]
                        Pst = [sb(P_ + f"Pst{b}", [128, 4, 64], BF16, st) for b in range(2)]
                        Xs = sb(P_ + "Xs", [128, 4, 64], BF16, st); Us = sb(P_ + "Us", [128, 4, 64], BF16, st)
                        Yb = sb(P_ + "Yb", [128, 4, 64], F32, st); STs = sb(P_ + "STs", [128, 4, 64], F32, st)
                        tmpS = sb(P_ + "tmpS", [128, 4, 64], F32, st)
                        STb = sb(P_ + "STb", [128, 4, 64], BF16, st)
                        S.op('pool', lambda e: e.memset(STb[:], 0.0), writes=[P_ + "STb"])
                        S.op('pool', lambda e: e.memset(STs[:], 0.0), writes=[P_ + "STs"])
                        r0 = hh * 512

                        def diag(eng, dst, kdst, src, ksrc, cols=64):
                            for half in range(2):
                                pslice = slice(half * 64, half * 64 + 64)
                                if eng == 'act':
                                    S.op('act', lambda e: e.activation(out=dst[pslice, :, half * 64:half * 64 + 64], in_=src[pslice], func=AF.Identity),
                                         reads=ksrc, writes=[kdst])
                                else:
                                    S.op(eng, lambda e: e.tensor_copy(out=dst[pslice, :, half * 64:half * 64 + 64], in_=src[pslice]),
                                         reads=ksrc, writes=[kdst])

                        def load(n):
                            g = order[n]
                            b = n % 2
                            cols = slice(g * 64, g * 64 + 64)
                            kin = P_ + f"in{b}"
                            for (dst, srcd) in [(AR[b][:, :, 0, :], ATd[d]), (AR[b][:, :, 1, :], RTd[d]), (Bi[b][:], BTd[d]), (Ki[b][:], KTd[d]), (Vi[b][:], VTb)]:
                                S.dma('sp', dst, srcd[r0:r0 + 512, cols].rearrange("(c p) n -> p c n", p=128), writes=[kin])

                        load(0)
                        for n in range(NCH):
                            g = order[n]
                            b = n % 2
                            kin = P_ + f"in{b}"
                            if n + 1 < NCH:
                                load(n + 1)
                            diag('dve', BD["bdA"], P_ + "bdA", AR[b][:, :, 0, :], [kin])
                            diag('dve', BD["T2"], P_ + "T2", Bi[b], [kin])
                            diag('act', BD["T3"], P_ + "T3", Ki[b], [kin])
                            diag('act', BD["T4"], P_ + "T4", Vi[b], [kin])
                            ARf = AR[b][:].rearrange("p c a t -> p c (a t)")
                            for hp in range(4):
                                S.op('pe', lambda e: e.matmul(pb[0][:, hp * 128:(hp + 1) * 128], lhsT=BD["T2"][:, hp, :], rhs=ARf[:, hp, :], start=True, stop=True),
                                     reads=[P_ + "T2", kin], writes=pk(0), inc=(hp == 3))
                            for hp in range(4):
                                S.op('pe', lambda e: e.matmul(pb[1][:, hp * 64:(hp + 1) * 64], lhsT=BD["bdA"][:, hp, :], rhs=Bi[b][:, hp, :], start=True, stop=True),
                                     reads=[P_ + "bdA", kin], writes=pk(1), inc=(hp == 3))
                            yield
                            S.op('dve', lambda e: e.tensor_tensor(out=ARm[:], in0=pb[0][:].rearrange("p (c t) -> p c t", t=128),
                                                                  in1=mAR.unsqueeze(1).to_broadcast([128, 4, 128]), op=ALU.mult),
                                 reads=pk(0) + ['c32'], writes=[P_ + "ARm"])
                            S.op('dve', lambda e: e.tensor_tensor(out=Pst[0][:], in0=pb[1][:, 0:256].rearrange("p (c t) -> p c t", t=64),
                                                                  in1=mN.unsqueeze(1).to_broadcast([128, 4, 64]), op=ALU.mult),
                                 reads=pk(1) + ['c32'], writes=[P_ + "Pst0"])
                            for hp in range(4):
                                S.op('pe', lambda e: e.matmul(pb[0][:, hp * 128:(hp + 1) * 128], lhsT=BD["T3"][:, hp, :], rhs=ARf[:, hp, :], start=True, stop=True),
                                     reads=[P_ + "T3", kin], writes=pk(0), inc=(hp == 3))
                            for hp in range(4):
                                S.op('pe', lambda e: e.matmul(pb[1][:, hp * 128:(hp + 1) * 128], lhsT=BD["T2"][:, hp, :], rhs=ident_bf[:], start=True, stop=True),
                                     reads=[P_ + "T2", 'ident_bf'], writes=pk(1), inc=(hp == 3))
                            yield
                            S.op('dve', lambda e: e.tensor_tensor(out=AKm[:], in0=pb[0][:].rearrange("p (c t) -> p c t", t=128),
                                                                  in1=mAR.unsqueeze(1).to_broadcast([128, 4, 128]), op=ALU.mult),
                                 reads=pk(0) + ['c32'], writes=[P_ + "AKm"])
                            S.op('act', lambda e: e.activation(out=BD["tbB"][:], in_=pb[1][:].rearrange("p (c t) -> p c t", t=128), func=AF.Identity),
                                 reads=pk(1), writes=[P_ + "tbB"])
                            for hp in range(4):
                                S.op('pe', lambda e: e.matmul(pb[0][:, hp * 128:(hp + 1) * 128], lhsT=BD["T3"][:, hp, :], rhs=ident_bf[:], start=True, stop=True),
                                     reads=[P_ + "T3", 'ident_bf'], writes=pk(0), inc=(hp == 3))
                            for hp in range(4):
                                S.op('pe', lambda e: e.matmul(pb[1][:, hp * 128:(hp + 1) * 128], lhsT=BD["T4"][:, hp, :], rhs=ident_bf[:], start=True, stop=True),
                                     reads=[P_ + "T4", 'ident_bf'], writes=pk(1), inc=(hp == 3))
                            S.op('pool', lambda e: e.tensor_copy(out=PG[0][:, :, 0:64], in_=ARm[:, :, 0:64]), reads=[P_ + "ARm"], writes=[P_ + "PG0"])
                            S.op('pool', lambda e: e.tensor_copy(out=PG[0][:, :, 64:128], in_=SI.unsqueeze(1).to_broadcast([128, 4, 64])),
                                 reads=['c32'], writes=[P_ + "PG0"])
                            diag('pool', BD["bdMak"], P_ + "bdMak", AKm[:, :, 0:64], [P_ + "AKm"])
                            yield
                            S.op('act', lambda e: e.activation(out=BD["tbK"][:], in_=pb[0][:].rearrange("p (c t) -> p c t", t=128), func=AF.Identity),
                                 reads=pk(0), writes=[P_ + "tbK"])
                            S.op('act', lambda e: e.activation(out=BD["tbV"][:], in_=pb[1][:].rearrange("p (c t) -> p c t", t=128), func=AF.Identity),
                                 reads=pk(1), writes=[P_ + "tbV"])
                            for half in range(2):
                                pslice = slice(half * 64, half * 64 + 64)
                                S.op('pool', lambda e: e.tensor_copy(out=Vs[pslice], in_=BD["tbV"][pslice, :, half * 64:half * 64 + 64]),
                                     reads=[P_ + "tbV"], writes=[P_ + "Vs"])
                            diag('pool', BD["T2"], P_ + "T2", Pst[0], [P_ + "Pst0"])
                            diag('pool', BD["T3"], P_ + "T3", PG[0][:, :, 0:64], [P_ + "PG0"])
                            sets = [("T2", "T3"), ("T4", "T8")]
                            for lv in range(6):
                                cur, nxt = lv % 2, (lv + 1) % 2
                                bP, bPT = sets[cur]
                                nP, nPT = sets[nxt]
                                kPG, kPGn = P_ + f"PG{cur}", P_ + f"PG{nxt}"
                                if lv < 5:
                                    for hp in range(4):
                                        S.op('pe', lambda e: e.matmul(pb[1][:, hp * 64:(hp + 1) * 64], lhsT=BD[bPT][:, hp, :], rhs=Pst[cur][:, hp, :], start=True, stop=True),
                                             reads=[P_ + bPT, P_ + f"Pst{cur}"], writes=pk(1, 0), inc=(hp == 3))
                                    for hp in range(4):
                                        S.op('pe', lambda e: e.matmul(pb[0][:, hp * 128:(hp + 1) * 128], lhsT=BD[bP][:, hp, :], rhs=PG[cur][:, hp, :], start=True, stop=True),
                                             reads=[P_ + bP, kPG], writes=pk(0), inc=(hp == 3))
                                else:
                                    for hp in range(4):
                                        S.op('pe', lambda e: e.matmul(pb[0][:, hp * 128 + 64:(hp + 1) * 128], lhsT=BD[bP][:, hp, :], rhs=PG[cur][:, hp, 64:128], start=True, stop=True),
                                             reads=[P_ + bP, kPG], writes=pk(0), inc=(hp == 3))
                                yield
                                psv = pb[0][:].rearrange("p (c t) -> p c t", t=128)
                                S.op('dve', lambda e: e.tensor_tensor(out=PG[nxt][:, :, 64:128], in0=PG[cur][:, :, 64:128], in1=psv[:, :, 64:128], op=ALU.add),
                                     reads=pk(0) + [kPG], writes=[kPGn])
                                if lv < 5:
                                    S.op('act', lambda e: e.activation(out=PG[nxt][:, :, 0:64], in_=psv[:, :, 0:64], func=AF.Identity), reads=pk(0), writes=[kPGn])
                                    S.op('dve', lambda e: e.tensor_copy(out=Pst[nxt][:], in_=pb[1][:, 0:256].rearrange("p (c t) -> p c t", t=64)),
                                         reads=pk(1, 0), writes=[P_ + f"Pst{nxt}"])
                                    diag('act', BD[nP], P_ + nP, Pst[nxt], [P_ + f"Pst{nxt}"])
                                    if lv < 4:
                                        diag('pool', BD[nPT], P_ + nPT, PG[nxt][:, :, 0:64], [kPGn])
                            diag('pool', BD["T8"], P_ + "T8", PG[0][:, :, 64:128], [P_ + "PG0"])
                            for hp in range(4):
                                S.op('pe', lambda e: e.matmul(pb[1][:, 256 + hp * 64:256 + (hp + 1) * 64], lhsT=BD["bdA"][:, hp, :], rhs=STb[:, hp, :], start=True, stop=False),
                                     reads=[P_ + "bdA", P_ + "STb"], writes=pk(1), inc=False)
                                S.op('pe', lambda e: e.matmul(pb[1][:, 256 + hp * 64:256 + (hp + 1) * 64], lhsT=BD["bdMak"][:, hp, :], rhs=Vs[:, hp, :], start=False, stop=True),
                                     reads=[P_ + "bdMak", P_ + "Vs"], writes=pk(1), inc=(hp == 3))
                            yield
                            S.op('act', lambda e: e.activation(out=Xs[:], in_=pb[1][:, 256:512].rearrange("p (c t) -> p c t", t=64), func=AF.Identity),
                                 reads=pk(1), writes=[P_ + "Xs"])
                            for hp in range(4):
                                S.op('pe', lambda e: e.matmul(pb[0][:, hp * 64:(hp + 1) * 64], lhsT=BD["T8"][:, hp, :], rhs=Xs[:, hp, :], start=True, stop=True),
                                     reads=[P_ + "T8", P_ + "Xs"], writes=pk(0), inc=(hp == 3))
                            yield
                            S.op('dve', lambda e: e.tensor_copy(out=Us[:], in_=pb[0][:, 0:256].rearrange("p (c t) -> p c t", t=64)), reads=pk(0), writes=[P_ + "Us"])
                            diag('act', BD["bdU"], P_ + "bdU", pb[0][:, 0:256].rearrange("p (c t) -> p c t", t=64), pk(0))
                            for hp in range(4):
                                S.op('pe', lambda e: e.matmul(pb[0][:, 256 + hp * 64:256 + (hp + 1) * 64], lhsT=BD["bdST"][:, hp, :], rhs=AR[b][:, hp, 1, :], start=True, stop=False),
                                     reads=[P_ + "bdST", kin], writes=pk(0), inc=False)
                                S.op('pe', lambda e: e.matmul(pb[0][:, 256 + hp * 64:256 + (hp + 1) * 64], lhsT=BD["bdU"][:, hp, :], rhs=ARm[:, hp, 64:128], start=False, stop=False),
                                     reads=[P_ + "bdU", P_ + "ARm"], writes=pk(0), inc=False)
                                S.op('pe', lambda e: e.matmul(pb[0][:, 256 + hp * 64:256 + (hp + 1) * 64], lhsT=BD["tbV"][:, hp, :], rhs=AKm[:, hp, 64:128], start=False, stop=True),
                                     reads=[P_ + "tbV", P_ + "AKm"], writes=pk(0), inc=(hp == 3))
                            for hp in range(4):
                                S.op('pe', lambda e: e.matmul(pb[1][:, 256 + hp * 64:256 + (hp + 1) * 64], lhsT=BD["tbB"][:, hp, :], rhs=Us[:, hp, :], start=True, stop=False),
                                     reads=[P_ + "tbB", P_ + "Us"], writes=pk(1, 1), inc=False)
                                S.op('pe', lambda e: e.matmul(pb[1][:, 256 + hp * 64:256 + (hp + 1) * 64], lhsT=BD["tbK"][:, hp, :], rhs=Vs[:, hp, :], start=False, stop=True),
                                     reads=[P_ + "tbK", P_ + "Vs"], writes=pk(1, 1), inc=(hp == 3))
                            yield
                            S.op('act', lambda e: e.activation(out=Yb[:], in_=pb[0][:, 256:512].rearrange("p (c t) -> p c t", t=64), func=AF.Identity),
                                 reads=pk(0), writes=[P_ + "Yb"])
                            S.dma('sp', YD[d][r0:r0 + 512, g * 64:g * 64 + 64].rearrange("(c p) n -> p c n", p=128), Yb[:], reads=[P_ + "Yb"], writes=['YD'])
                            S.op('dve', lambda e: e.tensor_tensor(out=tmpS[:], in0=pb[1][:, 256:512].rearrange("p (c t) -> p c t", t=64), in1=STs[:], op=ALU.add),
                                 reads=pk(1, 1) + [P_ + "STs"], writes=[P_ + "tmpS"])
                            S.op('dve', lambda e: e.tensor_tensor(out=STs[:], in0=tmpS[:], in1=WCall[:, :, g:g + 1].to_broadcast([128, 4, 64]), op=ALU.mult),
                                 reads=[P_ + "tmpS", P_ + "WCall"], writes=[P_ + "STs"])
                            diag('pool', BD["bdST"], P_ + "bdST", STs, [P_ + "STs"])
                            S.op('act', lambda e: e.activation(out=STb[:], in_=STs[:], func=AF.Identity), reads=[P_ + "STs"], writes=[P_ + "STb"])
                            yield

                    gens = [stream(0, 0), stream(0, 1), stream(1, 0), stream(1, 1)]
                    alive = [True] * 4
                    while any(alive):
                        for q in range(4):
                            if alive[q]:
                                try:
                                    next(gens[q])
                                except StopIteration:
                                    alive[q] = False
                S.barrier()
            if os.environ.get('RSTOP') == '3':
                return
            with contextlib.ExitStack() as st:
                wo = sb("r_wo", [128, 8, 1024], BF16, st)
                S.dma('pool', wo[:], rwkv_wo[j].rearrange("(k p) n -> p k n", p=128), writes=['r_wo'])
                y0 = sb("r_y0", [128, 8, 512], F32, st); y1 = sb("r_y1", [128, 8, 512], F32, st)
                bo = sb("r_bo", [128, 8, 512], F32, st); gg = sb("r_gg", [128, 8, 512], F32, st)
                xt = sb("r_xt", [128, 8, 512], F32, st)
                ob = sb("r_ob", [128, 8, 512], BF16, st)
                yc_ = [sb(f"r_yc{q}", [128, 512], F32, st) for q in range(2)]; y2_ = [sb(f"r_y2{q}", [128, 512], F32, st) for q in range(2)]; sd_ = [sb(f"r_sd{q}", [128, 512], F32, st) for q in range(2)]
                tiles = TILES[1:] if last else TILES
                for ti, (t0, N, mc) in enumerate(tiles):
                    for (dst, srcd, kk_) in [(y0, YD[0], 'y0'), (y1, YD[1], 'y1'), (bo, BON, 'bo'), (gg, GG, 'gg'), (xt, XS, 'r_xt')]:
                        S.dma('sp', dst[:, :, :N], srcd[:, t0:t0 + N].rearrange("(k p) n -> p k n", p=128), writes=[kk_])
                    for c in range(8):
                        pa, pv = c % 2, 2 + c % 2
                        yc, y2, sd = yc_[c % 2], y2_[c % 2], sd_[c % 2]
                        cq_ = str(c % 2)
                        S.op('dve', lambda e: e.tensor_tensor(out=y0[:, c, :N], in0=y0[:, c, :N], in1=y1[:, c, :N], op=ALU.add), reads=['y0', 'y1'], writes=['y0'])
                        S.op('pe', lambda e: e.matmul(ps[pa][:, :N], lhsT=blockones, rhs=y0[:, c, :N], start=True, stop=True), reads=['y0', 'c32'], writes=[('ps', pa)])
                        S.op('dve', lambda e: e.scalar_tensor_tensor(out=yc[:, :N], in0=ps[pa][:, :N], scalar=-1.0 / 64, in1=y0[:, c, :N], op0=ALU.mult, op1=ALU.add),
                             reads=[('ps', pa), 'y0'], writes=['yc' + cq_])
                        S.op('pool', lambda e: e.tensor_tensor(out=y2[:, :N], in0=yc[:, :N], in1=yc[:, :N], op=ALU.mult), reads=['yc' + cq_], writes=['y2' + cq_])
                        S.op('pe', lambda e: e.matmul(ps[pv][:, :N], lhsT=blockones, rhs=y2[:, :N], start=True, stop=True), reads=['y2' + cq_, 'c32'], writes=[('ps', pv)])
                        S.op('act', lambda e: e.activation(out=sd[:, :N], in_=ps[pv][:, :N], func=AF.Sqrt, bias=epsT[:, 5:6], scale=1.0 / 64),
                             reads=[('ps', pv), 'epsT'], writes=['sd' + cq_])
                        S.op('dve', lambda e: e.reciprocal(out=sd[:, :N], in_=sd[:, :N]), reads=['sd' + cq_], writes=['sd' + cq_])
                        S.op('pool', lambda e: e.tensor_tensor(out=yc[:, :N], in0=yc[:, :N], in1=sd[:, :N], op=ALU.mult), reads=['yc' + cq_, 'sd' + cq_], writes=['yc' + cq_])
                        S.op('dve', lambda e: e.tensor_scalar(out=yc[:, :N], in0=yc[:, :N], scalar1=V(f"lnw{j}", c, c + 1), scalar2=V(f"lnb{j}", c, c + 1),
                                                              op0=ALU.mult, op1=ALU.add), reads=['yc' + cq_, 'vecs'], writes=['yc' + cq_])
                        S.op('pool', lambda e: e.tensor_tensor(out=yc[:, :N], in0=yc[:, :N], in1=bo[:, c, :N], op=ALU.add), reads=['yc' + cq_, 'bo'], writes=['yc' + cq_])
                        S.op('pool', lambda e: e.tensor_tensor(out=ob[:, c, :N], in0=yc[:, :N], in1=gg[:, c, :N], op=ALU.mult), reads=['yc' + cq_, 'gg'], writes=['ob'])
                    for oc in range(8):
                        pb_ = 4 + oc % 4
                        for kc in range(8):
                            S.op('pe', lambda e: e.matmul(ps[pb_][:, :N], lhsT=wo[:, kc, oc * 128:(oc + 1) * 128], rhs=ob[:, kc, :N], start=(kc == 0), stop=(kc == 7)),
                                 reads=['r_wo', 'ob'], writes=[('ps', pb_)], inc=(kc == 7))
                        S.op('dve', lambda e: e.scalar_tensor_tensor(out=xt[:, oc, :N], in0=ps[pb_][:, :N], scalar=modt[:, i, 16 + oc, mc:mc + 1],
                                                                     in1=xt[:, oc, :N], op0=ALU.mult, op1=ALU.add),
                             reads=[('ps', pb_), 'r_xt', 'modt'], writes=['r_xt'])
                    S.dma('sp', XS[:, t0:t0 + N].rearrange("(k p) n -> p k n", p=128), xt[:, :, :N], reads=['r_xt'], writes=['XS'])
            S.barrier()

        def phase_ffn(i, last):
            moe = (i % 2 == 1)
            k = i // 2
            E = NE if moe else 1
            F = DFE if moe else DFF
            nchunk = F // 128
            blocks = [(c0, min(4, nchunk - c0)) for c0 in range(0, nchunk, 4)]
            tiles = TILES[1:] if last else TILES
            groups = [tiles[0:len(tiles) - 6], tiles[-6:-3], tiles[-3:]]
            for gi, grp in enumerate(groups):
                Sg = sum(t[1] for t in grp)
                offs = [sum(t[1] for t in grp[:a]) for a in range(len(grp))]
                with contextlib.ExitStack() as st:
                    hb = sb("f_hb", [128, 8, 1536], BF16, st)
                    yacc = sb("f_yacc", [128, 8, 1536], F32, st)
                    GT = sb("f_GT", [8, 1536], F32, st)
                    gbc = sb("f_gbc", [128, 1536], F32, st)
                    with contextlib.ExitStack() as st2:
                        xt = [sb(f"f_xt{b}", [128, 8, 512], F32, st2) for b in range(2)]
                        sq = sb("f_sq", [128, 8, 512], F32, st2)
                        rs = sb("f_rs", [128, 512], F32, st2)
                        if moe:
                            rt = sb("f_rt", [128, 8, 8], F32, st2)
                            S.dma('sp', rt[:], moe_router[k].rearrange("(k p) n -> p k n", p=128), writes=['rt'])
                            lg = sb("f_lg", [128, 8], F32, st2); m8 = sb("f_m8", [128, 8], F32, st2)
                            sel = sb("f_sel", [128, 8], F32, st2); ex = sb("f_ex", [128, 8], F32, st2)
                            sm = sb("f_sm", [128, 4], F32, st2); G = sb("f_G", [128, 4, 8], F32, st2)
                        for ti, (t0, N, mc) in enumerate(grp):
                            b = ti % 2
                            kx = ('fx', b)
                            o = offs[ti]
                            S.dma('sp', xt[b][:, :, :N], XS[:, t0:t0 + N].rearrange("(k p) n -> p k n", p=128), writes=[kx])
                            ksq = normmod(xt[b][:, :, :N], N, A2[:, i, :, mc], None, None, sq, rs, 7, kx, 'f')
                            S.op('dve', lambda e: e.tensor_tensor(out=sq[:, :, :N], in0=sq[:, :, :N],
                                                                  in1=modt[:, i, 24:32, mc].unsqueeze(2).to_broadcast([128, 8, N]), op=ALU.add),
                                 reads=[ksq, 'modt'], writes=[ksq])
                            S.op('act', lambda e: e.activation(out=hb[:, :, o:o + N], in_=sq[:, :, :N], func=AF.Identity),
                                 reads=[ksq], writes=['hb'])
                            if moe:
                                for blk in range(N // 128):
                                    for kc in range(8):
                                        S.op('pe', lambda e: e.matmul(ps[6][:, 0:8], lhsT=sq[:, kc, blk * 128:(blk + 1) * 128], rhs=rt[:, kc, :],
                                                                      start=(kc == 0), stop=(kc == 7)), reads=[ksq, 'rt'], writes=[('ps', 6)], inc=(kc == 7))
                                    S.op('dve', lambda e: e.tensor_copy(out=lg[:], in_=ps[6][:, 0:8]), reads=[('ps', 6)], writes=['lg'])
                                    S.op('dve', lambda e: e.max(out=m8[:], in_=lg[:]), reads=['lg'], writes=['m8'])
                                    S.op('dve', lambda e: e.tensor_scalar(out=sel[:], in0=lg[:], scalar1=m8[:, 1:2], scalar2=None, op0=ALU.is_ge),
                                         reads=['lg', 'm8'], writes=['sel'])
                                    S.op('dve', lambda e: e.tensor_scalar(out=sm[:, 0:1], in0=m8[:, 0:1], scalar1=-1.0, scalar2=None, op0=ALU.mult),
                                         reads=['m8'], writes=['sm'])
                                    S.op('act', lambda e: e.activation(out=ex[:], in_=lg[:], func=AF.Exp, bias=sm[:, 0:1], scale=1.0),
                                         reads=['lg', 'sm'], writes=['ex'])
                                    S.op('dve', lambda e: e.tensor_tensor(out=ex[:], in0=ex[:], in1=sel[:], op=ALU.mult), reads=['ex', 'sel'], writes=['ex'])
                                    S.op('dve', lambda e: e.tensor_reduce(out=sm[:, 1:2], in_=ex[:], axis=AX.X, op=ALU.add), reads=['ex'], writes=['sm'])
                                    S.op('dve', lambda e: e.reciprocal(out=sm[:, 2:3], in_=sm[:, 1:2]), reads=['sm'], writes=['sm'])
                                    S.op('dve', lambda e: e.tensor_scalar(out=G[:, blk, :], in0=ex[:], scalar1=sm[:, 2:3], scalar2=None, op0=ALU.mult),
                                         reads=['ex', 'sm'], writes=['G'])
                                    S.op('pe', lambda e: e.transpose(out=ps[5][0:8, blk * 128:(blk + 1) * 128], in_=G[:, blk, :], identity=ident),
                                         reads=['G', 'c32'], writes=[('ps', 5)])
                                S.op('act', lambda e: e.activation(out=GT[:, o:o + N], in_=ps[5][0:8, :N], func=AF.Identity),
                                     reads=[('ps', 5)], writes=['GT'])
                    S.barrier()
                    with contextlib.ExitStack() as st2:
                        w1b = [sb(f"f_w1{b}", [128, 8, 512], BF16, st2) for b in range(2)]
                        w3b = [sb(f"f_w3{b}", [128, 8, 512], BF16, st2) for b in range(2)]
                        w2b = [sb(f"f_w2{b}", [128, 4, 1024], BF16, st2) for b in range(2)]
                        gt = [sb(f"f_g{b}", [128, 4, 512], BF16, st2) for b in range(2)]
                        s1 = [sb(f"f_s1{b}", [128, 512], F32, st2) for b in range(2)]
                        s1g = [sb(f"f_s1g{b}", [128, 512], F32, st2) for b in range(2)]
                        nw = 0
                        ng = 0
                        nhc = 0
                        ny = 0
                        first = True
                        items = []
                        first = True
                        for ex_i in range(E):
                            for bi, (c0, nh) in enumerate(blocks):
                                wb = nw % 2
                                nw += 1
                                for ti, (t0, N, mc) in enumerate(grp):
                                    items.append(dict(ex=ex_i, bi=bi, c0=c0, nh=nh, wb=wb, ti=ti, o=offs[ti], N=N, gb=ng % 2, first=first))
                                    ng += 1
                                first = False

                        def emit_gate(ex_i):
                            for ti, (t0, N, mc) in enumerate(grp):
                                o = offs[ti]
                                S.op('pe', lambda e: e.matmul(ps[6][:, :N], lhsT=selE[:, ex_i * 128:(ex_i + 1) * 128], rhs=GT[:, o:o + N],
                                                              start=True, stop=True), reads=['GT', 'c32'], writes=[('ps', 6)])
                                S.op('act', lambda e: e.activation(out=gbc[:, o:o + N], in_=ps[6][:, :N], func=AF.Identity),
                                     reads=[('ps', 6)], writes=['gbc'])

                        def emit_wload(it):
                            wb, c0, nh, ex_i = it['wb'], it['c0'], it['nh'], it['ex']
                            kw = ('fw', wb)
                            if moe:
                                W1, W3, W2 = moe_w1[k, ex_i], moe_w3[k, ex_i], moe_w2[k, ex_i]
                            else:
                                W1, W3, W2 = ffn_w1[k], ffn_w3[k], ffn_w2[k]
                            S.dma('pool', w1b[wb][:, :, :nh * 128], W1[:, c0 * 128:(c0 + nh) * 128].rearrange("(k p) n -> p k n", p=128), writes=[kw])
                            S.dma('pool', w3b[wb][:, :, :nh * 128], W3[:, c0 * 128:(c0 + nh) * 128].rearrange("(k p) n -> p k n", p=128), writes=[kw])
                            S.dma('pool', w2b[wb][:, :nh, :], W2[c0 * 128:(c0 + nh) * 128, :].rearrange("(k p) n -> p k n", p=128), writes=[kw])

                        def emit_P(it):
                            nonlocal_nhc = cnts
                            wb, nh, o, N, gb = it['wb'], it['nh'], it['o'], it['N'], it['gb']
                            kw = ('fw', wb)
                            for hc in range(nh):
                                pa = (cnts[0] % 2) * 2
                                sbi = cnts[0] % 2
                                cnts[0] += 1
                                for kc in range(8):
                                    S.op('pe', lambda e: e.matmul(ps[pa][:, :N], lhsT=w1b[wb][:, kc, hc * 128:(hc + 1) * 128], rhs=hb[:, kc, o:o + N],
                                                                  start=(kc == 0), stop=(kc == 7)), reads=[kw, 'hb'], writes=[('ps', pa)], inc=(kc == 7))
                                for kc in range(8):
                                    S.op('pe', lambda e: e.matmul(ps[pa + 1][:, :N], lhsT=w3b[wb][:, kc, hc * 128:(hc + 1) * 128], rhs=hb[:, kc, o:o + N],
                                                                  start=(kc == 0), stop=(kc == 7)), reads=[kw, 'hb'], writes=[('ps', pa + 1)], inc=(kc == 7))
                                S.op('act', lambda e: e.activation(out=s1[sbi][:, :N], in_=ps[pa][:, :N], func=AF.Silu),
                                     reads=[('ps', pa)], writes=[('s1', sbi)])
                                src, ksrc = s1[sbi], ('s1', sbi)
                                if moe:
                                    S.op('dve', lambda e: e.tensor_tensor(out=s1g[sbi][:, :N], in0=s1[sbi][:, :N], in1=gbc[:, o:o + N], op=ALU.mult),
                                         reads=[('s1', sbi), 'gbc'], writes=[('s1g', sbi)])
                                    src, ksrc = s1g[sbi], ('s1g', sbi)
                                S.op('dve', lambda e: e.tensor_tensor(out=gt[gb][:, hc, :N], in0=src[:, :N], in1=ps[pa + 1][:, :N], op=ALU.mult),
                                     reads=[ksrc, ('ps', pa + 1)], writes=[('g', gb)])

                        def emit_W2(it):
                            wb, nh, o, N, gb = it['wb'], it['nh'], it['o'], it['N'], it['gb']
                            kw = ('fw', wb)
                            for oc in range(8):
                                py = 4 + cnts[1] % 2
                                cnts[1] += 1
                                for hc in range(nh):
                                    S.op('pe', lambda e: e.matmul(ps[py][:, :N], lhsT=w2b[wb][:, hc, oc * 128:(oc + 1) * 128], rhs=gt[gb][:, hc, :N],
                                                                  start=(hc == 0), stop=(hc == nh - 1)), reads=[kw, ('g', gb)], writes=[('ps', py)],
                                         inc=(hc == nh - 1))
                                ky = ('y', o, oc)
                                if it['first']:
                                    S.op('act', lambda e: e.activation(out=yacc[:, oc, o:o + N], in_=ps[py][:, :N], func=AF.Identity),
                                         reads=[('ps', py)], writes=[ky])
                                else:
                                    S.op('dve', lambda e: e.tensor_tensor(out=yacc[:, oc, o:o + N], in0=yacc[:, oc, o:o + N], in1=ps[py][:, :N], op=ALU.add),
                                         reads=[('ps', py), ky], writes=[ky])

                        cnts = [0, 0]
                        prev = None
                        for it in items:
                            if moe and it['bi'] == 0 and it['ti'] == 0:
                                emit_gate(it['ex'])
                            if it['ti'] == 0:
                                emit_wload(it)
                            emit_P(it)
                            if prev is not None:
                                emit_W2(prev)
                            prev = it
                        emit_W2(prev)
                    S.barrier()
                    with contextlib.ExitStack() as st2:
                        xt = [sb(f"f_cx{b}", [128, 8, 512], F32, st2) for b in range(2)]
                        for ti, (t0, N, mc) in enumerate(grp):
                            b = ti % 2
                            o = offs[ti]
                            S.dma('sp', xt[b][:, :, :N], XS[:, t0:t0 + N].rearrange("(k p) n -> p k n", p=128), writes=[('fcx', b)])
                            for oc in range(8):
                                S.op('dve', lambda e: e.scalar_tensor_tensor(
                                    out=xt[b][:, oc, :N], in0=yacc[:, oc, o:o + N], scalar=modt[:, i, 40 + oc, mc:mc + 1],
                                    in1=xt[b][:, oc, :N], op0=ALU.mult, op1=ALU.add), reads=[('fcx', b), 'modt'], writes=[('fcx', b)])
                            if last:
                                S.dma('sp', out_d[:, t0 - CTX:t0 - CTX + N].rearrange("(k p) n -> p k n", p=128), xt[b][:, :, :N],
                                      reads=[('fcx', b)], writes=['out'])
                            else:
                                S.dma('sp', XS[:, t0:t0 + N].rearrange("(k p) n -> p k n", p=128), xt[b][:, :, :N],
                                      reads=[('fcx', b)], writes=['XS'])
                    S.barrier()

        if dbg:
            dbg_d = nc.dram_tensor("dbg", [D, NT], F32, kind="ExternalOutput").ap()
        phase_mod()
        for i in range(depth):
            last = (i == depth - 1)
            if i == 0 and os.environ.get('SKIP0'):
                S.dma('sp', XS, xs_in, writes=['XS'])
                S.barrier()
                continue
            if i % 2 == 0:
                phase_mla(i, xs_in if i == 0 else XS)
            else:
                phase_rwkv(i, last and dbg != 'r')
            if not (dbg == 'r' and i == depth - 1):
                phase_ffn(i, last)
        if dbg:
            S.dma('sp', dbg_d, XS, writes=['dbg'])
        S.barrier()
    return nc, S


def host_consts():
    c = np.zeros((128, C32W), np.float32)
    c[:, 0:128] = 1.0
    c[:, 128:256] = np.eye(128, dtype=np.float32)
    P = np.zeros((64, 64), np.float32)
    for base in (0, 32):
        for f in range(16):
            P[base + 16 + f, base + f] = -1.0
            P[base + f, base + 16 + f] = 1.0
    c[0:64, 256:320] = P
    for e in range(8):
        c[e, 384 + e * 128:384 + (e + 1) * 128] = 1.0
    o_ = 384 + 1024
    p = np.arange(128)
    c[:, o_:o_ + 128] = (p[:, None] // 64 == p[None, :] // 64)
    tt = np.arange(64)
    c[:, o_ + 128:o_ + 192] = (p[:, None] % 64 == tt[None, :])
    sidx = (p % 64)[:, None]
    c[:, o_ + 192:o_ + 256] = (sidx < tt[None, :])
    c[:, o_ + 256:o_ + 320] = (sidx <= tt[None, :])
    c[:, o_ + 320:o_ + 384] = (tt[None, :] < sidx)
    c[:, o_ + 384:o_ + 448] = (sidx > tt[None, :])
    c[:, o_ + 448:o_ + 512] = (sidx >= tt[None, :])
    c[:, o_ + 512:o_ + 576] = (tt[None, :] > sidx)
    cm = np.ones(512, np.float32); cm[0::64] = 0.0
    c[:, o_ + 576:o_ + 1088] = cm[None, :]
    rows = TL // 64
    row_ids = np.repeat(np.arange(rows, dtype=np.float32), 64)
    col_ids = np.tile(np.arange(64, dtype=np.float32), rows)
    inv_freq = (1.0 / (np.float32(10000.0) ** (np.arange(16, dtype=np.float32) / np.float32(16)))).astype(np.float32)
    ang_r = (row_ids[:, None] * inv_freq[None, :]).astype(np.float32)
    ang_c = (col_ids[:, None] * inv_freq[None, :]).astype(np.float32)
    cosT = np.zeros((64, TL), np.float32)
    sinT = np.zeros((64, TL), np.float32)
    for base, ang in ((0, ang_r), (32, ang_c)):
        for half in (0, 16):
            cosT[base + half:base + half + 16] = np.cos(ang).T
            sinT[base + half:base + half + 16] = np.sin(ang).T
    return c, cosT, sinT


def host_vecs(inp, depth):
    voff, NV = vec_layout(depth)
    vecs = np.zeros((128, NV), np.float32)

    def put(name, arr):
        o, c = voff[name]
        assert arr.shape == (128, c), (name, arr.shape, c)
        vecs[:, o:o + c] = arr
    for i in range(depth):
        put(f"adab{i}", fm(inp["ada_b"][i]))
        put(f"n1g{i}", fm(inp["norm1_g"][i]))
        put(f"n2g{i}", fm(inp["norm2_g"][i]))
        j = i // 2
        if i % 2 == 0:
            put(f"qan{j}", fm(inp["mla_qa_norm"][j]))
            put(f"kvan{j}", fm(inp["mla_kva_norm"][j]))
            put(f"qnn{j}", fm(inp["mla_q_norm"][j][:128]))
            put(f"qnr{j}", fm(inp["mla_q_norm"][j][128:]))
            put(f"knn{j}", fm(inp["mla_k_norm"][j][:128]))
            put(f"knr{j}", fm(inp["mla_k_norm"][j][128:]))
        else:
            put(f"mix{j}", fm(inp["rwkv_mix"][j]))
            put(f"w0{j}", fm(inp["rwkv_w0"][j]))
            put(f"a0{j}", fm(inp["rwkv_a0"][j]))
            put(f"kk{j}", fm(inp["rwkv_k_k"][j]))
            put(f"ka{j}", fm(inp["rwkv_k_a"][j]))
            put(f"rk{j}", fm(inp["rwkv_r_k"][j]))
            put(f"lnw{j}", fm(inp["rwkv_ln_w"][j]))
            put(f"lnb{j}", fm(inp["rwkv_ln_b"][j]))
            if j >= 1:
                put(f"v0{j}", fm(inp["rwkv_v0"][j - 1]))
    return vecs


WNAMES = ["ada_w", "mla_wqa", "mla_wqb", "mla_wkva", "mla_wkvb", "mla_wo", "ffn_w1", "ffn_w3", "ffn_w2",
          "moe_router", "moe_w1", "moe_w3", "moe_w2",
          "rwkv_wr", "rwkv_wk", "rwkv_wv", "rwkv_wo", "rwkv_w1", "rwkv_w2", "rwkv_a1", "rwkv_a2", "rwkv_g1", "rwkv_g2", "rwkv_v1", "rwkv_v2"]


def make_in_maps(inp, depth, cores):
    c32, cosT, sinT = host_consts()
    vecs = host_vecs(inp, depth)
    shared = {n: np.ascontiguousarray(inp[n], dtype=np.float32) for n in WNAMES}
    maps = []
    for b in cores:
        xs = np.ascontiguousarray(np.concatenate([inp["ctx"][b], inp["x"][b]], axis=0).T.astype(np.float32))
        cv = np.zeros((128, 16), np.float32)
        cv[:, 0::2] = fm(inp["c"][b])
        cv[:, 1::2] = fm(inp["c_ctx"])
        m = dict(shared)
        m.update(xs=xs, cvec=cv, vecs=vecs, c32=c32, ropec=cosT, ropes=sinT)
        maps.append(m)
    return maps


def kernel(**inp):
    depth = 4
    nc, S = build(depth)
    maps = make_in_maps(inp, depth, list(range(8)))
    res = run_bass_kernel_spmd(nc, maps, core_ids=list(range(8)))
    out = np.stack([np.ascontiguousarray(r["out"].T) for r in res.results], axis=0)
    return out.astype(np.float32)
```
